# Optimizing a Trainium2 kernel written in Bass

```python
import math
import functools
import jax
import jax.numpy as jnp
from jax import lax
import numpy as np

D_MODEL = 1024
BATCH = 2
SEQ = 8192
DEPTH = 2

GRID_W = 64
CTX_LEN = 256
CHUNK = 64
NORM_EPS = 1e-6

GLA_HEADS = 4
GLA_DK = 64
GLA_DV = 128
GLA_KEY_W = GLA_HEADS * GLA_DK
GLA_VAL_W = GLA_HEADS * GLA_DV
GLA_LOW_RANK = 16
GLA_TAU = 16.0
HG_HEADS = 4
HG_EXPAND = 128
HG_DV = 128
HG_KEY_W = HG_HEADS * HG_EXPAND
HG_VAL_W = HG_HEADS * HG_DV
HY_CH = 512
HY_ORDER = 2
HY_SHORT = 3
HY_BANDS = 16
HY_POS_DIM = 1 + 2 * HY_BANDS
HY_FILT_HID = 64
HY_MIN_DECAY = math.log(1e-2) / 1.5
HY_MAX_DECAY = math.log(1e-2) / 0.3
MB_HEADS = 8
MB_HEAD_DIM = 64
MB_INNER = MB_HEADS * MB_HEAD_DIM
MB_GROUPS = 2
MB_STATE = 128
MB_CONV = 3
MB_CONV_CH = MB_INNER + 2 * MB_GROUPS * MB_STATE
AB_SPLITS = (GLA_KEY_W, GLA_KEY_W, GLA_VAL_W, GLA_VAL_W, GLA_LOW_RANK, GLA_LOW_RANK,
             HG_KEY_W, HG_KEY_W, HG_KEY_W, HG_VAL_W, HG_VAL_W)
AB_COLS = sum(AB_SPLITS)
AB_WIDTH = GLA_VAL_W + HG_VAL_W
CD_SPLITS = (3 * HY_CH, MB_INNER, MB_CONV_CH, MB_HEADS, MB_HEADS)
CD_COLS = sum(CD_SPLITS)
CD_WIDTH = HY_CH + MB_INNER
N_EXPERTS = 16
N_GROUPS = 4
EXPERTS_PER_GROUP = N_EXPERTS // N_GROUPS
TOP_K = 2
D_EXPERT = 1024
MOE_BLOCK = 256

kernel_name = "hybrid_gla_hgrn_hyena_ssd_moe_dit"


def rms_norm(x, g):
    xf = x.astype(jnp.float32)
    y = xf * lax.rsqrt(jnp.mean(xf * xf, axis=-1, keepdims=True) + NORM_EPS)
    return (y * g.astype(jnp.float32)).astype(x.dtype)


def modulate(h, g, shift, scale):
    return rms_norm(h, g) * (1.0 + scale) + shift


def adaln(cond, w, b):
    m = jnp.dot(jax.nn.silu(cond), w) + b
    return jnp.split(m[..., None, :], 6, axis=-1)


def split_cols(a, sizes):
    return jnp.split(a, np.cumsum(sizes)[:-1].tolist(), axis=-1)


def dir_view(a, n_ctx, reverse):
    if not reverse:
        return a
    return jnp.concatenate([jnp.flip(a[:, :n_ctx], 1), jnp.flip(a[:, n_ctx:], 1)], axis=1)


def centred_dwconv(u, w, b):
    kw = w.shape[-1]
    r = kw // 2
    n = u.shape[1]
    up = jnp.pad(u, ((0, 0), (r, r), (0, 0)))
    return sum(up[:, j:j + n] * w[:, j] for j in range(kw)) + b


def to_chunks(a):
    b, t = a.shape[:2]
    return jnp.moveaxis(a.reshape(b, t // CHUNK, CHUNK, *a.shape[2:]), 1, 0)


def from_chunks(a):
    n, b, c = a.shape[:3]
    return jnp.moveaxis(a, 0, 1).reshape(b, n * c, *a.shape[3:])


def gla_scan(q, k, v, log_a):
    b, _, h, dk = q.shape
    dv = v.shape[-1]
    mask = jnp.tril(jnp.ones((CHUNK, CHUNK), bool))[None, :, :, None, None]

    def step(s, blk):
        qc, kc, vc, ac = blk
        cum = jnp.cumsum(ac, axis=1)
        o = jnp.einsum('bthk,bhkv->bthv', qc * jnp.exp(cum), s)
        decay = jnp.exp(jnp.where(mask, cum[:, :, None] - cum[:, None], -jnp.inf))
        att = jnp.einsum('bthk,bshk,btshk->bths', qc, kc, decay)
        o = o + jnp.einsum('bths,bshv->bthv', att, vc)
        last = cum[:, -1]
        s = jnp.exp(last)[..., None] * s + jnp.einsum(
            'bshk,bshv->bhkv', kc * jnp.exp(last[:, None] - cum), vc)
        return s, o

    s0 = jnp.zeros((b, h, dk, dv), jnp.float32)
    _, o = lax.scan(step, s0, (to_chunks(q), to_chunks(k), to_chunks(v), to_chunks(log_a)))
    return from_chunks(o)


def ssd_scan(x, la, bm, cm):
    b, t, h, p = x.shape
    g, n = bm.shape[2:]
    r = h // g
    x = x.reshape(b, t, g, r, p)
    la = la.reshape(b, t, g, r)
    mask = jnp.tril(jnp.ones((CHUNK, CHUNK), bool))[None, :, :, None, None]

    def step(s, blk):
        xc, ac, bc, cc = blk
        cum = jnp.cumsum(ac, axis=1)
        decay = jnp.exp(jnp.where(mask, cum[:, :, None] - cum[:, None], -jnp.inf))
        y = jnp.einsum('btgn,bsgn,btsgr,bsgrp->btgrp', cc, bc, decay, xc)
        y = y + jnp.einsum('btgn,bgrnp->btgrp', cc, s) * jnp.exp(cum)[..., None]
        last = cum[:, -1]
        s = jnp.exp(last)[..., None, None] * s + jnp.einsum(
            'bsgn,bsgrp->bgrnp', bc, xc * jnp.exp(last[:, None] - cum)[..., None])
        return s, y

    s0 = jnp.zeros((b, g, r, n, p), jnp.float32)
    _, y = lax.scan(step, s0, (to_chunks(x), to_chunks(la), to_chunks(bm), to_chunks(cm)))
    return from_chunks(y).reshape(b, t, h, p)


def hyena_filters(n, w1, b1, w2, b2, w3, freq):
    t = jnp.linspace(0.0, 1.0, n, dtype=jnp.float32)[:, None]
    bands = jnp.linspace(1e-4, HY_BANDS - 1, HY_BANDS, dtype=jnp.float32)
    ang = (2.0 * math.pi / n) * jnp.arange(n, dtype=jnp.float32)[:, None] * bands
    z = jnp.concatenate([t, jnp.cos(ang), -jnp.sin(ang)], axis=-1)
    hid = jnp.sin(freq * (z @ w1 + b1))
    hid = jnp.sin(freq * (hid @ w2 + b2))
    filt = (hid @ w3).reshape(n, HY_ORDER, 2, HY_CH)
    rates = jnp.abs(jnp.linspace(HY_MIN_DECAY, HY_MAX_DECAY, HY_CH, dtype=jnp.float32))
    return filt * jnp.exp(-t * rates)[:, None, None, :]


def bidir_long_conv(u, h_fwd, h_bwd, bias):
    n = u.shape[1]
    k = jnp.concatenate([h_fwd[:1] + h_bwd[:1], h_fwd[1:], jnp.zeros_like(h_fwd[:1]),
                         jnp.flip(h_bwd[1:], 0)], axis=0)
    uf = jnp.fft.rfft(u.astype(jnp.float32), n=2 * n, axis=1)
    kf = jnp.fft.rfft(k.astype(jnp.float32), axis=0)
    y = jnp.fft.irfft(uf * kf[None], n=2 * n, axis=1)[:, :n]
    return y + u.astype(jnp.float32) * bias


def mixer_gla_hgrn(u, n_ctx, w_in, gate_w, gate_b, gla_norm_g, lb, hg_norm_g):
    bsz, t, _ = u.shape
    heads = lambda a, nh: a.reshape(bsz, t, nh, -1)
    gq, gk, gv, gg, glr_f, glr_b, hq, hf_f, hf_b, hi, hg = split_cols(u @ w_in, AB_SPLITS)
    gq = heads(gq, GLA_HEADS) * GLA_DK ** -0.5
    gk, gv = heads(gk, GLA_HEADS), heads(gv, GLA_HEADS)
    hq, hi = heads(jax.nn.silu(hq), HG_HEADS), heads(hi, HG_HEADS)
    o_gla, o_hg = 0.0, 0.0
    for d, (lr, fz) in enumerate(((glr_f, hf_f), (glr_b, hf_b))):
        view = functools.partial(dir_view, n_ctx=n_ctx, reverse=(d == 1))
        log_a = jax.nn.log_sigmoid((lr @ gate_w[d] + gate_b[d]).astype(jnp.float32)) / GLA_TAU
        f = lb[d] + (1.0 - lb[d]) * jax.nn.sigmoid(fz.astype(jnp.float32))
        o_gla = o_gla + view(gla_scan(view(gq), view(gk), view(gv), view(heads(log_a, GLA_HEADS))))
        o_hg = o_hg + view(gla_scan(view(hq), view(heads(1.0 - f, HG_HEADS)), view(hi),
                                    view(heads(jnp.log(f), HG_HEADS))))
    feat_gla = rms_norm(o_gla, gla_norm_g) * jax.nn.silu(heads(gg, GLA_HEADS))
    feat_hg = rms_norm(o_hg, hg_norm_g) * jax.nn.silu(heads(hg, HG_HEADS))
    feat = jnp.concatenate([feat_gla.reshape(bsz, t, -1), feat_hg.reshape(bsz, t, -1)],
                           axis=-1).astype(u.dtype)
    return feat[:, :n_ctx], feat[:, n_ctx:]


def mixer_hyena_ssd(u, n_ctx, need_ctx, w_in, short_w, short_b, fw1, fb1, fw2, fb2, fw3, freq,
                    conv_bias, conv_w, conv_b, dt_bias, a_log, d_skip, norm_g):
    bsz, t, _ = u.shape
    hy_in, z, xbc, dt_f, dt_b = split_cols(u @ w_in, CD_SPLITS)

    def hyena(seg):
        n = seg.shape[1]
        v, x1, x2 = jnp.split(centred_dwconv(seg, short_w, short_b), 3, axis=-1)
        filt = hyena_filters(n, fw1, fb1, fw2, fb2, fw3, freq)
        zz = x1 * bidir_long_conv(v, filt[:, 0, 0], filt[:, 0, 1], conv_bias[0])
        return x2 * bidir_long_conv(zz, filt[:, 1, 0], filt[:, 1, 1], conv_bias[1])

    hy_lat = hyena(hy_in[:, n_ctx:])
    xbc = jax.nn.silu(jnp.concatenate([centred_dwconv(xbc[:, :n_ctx], conv_w, conv_b),
                                       centred_dwconv(xbc[:, n_ctx:], conv_w, conv_b)], axis=1))
    xs, bm, cm = split_cols(xbc, (MB_INNER, MB_GROUPS * MB_STATE, MB_GROUPS * MB_STATE))
    xs = xs.reshape(bsz, t, MB_HEADS, MB_HEAD_DIM)
    bm = bm.reshape(bsz, t, MB_GROUPS, MB_STATE)
    cm = cm.reshape(bsz, t, MB_GROUPS, MB_STATE)
    y = d_skip[:, None] * xs
    for d, dt_raw in enumerate((dt_f, dt_b)):
        view = functools.partial(dir_view, n_ctx=n_ctx, reverse=(d == 1))
        dt = jax.nn.softplus(dt_raw.astype(jnp.float32) + dt_bias[d])
        la = -dt * jnp.exp(a_log[d].astype(jnp.float32))
        y = y + view(ssd_scan(view(xs * dt[..., None]), view(la), view(bm), view(cm)))
    y = (y.reshape(bsz, t, MB_INNER) * jax.nn.silu(z)).reshape(bsz, t, MB_GROUPS, -1)
    y = rms_norm(y, norm_g.reshape(MB_GROUPS, -1)).reshape(bsz, t, MB_INNER)
    f_lat = jnp.concatenate([hy_lat, y[:, n_ctx:]], axis=-1).astype(u.dtype)
    if not need_ctx:
        return None, f_lat
    f_ctx = jnp.concatenate([hyena(hy_in[:, :n_ctx]), y[:, :n_ctx]], axis=-1).astype(u.dtype)
    return f_ctx, f_lat


def moe_ffn(h, router_w, router_b, w_gate, w_up, w_down):
    n, d = h.shape
    scores = jax.nn.sigmoid(jnp.dot(h.astype(jnp.float32), router_w.astype(jnp.float32)))
    sel = scores + router_b
    group_score = lax.top_k(sel.reshape(n, N_GROUPS, EXPERTS_PER_GROUP), TOP_K)[0].sum(-1)
    best = jnp.argmax(group_score, axis=-1)
    in_group = (jnp.arange(N_EXPERTS) // EXPERTS_PER_GROUP)[None] == best[:, None]
    _, idx = lax.top_k(jnp.where(in_group, sel, -jnp.inf), TOP_K)
    w = jnp.take_along_axis(scores, idx, axis=1)
    w = w / jnp.sum(w, axis=-1, keepdims=True)
    e_flat = idx.reshape(-1)
    tok = jnp.repeat(jnp.arange(n, dtype=jnp.int32), TOP_K)
    order = jnp.argsort(e_flat)
    e_s, tok_s, w_s = e_flat[order], tok[order], w.reshape(-1)[order]
    counts = jnp.zeros(N_EXPERTS, jnp.int32).at[e_flat].add(1)
    start = jnp.cumsum(counts) - counts
    padded = (counts + MOE_BLOCK - 1) // MOE_BLOCK * MOE_BLOCK
    pend = jnp.cumsum(padded)
    dest = (pend - padded)[e_s] + jnp.arange(n * TOP_K, dtype=jnp.int32) - start[e_s]
    n_slots = (n * TOP_K + MOE_BLOCK - 1) // MOE_BLOCK * MOE_BLOCK + N_EXPERTS * MOE_BLOCK
    n_blocks = n_slots // MOE_BLOCK
    slot_tok = jnp.full((n_slots,), n, jnp.int32).at[dest].set(tok_s)
    slot_w = jnp.zeros((n_slots,), jnp.float32).at[dest].set(w_s)
    block_e = jnp.clip(jnp.searchsorted(pend, jnp.arange(n_blocks) * MOE_BLOCK, side='right'),
                       0, N_EXPERTS - 1)
    hp = jnp.concatenate([h, jnp.zeros((1, d), h.dtype)], axis=0)
    xb = hp[slot_tok].reshape(n_blocks, MOE_BLOCK, d)

    def expert_block(args):
        xblk, e = args
        return (jax.nn.silu(xblk @ w_gate[e]) * (xblk @ w_up[e])) @ w_down[e]

    yb = lax.map(expert_block, (xb, block_e)).reshape(n_slots, d)
    out = jax.ops.segment_sum(yb * slot_w[:, None], slot_tok, num_segments=n + 1)[:n]
    return out.astype(h.dtype)


def setup_inputs(seed: int = 0) -> dict:
    key = jax.random.key(seed)
    ks = iter(jax.random.split(key, 48))
    ne, no = (DEPTH + 1) // 2, DEPTH // 2

    def nrm(shape, scale):
        return scale * jax.random.normal(next(ks), shape, jnp.float32)

    dt0 = jnp.exp(jax.random.uniform(next(ks), (no, 2, MB_HEADS), jnp.float32,
                                     math.log(1e-3), math.log(1e-1)))
    mb_dt_bias = dt0 + jnp.log(-jnp.expm1(-dt0))
    mb_a_log = jnp.log(jax.random.uniform(next(ks), (no, 2, MB_HEADS), jnp.float32, 1.0, 16.0))
    return {
        "x": nrm((BATCH, SEQ, D_MODEL), 1.0),
        "c": nrm((BATCH, D_MODEL), 1.0),
        "ctx": nrm((BATCH, CTX_LEN, D_MODEL), 1.0),
        "c_ctx": nrm((D_MODEL,), 1.0),
        "ada_w": nrm((DEPTH, D_MODEL, 6 * D_MODEL), 0.5 * D_MODEL ** -0.5),
        "ada_b": nrm((DEPTH, 6 * D_MODEL), 0.02),
        "norm_mix_g": 1.0 + nrm((DEPTH, D_MODEL), 0.02),
        "norm_ffn_g": 1.0 + nrm((DEPTH, D_MODEL), 0.02),
        "norm_out_g": 1.0 + nrm((D_MODEL,), 0.02),
        "ab_w_in": nrm((ne, D_MODEL, AB_COLS), D_MODEL ** -0.5),
        "ab_w_out": nrm((ne, AB_WIDTH, D_MODEL), AB_WIDTH ** -0.5),
        "gla_gate_w": nrm((ne, 2, GLA_LOW_RANK, GLA_KEY_W), GLA_LOW_RANK ** -0.5),
        "gla_gate_b": nrm((ne, 2, GLA_KEY_W), 0.1),
        "gla_norm_g": 1.0 + nrm((ne, GLA_DV), 0.02),
        "hg_lb": nrm((2, ne + 1, HG_KEY_W), 0.1),
        "hg_norm_g": 1.0 + nrm((ne, HG_DV), 0.02),
        "cd_w_in": nrm((no, D_MODEL, CD_COLS), D_MODEL ** -0.5),
        "cd_w_out": nrm((no, CD_WIDTH, D_MODEL), CD_WIDTH ** -0.5),
        "hy_short_w": nrm((no, 3 * HY_CH, HY_SHORT), HY_SHORT ** -0.5),
        "hy_short_b": nrm((no, 3 * HY_CH), 0.02),
        "hy_w1": nrm((no, HY_POS_DIM, HY_FILT_HID), HY_POS_DIM ** -0.5),
        "hy_b1": nrm((no, HY_FILT_HID), 0.1),
        "hy_w2": nrm((no, HY_FILT_HID, HY_FILT_HID), HY_FILT_HID ** -0.5),
        "hy_b2": nrm((no, HY_FILT_HID), 0.1),
        "hy_w3": nrm((no, HY_FILT_HID, HY_ORDER * 2 * HY_CH), 0.03 * HY_FILT_HID ** -0.5),
        "hy_freq": 1.0 + nrm((no, HY_FILT_HID), 0.02),
        "hy_bias": nrm((no, HY_ORDER, HY_CH), 0.5),
        "mb_conv_w": nrm((no, MB_CONV_CH, MB_CONV), MB_CONV ** -0.5),
        "mb_conv_b": nrm((no, MB_CONV_CH), 0.02),
        "mb_dt_bias": mb_dt_bias,
        "mb_a_log": mb_a_log,
        "mb_d": 1.0 + nrm((no, MB_HEADS), 0.02),
        "mb_norm_g": 1.0 + nrm((no, MB_INNER), 0.02),
        "router_w": nrm((D_MODEL, N_EXPERTS), D_MODEL ** -0.5),
        "router_b": nrm((N_EXPERTS,), 0.01),
        "moe_w_gate": nrm((DEPTH, N_EXPERTS, D_MODEL, D_EXPERT), D_MODEL ** -0.5),
        "moe_w_up": nrm((DEPTH, N_EXPERTS, D_MODEL, D_EXPERT), D_MODEL ** -0.5),
        "moe_w_down": nrm((DEPTH, N_EXPERTS, D_EXPERT, D_MODEL), D_EXPERT ** -0.5),
    }


def reference(x, c, ctx, c_ctx, ada_w, ada_b, norm_mix_g, norm_ffn_g, norm_out_g,
              ab_w_in, ab_w_out, gla_gate_w, gla_gate_b, gla_norm_g, hg_lb, hg_norm_g,
              cd_w_in, cd_w_out, hy_short_w, hy_short_b, hy_w1, hy_b1, hy_w2, hy_b2, hy_w3,
              hy_freq, hy_bias, mb_conv_w, mb_conv_b, mb_dt_bias, mb_a_log, mb_d, mb_norm_g,
              router_w, router_b, moe_w_gate, moe_w_up, moe_w_down):
    bsz, n_lat, d = x.shape
    n_ctx = ctx.shape[1]
    lb_all = jnp.cumsum(jax.nn.softmax(hg_lb.astype(jnp.float32), axis=1), axis=1)
    h_ctx, h_lat = ctx, x
    for layer in range(DEPTH):
        need_ctx = layer < DEPTH - 1
        sh_m, sc_m, gt_m, sh_f, sc_f, gt_f = adaln(c, ada_w[layer], ada_b[layer])
        csh_m, csc_m, cgt_m, csh_f, csc_f, cgt_f = adaln(c_ctx, ada_w[layer], ada_b[layer])
        u = jnp.concatenate([modulate(h_ctx, norm_mix_g[layer], csh_m, csc_m),
                             modulate(h_lat, norm_mix_g[layer], sh_m, sc_m)], axis=1)
        j = layer // 2
        if layer % 2 == 0:
            f_ctx, f_lat = mixer_gla_hgrn(u, n_ctx, ab_w_in[j], gla_gate_w[j], gla_gate_b[j],
                                          gla_norm_g[j], lb_all[:, j], hg_norm_g[j])
            w_out = ab_w_out[j]
        else:
            f_ctx, f_lat = mixer_hyena_ssd(u, n_ctx, need_ctx, cd_w_in[j], hy_short_w[j], hy_short_b[j],
                                           hy_w1[j], hy_b1[j], hy_w2[j], hy_b2[j], hy_w3[j], hy_freq[j],
                                           hy_bias[j], mb_conv_w[j], mb_conv_b[j], mb_dt_bias[j],
                                           mb_a_log[j], mb_d[j], mb_norm_g[j])
            w_out = cd_w_out[j]
        h_lat = h_lat + gt_m * (f_lat @ w_out)
        if need_ctx:
            h_ctx = h_ctx + cgt_m * (f_ctx @ w_out)
            v = jnp.concatenate([modulate(h_ctx, norm_ffn_g[layer], csh_f, csc_f),
                                 modulate(h_lat, norm_ffn_g[layer], sh_f, sc_f)], axis=1)
            y = moe_ffn(v.reshape(-1, d), router_w, router_b, moe_w_gate[layer], moe_w_up[layer],
                        moe_w_down[layer]).reshape(bsz, n_ctx + n_lat, d)
            h_ctx = h_ctx + cgt_f * y[:, :n_ctx]
            h_lat = h_lat + gt_f * y[:, n_ctx:]
        else:
            v = modulate(h_lat, norm_ffn_g[layer], sh_f, sc_f)
            y = moe_ffn(v.reshape(-1, d), router_w, router_b, moe_w_gate[layer], moe_w_up[layer],
                        moe_w_down[layer]).reshape(bsz, n_lat, d)
            h_lat = h_lat + gt_f * y
    return rms_norm(h_lat, norm_out_g)
```

```python
import numpy as np
import ml_dtypes
import concourse.bass as bass
import concourse.mybir as mybir
from concourse.bass_utils import run_bass_kernel_spmd

F32 = mybir.dt.float32
BF16 = mybir.dt.bfloat16
AF = mybir.ActivationFunctionType
ALU = mybir.AluOpType
AX = mybir.AxisListType
NPBF = ml_dtypes.bfloat16

ENGS = ['pe', 'act', 'dve', 'pool', 'sp']
SAME_ENGINE_SYNC = True
NCORES = 8
TL = 2112
TILES = [(0, 512, 0), (512, 1024, 0), (1024, 1536, 0), (1536, 2048, 0), (2048, 2112, 1)]
EPS = 1e-6


class Prog:
    def __init__(self, nc, ndma_sems=6):
        self.nc = nc
        self.q = {e: [] for e in ENGS}
        self.cnt = {e: 0 for e in ENGS}
        self.seen = {e: {} for e in ENGS}
        self.lastw = {}
        self.reads = {}
        self.ndma = ndma_sems
        self.dma_n = {e: 0 for e in ENGS}
        self.stack = []
        self.uid = 0

    def enter(self, cm):
        v = cm.__enter__()
        self.stack.append(cm)
        return v

    def sb(self, name, shape, dt):
        return self.enter(self.nc.sbuf_tensor(name, list(shape), dt))

    def ps(self, name, shape, dt=F32):
        return self.enter(self.nc.psum_tensor(name, list(shape), dt))

    def close(self):
        while self.stack:
            self.stack.pop().__exit__(None, None, None)

    def _deps(self, eng, reads, writes):
        toks = set()
        for r in reads:
            t = self.lastw.get(r)
            if t is not None:
                toks.add(t)
        for w in writes:
            t = self.lastw.get(w)
            if t is not None:
                toks.add(t)
            for t in self.reads.get(w, ()):
                toks.add(t)
        need = {}
        for (k, v) in toks:
            if k == eng and (eng == 'pe' or not SAME_ENGINE_SYNC):
                continue
            if self.seen[eng].get(k, 0) >= v:
                continue
            if need.get(k, 0) < v:
                need[k] = v
        for k, v in need.items():
            self.seen[eng][k] = v
        return list(need.items())

    def _commit(self, tok, reads, writes):
        for r in reads:
            if r in writes:
                continue
            self.reads.setdefault(r, []).append(tok)
        for w in writes:
            self.lastw[w] = tok
            self.reads[w] = []

    def op(self, eng, name, reads=(), writes=(), **kw):
        reads = list(reads)
        writes = list(writes)
        waits = self._deps(eng, reads, writes)
        self.cnt[eng] += 1
        tok = (eng, self.cnt[eng])
        self.q[eng].append((waits, (name, kw), ('c', eng)))
        self._commit(tok, reads, writes)

    def mm(self, out, lhsT, rhs, start, stop, reads, writes):
        self.op('pe', 'matmul', reads, writes, out=out, lhsT=lhsT, rhs=rhs, start=start, stop=stop)

    def act(self, out, in_, func, reads, writes, **kw):
        self.op('act', 'activation', reads, writes, out=out, in_=in_, func=func, **kw)

    def dve(self, name, reads, writes, **kw):
        self.op('dve', name, reads, writes, **kw)

    def pool(self, name, reads, writes, **kw):
        self.op('pool', name, reads, writes, **kw)

    def dma(self, eng, reads=(), writes=(), **kw):
        fn = ('dma_start', kw)
        reads = list(reads)
        writes = list(writes)
        n = self.dma_n[eng]
        self.dma_n[eng] += 1
        si = n % self.ndma
        key = ('d', eng, si)
        val = 16 * (n // self.ndma + 1)
        waits = self._deps(eng, reads, writes)
        if n >= self.ndma and self.seen[eng].get(key, 0) < val - 16:
            waits.append((key, val - 16))
            self.seen[eng][key] = val - 16
        self.q[eng].append((waits, fn, key))
        self._commit((key, val), reads, writes)

    def wait_all(self, eng):
        need = {}
        for tok in self.lastw.values():
            k, v = tok
            if need.get(k, 0) < v:
                need[k] = v
        for toks in self.reads.values():
            for k, v in toks:
                if need.get(k, 0) < v:
                    need[k] = v
        waits = [(k, v) for k, v in need.items() if self.seen[eng].get(k, 0) < v and (k != eng or eng != 'sp')]
        for k, v in waits:
            self.seen[eng][k] = v
        self.q[eng].append((waits, None, None))

    def emit(self):
        nc = self.nc
        used = set()
        for e in ENGS:
            for waits, fn, inc in self.q[e]:
                for k, _ in waits:
                    used.add(k)
                if inc is not None:
                    used.add(inc if inc[0] == 'd' else inc[1])
        sem = {}
        for i, k in enumerate(sorted(used, key=str)):
            sem[k] = self.enter(nc.semaphore("sem%d" % i))
        block = self.enter(nc.Block())
        q = self.q

        def run(e, engine):
            for waits, fn, inc in q[e]:
                for k, v in waits:
                    engine.wait_ge(sem[k], v)
                if fn is None:
                    continue
                inst = getattr(engine, fn[0])(**fn[1])
                if inc[0] == 'd':
                    inst.then_inc(sem[inc], 16)
                else:
                    inst.then_inc(sem[inc[1]], 1)

        @block.tensor
        def _(eng):
            run('pe', eng)

        @block.scalar
        def _(eng):
            run('act', eng)

        @block.vector
        def _(eng):
            run('dve', eng)

        @block.gpsimd
        def _(eng):
            run('pool', eng)

        @block.sync
        def _(eng):
            run('sp', eng)


def emit_mods(P, condT, ada_w_l, adab, pm, tag):
    cond = P.sb("sbcond" + tag, [128, 8, 2], F32)
    sc = P.sb("sbsc" + tag, [128, 8, 2], F32)
    adab_sb = P.sb("sbadab" + tag, [128, 48], F32)
    mod = P.sb("sbmod" + tag, [128, 48, 2], F32)
    mark_ = len(P.stack)
    wb = [P.sb("sbadaw%d" % i + tag, [128, 8, 256], F32) for i in range(2)]
    P.dma('sp', writes=['cond' + tag], out=cond[:], in_=condT)
    P.dma('sp', writes=['adab' + tag], out=adab_sb[:], in_=adab)
    P.act(sc[:], cond[:], AF.Silu, ['cond' + tag], ['sc' + tag])
    wv = ada_w_l.rearrange("(k p) n -> p k n", p=128)
    for s in range(24):
        buf = wb[s % 2]
        bk = 'adaw%d' % (s % 2) + tag
        P.dma('sp' if s % 2 == 0 else 'act', writes=[bk], out=buf[:], in_=wv[:, :, s * 256:(s + 1) * 256])
        for m4 in range(2):
            m = s * 2 + m4
            for k in range(8):
                P.mm(pm[:, 2 * m:2 * m + 2], buf[:, k, m4 * 128:(m4 + 1) * 128], sc[:, k, :], k == 0, k == 7, [bk, 'sc' + tag], ['pm' + tag])
    P.dve('tensor_tensor', ['pm' + tag, 'adab' + tag], ['mod' + tag], out=mod[:], in0=pm[:, 0:96].rearrange("p (m r) -> p m r", r=2),
          in1=adab_sb[:].unsqueeze(2).to_broadcast([128, 48, 2]), op=ALU.add)
    barrier(P)
    while len(P.stack) > mark_:
        P.stack.pop().__exit__(None, None, None)
    return mod


def emit_affine(P, mod, modkey, gT_dram, kind_scale, tag):
    g = P.sb("sbg" + tag, [128, 8], F32)
    A = P.sb("sbA" + tag, [128, 8, 2], F32)
    P.dma('sp', writes=['g' + tag], out=g[:], in_=gT_dram)
    P.dve('scalar_tensor_tensor', [modkey, 'g' + tag], ['A' + tag], out=A[:], in0=mod[:, kind_scale * 8:kind_scale * 8 + 8, :], scalar=1.0,
          in1=g[:].unsqueeze(2).to_broadcast([128, 8, 2]), op0=ALU.add, op1=ALU.mult)
    return A


def emit_rstd(P, src, srckey, n, nk, ones_bf, ps_ssq, sq, rs, inv_n):
    P.act(sq[:, 0:nk, 0:n], src, AF.Square, [srckey], ['sq'])
    for k in range(nk):
        P.mm(ps_ssq[:, 0:n], ones_bf[:], sq[:, k, 0:n], k == 0, k == nk - 1, ['sq', 'ones'], ['ps_ssq'])
    P.dve('tensor_scalar', ['ps_ssq'], ['rs'], out=rs[:, 0:n], in0=ps_ssq[:, 0:n], scalar1=float(inv_n), scalar2=EPS, op0=ALU.mult, op1=ALU.add)
    P.act(rs[:, 0:n], rs[:, 0:n], AF.Sqrt, ['rs'], ['rs'])
    P.dve('reciprocal', ['rs'], ['rs'], out=rs[:, 0:n], in_=rs[:, 0:n])


def emit_modulate_tile(P, hT, hkey, A, Akey, mod, modkey, kind_shift, ti, tile, ones_bf, ps_ssq, work, uT=None, ukey=None, u32=None):
    sq, rs, tmp = work['sq'], work['rs'], work['tmp']
    c0, c1, r = tile
    n = c1 - c0
    emit_rstd(P, hT[:, :, c0:c1], hkey + ':%d' % ti, n, 8, ones_bf, ps_ssq, sq, rs, 1.0 / 1024)
    for k in range(8):
        tb = tmp[k % 2]
        tk = 'tmp%d' % (k % 2)
        P.dve('scalar_tensor_tensor', [hkey + ':%d' % ti, Akey, 'rs'], [tk], out=tb[:, 0:n], in0=hT[:, k, c0:c1], scalar=A[:, k, r:r + 1], in1=rs[:, 0:n], op0=ALU.mult, op1=ALU.mult)
        sh = mod[:, kind_shift * 8 + k, r:r + 1]
        if u32 is not None:
            P.act(u32[:, k, 0:n], tb[:, 0:n], AF.Identity, [tk, modkey], ['u32'], bias=sh, scale=1.0)
            if uT is not None:
                P.pool('tensor_copy', ['u32'], [ukey + ':%d' % ti], out=uT[:, k, c0:c1], in_=u32[:, k, 0:n])
        else:
            P.act(uT[:, k, c0:c1], tb[:, 0:n], AF.Identity, [tk, modkey], [ukey + ':%d' % ti], bias=sh, scale=1.0)


AB = dict(gq=0, gk=256, gv=512, gg=1024, glr_f=1536, glr_b=1552, hq=1568, hf_f=2080, hf_b=2592, hi=3104, hg=3616, end=4128)
A0_BF = dict(gq=0, gk=256, gv=512, hq=1024, hk_f=1536, hk_b=2048, hi=2560, end=3072)
A0_F = dict(gg=0, hg=512, gla_f=1024, gla_b=1280, hla_f=1536, hla_b=2048, end=2560)


class ProjCtx:
    def __init__(self, P, w_dram, uT, ukey, gb, o_f, o_bf, nst=3):
        self.P, self.uT, self.ukey, self.gb, self.o_f, self.o_bf = P, uT, ukey, gb, o_f, o_bf
        self.wv = w_dram.rearrange("(k p) n -> p k n", p=128)
        self.wblk = [P.sb("wblk%d" % i, [128, 8, 512], BF16) for i in range(2)]
        self.nw = 0
        self.W = None
        self.nst = nst
        self.stg_f = [P.sb("stgf%d" % i, [128, TL], F32) for i in range(nst)]
        self.stg_b = [P.sb("stgb%d" % i, [128, TL], BF16) for i in range(nst)]
        self.cnt = {'f': 0, 'b': 0, 'g': 0, 'q': 0}

    def stage(self, kind):
        i = self.cnt[kind] % self.nst
        self.cnt[kind] += 1
        return ((self.stg_f if kind == 'f' else self.stg_b)[i], 'stg%s%d' % (kind, i))

    def bank(self):
        i = self.cnt['g'] % len(self.gb)
        self.cnt['g'] += 1
        return self.gb[i], 'gb%d' % i

    def outdma(self, kind, st, sk, row0, nrows, ncols=TL):
        dst = (self.o_f if kind == 'f' else self.o_bf)
        q = ['sp', 'act'][self.cnt['q'] % 2]
        self.cnt['q'] += 1
        self.P.dma(q, reads=[sk], out=dst[row0:row0 + nrows, 0:ncols], in_=st[0:nrows, 0:ncols])

    def load_w(self, col0, ncols):
        i = self.nw % 2
        self.nw += 1
        self.W, self.Wkey, self.wcol0 = self.wblk[i], 'wblk%d' % i, col0
        self.P.dma('pool', writes=[self.Wkey], out=self.W[:, :, 0:ncols], in_=self.wv[:, :, col0:col0 + ncols])

    def chunk(self, col0, ncols, post, tiles=TILES):
        P = self.P
        col0 = col0 - self.wcol0
        for ti, (c0, c1, r) in enumerate(tiles):
            n = c1 - c0
            b, bkey = self.bank()
            for k in range(8):
                P.mm(b[0:ncols, 0:n], self.W[:, k, col0:col0 + ncols], self.uT[:, k, c0:c1], k == 0, k == 7, [self.Wkey, self.ukey + ':%d' % ti], [bkey])
            post(b[0:ncols, 0:n], n, c0, c1, ti, bkey)

    def job_lin(self, col0, ncols_total, row0, scale, kind='b', tiles=TILES, ncols_out=TL):
        P = self.P
        for j in range(ncols_total // 128):
            if j % 4 == 0:
                self.load_w(col0 + j * 128, min(512, ncols_total - j * 128))
            st, sk = self.stage(kind)

            def post(ps, n, c0, c1, ti, bkey, st=st, sk=sk):
                P.dve('tensor_scalar', [bkey], [sk, bkey], out=st[:, c0:c1], in0=ps, scalar1=float(scale), scalar2=None, op0=ALU.mult)
            self.chunk(col0 + j * 128, 128, post, tiles)
            self.outdma(kind, st, sk, row0 + j * 128, 128, ncols_out)

    def job_silu(self, col0, ncols_total, row0, kind, tiles=TILES, ncols_out=TL):
        P = self.P
        for j in range(ncols_total // 128):
            if j % 4 == 0:
                self.load_w(col0 + j * 128, min(512, ncols_total - j * 128))
            st, sk = self.stage(kind)

            def post(ps, n, c0, c1, ti, bkey, st=st, sk=sk):
                P.act(st[:, c0:c1], ps, AF.Silu, [bkey], [sk, bkey])
            self.chunk(col0 + j * 128, 128, post, tiles)
            self.outdma(kind, st, sk, row0 + j * 128, 128, ncols_out)


def build_A0():
    nc = bass.Bass("TRN2", target_bir_lowering=False)
    xT = nc.dram_tensor("xT", [128, 8, TL], F32, kind="ExternalInput").ap()
    condT = nc.dram_tensor("condT", [128, 8, 2], F32, kind="ExternalInput").ap()
    ada_w = nc.dram_tensor("ada_w", [1024, 6144], F32, kind="ExternalInput").ap()
    adab = nc.dram_tensor("adab", [128, 48], F32, kind="ExternalInput").ap()
    gmix = nc.dram_tensor("gmix", [128, 8], F32, kind="ExternalInput").ap()
    w_in = nc.dram_tensor("w_in", [1024, 4128], F32, kind="ExternalInput").ap()
    gate_w = nc.dram_tensor("gate_w", [16, 2, 256], F32, kind="ExternalInput").ap()
    gate_b = nc.dram_tensor("gate_b", [128, 2, 2], F32, kind="ExternalInput").ap()
    hglb = nc.dram_tensor("hglb", [128, 2, 2, 4], F32, kind="ExternalInput").ap()
    o_bf = nc.dram_tensor("o_bf", [A0_BF['end'], TL], BF16, kind="ExternalOutput").ap()
    o_f = nc.dram_tensor("o_f", [A0_F['end'], TL], F32, kind="ExternalOutput").ap()
    ada_w1 = nc.dram_tensor("ada_w1", [1024, 6144], F32, kind="ExternalInput").ap()
    adab1 = nc.dram_tensor("adab1", [128, 48], F32, kind="ExternalInput").ap()
    mods_out = nc.dram_tensor("mods_out", [2, 128, 48, 2], F32, kind="ExternalOutput").ap()

    P = Prog(nc)
    hT = P.sb("hT", [128, 8, TL], F32)
    uT = P.sb("uT", [128, 8, TL], BF16)
    ones_bf = P.sb("ones_bf", [128, 128], BF16)
    work = dict(sq=P.sb("sq", [128, 8, 512], BF16), rs=P.sb("rs", [128, 512], F32),
                tmp=[P.sb("tmp%d" % i, [128, 512], F32) for i in range(2)])
    banks = [P.ps("bank%d" % i, [128, 512], F32) for i in range(6)]
    pm, ps_ssq = banks[0], banks[1]
    gb = banks[2:6]

    for k in range(8):
        P.dma('sp' if k % 2 == 0 else 'act', writes=['h:%d' % t for t in range(5)], out=hT[:, k, :], in_=xT[:, k, :])
    P.pool('memset', [], ['ones'], ap=ones_bf[:], constant=1.0)

    mod = emit_mods(P, condT, ada_w, adab, pm, '0')
    modB = emit_mods(P, condT, ada_w1, adab1, pm, '1')
    P.dma('sp', reads=['mod0'], out=mods_out[0], in_=mod[:])
    P.dma('sp', reads=['mod1'], out=mods_out[1], in_=modB[:])
    A = emit_affine(P, mod, 'mod0', gmix, 1, 'm0')
    for ti, tile in enumerate(TILES):
        emit_modulate_tile(P, hT, 'h', A, 'Am0', mod, 'mod0', 0, ti, tile, ones_bf, ps_ssq, work, uT=uT, ukey='u')

    gw_f = P.sb("gw_f", [16, 2, 256], F32)
    gw = P.sb("gw", [16, 2, 256], BF16)
    gbias = P.sb("gbias", [128, 2, 2], F32)
    ngb = P.sb("ngb", [128, 2, 2], F32)
    lbr = P.sb("lbr", [128, 2, 2, 4], F32)
    lbe = P.sb("lbe", [128, 2, 2, 4], F32)
    lb = P.sb("lb", [128, 2, 4], F32)
    oml = P.sb("oml", [128, 2, 4], F32)
    den = P.sb("den", [128, 2, 4], F32)
    P.dma('sp', writes=['gw_f'], out=gw_f[:], in_=gate_w)
    P.dma('sp', writes=['gbias'], out=gbias[:], in_=gate_b)
    P.dma('sp', writes=['lbr'], out=lbr[:], in_=hglb)
    P.dve('tensor_copy', ['gw_f'], ['gw'], out=gw[:], in_=gw_f[:])
    P.dve('tensor_scalar', ['gbias'], ['ngb'], out=ngb[:], in0=gbias[:], scalar1=-1.0, scalar2=None, op0=ALU.mult)
    P.act(lbe[:], lbr[:], AF.Exp, ['lbr'], ['lbe'])
    P.dve('tensor_tensor', ['lbe'], ['den'], out=den[:], in0=lbe[:, :, 0, :], in1=lbe[:, :, 1, :], op=ALU.add)
    P.dve('reciprocal', ['den'], ['den'], out=den[:], in_=den[:])
    P.dve('tensor_tensor', ['lbe', 'den'], ['lb'], out=lb[:], in0=lbe[:, :, 0, :], in1=den[:], op=ALU.mult)
    P.dve('tensor_scalar', ['lb'], ['oml'], out=oml[:], in0=lb[:], scalar1=-1.0, scalar2=1.0, op0=ALU.mult, op1=ALU.add)

    glrT = [P.sb("glrT%d" % d, [16, TL], BF16) for d in range(2)]
    ftmp = [P.sb("ftmp%d" % i, [128, 512], F32) for i in range(2)]
    C = ProjCtx(P, w_in, uT, 'u', gb, o_f, o_bf)

    def job_glr(col0, d):
        def post(ps, n, c0, c1, ti, bkey):
            P.dve('tensor_copy', [bkey], ['glrT%d' % d], out=glrT[d][:, c0:c1], in_=ps)
        C.load_w(col0, 16)
        C.chunk(col0, 16, post)

    def job_gate(d, row0):
        for j in range(2):
            st, sk = C.stage('f')
            for ti, (c0, c1, r) in enumerate(TILES):
                n = c1 - c0
                b, bkey = C.bank()
                P.mm(b[:, 0:n], gw[:, d, j * 128:(j + 1) * 128], glrT[d][:, c0:c1], True, True, ['gw', 'glrT%d' % d], [bkey])
                ft = ftmp[ti % 2]
                fk = 'ftmp%d' % (ti % 2)
                P.act(ft[:, 0:n], b[:, 0:n], AF.Exp, [bkey, 'ngb'], [fk], bias=ngb[:, d, j:j + 1], scale=-1.0)
                P.act(ft[:, 0:n], ft[:, 0:n], AF.Ln, [fk], [fk], bias=1.0, scale=1.0)
                P.dve('tensor_scalar', [fk], [sk], out=st[:, c0:c1], in0=ft[:, 0:n], scalar1=-1.0 / 16.0, scalar2=None, op0=ALU.mult)
            C.outdma('f', st, sk, row0 + j * 128, 128)

    def job_hgf(col0, d, row_k, row_la):
        C.load_w(col0, 512)
        for j in range(4):
            stb, skb = C.stage('b')
            stf, skf = C.stage('f')

            def post(ps, n, c0, c1, ti, bkey, stb=stb, skb=skb, stf=stf, skf=skf, j=j):
                ft = ftmp[ti % 2]
                fk = 'ftmp%d' % (ti % 2)
                P.act(ft[:, 0:n], ps, AF.Sigmoid, [bkey], [fk])
                P.dve('tensor_scalar', [fk, 'oml', 'lb'], [fk], out=ft[:, 0:n], in0=ft[:, 0:n], scalar1=oml[:, d, j:j + 1], scalar2=lb[:, d, j:j + 1], op0=ALU.mult, op1=ALU.add)
                P.pool('tensor_scalar', [fk], [skb], out=stb[:, c0:c1], in0=ft[:, 0:n], scalar1=-1.0, scalar2=1.0, op0=ALU.mult, op1=ALU.add)
                P.act(stf[:, c0:c1], ft[:, 0:n], AF.Ln, [fk], [skf])
            C.chunk(col0 + j * 128, 128, post)
            C.outdma('b', stb, skb, row_k + j * 128, 128)
            C.outdma('f', stf, skf, row_la + j * 128, 128)

    job_glr(AB['glr_f'], 0)
    job_glr(AB['glr_b'], 1)
    C.job_lin(AB['gq'], 256, A0_BF['gq'], 64 ** -0.5)
    C.job_lin(AB['gk'], 256, A0_BF['gk'], 1.0)
    C.job_lin(AB['gv'], 512, A0_BF['gv'], 1.0)
    C.job_silu(AB['gg'], 512, A0_F['gg'], 'f')
    job_gate(0, A0_F['gla_f'])
    job_gate(1, A0_F['gla_b'])
    C.job_silu(AB['hq'], 512, A0_BF['hq'], 'b')
    job_hgf(AB['hf_f'], 0, A0_BF['hk_f'], A0_F['hla_f'])
    job_hgf(AB['hf_b'], 1, A0_BF['hk_b'], A0_F['hla_b'])
    C.job_lin(AB['hi'], 512, A0_BF['hi'], 1.0)
    C.job_silu(AB['hg'], 512, A0_F['hg'], 'f')
    P.wait_all('sp')
    P.emit()
    P.close()
    return nc


def fm(a):
    T = a.shape[0]
    return np.ascontiguousarray(a.T.reshape(8, 128, T).transpose(1, 0, 2))


def colT(v, nk):
    return np.ascontiguousarray(np.asarray(v).reshape(nk, 128).T)


def core_tokens(x, ctx, i):
    b, q = i // 4, i % 4
    return np.concatenate([x[b, 2048 * q:2048 * (q + 1)], ctx[b, 64 * q:64 * (q + 1)]], axis=0)


def cond_T(inp, b):
    cond = np.stack([inp['c'][b], inp['c_ctx']], axis=-1)
    return np.ascontiguousarray(cond.reshape(8, 128, 2).transpose(1, 0, 2))


def host_A0_inputs(inp):
    maps = []
    x, ctx = inp['x'], inp['ctx']
    for i in range(NCORES):
        b = i // 4
        m = dict(
            xT=fm(core_tokens(x, ctx, i)),
            condT=cond_T(inp, b),
            ada_w=inp['ada_w'][0], adab=colT(inp['ada_b'][0], 48), gmix=colT(inp['norm_mix_g'][0], 8),
            ada_w1=inp['ada_w'][1], adab1=colT(inp['ada_b'][1], 48),
            w_in=inp['ab_w_in'][0],
            gate_w=np.ascontiguousarray(inp['gla_gate_w'][0].transpose(1, 0, 2)),
            gate_b=np.ascontiguousarray(inp['gla_gate_b'][0].reshape(2, 2, 128).transpose(2, 0, 1)),
            hglb=np.ascontiguousarray(inp['hg_lb'].reshape(2, 2, 4, 128).transpose(3, 0, 1, 2)),
        )
        maps.append(m)
    return maps


STOP_STAGE = 9
NSCAN = 4
VAR7 = 7
NCH = 132
SCH = 4
NSC = NCH // SCH


def scan_consts(P, tri_dram):
    tri = P.sb("tri_sb", [64, 6, 64], F32)
    P.dma('sp', writes=['tri'], out=tri[:], in_=tri_dram)
    return tri


def emit_scans(P, scans, tri, banks, nsteps=None):
    ns = len(scans)
    st = []
    for i, sc in enumerate(scans):
        dk, dv = sc['dk'], sc['dv']
        d = dict(sc)
        d['i'] = i
        d['in'] = []
        for par in range(2):
            n = "s%dp%d" % (i, par)
            d['in'].append(dict(
                qT=P.sb("qT" + n, [dk, SCH * 64], BF16), kT=P.sb("kT" + n, [dk, SCH * 64], BF16),
                ktm=P.sb("ktm" + n, [64, SCH, dk], BF16), vtm=P.sb("vtm" + n, [64, SCH, dv], BF16),
                latm=P.sb("latm" + n, [64, SCH, dk], F32), o=P.sb("o" + n, [64, SCH, dv], F32), key="in" + n, okey="o" + n))
        d['scr'] = []
        for par in range(2):
            n = "s%dq%d" % (i, par)
            d['scr'].append(dict(
                e1=P.sb("e1" + n, [dk, 64], F32), e2=P.sb("e2" + n, [dk, 64], F32), e3=P.sb("e3" + n, [64, dk], F32),
                qe=P.sb("qe" + n, [dk, 64], BF16), ke=P.sb("ke" + n, [dk, 64], BF16), kd=P.sb("kd" + n, [64, dk], BF16),
                attm=P.sb("attm" + n, [64, 64], BF16), dS=P.sb("dS" + n, [64, 64], F32), cc=P.sb("cc" + n, [64, 2], F32), n=n, bank=banks[2 * i + par]))
        d['Sf'] = P.sb("Sf%d" % i, [dk, dv], F32)
        d['Sb'] = P.sb("Sb%d" % i, [dk, dv], BF16)
        P.pool('memset', [], ['Sf%d' % i], ap=d['Sf'][:], constant=0.0)
        P.pool('memset', [], ['Sb%d' % i], ap=d['Sb'][:], constant=0.0)
        st.append(d)

    def sc_order(rev):
        if not rev:
            return [(s, list(range(SCH))) for s in range(NSC)]
        return [(0, list(range(SCH - 1, -1, -1)))] + [(s, list(range(SCH - 1, -1, -1))) for s in range(NSC - 1, 0, -1)]

    orders = [sc_order(d['rev']) for d in st]
    dq = [0]

    def load(d, step):
        s, _ = orders[d['i']][step]
        b = d['in'][step % 2]
        t0, t1 = s * SCH * 64, (s + 1) * SCH * 64
        c0, c1 = s * SCH, (s + 1) * SCH
        for nm, src in (('qT', d['qT'][:, t0:t1]), ('kT', d['kT'][:, t0:t1]), ('ktm', d['ktm'][:, c0:c1, :]), ('vtm', d['vtm'][:, c0:c1, :]), ('latm', d['latm'][:, c0:c1, :])):
            q = ['sp', 'act', 'pool'][dq[0] % 3]
            dq[0] += 1
            P.dma(q, reads=d.get('rkeys', []), writes=[b['key'] + nm], out=b[nm][:], in_=src)

    def store(d, step):
        s, _ = orders[d['i']][step]
        b = d['in'][step % 2]
        q = ['sp', 'act', 'pool'][dq[0] % 3]
        dq[0] += 1
        P.dma(q, reads=[b['okey']], writes=[d['okey']], out=d['o'][:, s * SCH:(s + 1) * SCH, :], in_=b['o'][:])

    for d in st:
        load(d, 0)
    cidx = 0
    NS = NSC if nsteps is None else nsteps
    for step in range(NS):
        for d in st:
            if step + 1 < NS:
                load(d, step + 1)
        for j in range(SCH):
            work = []
            for d in st:
                s, chs = orders[d['i']][step]
                c = chs[j]
                b = d['in'][step % 2]
                w = d['scr'][cidx % 2]
                work.append((d, b, w, c))
            cidx += 1
            for d, b, w, c in work:
                dk, dv, rev = d['dk'], d['dv'], d['rev']
                bk = w['bank']
                kb = 'bank' + w['n']
                lac = b['latm'][:, c, :]
                triI = tri[:, 1 if rev else 0, :]
                triS = tri[:, 3 if rev else 2, :]
                P.mm(bk[0:dk, 0:64], lac, triI, True, True, [b['key'] + 'latm', 'tri'], [kb])
                P.mm(bk[0:64, 128:128 + dk], triS, lac, True, True, [b['key'] + 'latm', 'tri'], [kb])
                if d['mode'] == 'scalar':
                    P.mm(bk[0:64, 320:322], triI, lac[:, 0:2], True, True, [b['key'] + 'latm', 'tri'], [kb])
            for d, b, w, c in (work if STOP_STAGE >= 2 else []):
                dk, dv, rev = d['dk'], d['dv'], d['rev']
                bk = w['bank']
                kb = 'bank' + w['n']
                n = w['n']
                P.act(w['e1'][:], bk[0:dk, 0:64], AF.Exp, [kb], ['e1' + n, kb])
                if d['mode'] == 'vec':
                    P.act(w['e2'][:], bk[0:dk, 0:64], AF.Exp, [kb], ['e2' + n, kb], scale=-1.0)
                P.act(w['e3'][:], bk[0:64, 128:128 + dk], AF.Exp, [kb], ['e3' + n, kb])
            for d, b, w, c in (work if STOP_STAGE >= 3 else []):
                dk, dv, rev = d['dk'], d['dv'], d['rev']
                n = w['n']
                bk = w['bank']
                kb = 'bank' + w['n']
                qc = b['qT'][:, c * 64:(c + 1) * 64]
                kc = b['kT'][:, c * 64:(c + 1) * 64]
                P.dve('tensor_tensor', [b['key'] + 'qT', 'e1' + n], ['qe' + n], out=w['qe'][:], in0=qc, in1=w['e1'][:], op=ALU.mult)
                if d['mode'] == 'vec':
                    P.dve('tensor_tensor', [b['key'] + 'kT', 'e2' + n], ['ke' + n], out=w['ke'][:], in0=kc, in1=w['e2'][:], op=ALU.mult)
                else:
                    P.dve('tensor_copy', [kb], ['cc' + n, kb], out=w['cc'][:], in_=bk[0:64, 320:322])
                    P.dve('scalar_tensor_tensor', [kb, 'cc' + n, 'tri'], ['dS' + n, kb], out=w['dS'][:], in0=bk[0:64, 0:64], scalar=w['cc'][:, 0:1],
                          in1=tri[:, 5 if rev else 4, :], op0=ALU.subtract, op1=ALU.add)
                P.pool('tensor_tensor', [b['key'] + 'ktm', 'e3' + n], ['kd' + n], out=w['kd'][:], in0=b['ktm'][:, c, :], in1=w['e3'][:], op=ALU.mult)
            for d, b, w, c in (work if STOP_STAGE >= 4 else []):
                dk = d['dk']
                n = w['n']
                bk = w['bank']
                kb = 'bank' + w['n']
                if d['mode'] == 'vec':
                    P.mm(bk[0:64, 64:128], w['ke'][:], w['qe'][:], True, True, ['ke' + n, 'qe' + n], [kb])
                else:
                    P.mm(bk[0:64, 64:128], b['kT'][:, c * 64:(c + 1) * 64], b['qT'][:, c * 64:(c + 1) * 64], True, True, [b['key'] + 'kT', b['key'] + 'qT'], [kb])
                    P.act(w['dS'][:], w['dS'][:], AF.Exp, ['dS' + n], ['dS' + n])
            for d, b, w, c in (work if STOP_STAGE >= 5 else []):
                n = w['n']
                bk = w['bank']
                kb = 'bank' + w['n']
                if d['mode'] == 'vec':
                    P.dve('tensor_tensor', [kb, 'tri'], ['attm' + n, kb], out=w['attm'][:], in0=bk[0:64, 64:128], in1=tri[:, 1 if d['rev'] else 0, :], op=ALU.mult)
                else:
                    P.dve('tensor_tensor', [kb, 'dS' + n], ['attm' + n, kb], out=w['attm'][:], in0=bk[0:64, 64:128], in1=w['dS'][:], op=ALU.mult)
            for d, b, w, c in (work if STOP_STAGE >= 6 else []):
                dk, dv = d['dk'], d['dv']
                n = w['n']
                i = d['i']
                bk = w['bank']
                kb = 'bank' + w['n']
                P.mm(bk[0:64, 384:384 + dv], w['qe'][:], d['Sb'][:], True, False, ['qe' + n, 'Sb%d' % i], [kb])
                P.mm(bk[0:64, 384:384 + dv], w['attm'][:], b['vtm'][:, c, :], False, True, ['attm' + n, b['key'] + 'vtm'], [kb])
                P.mm(bk[0:dk, 256:256 + dv], w['kd'][:], b['vtm'][:, c, :], True, True, ['kd' + n, b['key'] + 'vtm'], [kb])
            for d, b, w, c in (work if STOP_STAGE >= 7 else []):
                dk, dv, rev = d['dk'], d['dv'], d['rev']
                n = w['n']
                i = d['i']
                bk = w['bank']
                kb = 'bank' + w['n']
                if VAR7 & 1:
                    P.act(b['o'][:, c, :], bk[0:64, 384:384 + dv], AF.Copy, [kb], [b['okey'], kb])
                el = w['e1'][:, 0:1] if rev else w['e1'][:, 63:64]
                if VAR7 & 2:
                    P.dve('scalar_tensor_tensor', [kb, 'e1' + n, 'Sf%d' % i], ['Sf%d' % i, kb], out=d['Sf'][:], in0=d['Sf'][:], scalar=el, in1=bk[0:dk, 256:256 + dv], op0=ALU.mult, op1=ALU.add)
                if VAR7 & 4:
                    P.pool('tensor_copy', ['Sf%d' % i], ['Sb%d' % i], out=d['Sb'][:], in_=d['Sf'][:])
        for d in st:
            store(d, step)


def tri_consts():
    s = np.arange(64)[:, None]
    t = np.arange(64)[None, :]
    NEG = -30000.0
    m = np.stack([(s <= t), (s >= t), (s > t), (s < t)]).astype(np.float32)
    n = np.stack([np.where(s <= t, 0.0, NEG), np.where(s >= t, 0.0, NEG)]).astype(np.float32)
    return np.ascontiguousarray(np.concatenate([m, n], axis=0).transpose(1, 0, 2))


def build_B0(nsteps=None):
    nc = bass.Bass("TRN2", target_bir_lowering=False)
    T = NCH * 64
    tri_d = nc.dram_tensor("tri", [64, 6, 64], F32, kind="ExternalInput").ap()
    scans = []
    for nm, dk in (('g', 64), ('h', 128)):
        qT = nc.dram_tensor(nm + "_qT", [dk, T], BF16, kind="ExternalInput").ap()
        vtm = nc.dram_tensor(nm + "_vtm", [64, NCH, 128], BF16, kind="ExternalInput").ap()
        nk = 1 if nm == 'g' else 2
        kT = [nc.dram_tensor(nm + "_kT%d" % d, [dk, T], BF16, kind="ExternalInput").ap() for d in range(nk)]
        ktm = [nc.dram_tensor(nm + "_ktm%d" % d, [64, NCH, dk], BF16, kind="ExternalInput").ap() for d in range(nk)]
        for d in range(2):
            latm = nc.dram_tensor(nm + "_latm%d" % d, [64, NCH, dk], F32, kind="ExternalInput").ap()
            o = nc.dram_tensor(nm + "_o%d" % d, [64, NCH, 128], F32, kind="ExternalOutput").ap()
            scans.append(dict(dk=dk, dv=128, mode='vec', rev=(d == 1), qT=qT, kT=kT[d % nk], ktm=ktm[d % nk], vtm=vtm, latm=latm, o=o, okey='out_%s%d' % (nm, d)))
    P = Prog(nc)
    tri = scan_consts(P, tri_d)
    banks = [P.ps("bank%d" % i, [128, 512], F32) for i in range(8)]
    emit_scans(P, scans[:NSCAN], tri, banks, nsteps)
    P.wait_all('sp')
    P.emit()
    P.close()
    return nc


def joint_fm(outs, row0, nrows, b):
    parts_ctx = [outs[4 * b + q][row0:row0 + nrows, 2048:2112] for q in range(4)]
    parts_lat = [outs[4 * b + q][row0:row0 + nrows, 0:2048] for q in range(4)]
    return np.concatenate(parts_ctx + parts_lat, axis=1)


def to_tm(aT):
    w, T = aT.shape
    return np.ascontiguousarray(aT.T.reshape(T // 64, 64, w).transpose(1, 0, 2))


def host_B0_inputs(a0):
    obf = [r['o_bf'] for r in a0]
    of = [r['o_f'] for r in a0]
    tri = tri_consts()
    maps = []
    for i in range(NCORES):
        b, hd = i // 4, i % 4
        m = dict(tri=tri)
        gq = joint_fm(obf, A0_BF['gq'] + 64 * hd, 64, b)
        gk = joint_fm(obf, A0_BF['gk'] + 64 * hd, 64, b)
        gv = joint_fm(obf, A0_BF['gv'] + 128 * hd, 128, b)
        m['g_qT'] = gq
        m['g_kT0'] = gk
        m['g_ktm0'] = to_tm(gk)
        m['g_vtm'] = to_tm(gv)
        for d, nm in enumerate(('gla_f', 'gla_b')):
            m['g_latm%d' % d] = to_tm(joint_fm(of, A0_F[nm] + 64 * hd, 64, b))
        m['h_qT'] = joint_fm(obf, A0_BF['hq'] + 128 * hd, 128, b)
        m['h_vtm'] = to_tm(joint_fm(obf, A0_BF['hi'] + 128 * hd, 128, b))
        for d, (nk, nl) in enumerate((('hk_f', 'hla_f'), ('hk_b', 'hla_b'))):
            hk = joint_fm(obf, A0_BF[nk] + 128 * hd, 128, b)
            m['h_kT%d' % d] = hk
            m['h_ktm%d' % d] = to_tm(hk)
            m['h_latm%d' % d] = to_tm(joint_fm(of, A0_F[nl] + 128 * hd, 128, b))
        maps.append(m)
    return maps


CD = dict(hy=0, z=1536, xbc=2048, dt=3072, end=3088)
A1_BF = dict(hy=0, xbc=1536, end=2560)
A1_F = dict(zs=0, dt=512, la=528, end=544)


def barrier(P):
    for e in ENGS:
        P.wait_all(e)


def build_C(layer, nexp=16):
    nc = bass.Bass("TRN2", target_bir_lowering=False)
    T = TL if layer == 0 else 2048
    tiles = TILES if layer == 0 else TILES[:4]
    NT = len(tiles)
    din = lambda name, shape, dt=F32: nc.dram_tensor(name, list(shape), dt, kind="ExternalInput").ap()
    dout = lambda name, shape, dt=F32: nc.dram_tensor(name, list(shape), dt, kind="ExternalOutput").ap()
    hT_in = din("hT_in", [128, 8, T])
    mod_in = din("mod_in", [128, 48, 2])
    gffn = din("gffn", [128, 8])
    w_out = din("w_out", [1024, 1024])
    rw_d = din("router_w", [128, 8, 16])
    rb_d = din("router_b", [128, 16])
    sel_d = din("sel16", [16, 16, 128])
    identd = din("ident", [128, 128])
    wg_d = din("moe_wg", [16, 1024, 1024])
    wu_d = din("moe_wu", [16, 1024, 1024])
    wd_d = din("moe_wd", [16, 1024, 1024])
    if layer == 0:
        oT_d = din("oT", [8, 128, 2, T])
        gate_d = din("gateT", [8, 128, T])
        ng_d = din("ng", [128, 2])
        mod1_in = din("mod1_in", [128, 48, 2])
        gmix1 = din("gmix1", [128, 8])
        w_in1 = din("w_in1", [1024, 3088])
        dtp_d = din("dtp", [16, 2])
        hT_out = dout("hT_out", [128, 8, T])
        o_bf = dout("o_bf", [A1_BF['end'], T], BF16)
        o_f = dout("o_f", [A1_F['end'], T])
    else:
        hy_d = din("hyT", [128, 4, T])
        so_d = din("ssd_oT", [2, 128, 4, T])
        xs_d = din("xsT", [128, 4, T])
        zs_d = din("zsT", [128, 4, T])
        dsk_d = din("dsk", [128, 4])
        mg_d = din("mbg", [128, 4])
        gout_d = din("gout", [128, 8])
        y_out = dout("y_out", [128, 8, T])

    P = Prog(nc)
    hT = P.sb("hT", [128, 8, T], F32)
    fT = P.sb("fT", [128, 8, T], BF16)
    ones_bf = P.sb("ones_bf", [128, 128], BF16)
    ident = P.sb("ident_sb", [128, 128], F32)
    work = dict(sq=P.sb("sq", [128, 8, 512], BF16), rs=P.sb("rs", [128, 512], F32),
                tmp=[P.sb("tmp%d" % i, [128, 512], F32) for i in range(2)])
    banks = [P.ps("bank%d" % i, [128, 512], F32) for i in range(8)]
    pm, ps_ssq = banks[0], banks[1]
    for k in range(8):
        P.dma('sp' if k % 2 == 0 else 'act', writes=['h:%d' % t for t in range(NT)], out=hT[:, k, :], in_=hT_in[:, k, :])
    P.pool('memset', [], ['ones'], ap=ones_bf[:], constant=1.0)
    P.dma('sp', writes=['ident'], out=ident[:], in_=identd)
    mod = P.sb("sbmodL", [128, 48, 2], F32)
    P.dma('sp', writes=['modL'], out=mod[:], in_=mod_in)

    mark = len(P.stack)
    if layer == 0:
        ng = P.sb("ng_sb", [128, 2], F32)
        P.dma('sp', writes=['ng'], out=ng[:], in_=ng_d)
        ob = [P.sb("ob%d" % i, [128, 2, 512], F32) for i in range(2)]
        gbf = [P.sb("gbf%d" % i, [128, 512], F32) for i in range(2)]
        osum = [P.sb("osum%d" % i, [128, 512], F32) for i in range(2)]
        it = 0
        for hh in range(8):
            for ti, (c0, c1, r) in enumerate(tiles):
                n = c1 - c0
                i2 = it % 2
                it += 1
                P.dma('sp', writes=['ob%d' % i2], out=ob[i2][:, :, 0:n], in_=oT_d[hh, :, :, c0:c1])
                P.dma('act', writes=['gbf%d' % i2], out=gbf[i2][:, 0:n], in_=gate_d[hh, :, c0:c1])
                P.pool('tensor_tensor', ['ob%d' % i2], ['osum%d' % i2], out=osum[i2][:, 0:n], in0=ob[i2][:, 0, 0:n], in1=ob[i2][:, 1, 0:n], op=ALU.add)
                emit_rstd(P, osum[i2][:, 0:n].unsqueeze(1), 'osum%d' % i2, n, 1, ones_bf, ps_ssq, work['sq'], work['rs'], 1.0 / 128)
                tb = work['tmp'][i2]
                P.dve('scalar_tensor_tensor', ['osum%d' % i2, 'ng', 'rs'], ['tmp%d' % i2], out=tb[:, 0:n], in0=osum[i2][:, 0:n], scalar=ng[:, hh // 4:hh // 4 + 1], in1=work['rs'][:, 0:n], op0=ALU.mult, op1=ALU.mult)
                P.pool('tensor_tensor', ['tmp%d' % i2, 'gbf%d' % i2], ['f:%d' % ti], out=fT[:, hh, c0:c1], in0=tb[:, 0:n], in1=gbf[i2][:, 0:n], op=ALU.mult)
    else:
        dsk = P.sb("dsk_sb", [128, 4], F32)
        mg = P.sb("mg_sb", [128, 4], F32)
        P.dma('sp', writes=['dsk'], out=dsk[:], in_=dsk_d)
        P.dma('sp', writes=['mg'], out=mg[:], in_=mg_d)
        lb4 = [P.sb("lb4_%d" % i, [128, 4, 512], F32) for i in range(3)]
        yb = P.sb("yb", [128, 4, 512], F32)
        for ti, (c0, c1, r) in enumerate(tiles):
            n = c1 - c0
            P.dma('sp', writes=['lb4_0'], out=lb4[0][:, :, 0:n], in_=hy_d[:, :, c0:c1])
            P.pool('tensor_copy', ['lb4_0'], ['f:%d' % ti], out=fT[:, 0:4, c0:c1], in_=lb4[0][:, :, 0:n])
            P.dma('sp', writes=['lb4_1'], out=lb4[1][:, :, 0:n], in_=so_d[0, :, :, c0:c1])
            P.dma('act', writes=['lb4_2'], out=lb4[2][:, :, 0:n], in_=so_d[1, :, :, c0:c1])
            P.pool('tensor_tensor', ['lb4_1', 'lb4_2'], ['yb'], out=yb[:, :, 0:n], in0=lb4[1][:, :, 0:n], in1=lb4[2][:, :, 0:n], op=ALU.add)
            P.dma('sp', writes=['lb4_1'], out=lb4[1][:, :, 0:n], in_=xs_d[:, :, c0:c1])
            P.dma('act', writes=['lb4_2'], out=lb4[2][:, :, 0:n], in_=zs_d[:, :, c0:c1])
            for j in range(4):
                P.dve('scalar_tensor_tensor', ['lb4_1', 'dsk', 'yb'], ['yb'], out=yb[:, j, 0:n], in0=lb4[1][:, j, 0:n], scalar=dsk[:, j:j + 1], in1=yb[:, j, 0:n], op0=ALU.mult, op1=ALU.add)
            P.pool('tensor_tensor', ['yb', 'lb4_2'], ['yb'], out=yb[:, :, 0:n], in0=yb[:, :, 0:n], in1=lb4[2][:, :, 0:n], op=ALU.mult)
            for g in range(2):
                emit_rstd(P, yb[:, 2 * g:2 * g + 2, 0:n], 'yb', n, 2, ones_bf, ps_ssq, work['sq'], work['rs'], 1.0 / 256)
                for j in (2 * g, 2 * g + 1):
                    P.dve('scalar_tensor_tensor', ['yb', 'mg', 'rs'], ['f:%d' % ti], out=fT[:, 4 + j, c0:c1], in0=yb[:, j, 0:n], scalar=mg[:, j:j + 1], in1=work['rs'][:, 0:n], op0=ALU.mult, op1=ALU.mult)
    barrier(P)
    while len(P.stack) > mark:
        P.stack.pop().__exit__(None, None, None)

    wo = P.sb("wo", [128, 8, 1024], BF16)
    wov = w_out.rearrange("(k p) n -> p k n", p=128)
    for k in range(8):
        P.dma('pool', writes=['wo'], out=wo[:, k, :], in_=wov[:, k, :])
    gcnt = [0]
    gbk = banks[2:6]

    def bank():
        i = gcnt[0] % 4
        gcnt[0] += 1
        return gbk[i], 'gb%d' % i
    for dc in range(8):
        for ti, (c0, c1, r) in enumerate(tiles):
            n = c1 - c0
            b, bk = bank()
            for k in range(8):
                P.mm(b[:, 0:n], wo[:, k, dc * 128:(dc + 1) * 128], fT[:, k, c0:c1], k == 0, k == 7, ['wo', 'f:%d' % ti], [bk])
            P.dve('scalar_tensor_tensor', [bk, 'modL', 'h:%d' % ti], ['h:%d' % ti, bk], out=hT[:, dc, c0:c1], in0=b[:, 0:n], scalar=mod[:, 16 + dc, r:r + 1], in1=hT[:, dc, c0:c1], op0=ALU.mult, op1=ALU.add)
    barrier(P)
    P.stack.pop().__exit__(None, None, None)

    Af = emit_affine(P, mod, 'modL', gffn, 4, 'f')
    sel = P.sb("sel_sb", [16, 16, 128], F32)
    WT = P.sb("WT", [16, T], F32)
    mark = len(P.stack)
    u32 = P.sb("u32", [128, 8, 512], F32)
    rw = P.sb("rw", [128, 8, 16], F32)
    rb = P.sb("rb", [128, 16], F32)
    P.dma('sp', writes=['rw'], out=rw[:], in_=rw_d)
    P.dma('sp', writes=['rb'], out=rb[:], in_=rb_d)
    P.dma('sp', writes=['sel'], out=sel[:], in_=sel_d)
    rt = {nm: P.sb("rt_" + nm, [128, 16], F32) for nm in ('sc', 'sl', 'eq', 's2', 'ch', 'w')}
    r4 = {nm: P.sb("r4_" + nm, [128, 4], F32) for nm in ('m1', 'm2', 'gs', 'gm')}
    r1 = {nm: P.sb("r1_" + nm, [128, 1], F32) for nm in ('gx', 'ss')}
    v4 = lambda t_: t_[:].rearrange("p (g j) -> p g j", j=4)
    b4 = lambda t_: t_[:].unsqueeze(2).to_broadcast([128, 4, 4])
    rbank, rbk = banks[6], 'rbank'
    for ti, tile in enumerate(tiles):
        c0, c1, r = tile
        n = c1 - c0
        emit_modulate_tile(P, hT, 'h', Af, 'Af', mod, 'modL', 3, ti, tile, ones_bf, ps_ssq, work, uT=fT, ukey='v', u32=u32)
        for sub in range(n // 128 if n >= 128 else 1):
            m = min(128, n)
            s0 = sub * 128
            for k in range(8):
                P.mm(rbank[0:m, 0:16], u32[:, k, s0:s0 + m], rw[:, k, :], k == 0, k == 7, ['u32', 'rw'], [rbk])
            P.act(rt['sc'][0:m, :], rbank[0:m, 0:16], AF.Sigmoid, [rbk], ['rt_sc', rbk])
            P.dve('tensor_tensor', ['rt_sc', 'rb'], ['rt_sl'], out=rt['sl'][0:m], in0=rt['sc'][0:m], in1=rb[0:m], op=ALU.add)
            P.dve('tensor_reduce', ['rt_sl'], ['r4_m1'], out=r4['m1'][0:m], in_=v4(rt['sl'])[0:m], axis=AX.X, op=ALU.max)
            P.dve('tensor_tensor', ['rt_sl', 'r4_m1'], ['rt_eq'], out=v4(rt['eq'])[0:m], in0=v4(rt['sl'])[0:m], in1=b4(r4['m1'])[0:m], op=ALU.is_equal)
            P.dve('scalar_tensor_tensor', ['rt_eq', 'rt_sl'], ['rt_s2'], out=rt['s2'][0:m], in0=rt['eq'][0:m], scalar=-1e9, in1=rt['sl'][0:m], op0=ALU.mult, op1=ALU.add)
            P.dve('tensor_reduce', ['rt_s2'], ['r4_m2'], out=r4['m2'][0:m], in_=v4(rt['s2'])[0:m], axis=AX.X, op=ALU.max)
            P.dve('tensor_tensor', ['r4_m1', 'r4_m2'], ['r4_gs'], out=r4['gs'][0:m], in0=r4['m1'][0:m], in1=r4['m2'][0:m], op=ALU.add)
            P.dve('tensor_reduce', ['r4_gs'], ['r1_gx'], out=r1['gx'][0:m], in_=r4['gs'][0:m], axis=AX.X, op=ALU.max)
            P.dve('tensor_scalar', ['r4_gs', 'r1_gx'], ['r4_gm'], out=r4['gm'][0:m], in0=r4['gs'][0:m], scalar1=r1['gx'][0:m, 0:1], scalar2=None, op0=ALU.is_equal)
            P.dve('tensor_tensor', ['rt_sl', 'r4_m2'], ['rt_ch'], out=v4(rt['ch'])[0:m], in0=v4(rt['sl'])[0:m], in1=b4(r4['m2'])[0:m], op=ALU.is_ge)
            P.dve('tensor_tensor', ['rt_ch', 'r4_gm'], ['rt_ch'], out=v4(rt['ch'])[0:m], in0=v4(rt['ch'])[0:m], in1=b4(r4['gm'])[0:m], op=ALU.mult)
            P.dve('tensor_tensor', ['rt_ch', 'rt_sc'], ['rt_w'], out=rt['w'][0:m], in0=rt['ch'][0:m], in1=rt['sc'][0:m], op=ALU.mult)
            P.dve('tensor_reduce', ['rt_w'], ['r1_ss'], out=r1['ss'][0:m], in_=rt['w'][0:m], axis=AX.X, op=ALU.add)
            P.dve('reciprocal', ['r1_ss'], ['r1_ss'], out=r1['ss'][0:m], in_=r1['ss'][0:m])
            P.dve('tensor_scalar', ['rt_w', 'r1_ss'], ['rt_w'], out=rt['w'][0:m], in0=rt['w'][0:m], scalar1=r1['ss'][0:m, 0:1], scalar2=None, op0=ALU.mult)
            P.op('pe', 'transpose', ['rt_w', 'ident'], [rbk], out=rbank[0:16, 128:128 + m], in_=rt['w'][0:m, :], identity=ident[0:m, 0:m])
            P.act(WT[:, c0 + s0:c0 + s0 + m], rbank[0:16, 128:128 + m], AF.Copy, [rbk], ['WT', rbk])
    barrier(P)
    while len(P.stack) > mark:
        P.stack.pop().__exit__(None, None, None)

    barrier(P)
    while len(P.stack) > mark:
        P.stack.pop().__exit__(None, None, None)
    hid = P.sb("hid", [128, 4, T], BF16)
    wbc = P.sb("wbc", [128, T], F32)
    WG = [P.sb("WG%d" % k, [128, 512], BF16) for k in range(8)]
    WU = [P.sb("WU%d" % k, [128, 512], BF16) for k in range(8)]
    WD = [P.sb("WD%d" % k, [128, 1024], BF16) for k in range(4)]
    sg = [P.sb("sg%d" % i, [128, 512], F32) for i in range(2)]
    bA = [banks[2], banks[3]]
    bB = [banks[4], banks[5]]
    bY = [banks[6], banks[7]]
    it = 0
    for e in range(nexp):
        for ti, (c0, c1, r) in enumerate(tiles):
            n = c1 - c0
            P.mm(ps_ssq[:, 0:n], sel[:, e, :], WT[:, c0:c1], True, True, ['sel', 'WT'], ['ps_ssq'])
            P.act(wbc[:, c0:c1], ps_ssq[:, 0:n], AF.Copy, ['ps_ssq'], ['wbc:%d' % ti, 'ps_ssq'])
        for hf in range(2):
            for k in range(8):
                P.dma('pool', writes=['WG%d' % k], out=WG[k][:], in_=wg_d[e, k * 128:(k + 1) * 128, hf * 512:(hf + 1) * 512])
                P.dma('pool', writes=['WU%d' % k], out=WU[k][:], in_=wu_d[e, k * 128:(k + 1) * 128, hf * 512:(hf + 1) * 512])
            for k in range(4):
                P.dma('pool', writes=['WD%d' % k], out=WD[k][:], in_=wd_d[e, hf * 512 + k * 128:hf * 512 + (k + 1) * 128, :])
            for fc in range(4):
                for ti, (c0, c1, r) in enumerate(tiles):
                    n = c1 - c0
                    i2 = it % 2
                    it += 1
                    for k in range(8):
                        P.mm(bA[i2][:, 0:n], WG[k][:, fc * 128:(fc + 1) * 128], fT[:, k, c0:c1], k == 0, k == 7, ['WG%d' % k, 'v:%d' % ti], ['bA%d' % i2])
                    for k in range(8):
                        P.mm(bB[i2][:, 0:n], WU[k][:, fc * 128:(fc + 1) * 128], fT[:, k, c0:c1], k == 0, k == 7, ['WU%d' % k, 'v:%d' % ti], ['bB%d' % i2])
                    P.act(sg[i2][:, 0:n], bA[i2][:, 0:n], AF.Silu, ['bA%d' % i2], ['sg%d' % i2, 'bA%d' % i2])
                    P.dve('tensor_tensor', ['bB%d' % i2, 'sg%d' % i2], ['sg%d' % i2, 'bB%d' % i2], out=sg[i2][:, 0:n], in0=bB[i2][:, 0:n], in1=sg[i2][:, 0:n], op=ALU.mult)
                    P.pool('tensor_tensor', ['sg%d' % i2, 'wbc:%d' % ti], ['hid:%d' % ti], out=hid[:, fc, c0:c1], in0=sg[i2][:, 0:n], in1=wbc[:, c0:c1], op=ALU.mult)
            for dc in range(8):
                for ti, (c0, c1, r) in enumerate(tiles):
                    n = c1 - c0
                    i2 = it % 2
                    it += 1
                    for k in range(4):
                        P.mm(bY[i2][:, 0:n], WD[k][:, dc * 128:(dc + 1) * 128], hid[:, k, c0:c1], k == 0, k == 3, ['WD%d' % k, 'hid:%d' % ti], ['bY%d' % i2])
                    P.dve('scalar_tensor_tensor', ['bY%d' % i2, 'modL', 'h:%d' % ti], ['h:%d' % ti, 'bY%d' % i2], out=hT[:, dc, c0:c1], in0=bY[i2][:, 0:n], scalar=mod[:, 40 + dc, r:r + 1], in1=hT[:, dc, c0:c1], op0=ALU.mult, op1=ALU.add)
    barrier(P)
    while len(P.stack) > mark:
        P.stack.pop().__exit__(None, None, None)

    if layer == 0:
        for k in range(8):
            P.dma('sp' if k % 2 == 0 else 'act', reads=['h:%d' % t for t in range(NT)], out=hT_out[:, k, :], in_=hT[:, k, :])
        mod1 = P.sb("sbmod1", [128, 48, 2], F32)
        P.dma('sp', writes=['mod1'], out=mod1[:], in_=mod1_in)
        A1 = emit_affine(P, mod1, 'mod1', gmix1, 1, 'm1')
        for ti, tile in enumerate(tiles):
            emit_modulate_tile(P, hT, 'h', A1, 'Am1', mod1, 'mod1', 0, ti, tile, ones_bf, ps_ssq, work, uT=fT, ukey='u1')
        C = ProjCtx(P, w_in1, fT, 'u1', banks[2:6], o_f, o_bf)
        C.job_lin(CD['hy'], 1536, A1_BF['hy'], 1.0)
        C.job_lin(CD['xbc'], 1024, A1_BF['xbc'], 1.0)
        C.job_silu(CD['z'], 512, A1_F['zs'], 'f')
        dtp = P.sb("dtp_sb", [16, 2], F32)
        negA = P.sb("negA", [16, 1], F32)
        P.dma('sp', writes=['dtp'], out=dtp[:], in_=dtp_d)
        P.act(negA[:], dtp[:, 1:2], AF.Exp, ['dtp'], ['negA'])
        P.dve('tensor_scalar', ['negA'], ['negA'], out=negA[:], in0=negA[:], scalar1=-1.0, scalar2=None, op0=ALU.mult)
        st1, sk1 = C.stage('f')
        st2, sk2 = C.stage('f')
        C.load_w(CD['dt'], 16)

        def post(ps, n, c0, c1, ti, bkey):
            P.act(st1[0:16, c0:c1], ps, AF.Exp, [bkey, 'dtp'], [sk1, bkey], bias=dtp[:, 0:1], scale=1.0)
            P.act(st1[0:16, c0:c1], st1[0:16, c0:c1], AF.Ln, [sk1], [sk1], bias=1.0, scale=1.0)
            P.dve('tensor_scalar', [sk1, 'negA'], [sk2], out=st2[0:16, c0:c1], in0=st1[0:16, c0:c1], scalar1=negA[:, 0:1], scalar2=None, op0=ALU.mult)
        C.chunk(CD['dt'], 16, post)
        C.outdma('f', st1, sk1, A1_F['dt'], 16)
        C.outdma('f', st2, sk2, A1_F['la'], 16)
    else:
        gout = P.sb("gout_sb", [128, 8], F32)
        P.dma('sp', writes=['gout'], out=gout[:], in_=gout_d)
        yst = [P.sb("yst%d" % i, [128, 8, 512], F32) for i in range(2)]
        for ti, (c0, c1, r) in enumerate(tiles):
            n = c1 - c0
            emit_rstd(P, hT[:, :, c0:c1], 'h:%d' % ti, n, 8, ones_bf, ps_ssq, work['sq'], work['rs'], 1.0 / 1024)
            for k in range(8):
                P.dve('scalar_tensor_tensor', ['h:%d' % ti, 'gout', 'rs'], ['yst%d' % (ti % 2)], out=yst[ti % 2][:, k, 0:n], in0=hT[:, k, c0:c1], scalar=gout[:, k:k + 1], in1=work['rs'][:, 0:n], op0=ALU.mult, op1=ALU.mult)
            P.dma('sp' if ti % 2 == 0 else 'act', reads=['yst%d' % (ti % 2)], out=y_out[:, :, c0:c1], in_=yst[ti % 2][:, :, 0:n])
    P.wait_all('sp')
    P.emit()
    P.close()
    return nc


def build_C2(layer, nexp=16, CAP=640):
    nc = bass.Bass("TRN2", target_bir_lowering=False)
    T = TL if layer == 0 else 2048
    tiles = TILES if layer == 0 else TILES[:4]
    NT = len(tiles)
    din = lambda name, shape, dt=F32: nc.dram_tensor(name, list(shape), dt, kind="ExternalInput").ap()
    dout = lambda name, shape, dt=F32: nc.dram_tensor(name, list(shape), dt, kind="ExternalOutput").ap()
    hT_in = din("hT_in", [128, 8, T])
    mod_in = din("mod_in", [128, 48, 2])
    gffn = din("gffn", [128, 8])
    w_out = din("w_out", [1024, 1024])
    NSUB = (T + 127) // 128
    NSL = CAP // 128
    I32 = mybir.dt.int32
    tris_d = din("tris", [128, 128], BF16)
    tokhl_d = din("tokhl", [128, NSUB, 2], BF16)
    iota_d = din("iota_s", [128, CAP])
    eoff_d = din("eoff", [128, 16])
    vtm = nc.dram_tensor("vtm_scr", [NSUB * 128, 1024], BF16, kind="Internal").ap()
    y_all = nc.dram_tensor("yall_scr", [16 * CAP, 1024], F32, kind="Internal").ap()
    rw_d = din("router_w", [128, 8, 16])
    rb_d = din("router_b", [128, 16])
    identd = din("ident", [128, 128])
    wg_d = din("moe_wg", [16, 1024, 1024])
    wu_d = din("moe_wu", [16, 1024, 1024])
    wd_d = din("moe_wd", [16, 1024, 1024])
    if layer == 0:
        oT_d = din("oT", [8, 128, 2, T])
        gate_d = din("gateT", [8, 128, T])
        ng_d = din("ng", [128, 2])
        mod1_in = din("mod1_in", [128, 48, 2])
        gmix1 = din("gmix1", [128, 8])
        w_in1 = din("w_in1", [1024, 3088])
        dtp_d = din("dtp", [16, 2])
        hT_out = dout("hT_out", [128, 8, T])
        o_bf = dout("o_bf", [A1_BF['end'], T], BF16)
        o_f = dout("o_f", [A1_F['end'], T])
    else:
        hy_d = din("hyT", [128, 4, T])
        so_d = din("ssd_oT", [2, 128, 4, T])
        xs_d = din("xsT", [128, 4, T])
        zs_d = din("zsT", [128, 4, T])
        dsk_d = din("dsk", [128, 4])
        mg_d = din("mbg", [128, 4])
        gout_d = din("gout", [128, 8])
        y_out = dout("y_out", [128, 8, T])

    P = Prog(nc)
    hT = P.sb("hT", [128, 8, T], F32)
    ones_bf = P.sb("ones_bf", [128, 128], BF16)
    ident = P.sb("ident_sb", [128, 128], F32)
    work = dict(sq=P.sb("sq", [128, 8, 512], BF16), rs=P.sb("rs", [128, 512], F32),
                tmp=[P.sb("tmp%d" % i, [128, 512], F32) for i in range(2)])
    banks = [P.ps("bank%d" % i, [128, 512], F32) for i in range(8)]
    pm, ps_ssq = banks[0], banks[1]
    for k in range(8):
        P.dma('sp' if k % 2 == 0 else 'act', writes=['h:%d' % t for t in range(NT)], out=hT[:, k, :], in_=hT_in[:, k, :])
    P.pool('memset', [], ['ones'], ap=ones_bf[:], constant=1.0)
    P.dma('sp', writes=['ident'], out=ident[:], in_=identd)
    mod = P.sb("sbmodL", [128, 48, 2], F32)
    P.dma('sp', writes=['modL'], out=mod[:], in_=mod_in)

    mark_f = len(P.stack)
    fT = P.sb("fT", [128, 8, T], BF16)
    mark = len(P.stack)
    if layer == 0:
        ng = P.sb("ng_sb", [128, 2], F32)
        P.dma('sp', writes=['ng'], out=ng[:], in_=ng_d)
        ob = [P.sb("ob%d" % i, [128, 2, 512], F32) for i in range(2)]
        gbf = [P.sb("gbf%d" % i, [128, 512], F32) for i in range(2)]
        osum = [P.sb("osum%d" % i, [128, 512], F32) for i in range(2)]
        it = 0
        for hh in range(8):
            for ti, (c0, c1, r) in enumerate(tiles):
                n = c1 - c0
                i2 = it % 2
                it += 1
                P.dma('sp', writes=['ob%d' % i2], out=ob[i2][:, :, 0:n], in_=oT_d[hh, :, :, c0:c1])
                P.dma('act', writes=['gbf%d' % i2], out=gbf[i2][:, 0:n], in_=gate_d[hh, :, c0:c1])
                P.pool('tensor_tensor', ['ob%d' % i2], ['osum%d' % i2], out=osum[i2][:, 0:n], in0=ob[i2][:, 0, 0:n], in1=ob[i2][:, 1, 0:n], op=ALU.add)
                emit_rstd(P, osum[i2][:, 0:n].unsqueeze(1), 'osum%d' % i2, n, 1, ones_bf, ps_ssq, work['sq'], work['rs'], 1.0 / 128)
                tb = work['tmp'][i2]
                P.dve('scalar_tensor_tensor', ['osum%d' % i2, 'ng', 'rs'], ['tmp%d' % i2], out=tb[:, 0:n], in0=osum[i2][:, 0:n], scalar=ng[:, hh // 4:hh // 4 + 1], in1=work['rs'][:, 0:n], op0=ALU.mult, op1=ALU.mult)
                P.pool('tensor_tensor', ['tmp%d' % i2, 'gbf%d' % i2], ['f:%d' % ti], out=fT[:, hh, c0:c1], in0=tb[:, 0:n], in1=gbf[i2][:, 0:n], op=ALU.mult)
    else:
        dsk = P.sb("dsk_sb", [128, 4], F32)
        mg = P.sb("mg_sb", [128, 4], F32)
        P.dma('sp', writes=['dsk'], out=dsk[:], in_=dsk_d)
        P.dma('sp', writes=['mg'], out=mg[:], in_=mg_d)
        lb4 = [P.sb("lb4_%d" % i, [128, 4, 512], F32) for i in range(3)]
        yb = P.sb("yb", [128, 4, 512], F32)
        for ti, (c0, c1, r) in enumerate(tiles):
            n = c1 - c0
            P.dma('sp', writes=['lb4_0'], out=lb4[0][:, :, 0:n], in_=hy_d[:, :, c0:c1])
            P.pool('tensor_copy', ['lb4_0'], ['f:%d' % ti], out=fT[:, 0:4, c0:c1], in_=lb4[0][:, :, 0:n])
            P.dma('sp', writes=['lb4_1'], out=lb4[1][:, :, 0:n], in_=so_d[0, :, :, c0:c1])
            P.dma('act', writes=['lb4_2'], out=lb4[2][:, :, 0:n], in_=so_d[1, :, :, c0:c1])
            P.pool('tensor_tensor', ['lb4_1', 'lb4_2'], ['yb'], out=yb[:, :, 0:n], in0=lb4[1][:, :, 0:n], in1=lb4[2][:, :, 0:n], op=ALU.add)
            P.dma('sp', writes=['lb4_1'], out=lb4[1][:, :, 0:n], in_=xs_d[:, :, c0:c1])
            P.dma('act', writes=['lb4_2'], out=lb4[2][:, :, 0:n], in_=zs_d[:, :, c0:c1])
            for j in range(4):
                P.dve('scalar_tensor_tensor', ['lb4_1', 'dsk', 'yb'], ['yb'], out=yb[:, j, 0:n], in0=lb4[1][:, j, 0:n], scalar=dsk[:, j:j + 1], in1=yb[:, j, 0:n], op0=ALU.mult, op1=ALU.add)
            P.pool('tensor_tensor', ['yb', 'lb4_2'], ['yb'], out=yb[:, :, 0:n], in0=yb[:, :, 0:n], in1=lb4[2][:, :, 0:n], op=ALU.mult)
            for g in range(2):
                emit_rstd(P, yb[:, 2 * g:2 * g + 2, 0:n], 'yb', n, 2, ones_bf, ps_ssq, work['sq'], work['rs'], 1.0 / 256)
                for j in (2 * g, 2 * g + 1):
                    P.dve('scalar_tensor_tensor', ['yb', 'mg', 'rs'], ['f:%d' % ti], out=fT[:, 4 + j, c0:c1], in0=yb[:, j, 0:n], scalar=mg[:, j:j + 1], in1=work['rs'][:, 0:n], op0=ALU.mult, op1=ALU.mult)
    barrier(P)
    while len(P.stack) > mark:
        P.stack.pop().__exit__(None, None, None)

    wo = P.sb("wo", [128, 8, 1024], BF16)
    wov = w_out.rearrange("(k p) n -> p k n", p=128)
    for k in range(8):
        P.dma('pool', writes=['wo'], out=wo[:, k, :], in_=wov[:, k, :])
    gcnt = [0]
    gbk = banks[2:6]

    def bank():
        i = gcnt[0] % 4
        gcnt[0] += 1
        return gbk[i], 'gb%d' % i
    for dc in range(8):
        for ti, (c0, c1, r) in enumerate(tiles):
            n = c1 - c0
            b, bk = bank()
            for k in range(8):
                P.mm(b[:, 0:n], wo[:, k, dc * 128:(dc + 1) * 128], fT[:, k, c0:c1], k == 0, k == 7, ['wo', 'f:%d' % ti], [bk])
            P.dve('scalar_tensor_tensor', [bk, 'modL', 'h:%d' % ti], ['h:%d' % ti, bk], out=hT[:, dc, c0:c1], in0=b[:, 0:n], scalar=mod[:, 16 + dc, r:r + 1], in1=hT[:, dc, c0:c1], op0=ALU.mult, op1=ALU.add)
    barrier(P)
    while len(P.stack) > mark_f:
        P.stack.pop().__exit__(None, None, None)

    Af = emit_affine(P, mod, 'modL', gffn, 4, 'f')
    CH1 = P.sb("CH1", [128, NSUB, 16], F32)
    CH2 = P.sb("CH2", [128, NSUB, 16], F32)
    WW = P.sb("WW", [128, NSUB, 16], F32)
    CHb = P.sb("CHb", [128, NSUB, 16], BF16)
    RP = P.sb("RP", [128, NSUB, 16], F32)
    tris = P.sb("tris_sb", [128, 128], BF16)
    tokhl = P.sb("tokhl_sb", [128, NSUB, 2], BF16)
    iota_s = P.sb("iota_sb", [128, CAP], F32)
    eoff = P.sb("eoff_sb", [128, 16], F32)
    for t_, d_, k_ in ((tris, tris_d, 'tris'), (tokhl, tokhl_d, 'tokhl'), (iota_s, iota_d, 'iota'), (eoff, eoff_d, 'eoff')):
        P.dma('sp', writes=[k_], out=t_[:], in_=d_)
    for t_, k_ in ((CH1, 'CH1'), (CH2, 'CH2'), (WW, 'WW'), (CHb, 'CHb')):
        P.pool('memset', [], [k_], ap=t_[:], constant=0.0)
    mark = len(P.stack)
    u32 = P.sb("u32", [128, 8, 512], F32)
    rw = P.sb("rw", [128, 8, 16], F32)
    rb = P.sb("rb", [128, 16], F32)
    vrow = [P.sb("vrow%d" % i, [128, 1024], BF16) for i in range(2)]
    P.dma('sp', writes=['rw'], out=rw[:], in_=rw_d)
    P.dma('sp', writes=['rb'], out=rb[:], in_=rb_d)
    rt = {nm: P.sb("rt_" + nm, [128, 16], F32) for nm in ('sc', 'sl', 'eq', 's2', 'ch', 'w')}
    r4 = {nm: P.sb("r4_" + nm, [128, 4], F32) for nm in ('m1', 'm2', 'gs', 'gm')}
    r1 = {nm: P.sb("r1_" + nm, [128, 1], F32) for nm in ('gx', 'ss')}
    v4 = lambda t_: t_[:].rearrange("p (g j) -> p g j", j=4)
    b4 = lambda t_: t_[:].unsqueeze(2).to_broadcast([128, 4, 4])
    rbank, rbk = banks[6], 'rbank'
    jsub = 0
    for ti, tile in enumerate(tiles):
        c0, c1, r = tile
        n = c1 - c0
        emit_modulate_tile(P, hT, 'h', Af, 'Af', mod, 'modL', 3, ti, tile, ones_bf, ps_ssq, work, uT=None, ukey=None, u32=u32)
        for sub in range(n // 128 if n >= 128 else 1):
            m = min(128, n)
            s0 = sub * 128
            j = jsub
            jsub += 1
            vr, vk = vrow[j % 2], 'vrow%d' % (j % 2)
            for half in range(2):
                tb_, tk_ = banks[4 + half], 'tpb%d' % half
                for kk in range(4):
                    k = half * 4 + kk
                    P.op('pe', 'transpose', ['u32', 'ident'], [tk_], out=tb_[0:m, kk * 128:(kk + 1) * 128], in_=u32[:, k, s0:s0 + m], identity=ident[:])
                P.act(vr[0:m, half * 512:(half + 1) * 512], tb_[0:m, :], AF.Copy, [tk_], [vk, tk_])
            P.dma('sp' if j % 2 == 0 else 'act', reads=[vk], writes=['vtm'], out=vtm[j * 128:j * 128 + 128, :], in_=vr[:, :])
            for k in range(8):
                P.mm(rbank[0:m, 0:16], u32[:, k, s0:s0 + m], rw[:, k, :], k == 0, k == 7, ['u32', 'rw'], [rbk])
            P.act(rt['sc'][0:m, :], rbank[0:m, 0:16], AF.Sigmoid, [rbk], ['rt_sc', rbk])
            P.dve('tensor_tensor', ['rt_sc', 'rb'], ['rt_sl'], out=rt['sl'][0:m], in0=rt['sc'][0:m], in1=rb[0:m], op=ALU.add)
            P.dve('tensor_reduce', ['rt_sl'], ['r4_m1'], out=r4['m1'][0:m], in_=v4(rt['sl'])[0:m], axis=AX.X, op=ALU.max)
            P.dve('tensor_tensor', ['rt_sl', 'r4_m1'], ['rt_eq'], out=v4(rt['eq'])[0:m], in0=v4(rt['sl'])[0:m], in1=b4(r4['m1'])[0:m], op=ALU.is_equal)
            P.dve('scalar_tensor_tensor', ['rt_eq', 'rt_sl'], ['rt_s2'], out=rt['s2'][0:m], in0=rt['eq'][0:m], scalar=-1e9, in1=rt['sl'][0:m], op0=ALU.mult, op1=ALU.add)
            P.dve('tensor_reduce', ['rt_s2'], ['r4_m2'], out=r4['m2'][0:m], in_=v4(rt['s2'])[0:m], axis=AX.X, op=ALU.max)
            P.dve('tensor_tensor', ['r4_m1', 'r4_m2'], ['r4_gs'], out=r4['gs'][0:m], in0=r4['m1'][0:m], in1=r4['m2'][0:m], op=ALU.add)
            P.dve('tensor_reduce', ['r4_gs'], ['r1_gx'], out=r1['gx'][0:m], in_=r4['gs'][0:m], axis=AX.X, op=ALU.max)
            P.dve('tensor_scalar', ['r4_gs', 'r1_gx'], ['r4_gm'], out=r4['gm'][0:m], in0=r4['gs'][0:m], scalar1=r1['gx'][0:m, 0:1], scalar2=None, op0=ALU.is_equal)
            P.dve('tensor_tensor', ['rt_sl', 'r4_m2'], ['rt_ch'], out=v4(rt['ch'])[0:m], in0=v4(rt['sl'])[0:m], in1=b4(r4['m2'])[0:m], op=ALU.is_ge)
            P.dve('tensor_tensor', ['rt_ch', 'r4_gm'], ['rt_ch'], out=v4(rt['ch'])[0:m], in0=v4(rt['ch'])[0:m], in1=b4(r4['gm'])[0:m], op=ALU.mult)
            P.dve('tensor_tensor', ['rt_ch', 'rt_sc'], ['rt_w'], out=rt['w'][0:m], in0=rt['ch'][0:m], in1=rt['sc'][0:m], op=ALU.mult)
            P.dve('tensor_reduce', ['rt_w'], ['r1_ss'], out=r1['ss'][0:m], in_=rt['w'][0:m], axis=AX.X, op=ALU.add)
            P.dve('reciprocal', ['r1_ss'], ['r1_ss'], out=r1['ss'][0:m], in_=r1['ss'][0:m])
            P.dve('tensor_scalar', ['rt_w', 'r1_ss'], ['WW'], out=WW[0:m, j, :], in0=rt['w'][0:m], scalar1=r1['ss'][0:m, 0:1], scalar2=None, op0=ALU.mult)
            P.dve('tensor_tensor', ['rt_eq', 'r4_gm'], ['CH1'], out=CH1[0:m, j, :].rearrange("p (g j) -> p g j", j=4), in0=v4(rt['eq'])[0:m], in1=b4(r4['gm'])[0:m], op=ALU.mult)
            P.dve('tensor_tensor', ['rt_ch', 'CH1'], ['CH2'], out=CH2[0:m, j, :], in0=rt['ch'][0:m], in1=CH1[0:m, j, :], op=ALU.subtract)
            P.dve('tensor_copy', ['rt_ch'], ['CHb'], out=CHb[0:m, j, :], in_=rt['ch'][0:m])
    pbank, pbk = banks[6], 'rbank'
    for j in range(NSUB):
        P.mm(pbank[:, 0:16], tris[:], CHb[:, j, :], True, j == 0, ['tris', 'CHb'], [pbk])
        for jj in range(j):
            P.mm(pbank[:, 0:16], ones_bf[:], CHb[:, jj, :], False, jj == j - 1, ['ones', 'CHb'], [pbk])
        P.dve('tensor_tensor', [pbk, 'eoff'], ['RP', pbk], out=RP[:, j, :], in0=pbank[:, 0:16], in1=eoff[:], op=ALU.add)
    barrier(P)
    while len(P.stack) > mark:
        P.stack.pop().__exit__(None, None, None)

    identb = P.sb("identb_sb", [128, 128], BF16)
    P.dve('tensor_copy', ['ident'], ['identb'], out=identb[:], in_=ident[:])
    WG = [P.sb("WG%d" % k, [128, 1024], BF16) for k in range(8)]
    WU = [P.sb("WU%d" % k, [128, 1024], BF16) for k in range(8)]
    WD = [P.sb("WD%d" % k, [128, 1024], BF16) for k in range(8)]
    OH = [P.sb("OH%d" % i, [128, CAP], BF16) for i in range(3)]
    wst = [P.sb("wst%d" % i, [128, 1024], F32) for i in range(4)]
    wcnt = [0]
    pe_ = P.sb("pe_e", [128, NSUB], F32)
    hl = P.sb("hl", [2, CAP], F32)
    idxf = P.sb("idxf", [128, NSL], F32)
    hls = P.sb("hls", [128, 2 * NSL], F32)
    idxi = [P.sb("idxi%d" % i, [128, NSL], I32) for i in range(2)]
    xg = [P.sb("xg%d" % i, [128, 1024], BF16) for i in range(2)]
    xsT = P.sb("xsTe", [128, 8, CAP], BF16)
    hid = P.sb("hid", [128, 8, CAP], BF16)
    sg = [P.sb("sg%d" % i, [128, 512], F32) for i in range(2)]
    ys = [P.sb("ys%d" % i, [128, 1024], F32) for i in range(2)]
    invA, invB = banks[0], banks[1]
    bA = [banks[2], banks[3]]
    bB = [banks[4], banks[5]]
    bY = [banks[6], banks[7]]
    ntl = [(0, min(512, CAP))] + ([(512, CAP)] if CAP > 512 else [])

    def indirect(out, in_, idx_ap, reads, writes):
        n_ = P.dma_n['pool']
        P.dma_n['pool'] += 1
        si = n_ % P.ndma
        key = ('d', 'pool', si)
        val = 16 * (n_ // P.ndma + 1)
        waits = P._deps('pool', list(reads), list(writes))
        if n_ >= P.ndma and P.seen['pool'].get(key, 0) < val - 16:
            waits.append((key, val - 16))
            P.seen['pool'][key] = val - 16
        P.q['pool'].append((waits, ('indirect_dma_start', dict(out=out, out_offset=None, in_=in_, in_offset=bass.IndirectOffsetOnAxis(ap=idx_ap, axis=0))), key))
        P._commit((key, val), list(reads), list(writes))

    it = 0
    gcount = 0
    for e in range(nexp):
        for k in range(8):
            for (Wt_, wk_, src_) in ((WG, 'WG%d' % k, wg_d), (WU, 'WU%d' % k, wu_d)):
                wi = wcnt[0] % 4
                wcnt[0] += 1
                P.dma('sp' if wi % 2 == 0 else 'act', writes=['wst%d' % wi], out=wst[wi][:], in_=src_[e, k * 128:(k + 1) * 128, :])
                P.pool('tensor_copy', ['wst%d' % wi], [wk_], out=Wt_[k][:], in_=wst[wi][:])
        for k in range(8):
            wi = wcnt[0] % 4
            wcnt[0] += 1
            P.dma('sp' if wi % 2 == 0 else 'act', writes=['wst%d' % wi], out=wst[wi][:], in_=wd_d[e, k * 128:(k + 1) * 128, :])
            P.pool('tensor_copy', ['wst%d' % wi], ['WD%d' % k], out=WD[k][:], in_=wst[wi][:])
        P.dve('tensor_scalar', ['RP'], ['pe_e'], out=pe_[:], in0=RP[:, :, e], scalar1=float(1 - e * CAP), scalar2=None, op0=ALU.add)
        P.dve('tensor_tensor', ['pe_e', 'CHb'], ['pe_e'], out=pe_[:], in0=pe_[:], in1=CHb[:, :, e], op=ALU.mult)
        P.dve('tensor_scalar', ['pe_e'], ['pe_e'], out=pe_[:], in0=pe_[:], scalar1=-1.0, scalar2=None, op0=ALU.add)
        for j in range(NSUB):
            oh, ok_ = OH[j % 3], 'OH%d' % (j % 3)
            P.dve('tensor_scalar', ['iota', 'pe_e'], [ok_], out=oh[:], in0=iota_s[:], scalar1=pe_[:, j:j + 1], scalar2=None, op0=ALU.is_equal)
            for ni, (n0, n1) in enumerate(ntl):
                bk_, bkk = (invA, 'invA') if ni == 0 else (invB, 'invB')
                P.mm(bk_[0:2, 0:n1 - n0], tokhl[:, j, :], oh[:, n0:n1], j == 0, j == NSUB - 1, ['tokhl', ok_], [bkk])
        for ni, (n0, n1) in enumerate(ntl):
            bk_, bkk = (invA, 'invA') if ni == 0 else (invB, 'invB')
            P.act(hl[:, n0:n1], bk_[0:2, 0:n1 - n0], AF.Copy, [bkk], ['hl', bkk])
        for st in range(NSL):
            P.op('pe', 'transpose', ['hl', 'ident'], ['invB'], out=invB[:, 256 + 2 * st:256 + 2 * st + 2], in_=hl[0:2, st * 128:(st + 1) * 128], identity=ident[0:2, 0:2])
        P.act(hls[:], invB[:, 256:256 + 2 * NSL], AF.Copy, ['invB'], ['hls', 'invB'])
        hv = hls[:].rearrange("p (s c) -> p s c", c=2)
        P.dve('scalar_tensor_tensor', ['hls'], ['idxf'], out=idxf[:], in0=hv[:, :, 0], scalar=64.0, in1=hv[:, :, 1], op0=ALU.mult, op1=ALU.add)
        ii, ik = idxi[e % 2], 'idxi%d' % (e % 2)
        P.dve('tensor_copy', ['idxf'], [ik], out=ii[:], in_=idxf[:])
        for st in range(NSL):
            g_, gk_ = xg[gcount % 2], 'xg%d' % (gcount % 2)
            gcount += 1
            indirect(g_[:], vtm[:, :], ii[:, st:st + 1], [ik, 'vtm'], [gk_])
            tb_, tk_ = bA[st % 2], 'bA%d' % (st % 2)
            tbb = tb_[:].bitcast(BF16)
            for k in range(8):
                P.op('pe', 'transpose', [gk_, 'identb'], [tk_], out=tbb[:, k * 128:(k + 1) * 128], in_=g_[:, k * 128:(k + 1) * 128], identity=identb[:])
            P.act(xsT[:, :, st * 128:(st + 1) * 128], tbb[:, 0:1024].rearrange("p (k s) -> p k s", k=8), AF.Copy, [tk_], ['xsT', tk_])
        for fc in range(8):
            for (n0, n1) in ntl:
                n = n1 - n0
                i2 = it % 2
                it += 1
                for k in range(8):
                    P.mm(bA[i2][:, 0:n], WG[k][:, fc * 128:(fc + 1) * 128], xsT[:, k, n0:n1], k == 0, k == 7, ['WG%d' % k, 'xsT'], ['bA%d' % i2])
                for k in range(8):
                    P.mm(bB[i2][:, 0:n], WU[k][:, fc * 128:(fc + 1) * 128], xsT[:, k, n0:n1], k == 0, k == 7, ['WU%d' % k, 'xsT'], ['bB%d' % i2])
                P.act(sg[i2][:, 0:n], bA[i2][:, 0:n], AF.Silu, ['bA%d' % i2], ['sg%d' % i2, 'bA%d' % i2])
                P.dve('tensor_tensor', ['bB%d' % i2, 'sg%d' % i2], ['hid', 'bB%d' % i2], out=hid[:, fc, n0:n1], in0=bB[i2][:, 0:n], in1=sg[i2][:, 0:n], op=ALU.mult)
        for st in range(NSL):
            y_, yk = ys[st % 2], 'ys%d' % (st % 2)
            for dh in range(2):
                i2 = it % 2
                it += 1
                for k in range(8):
                    P.mm(bY[i2][:, :], hid[:, k, st * 128:(st + 1) * 128], WD[k][:, dh * 512:(dh + 1) * 512], k == 0, k == 7, ['WD%d' % k, 'hid'], ['bY%d' % i2])
                P.act(y_[:, dh * 512:(dh + 1) * 512], bY[i2][:, :], AF.Copy, ['bY%d' % i2], [yk, 'bY%d' % i2])
            P.dma('sp' if st % 2 == 0 else 'act', reads=[yk], writes=['yall'], out=y_all[e * CAP + st * 128:e * CAP + (st + 1) * 128, :], in_=y_[:])
    barrier(P)
    while len(P.stack) > mark:
        P.stack.pop().__exit__(None, None, None)

    gk2 = [P.sb("gk%d" % i, [128, 1024], F32) for i in range(4)]
    ytok = [P.sb("ytok%d" % i, [128, 1024], F32) for i in range(2)]
    t16 = P.sb("t16", [128, 16], F32)
    cs = {nm: P.sb("cs_" + nm, [128, 1], F32) for nm in ('row0', 'row1', 'w0', 'w1', 'eo', 'vl')}
    rowi = [P.sb("rowi%d" % i, [128, 1], I32) for i in range(4)]
    for j in range(NSUB):
        if layer == 0 and j == NSUB - 1:
            m, c0, r, ti = 64, 2048, 1, 4
        else:
            m, c0, r, ti = 128, j * 128, 0, j // 4
        for kk, CHk in enumerate((CH1, CH2)):
            rw_, w_ = cs['row%d' % kk], cs['w%d' % kk]
            P.dve('tensor_tensor', ['CH1', 'CH2', 'RP'], ['t16'], out=t16[:], in0=CHk[:, j, :], in1=RP[:, j, :], op=ALU.mult)
            P.dve('tensor_reduce', ['t16'], ['cs_row%d' % kk], out=rw_[:], in_=t16[:], axis=AX.X, op=ALU.add)
            P.dve('tensor_tensor', ['CH1', 'CH2', 'eoff'], ['t16'], out=t16[:], in0=CHk[:, j, :], in1=eoff[:], op=ALU.mult)
            P.dve('tensor_reduce', ['t16'], ['cs_eo'], out=cs['eo'][:], in_=t16[:], axis=AX.X, op=ALU.add)
            P.dve('tensor_tensor', ['CH1', 'CH2', 'WW'], ['t16'], out=t16[:], in0=CHk[:, j, :], in1=WW[:, j, :], op=ALU.mult)
            P.dve('tensor_reduce', ['t16'], ['cs_w%d' % kk], out=w_[:], in_=t16[:], axis=AX.X, op=ALU.add)
            P.dve('tensor_tensor', ['cs_row%d' % kk, 'cs_eo'], ['cs_row%d' % kk], out=rw_[:], in0=rw_[:], in1=cs['eo'][:], op=ALU.subtract)
            P.dve('tensor_scalar', ['cs_row%d' % kk], ['cs_vl'], out=cs['vl'][:], in0=rw_[:], scalar1=float(CAP) - 0.5, scalar2=None, op0=ALU.is_lt)
            P.dve('tensor_tensor', ['cs_w%d' % kk, 'cs_vl'], ['cs_w%d' % kk], out=w_[:], in0=w_[:], in1=cs['vl'][:], op=ALU.mult)
            P.dve('tensor_scalar', ['cs_row%d' % kk], ['cs_row%d' % kk], out=rw_[:], in0=rw_[:], scalar1=float(CAP - 1), scalar2=None, op0=ALU.min)
            P.dve('tensor_tensor', ['cs_row%d' % kk, 'cs_eo'], ['cs_row%d' % kk], out=rw_[:], in0=rw_[:], in1=cs['eo'][:], op=ALU.add)
            ri, rk_ = rowi[(2 * j + kk) % 4], 'rowi%d' % ((2 * j + kk) % 4)
            P.dve('tensor_copy', ['cs_row%d' % kk], [rk_], out=ri[:], in_=rw_[:])
            g_, gk_ = gk2[(2 * j + kk) % 4], 'gk%d' % ((2 * j + kk) % 4)
            indirect(g_[:], y_all[:, :], ri[:, 0:1], [rk_, 'yall'], [gk_])
        g0, g1 = gk2[(2 * j) % 4], gk2[(2 * j + 1) % 4]
        yt, ytk = ytok[j % 2], 'ytok%d' % (j % 2)
        P.dve('tensor_scalar', ['gk%d' % ((2 * j) % 4), 'cs_w0'], [ytk], out=yt[:], in0=g0[:], scalar1=cs['w0'][:, 0:1], scalar2=None, op0=ALU.mult)
        P.dve('scalar_tensor_tensor', ['gk%d' % ((2 * j + 1) % 4), 'cs_w1', ytk], [ytk], out=yt[:], in0=g1[:], scalar=cs['w1'][:, 0:1], in1=yt[:], op0=ALU.mult, op1=ALU.add)
        pb = (bA, bB)[j % 2]
        pk = ('bA%d', 'bB%d')[j % 2]
        for k in range(8):
            bb_, bbk = pb[k // 4], pk % (k // 4)
            P.op('pe', 'transpose', [ytk, 'ident'], [bbk], out=bb_[:, (k % 4) * 128:(k % 4) * 128 + m], in_=yt[0:m, k * 128:(k + 1) * 128], identity=ident[0:m, 0:m])
        for k in range(8):
            bb_, bbk = pb[k // 4], pk % (k // 4)
            P.dve('scalar_tensor_tensor', [bbk, 'modL', 'h:%d' % ti], ['h:%d' % ti, bbk], out=hT[:, k, c0:c0 + m], in0=bb_[:, (k % 4) * 128:(k % 4) * 128 + m], scalar=mod[:, 40 + k, r:r + 1], in1=hT[:, k, c0:c0 + m], op0=ALU.mult, op1=ALU.add)
    barrier(P)
    while len(P.stack) > mark:
        P.stack.pop().__exit__(None, None, None)

    if layer == 0:
        for k in range(8):
            P.dma('sp' if k % 2 == 0 else 'act', reads=['h:%d' % t for t in range(NT)], out=hT_out[:, k, :], in_=hT[:, k, :])
        mod1 = P.sb("sbmod1", [128, 48, 2], F32)
        P.dma('sp', writes=['mod1'], out=mod1[:], in_=mod1_in)
        A1 = emit_affine(P, mod1, 'mod1', gmix1, 1, 'm1')
        uT1 = P.sb("uT1", [128, 8, T], BF16)
        for ti, tile in enumerate(tiles):
            emit_modulate_tile(P, hT, 'h', A1, 'Am1', mod1, 'mod1', 0, ti, tile, ones_bf, ps_ssq, work, uT=uT1, ukey='u1')
        C = ProjCtx(P, w_in1, uT1, 'u1', banks[2:6], o_f, o_bf)
        C.job_lin(CD['hy'], 1536, A1_BF['hy'], 1.0)
        C.job_lin(CD['xbc'], 1024, A1_BF['xbc'], 1.0)
        C.job_silu(CD['z'], 512, A1_F['zs'], 'f')
        dtp = P.sb("dtp_sb", [16, 2], F32)
        negA = P.sb("negA", [16, 1], F32)
        P.dma('sp', writes=['dtp'], out=dtp[:], in_=dtp_d)
        P.act(negA[:], dtp[:, 1:2], AF.Exp, ['dtp'], ['negA'])
        P.dve('tensor_scalar', ['negA'], ['negA'], out=negA[:], in0=negA[:], scalar1=-1.0, scalar2=None, op0=ALU.mult)
        st1, sk1 = C.stage('f')
        st2, sk2 = C.stage('f')
        C.load_w(CD['dt'], 16)

        def post(ps, n, c0, c1, ti, bkey):
            P.act(st1[0:16, c0:c1], ps, AF.Exp, [bkey, 'dtp'], [sk1, bkey], bias=dtp[:, 0:1], scale=1.0)
            P.act(st1[0:16, c0:c1], st1[0:16, c0:c1], AF.Ln, [sk1], [sk1], bias=1.0, scale=1.0)
            P.dve('tensor_scalar', [sk1, 'negA'], [sk2], out=st2[0:16, c0:c1], in0=st1[0:16, c0:c1], scalar1=negA[:, 0:1], scalar2=None, op0=ALU.mult)
        C.chunk(CD['dt'], 16, post)
        C.outdma('f', st1, sk1, A1_F['dt'], 16)
        C.outdma('f', st2, sk2, A1_F['la'], 16)
    else:
        gout = P.sb("gout_sb", [128, 8], F32)
        P.dma('sp', writes=['gout'], out=gout[:], in_=gout_d)
        yst = [P.sb("yst%d" % i, [128, 8, 512], F32) for i in range(2)]
        for ti, (c0, c1, r) in enumerate(tiles):
            n = c1 - c0
            emit_rstd(P, hT[:, :, c0:c1], 'h:%d' % ti, n, 8, ones_bf, ps_ssq, work['sq'], work['rs'], 1.0 / 1024)
            for k in range(8):
                P.dve('scalar_tensor_tensor', ['h:%d' % ti, 'gout', 'rs'], ['yst%d' % (ti % 2)], out=yst[ti % 2][:, k, 0:n], in0=hT[:, k, c0:c1], scalar=gout[:, k:k + 1], in1=work['rs'][:, 0:n], op0=ALU.mult, op1=ALU.mult)
            P.dma('sp' if ti % 2 == 0 else 'act', reads=['yst%d' % (ti % 2)], out=y_out[:, :, c0:c1], in_=yst[ti % 2][:, :, 0:n])
    P.wait_all('sp')
    P.emit()
    P.close()
    return nc


def local_from_joint(a, q):
    return np.concatenate([a[256 + 2048 * q:256 + 2048 * (q + 1)], a[64 * q:64 * (q + 1)]], axis=0)


def tm_to_joint(o):
    return o.transpose(1, 0, 2).reshape(NCH * 64, o.shape[2])


CAP_ = 640


def tokhl_const(nsub):
    p = np.arange(128)[:, None]
    j = np.arange(nsub)[None, :]
    tid = j * 128 + p
    return np.ascontiguousarray(np.stack([tid // 64, tid % 64], axis=-1).astype(np.float32).astype(NPBF))


def common_C_inputs(inp, layer):
    sel = np.zeros((16, 16, 128), np.float32)
    for e in range(16):
        sel[e, e, :] = 1.0
    return dict(
        gffn=colT(inp['norm_ffn_g'][layer], 8),
        router_w=np.ascontiguousarray(inp['router_w'].reshape(8, 128, 16).transpose(1, 0, 2)),
        router_b=np.ascontiguousarray(np.broadcast_to(inp['router_b'][None, :], (128, 16))),
        sel16=sel, ident=np.eye(128, dtype=np.float32),
        tris=np.triu(np.ones((128, 128), np.float32), 1).astype(NPBF),
        iota_s=np.ascontiguousarray(np.broadcast_to(np.arange(CAP_, dtype=np.float32)[None, :], (128, CAP_))),
        eoff=np.ascontiguousarray(np.broadcast_to((np.arange(16, dtype=np.float32) * CAP_)[None, :], (128, 16))),
        tokhl=tokhl_const(17 if layer == 0 else 16),
        moe_wg=inp['moe_w_gate'][layer], moe_wu=inp['moe_w_up'][layer], moe_wd=inp['moe_w_down'][layer])


def host_C0_inputs(inp, a0, b0):
    maps = []
    com = common_C_inputs(inp, 0)
    ng = np.ascontiguousarray(np.stack([inp['gla_norm_g'][0], inp['hg_norm_g'][0]], axis=1))
    dtp = np.ascontiguousarray(np.stack([inp['mb_dt_bias'][0].reshape(16), inp['mb_a_log'][0].reshape(16)], axis=1))
    for i in range(NCORES):
        b, q = i // 4, i % 4
        oT = np.empty((8, 128, 2, TL), np.float32)
        for hh in range(8):
            src = b0[4 * b + hh % 4]
            for d in range(2):
                o = tm_to_joint(src[('g_o%d' if hh < 4 else 'h_o%d') % d])
                oT[hh, :, d, :] = local_from_joint(o, q).T
        m = dict(com)
        m.update(hT_in=fm(core_tokens(inp['x'], inp['ctx'], i)), mod_in=a0[i]['mods_out'][0], mod1_in=a0[i]['mods_out'][1], w_out=inp['ab_w_out'][0],
                 oT=oT, gateT=np.ascontiguousarray(a0[i]['o_f'][0:1024].reshape(8, 128, TL)), ng=ng,
                 gmix1=colT(inp['norm_mix_g'][1], 8),
                 w_in1=inp['cd_w_in'][0], dtp=dtp)
        maps.append(m)
    return maps


NFFT = 16384
PI = float(np.pi)


def fft_consts():
    a = np.arange(128, dtype=np.float64)
    th = 2 * np.pi * np.outer(a, a) / 128.0
    tw = 2 * np.pi * np.outer(a, a) / NFFT
    c, s = np.cos(th), np.sin(th)
    F64 = np.concatenate([c[:64], -s[:64]], axis=1)
    Fst = np.stack([c, -s, s], axis=1)
    G12 = np.stack([np.concatenate([c, s], 1), np.concatenate([-s, c], 1)], axis=1)
    Gst = np.stack([c[:, :64] / NFFT, -s[:, :64] / NFFT], axis=1)
    tr, ti = np.cos(tw), -np.sin(tw)
    TW = np.stack([np.tile(np.concatenate([tr, tr], 1), (1, 2)), np.tile(np.concatenate([ti, ti], 1), (1, 2))], axis=1)
    pr, pi_ = np.cos(tw), np.sin(tw)
    TWI = np.stack([np.tile(np.concatenate([pr, pr], 1), (1, 2)), np.tile(np.concatenate([pi_, pi_], 1), (1, 2))], axis=1)
    return dict(F64=F64.astype(NPBF), Fst=Fst.astype(NPBF), G12=G12.astype(NPBF), Gst=Gst.astype(NPBF),
                TW=TW.astype(np.float32), TWI=TWI.astype(np.float32))


def build_B1(nscan_steps=None, hy_groups=32, do_ssd=True, do_hy=True):
    nc = bass.Bass("TRN2", target_bir_lowering=False)
    T = NCH * 64
    din = lambda name, shape, dt=F32: nc.dram_tensor(name, list(shape), dt, kind="ExternalInput").ap()
    dout = lambda name, shape, dt=F32: nc.dram_tensor(name, list(shape), dt, kind="ExternalOutput").ap()
    dscr = lambda name, shape, dt=F32: nc.dram_tensor(name, list(shape), dt, kind="Internal").ap()
    tri_d = din("tri", [64, 6, 64])
    ident_d = din("identb", [128, 128], BF16)
    TP = 258 + 8194
    xin_d = din("ssd_xin", [128, 3, TP], BF16)
    scw_d = din("ssd_cw", [128, 3, 4])
    dttm_d = din("ssd_dttm", [64, NCH, 4])
    latm_d = [din("ssd_latm%d" % k, [64, NCH, 128]) for k in range(4)]
    s_qT = dscr("s_qT", [128, T], BF16)
    s_kT = dscr("s_kT", [128, T], BF16)
    s_ktm = dscr("s_ktm", [64, NCH, 128], BF16)
    s_vtm = [dscr("s_vtm%d" % k, [64, NCH, 64], BF16) for k in range(4)]
    ssd_o = [dout("ssd_o%d" % k, [64, NCH, 64]) for k in range(4)]
    xs_out = dout("xs_out", [128, T])
    hy_in_d = din("hy_in", [128, 3, 8194], BF16)
    hcw_d = din("hy_cw", [128, 3, 4])
    zT_d = din("hy_zT", [33, 8192])
    w1_d = din("hy_w1", [33, 64])
    w2_d = din("hy_w2", [64, 64])
    w3_d = din("hy_w3", [64, 4, 128])
    fp_d = din("hy_fp", [64, 3])
    rate_d = din("hy_rate", [64, 128])
    negt_d = din("hy_negt", [64, 128])
    hb_d = din("hy_bias", [64, 2, 128])
    F64_d = din("F64", [64, 256], BF16)
    Fst_d = din("Fst", [128, 3, 128], BF16)
    G12_d = din("G12", [128, 2, 256], BF16)
    Gst_d = din("Gst", [128, 2, 64], BF16)
    TW_d = din("TW", [128, 2, 512])
    TWI_d = din("TWI", [128, 2, 512])
    s_hy = dscr("s_hy", [3, 128, 8192], BF16)
    s_kf = dscr("s_kf", [2, 32, 128, 4, 2, 128])
    hy_out = dout("hy_out", [64, 128, 128])

    P = Prog(nc)
    banks = [P.ps("bank%d" % i, [128, 512], F32) for i in range(8)]
    dq = [0]

    def q3():
        dq[0] += 1
        return ['sp', 'act', 'pool'][dq[0] % 3]

    def dwconv(x, xkey, cw, part, col0, n, acc, acckey):
        P.act(acc[:, 0:n], x[:, part, col0 + 1:col0 + 1 + n], AF.Identity, [xkey, 'cw'], [acckey], scale=cw[:, part, 1:2], bias=cw[:, part, 3:4])
        P.dve('scalar_tensor_tensor', [xkey, 'cw', acckey], [acckey], out=acc[:, 0:n], in0=x[:, part, col0:col0 + n], scalar=cw[:, part, 0:1], in1=acc[:, 0:n], op0=ALU.mult, op1=ALU.add)
        P.dve('scalar_tensor_tensor', [xkey, 'cw', acckey], [acckey], out=acc[:, 0:n], in0=x[:, part, col0 + 2:col0 + 2 + n], scalar=cw[:, part, 2:3], in1=acc[:, 0:n], op0=ALU.mult, op1=ALU.add)

    if do_hy:
        mark0 = len(P.stack)
        xin = P.sb("hy_xin", [128, 3, 8194], BF16)
        cw = P.sb("hy_cw_sb", [128, 3, 4], F32)
        acc = [P.sb("hy_acc%d" % i, [128, 2048], F32) for i in range(2)]
        accb = [P.sb("hy_accb%d" % i, [128, 2048], BF16) for i in range(2)]
        for p in range(3):
            P.dma(q3(), writes=['hyxin'], out=xin[:, p, :], in_=hy_in_d[:, p, :])
        P.dma('sp', writes=['cw'], out=cw[:], in_=hcw_d)
        it = 0
        for p in range(3):
            for blk in range(4):
                i2 = it % 2
                it += 1
                dwconv(xin, 'hyxin', cw, p, blk * 2048, 2048, acc[i2], 'hyacc%d' % i2)
                P.pool('tensor_copy', ['hyacc%d' % i2], ['hyaccb%d' % i2], out=accb[i2][:], in_=acc[i2][:])
                P.dma(q3(), reads=['hyaccb%d' % i2], writes=['s_hy'], out=s_hy[p, :, blk * 2048:(blk + 1) * 2048], in_=accb[i2][:])
        barrier(P)
        while len(P.stack) > mark0:
            P.stack.pop().__exit__(None, None, None)

        F64 = P.sb("F64_sb", [64, 256], BF16)
        Fst = P.sb("Fst_sb", [128, 3, 128], BF16)
        G12 = P.sb("G12_sb", [128, 2, 256], BF16)
        Gst = P.sb("Gst_sb", [128, 2, 64], BF16)
        TW = P.sb("TW_sb", [128, 2, 512], F32)
        TWI = P.sb("TWI_sb", [128, 2, 512], F32)
        for t_, d_ in ((F64, F64_d), (Fst, Fst_d), (G12, G12_d), (Gst, Gst_d), (TW, TW_d), (TWI, TWI_d)):
            P.dma(q3(), writes=['fftc'], out=t_[:], in_=d_)
        m1 = [P.sb("m1_%d" % i, [128, 512], F32) for i in range(2)]
        m2 = [P.sb("m2_%d" % i, [128, 512], F32) for i in range(2)]
        Bt = [P.sb("Bt%d" % i, [128, 4, 2, 128], BF16) for i in range(2)]
        cnt = {'a': 0, 'b': 0}

        def twiddle(psrc, pkey, table, dst, dkey, pair):
            i2 = cnt['a'] % 2
            cnt['a'] += 1
            a1, a2 = m1[i2], m2[i2]
            P.dve('tensor_tensor', [pkey, 'fftc'], ['m1_%d' % i2, pkey], out=a1[:], in0=psrc, in1=table[:, 0, :], op=ALU.mult)
            yield
            P.dve('tensor_tensor', [pkey, 'fftc'], ['m2_%d' % i2, pkey], out=a2[:], in0=psrc, in1=table[:, 1, :], op=ALU.mult)
            yield
            v1 = a1[:].rearrange("p (c h k) -> p c h k", c=2, h=2)
            v2 = a2[:].rearrange("p (c h k) -> p c h k", c=2, h=2)
            P.pool('tensor_tensor', ['m1_%d' % i2, 'm2_%d' % i2], [dkey + 'r%d' % pair], out=dst[:, pair * 2:pair * 2 + 2, 0, :], in0=v1[:, :, 0, :], in1=v2[:, :, 1, :], op=ALU.subtract)
            yield
            P.pool('tensor_tensor', ['m1_%d' % i2, 'm2_%d' % i2], [dkey + 'i%d' % pair], out=dst[:, pair * 2:pair * 2 + 2, 1, :], in0=v2[:, :, 0, :], in1=v1[:, :, 1, :], op=ALU.add)
            yield

        def interleave(*gens):
            gens = [g_ for g_ in gens if g_ is not None]
            while gens:
                for g_ in list(gens):
                    try:
                        next(g_)
                    except StopIteration:
                        gens.remove(g_)

        def fft_fwd4(src, skey, c0, bXr, kXr, bXi, kXi):
            interleave(fft_fwd4_g(src, skey, c0, bXr, kXr, bXi, kXi))

        def fft_fwd4_g(src, skey, c0, bXr, kXr, bXi, kXi, extra=()):
            i2 = cnt['b'] % 2
            cnt['b'] += 1
            B = Bt[i2]
            bk = 'Bt%d' % i2
            for pair in range(2):
                pa, pk = banks[pair], 'psA%d' % pair
                for cc in range(2):
                    P.mm(pa[:, cc * 256:(cc + 1) * 256], src[0:64, c0 + pair * 2 + cc, :], F64[:], True, True, [skey, 'fftc'] + list(extra), [pk])
                yield
                yield from twiddle(pa[:], pk, TW, B, bk, pair)
            rk = [bk + 'r0', bk + 'r1', bk + 'i0', bk + 'i1', 'fftc']
            Br = B[:, :, 0, :]
            Bi = B[:, :, 1, :]
            P.mm(bXr[:], Fst[:, 0, :], Br, True, False, rk, [kXr])
            P.mm(bXr[:], Fst[:, 2, :], Bi, False, True, rk, [kXr])
            yield
            P.mm(bXi[:], Fst[:, 1, :], Br, True, False, rk, [kXi])
            P.mm(bXi[:], Fst[:, 0, :], Bi, False, True, rk, [kXi])
            yield

        mark1 = len(P.stack)
        zT = P.sb("zT_sb", [33, 8192], F32)
        w1 = P.sb("w1_sb", [33, 64], F32)
        w2 = P.sb("w2_sb", [64, 64], F32)
        w3 = P.sb("w3_sb", [64, 4, 128], F32)
        fp = P.sb("fp_sb", [64, 3], F32)
        fb = P.sb("fb_sb", [64, 2], F32)
        rate = P.sb("rate_sb", [64, 128], F32)
        negt = P.sb("negt_sb", [64, 128], F32)
        h1 = P.sb("h1_sb", [64, 8192], F32)
        h2 = P.sb("h2_sb", [64, 8192], F32)
        for t_, d_, k_ in ((zT, zT_d, 'zT'), (w1, w1_d, 'w1'), (w2, w2_d, 'w2'), (w3, w3_d, 'w3'), (fp, fp_d, 'fp'), (rate, rate_d, 'rate'), (negt, negt_d, 'negt')):
            P.dma(q3(), writes=[k_], out=t_[:], in_=d_)
        P.dve('tensor_scalar', ['fp'], ['fb'], out=fb[:], in0=fp[:, 1:3], scalar1=fp[:, 0:1], scalar2=None, op0=ALU.mult)
        gbank, gk = banks[6], 'gbank'
        wrp = [P.sb("wrp%d" % i, [64, 512], F32) for i in range(2)]
        for lyr, (wsb, wk, src, sk, dst, dk_, K_) in enumerate(((w1, 'w1', zT, 'zT', h1, 'h1', 33), (w2, 'w2', h1, 'h1', h2, 'h2', 64))):
            for blk in range(16):
                cs = slice(blk * 512, (blk + 1) * 512)
                P.mm(gbank[0:64, :], wsb[0:K_, :], src[0:K_, cs], True, True, [wk, sk], [gk])
                P.dve('tensor_scalar', [gk, 'fp', 'fb'], [dk_, gk], out=dst[:, cs], in0=gbank[0:64, :], scalar1=fp[:, 0:1], scalar2=fb[:, lyr:lyr + 1], op0=ALU.mult, op1=ALU.add)
                wa, wb_ = wrp[0][:, 0:512], wrp[1][:, 0:512]
                P.dve('tensor_scalar', [dk_], ['wrp0'], out=wa, in0=dst[:, cs], scalar1=PI, scalar2=-2 * PI, op0=ALU.is_gt, op1=ALU.mult)
                P.dve('tensor_scalar', [dk_], ['wrp1'], out=wb_, in0=dst[:, cs], scalar1=-PI, scalar2=2 * PI, op0=ALU.is_lt, op1=ALU.mult)
                P.dve('tensor_tensor', ['wrp0', 'wrp1'], ['wrp0'], out=wa, in0=wa, in1=wb_, op=ALU.add)
                P.dve('tensor_tensor', ['wrp0', dk_], [dk_], out=dst[:, cs], in0=dst[:, cs], in1=wa, op=ALU.add)
                P.dve('tensor_scalar', [dk_], [dk_], out=dst[:, cs], in0=dst[:, cs], scalar1=3.1415925, scalar2=-3.1415925, op0=ALU.min, op1=ALU.max)
                P.act(dst[:, cs], dst[:, cs], AF.Sin, [dk_], [dk_])
        hf = [P.sb("hf%d" % d, [64, 128, 128], BF16) for d in range(2)]
        dec = [P.sb("dec%d" % i, [64, 128], F32) for i in range(2)]
        kst = [P.sb("kst%d" % i, [128, 4, 2, 128], F32) for i in range(2)]
        xev = [P.sb("xev%d" % i, [128, 512], F32) for i in range(2)]
        h2v = h2[:].rearrange("j (a b) -> j a b", b=128)
        for o in range(2):
            for n2 in range(128):
                i2 = n2 % 2
                P.act(dec[i2][:], rate[:], AF.Exp, ['rate', 'negt'], ['dec%d' % i2], scale=negt[:, n2:n2 + 1])
                for d in range(2):
                    P.mm(gbank[0:64, 0:128], h2v[:, :, n2], w3[:, o * 2 + d, :], True, True, ['h2', 'w3'], [gk])
                    P.dve('tensor_tensor', [gk, 'dec%d' % i2], ['hf%d' % d, gk], out=hf[d][:, :, n2], in0=gbank[0:64, 0:128], in1=dec[i2][:], op=ALU.mult)
            for g in range(hy_groups):
                fft_fwd4(hf[0], 'hf0', g * 4, banks[2], 'bX0', banks[3], 'bX1')
                fft_fwd4(hf[1], 'hf1', g * 4, banks[4], 'bX2', banks[5], 'bX3')
                i2 = g % 2
                ks = kst[i2]
                kk = 'kst%d' % i2
                P.act(xev[0][:], banks[4][:], AF.Copy, ['bX2'], ['xev0', 'bX2'])
                P.act(xev[1][:], banks[5][:], AF.Copy, ['bX3'], ['xev1', 'bX3'])
                P.dve('tensor_tensor', ['bX0', 'xev0'], [kk, 'bX0'], out=ks[:, :, 0, :], in0=banks[2][:].rearrange("p (c k) -> p c k", c=4), in1=xev[0][:].rearrange("p (c k) -> p c k", c=4), op=ALU.add)
                P.dve('tensor_tensor', ['bX1', 'xev1'], [kk, 'bX1'], out=ks[:, :, 1, :], in0=banks[3][:].rearrange("p (c k) -> p c k", c=4), in1=xev[1][:].rearrange("p (c k) -> p c k", c=4), op=ALU.subtract)
                P.dma(q3(), reads=[kk], writes=['s_kf'], out=s_kf[o, g], in_=ks[:])
        barrier(P)
        while len(P.stack) > mark1:
            P.stack.pop().__exit__(None, None, None)

        xv = P.sb("xv", [64, 128, 128], BF16)
        x1 = P.sb("x1", [64, 128, 128], BF16)
        x2 = P.sb("x2", [64, 128, 128], BF16)
        hbias = P.sb("hbias", [64, 2, 128], F32)
        for p, t_ in enumerate((xv, x1, x2)):
            for hh in range(2):
                P.dma(q3(), reads=['s_hy'], writes=['xd%d' % p], out=t_[:, hh * 64:(hh + 1) * 64, :], in_=s_hy[p, hh * 64:(hh + 1) * 64, :].rearrange("c (a b) -> a c b", b=128))
        P.dma('sp', writes=['hbias'], out=hbias[:], in_=hb_d)
        kfb = [P.sb("kfb%d" % i, [128, 4, 2, 128], F32) for i in range(2)]
        Yt = [P.sb("Yt%d" % i, [128, 4, 2, 128], BF16) for i in range(2)]
        Dt = [P.sb("Dt%d" % i, [128, 4, 2, 128], BF16) for i in range(2)]
        pw = [P.sb("pw%d" % i, [128, 512], F32) for i in range(4)]
        ep = [P.sb("ep%d" % i, [64, 512], F32) for i in range(2)]
        ost = [P.sb("ost%d" % i, [64, 4, 128], F32) for i in range(2)]
        def fwd_gen(o, g):
            i2 = g % 2
            kf, kfk = kfb[i2], 'kfb%d' % i2
            P.dma(q3(), reads=['s_kf'], writes=[kfk], out=kf[:], in_=s_kf[o, g])
            yield from fft_fwd4_g(xv, 'xd0', g * 4, banks[2], 'bX0', banks[3], 'bX1', extra=['zz:%d' % g])
            Xr = banks[2][:].rearrange("p (c k) -> p c k", c=4)
            Xi = banks[3][:].rearrange("p (c k) -> p c k", c=4)
            Y = Yt[i2]
            yk = 'Yt%d' % i2
            pwv = [pw[i][:].rearrange("p (c k) -> p c k", c=4) for i in range(4)]
            P.dve('tensor_tensor', ['bX0', kfk, 'zz:%d' % g], ['pw0', 'bX0'], out=pwv[0], in0=Xr, in1=kf[:, :, 0, :], op=ALU.mult)
            yield
            P.dve('tensor_tensor', ['bX1', kfk], ['pw1', 'bX1'], out=pwv[1], in0=Xi, in1=kf[:, :, 1, :], op=ALU.mult)
            yield
            P.dve('tensor_tensor', ['bX0', kfk], ['pw2', 'bX0'], out=pwv[2], in0=Xr, in1=kf[:, :, 1, :], op=ALU.mult)
            yield
            P.dve('tensor_tensor', ['bX1', kfk], ['pw3', 'bX1'], out=pwv[3], in0=Xi, in1=kf[:, :, 0, :], op=ALU.mult)
            yield
            P.pool('tensor_tensor', ['pw0', 'pw1'], [yk], out=Y[:, :, 0, :], in0=pwv[0], in1=pwv[1], op=ALU.subtract)
            yield
            P.pool('tensor_tensor', ['pw2', 'pw3'], [yk], out=Y[:, :, 1, :], in0=pwv[2], in1=pwv[3], op=ALU.add)
            yield

        def inv_gen(o, g):
            i2 = g % 2
            Y = Yt[i2]
            yk = 'Yt%d' % i2
            D = Dt[i2]
            dk_ = 'Dt%d' % i2
            for pair in range(2):
                pc, pck = banks[4 + pair], 'psC%d' % pair
                for cc in range(2):
                    c = pair * 2 + cc
                    P.mm(pc[:, cc * 256:(cc + 1) * 256], Y[:, c, 0, :], G12[:, 0, :], True, False, [yk, 'fftc'], [pck])
                    P.mm(pc[:, cc * 256:(cc + 1) * 256], Y[:, c, 1, :], G12[:, 1, :], False, True, [yk, 'fftc'], [pck])
                    yield
                yield from twiddle(pc[:], pck, TWI, D, dk_, pair)
            rk = [dk_ + 'r0', dk_ + 'r1', dk_ + 'i0', dk_ + 'i1', 'fftc']
            yb, ybk = banks[6], 'ybank'
            P.mm(yb[0:64, :], Gst[:, 0, :], D[:, :, 0, :], True, False, rk, [ybk])
            P.mm(yb[0:64, :], Gst[:, 1, :], D[:, :, 1, :], False, True, rk, [ybk])
            yield
            cs = slice(g * 4, g * 4 + 4)
            e_ = ep[i2]
            ek = 'ep%d' % i2
            ev = e_[:].rearrange("p (c k) -> p c k", c=4)
            P.pool('tensor_tensor', ['xd0', 'zz:%d' % g, 'hbias'], [ek], out=ev, in0=xv[:, cs, :], in1=hbias[:, o, cs].unsqueeze(2).to_broadcast([64, 4, 128]), op=ALU.mult)
            yield
            P.dve('tensor_tensor', [ybk, ek], [ek, ybk], out=e_[:], in0=yb[0:64, :], in1=e_[:], op=ALU.add)
            yield
            if o == 0:
                P.dve('tensor_tensor', [ek, 'xd1'], ['zz:%d' % g], out=xv[:, cs, :], in0=ev, in1=x1[:, cs, :], op=ALU.mult)
            else:
                os_ = ost[i2]
                P.dve('tensor_tensor', [ek, 'xd2'], ['ost%d' % i2], out=os_[:], in0=ev, in1=x2[:, cs, :], op=ALU.mult)
                P.dma(q3(), reads=['ost%d' % i2], out=hy_out[:, cs, :], in_=os_[:])
            yield

        prev = None
        for o in range(2):
            for g in range(hy_groups):
                interleave(fwd_gen(o, g), prev)
                prev = inv_gen(o, g)
        interleave(prev)
        barrier(P)
        while len(P.stack) > mark0:
            P.stack.pop().__exit__(None, None, None)

    if do_ssd:
        mark2 = len(P.stack)
        cw = P.sb("ssd_cw_sb", [128, 3, 4], F32)
        identb = P.sb("identb_sb", [128, 128], BF16)
        ybf = P.sb("ssd_ybf", [128, 3, T], BF16)
        dttm = P.sb("dttm_sb", [64, NCH, 4], F32)
        mark3 = len(P.stack)
        xin = P.sb("ssd_xin_sb", [128, 3, TP], BF16)
        acc = [P.sb("ssd_acc%d" % i, [128, 2048], F32) for i in range(2)]
        for p in range(3):
            P.dma(q3(), writes=['sxin'], out=xin[:, p, :], in_=xin_d[:, p, :])
        P.dma('sp', writes=['cw'], out=cw[:], in_=scw_d)
        P.dma('sp', writes=['identb'], out=identb[:], in_=ident_d)
        P.dma('sp', writes=['dttm'], out=dttm[:], in_=dttm_d)
        segs = [(0, 0, 256)] + [(258 + i * 2048, 256 + i * 2048, 2048) for i in range(4)]
        it = 0
        for p in range(3):
            for (pc0, oc0, n) in segs:
                i2 = it % 2
                it += 1
                dwconv(xin, 'sxin', cw, p, pc0, n, acc[i2], 'sacc%d' % i2)
                P.act(acc[i2][:, 0:n], acc[i2][:, 0:n], AF.Silu, ['sacc%d' % i2], ['sacc%d' % i2])
                P.pool('tensor_copy', ['sacc%d' % i2], ['ybf%d' % p], out=ybf[:, p, oc0:oc0 + n], in_=acc[i2][:, 0:n])
                if p == 0:
                    P.dma(q3(), reads=['sacc%d' % i2], out=xs_out[:, oc0:oc0 + n], in_=acc[i2][:, 0:n])
        P.dma(q3(), reads=['ybf2'], writes=['s_qT'], out=s_qT, in_=ybf[:, 2, :])
        P.dma(q3(), reads=['ybf1'], writes=['s_kT'], out=s_kT, in_=ybf[:, 1, :])
        barrier(P)
        while len(P.stack) > mark3:
            P.stack.pop().__exit__(None, None, None)
        btm = P.sb("btm", [64, NCH, 128], BF16)
        xtm = P.sb("xtm", [64, NCH, 128], BF16)
        vt = [P.sb("vt%d" % i, [64, NCH, 64], BF16) for i in range(2)]
        tb = [banks[0], banks[1]]
        for part, dst, dk_ in ((1, btm, 'btm'), (0, xtm, 'xtm')):
            for c4 in range(NCH // 4):
                i2 = c4 % 2
                bkb = tb[i2][0:64, :].bitcast(BF16)
                for j in range(4):
                    c = c4 * 4 + j
                    P.op('pe', 'transpose', ['ybf%d' % part, 'identb'], ['tb%d' % i2], out=bkb[:, j * 128:(j + 1) * 128], in_=ybf[:, part, c * 64:(c + 1) * 64], identity=identb[:])
                P.act(dst[:, c4 * 4:(c4 + 1) * 4, :], bkb[:, 0:512].rearrange("p (c k) -> p c k", c=4), AF.Copy, ['tb%d' % i2], [dk_, 'tb%d' % i2])
        P.dma(q3(), reads=['btm'], writes=['s_ktm'], out=s_ktm, in_=btm[:])
        for k in range(4):
            d, j = k // 2, k % 2
            v_ = vt[k % 2]
            P.dve('tensor_tensor', ['xtm', 'dttm'], ['vt%d' % (k % 2)], out=v_[:], in0=xtm[:, :, j * 64:(j + 1) * 64], in1=dttm[:, :, k:k + 1].to_broadcast([64, NCH, 64]), op=ALU.mult)
            P.dma(q3(), reads=['vt%d' % (k % 2)], writes=['s_vtm%d' % k], out=s_vtm[k], in_=v_[:])
        barrier(P)
        while len(P.stack) > mark2:
            P.stack.pop().__exit__(None, None, None)
        tri = scan_consts(P, tri_d)
        scans = []
        for k in range(4):
            d, j = k // 2, k % 2
            scans.append(dict(dk=128, dv=64, mode='scalar', rev=(d == 1), qT=s_qT, kT=s_kT, ktm=s_ktm, vtm=s_vtm[k], latm=latm_d[k], o=ssd_o[k],
                              rkeys=['s_qT', 's_kT', 's_ktm', 's_vtm%d' % k], okey='ssd_out%d' % k))
        emit_scans(P, scans, tri, banks, nscan_steps)
    P.wait_all('sp')
    P.emit()
    P.close()
    return nc


HY_MIN_DECAY = float(np.log(1e-2) / 1.5)
HY_MAX_DECAY = float(np.log(1e-2) / 0.3)


def pad1(a):
    return np.concatenate([np.zeros_like(a[:, :1]), a, np.zeros_like(a[:, :1])], axis=1)


def hyena_pos_consts():
    n = 8192
    t = np.linspace(0.0, 1.0, n, dtype=np.float32)[:, None]
    bands = np.linspace(1e-4, 15, 16, dtype=np.float32)
    ang = (np.float32(2.0 * np.pi / n) * np.arange(n, dtype=np.float32)[:, None]) * bands
    z = np.concatenate([t, np.cos(ang), -np.sin(ang)], axis=-1).astype(np.float32)
    rates = np.abs(np.linspace(HY_MIN_DECAY, HY_MAX_DECAY, 512, dtype=np.float32))
    negt = -(np.arange(8192, dtype=np.float32) / np.float32(8191.0)).reshape(64, 128)
    return np.ascontiguousarray(z.T), rates, np.ascontiguousarray(negt)


def host_B1_inputs(inp, c0):
    obf = [r['o_bf'] for r in c0]
    of = [r['o_f'] for r in c0]
    fc = fft_consts()
    zT, rates, negt = hyena_pos_consts()
    tri = tri_consts()
    identb = np.eye(128, dtype=np.float32).astype(NPBF)
    cwall = np.concatenate([inp['mb_conv_w'][0], inp['mb_conv_b'][0][:, None]], axis=1)
    hcwall = np.concatenate([inp['hy_short_w'][0], inp['hy_short_b'][0][:, None]], axis=1)
    maps = []
    for i in range(NCORES):
        b, q = i // 4, i % 4
        g, hp = q // 2, q % 2
        h0 = 4 * g + 2 * hp
        rows = [64 * h0, 512 + 128 * g, 768 + 128 * g]
        m = dict(tri=tri, identb=identb)
        m.update(fc)
        xin = []
        for r0 in rows:
            a = joint_fm(obf, A1_BF['xbc'] + r0, 128, b)
            xin.append(np.concatenate([pad1(a[:, :256]), pad1(a[:, 256:])], axis=1))
        m['ssd_xin'] = np.ascontiguousarray(np.stack(xin, axis=1))
        m['ssd_cw'] = np.ascontiguousarray(np.stack([cwall[r0:r0 + 128] for r0 in rows], axis=1))
        dt4, la4 = [], []
        for k in range(4):
            d, j = k // 2, k % 2
            dt4.append(to_tm(joint_fm(of, A1_F['dt'] + d * 8 + h0 + j, 1, b)))
            la = to_tm(joint_fm(of, A1_F['la'] + d * 8 + h0 + j, 1, b))
            m['ssd_latm%d' % k] = np.ascontiguousarray(np.broadcast_to(la, (64, NCH, 128)))
        m['ssd_dttm'] = np.ascontiguousarray(np.concatenate(dt4, axis=2))
        hy = [pad1(joint_fm(obf, A1_BF['hy'] + p * 512 + 128 * q, 128, b)[:, 256:]) for p in range(3)]
        m['hy_in'] = np.ascontiguousarray(np.stack(hy, axis=1))
        m['hy_cw'] = np.ascontiguousarray(np.stack([hcwall[p * 512 + 128 * q:p * 512 + 128 * q + 128] for p in range(3)], axis=1))
        m['hy_zT'] = zT
        m['hy_w1'] = inp['hy_w1'][0]
        m['hy_w2'] = inp['hy_w2'][0]
        m['hy_w3'] = np.ascontiguousarray(inp['hy_w3'][0].reshape(64, 4, 512)[:, :, 128 * q:128 * q + 128])
        m['hy_fp'] = np.ascontiguousarray(np.stack([inp['hy_freq'][0], inp['hy_b1'][0], inp['hy_b2'][0]], axis=1))
        m['hy_rate'] = np.ascontiguousarray(np.broadcast_to(rates[128 * q:128 * q + 128][None, :], (64, 128)))
        m['hy_negt'] = negt
        m['hy_bias'] = np.ascontiguousarray(np.broadcast_to(inp['hy_bias'][0][:, 128 * q:128 * q + 128][None], (64, 2, 128)))
        maps.append(m)
    return maps


def host_C1_inputs(inp, a0, c0, b1):
    maps = []
    com = common_C_inputs(inp, 1)
    dsk = np.ascontiguousarray(np.repeat(inp['mb_d'][0], 64).reshape(4, 128).T)
    for i in range(NCORES):
        b, q = i // 4, i % 4
        ls = slice(256 + 2048 * q, 256 + 2048 * (q + 1))
        hyT = np.empty((128, 4, 2048), np.float32)
        soT = np.empty((2, 128, 4, 2048), np.float32)
        xsT = np.empty((128, 4, 2048), np.float32)
        for j in range(4):
            src = b1[4 * b + j]
            hy_ct = src['hy_out'].transpose(1, 0, 2).reshape(128, 8192)
            hyT[:, j, :] = hy_ct[:, 2048 * q:2048 * (q + 1)]
            xsT[:, j, :] = src['xs_out'][:, ls]
            for d in range(2):
                for jj in range(2):
                    o = tm_to_joint(src['ssd_o%d' % (d * 2 + jj)])
                    soT[d, jj * 64:(jj + 1) * 64, j, :] = o[ls].T
        m = dict(com)
        m.update(hT_in=np.ascontiguousarray(c0[i]['hT_out'][:, :, 0:2048]), mod_in=a0[i]['mods_out'][1], w_out=inp['cd_w_out'][0],
                 hyT=hyT, ssd_oT=soT, xsT=xsT,
                 zsT=np.ascontiguousarray(c0[i]['o_f'][0:512, 0:2048].reshape(4, 128, 2048).transpose(1, 0, 2)),
                 dsk=dsk, mbg=colT(inp['mb_norm_g'][0], 4), gout=colT(inp['norm_out_g'], 8))
        maps.append(m)
    return maps


_CACHE = {}


def _prog(name, fn):
    if name not in _CACHE:
        _CACHE[name] = fn()
    return _CACHE[name]


def _run(nc, maps):
    res = run_bass_kernel_spmd(nc, maps, core_ids=list(range(NCORES)))
    return [dict(r) for r in res.results]


def kernel(**inputs):
    inp = {k: np.asarray(v) for k, v in inputs.items()}
    a0 = _run(_prog('A0', build_A0), host_A0_inputs(inp))
    b0 = _run(_prog('B0', build_B0), host_B0_inputs(a0))
    c0 = _run(_prog('C0', lambda: build_C2(0)), host_C0_inputs(inp, a0, b0))
    b1 = _run(_prog('B1', build_B1), host_B1_inputs(inp, c0))
    c1 = _run(_prog('C1', lambda: build_C2(1)), host_C1_inputs(inp, a0, c0, b1))
    out = np.empty((2, 8192, 1024), np.float32)
    for i in range(NCORES):
        b, q = i // 4, i % 4
        out[b, 2048 * q:2048 * (q + 1), :] = c1[i]['y_out'].transpose(2, 1, 0).reshape(2048, 1024)
    return out
```

```python
import numpy as np
import ml_dtypes
import concourse.bass as bass
import concourse.mybir as mybir
from concourse.bass_utils import run_bass_kernel_spmd

F32 = mybir.dt.float32
BF16 = mybir.dt.bfloat16
AF = mybir.ActivationFunctionType
ALU = mybir.AluOpType
AX = mybir.AxisListType
NPBF = ml_dtypes.bfloat16

ENGS = ['pe', 'act', 'dve', 'pool', 'sp']
SAME_ENGINE_SYNC = True
NCORES = 8
TL = 2112
TILES = [(0, 512, 0), (512, 1024, 0), (1024, 1536, 0), (1536, 2048, 0), (2048, 2112, 1)]
EPS = 1e-6


class Prog:
    def __init__(self, nc, ndma_sems=6):
        self.nc = nc
        self.q = {e: [] for e in ENGS}
        self.cnt = {e: 0 for e in ENGS}
        self.seen = {e: {} for e in ENGS}
        self.lastw = {}
        self.reads = {}
        self.ndma = ndma_sems
        self.dma_n = {e: 0 for e in ENGS}
        self.stack = []
        self.uid = 0

    def enter(self, cm):
        v = cm.__enter__()
        self.stack.append(cm)
        return v

    def sb(self, name, shape, dt):
        return self.enter(self.nc.sbuf_tensor(name, list(shape), dt))

    def ps(self, name, shape, dt=F32):
        return self.enter(self.nc.psum_tensor(name, list(shape), dt))

    def close(self):
        while self.stack:
            self.stack.pop().__exit__(None, None, None)

    def _deps(self, eng, reads, writes):
        toks = set()
        for r in reads:
            t = self.lastw.get(r)
            if t is not None:
                toks.add(t)
        for w in writes:
            t = self.lastw.get(w)
            if t is not None:
                toks.add(t)
            for t in self.reads.get(w, ()):
                toks.add(t)
        need = {}
        for (k, v) in toks:
            if k == eng and (eng == 'pe' or not SAME_ENGINE_SYNC):
                continue
            if self.seen[eng].get(k, 0) >= v:
                continue
            if need.get(k, 0) < v:
                need[k] = v
        for k, v in need.items():
            self.seen[eng][k] = v
        return list(need.items())

    def _commit(self, tok, reads, writes):
        for r in reads:
            if r in writes:
                continue
            self.reads.setdefault(r, []).append(tok)
        for w in writes:
            self.lastw[w] = tok
            self.reads[w] = []

    def op(self, eng, name, reads=(), writes=(), **kw):
        reads = list(reads)
        writes = list(writes)
        waits = self._deps(eng, reads, writes)
        self.cnt[eng] += 1
        tok = (eng, self.cnt[eng])
        self.q[eng].append((waits, (name, kw), ('c', eng)))
        self._commit(tok, reads, writes)

    def mm(self, out, lhsT, rhs, start, stop, reads, writes):
        self.op('pe', 'matmul', reads, writes, out=out, lhsT=lhsT, rhs=rhs, start=start, stop=stop)

    def act(self, out, in_, func, reads, writes, **kw):
        self.op('act', 'activation', reads, writes, out=out, in_=in_, func=func, **kw)

    def dve(self, name, reads, writes, **kw):
        self.op('dve', name, reads, writes, **kw)

    def pool(self, name, reads, writes, **kw):
        self.op('pool', name, reads, writes, **kw)

    def dma(self, eng, reads=(), writes=(), **kw):
        fn = ('dma_start', kw)
        reads = list(reads)
        writes = list(writes)
        n = self.dma_n[eng]
        self.dma_n[eng] += 1
        si = n % self.ndma
        key = ('d', eng, si)
        val = 16 * (n // self.ndma + 1)
        waits = self._deps(eng, reads, writes)
        if n >= self.ndma and self.seen[eng].get(key, 0) < val - 16:
            waits.append((key, val - 16))
            self.seen[eng][key] = val - 16
        self.q[eng].append((waits, fn, key))
        self._commit((key, val), reads, writes)

    def wait_all(self, eng):
        need = {}
        for tok in self.lastw.values():
            k, v = tok
            if need.get(k, 0) < v:
                need[k] = v
        for toks in self.reads.values():
            for k, v in toks:
                if need.get(k, 0) < v:
                    need[k] = v
        waits = [(k, v) for k, v in need.items() if self.seen[eng].get(k, 0) < v and (k != eng or eng != 'sp')]
        for k, v in waits:
            self.seen[eng][k] = v
        self.q[eng].append((waits, None, None))

    def emit(self):
        nc = self.nc
        used = set()
        for e in ENGS:
            for waits, fn, inc in self.q[e]:
                for k, _ in waits:
                    used.add(k)
                if inc is not None:
                    used.add(inc if inc[0] == 'd' else inc[1])
        sem = {}
        for i, k in enumerate(sorted(used, key=str)):
            sem[k] = self.enter(nc.semaphore("sem%d" % i))
        block = self.enter(nc.Block())
        q = self.q

        def run(e, engine):
            for waits, fn, inc in q[e]:
                for k, v in waits:
                    engine.wait_ge(sem[k], v)
                if fn is None:
                    continue
                inst = getattr(engine, fn[0])(**fn[1])
                if inc[0] == 'd':
                    inst.then_inc(sem[inc], 16)
                else:
                    inst.then_inc(sem[inc[1]], 1)

        @block.tensor
        def _(eng):
            run('pe', eng)

        @block.scalar
        def _(eng):
            run('act', eng)

        @block.vector
        def _(eng):
            run('dve', eng)

        @block.gpsimd
        def _(eng):
            run('pool', eng)

        @block.sync
        def _(eng):
            run('sp', eng)


def emit_mods(P, condT, ada_w_l, adab, pm, tag):
    cond = P.sb("sbcond" + tag, [128, 8, 2], F32)
    sc = P.sb("sbsc" + tag, [128, 8, 2], F32)
    adab_sb = P.sb("sbadab" + tag, [128, 48], F32)
    mod = P.sb("sbmod" + tag, [128, 48, 2], F32)
    mark_ = len(P.stack)
    wb = [P.sb("sbadaw%d" % i + tag, [128, 8, 256], F32) for i in range(2)]
    P.dma('sp', writes=['cond' + tag], out=cond[:], in_=condT)
    P.dma('sp', writes=['adab' + tag], out=adab_sb[:], in_=adab)
    P.act(sc[:], cond[:], AF.Silu, ['cond' + tag], ['sc' + tag])
    wv = ada_w_l.rearrange("(k p) n -> p k n", p=128)
    for s in range(24):
        buf = wb[s % 2]
        bk = 'adaw%d' % (s % 2) + tag
        P.dma('sp' if s % 2 == 0 else 'act', writes=[bk], out=buf[:], in_=wv[:, :, s * 256:(s + 1) * 256])
        for m4 in range(2):
            m = s * 2 + m4
            for k in range(8):
                P.mm(pm[:, 2 * m:2 * m + 2], buf[:, k, m4 * 128:(m4 + 1) * 128], sc[:, k, :], k == 0, k == 7, [bk, 'sc' + tag], ['pm' + tag])
    P.dve('tensor_tensor', ['pm' + tag, 'adab' + tag], ['mod' + tag], out=mod[:], in0=pm[:, 0:96].rearrange("p (m r) -> p m r", r=2),
          in1=adab_sb[:].unsqueeze(2).to_broadcast([128, 48, 2]), op=ALU.add)
    barrier(P)
    while len(P.stack) > mark_:
        P.stack.pop().__exit__(None, None, None)
    return mod


def emit_affine(P, mod, modkey, gT_dram, kind_scale, tag):
    g = P.sb("sbg" + tag, [128, 8], F32)
    A = P.sb("sbA" + tag, [128, 8, 2], F32)
    P.dma('sp', writes=['g' + tag], out=g[:], in_=gT_dram)
    P.dve('scalar_tensor_tensor', [modkey, 'g' + tag], ['A' + tag], out=A[:], in0=mod[:, kind_scale * 8:kind_scale * 8 + 8, :], scalar=1.0,
          in1=g[:].unsqueeze(2).to_broadcast([128, 8, 2]), op0=ALU.add, op1=ALU.mult)
    return A


def emit_rstd(P, src, srckey, n, nk, ones_bf, ps_ssq, sq, rs, inv_n):
    P.act(sq[:, 0:nk, 0:n], src, AF.Square, [srckey], ['sq'])
    for k in range(nk):
        P.mm(ps_ssq[:, 0:n], ones_bf[:], sq[:, k, 0:n], k == 0, k == nk - 1, ['sq', 'ones'], ['ps_ssq'])
    P.dve('tensor_scalar', ['ps_ssq'], ['rs'], out=rs[:, 0:n], in0=ps_ssq[:, 0:n], scalar1=float(inv_n), scalar2=EPS, op0=ALU.mult, op1=ALU.add)
    P.act(rs[:, 0:n], rs[:, 0:n], AF.Sqrt, ['rs'], ['rs'])
    P.dve('reciprocal', ['rs'], ['rs'], out=rs[:, 0:n], in_=rs[:, 0:n])


def emit_modulate_tile(P, hT, hkey, A, Akey, mod, modkey, kind_shift, ti, tile, ones_bf, ps_ssq, work, uT=None, ukey=None, u32=None):
    sq, rs, tmp = work['sq'], work['rs'], work['tmp']
    c0, c1, r = tile
    n = c1 - c0
    emit_rstd(P, hT[:, :, c0:c1], hkey + ':%d' % ti, n, 8, ones_bf, ps_ssq, sq, rs, 1.0 / 1024)
    for k in range(8):
        tb = tmp[k % 2]
        tk = 'tmp%d' % (k % 2)
        P.dve('scalar_tensor_tensor', [hkey + ':%d' % ti, Akey, 'rs'], [tk], out=tb[:, 0:n], in0=hT[:, k, c0:c1], scalar=A[:, k, r:r + 1], in1=rs[:, 0:n], op0=ALU.mult, op1=ALU.mult)
        sh = mod[:, kind_shift * 8 + k, r:r + 1]
        if u32 is not None:
            P.act(u32[:, k, 0:n], tb[:, 0:n], AF.Identity, [tk, modkey], ['u32'], bias=sh, scale=1.0)
            if uT is not None:
                P.pool('tensor_copy', ['u32'], [ukey + ':%d' % ti], out=uT[:, k, c0:c1], in_=u32[:, k, 0:n])
        else:
            P.act(uT[:, k, c0:c1], tb[:, 0:n], AF.Identity, [tk, modkey], [ukey + ':%d' % ti], bias=sh, scale=1.0)


AB = dict(gq=0, gk=256, gv=512, gg=1024, glr_f=1536, glr_b=1552, hq=1568, hf_f=2080, hf_b=2592, hi=3104, hg=3616, end=4128)
A0_BF = dict(gq=0, gk=256, gv=512, hq=1024, hk_f=1536, hk_b=2048, hi=2560, end=3072)
A0_F = dict(gg=0, hg=512, gla_f=1024, gla_b=1280, hla_f=1536, hla_b=2048, end=2560)


class ProjCtx:
    def __init__(self, P, w_dram, uT, ukey, gb, o_f, o_bf, nst=3):
        self.P, self.uT, self.ukey, self.gb, self.o_f, self.o_bf = P, uT, ukey, gb, o_f, o_bf
        self.wv = w_dram.rearrange("(k p) n -> p k n", p=128)
        self.wblk = [P.sb("wblk%d" % i, [128, 8, 512], BF16) for i in range(2)]
        self.nw = 0
        self.W = None
        self.nst = nst
        self.stg_f = [P.sb("stgf%d" % i, [128, TL], F32) for i in range(nst)]
        self.stg_b = [P.sb("stgb%d" % i, [128, TL], BF16) for i in range(nst)]
        self.cnt = {'f': 0, 'b': 0, 'g': 0, 'q': 0}

    def stage(self, kind):
        i = self.cnt[kind] % self.nst
        self.cnt[kind] += 1
        return ((self.stg_f if kind == 'f' else self.stg_b)[i], 'stg%s%d' % (kind, i))

    def bank(self):
        i = self.cnt['g'] % len(self.gb)
        self.cnt['g'] += 1
        return self.gb[i], 'gb%d' % i

    def outdma(self, kind, st, sk, row0, nrows, ncols=TL):
        dst = (self.o_f if kind == 'f' else self.o_bf)
        q = ['sp', 'act'][self.cnt['q'] % 2]
        self.cnt['q'] += 1
        self.P.dma(q, reads=[sk], out=dst[row0:row0 + nrows, 0:ncols], in_=st[0:nrows, 0:ncols])

    def load_w(self, col0, ncols):
        i = self.nw % 2
        self.nw += 1
        self.W, self.Wkey, self.wcol0 = self.wblk[i], 'wblk%d' % i, col0
        self.P.dma('pool', writes=[self.Wkey], out=self.W[:, :, 0:ncols], in_=self.wv[:, :, col0:col0 + ncols])

    def chunk(self, col0, ncols, post, tiles=TILES):
        P = self.P
        col0 = col0 - self.wcol0
        for ti, (c0, c1, r) in enumerate(tiles):
            n = c1 - c0
            b, bkey = self.bank()
            for k in range(8):
                P.mm(b[0:ncols, 0:n], self.W[:, k, col0:col0 + ncols], self.uT[:, k, c0:c1], k == 0, k == 7, [self.Wkey, self.ukey + ':%d' % ti], [bkey])
            post(b[0:ncols, 0:n], n, c0, c1, ti, bkey)

    def job_lin(self, col0, ncols_total, row0, scale, kind='b', tiles=TILES, ncols_out=TL):
        P = self.P
        for j in range(ncols_total // 128):
            if j % 4 == 0:
                self.load_w(col0 + j * 128, min(512, ncols_total - j * 128))
            st, sk = self.stage(kind)

            def post(ps, n, c0, c1, ti, bkey, st=st, sk=sk):
                P.dve('tensor_scalar', [bkey], [sk, bkey], out=st[:, c0:c1], in0=ps, scalar1=float(scale), scalar2=None, op0=ALU.mult)
            self.chunk(col0 + j * 128, 128, post, tiles)
            self.outdma(kind, st, sk, row0 + j * 128, 128, ncols_out)

    def job_silu(self, col0, ncols_total, row0, kind, tiles=TILES, ncols_out=TL):
        P = self.P
        for j in range(ncols_total // 128):
            if j % 4 == 0:
                self.load_w(col0 + j * 128, min(512, ncols_total - j * 128))
            st, sk = self.stage(kind)

            def post(ps, n, c0, c1, ti, bkey, st=st, sk=sk):
                P.act(st[:, c0:c1], ps, AF.Silu, [bkey], [sk, bkey])
            self.chunk(col0 + j * 128, 128, post, tiles)
            self.outdma(kind, st, sk, row0 + j * 128, 128, ncols_out)


def build_A0():
    nc = bass.Bass("TRN2", target_bir_lowering=False)
    xT = nc.dram_tensor("xT", [128, 8, TL], F32, kind="ExternalInput").ap()
    condT = nc.dram_tensor("condT", [128, 8, 2], F32, kind="ExternalInput").ap()
    ada_w = nc.dram_tensor("ada_w", [1024, 6144], F32, kind="ExternalInput").ap()
    adab = nc.dram_tensor("adab", [128, 48], F32, kind="ExternalInput").ap()
    gmix = nc.dram_tensor("gmix", [128, 8], F32, kind="ExternalInput").ap()
    w_in = nc.dram_tensor("w_in", [1024, 4128], F32, kind="ExternalInput").ap()
    gate_w = nc.dram_tensor("gate_w", [16, 2, 256], F32, kind="ExternalInput").ap()
    gate_b = nc.dram_tensor("gate_b", [128, 2, 2], F32, kind="ExternalInput").ap()
    hglb = nc.dram_tensor("hglb", [128, 2, 2, 4], F32, kind="ExternalInput").ap()
    o_bf = nc.dram_tensor("o_bf", [A0_BF['end'], TL], BF16, kind="ExternalOutput").ap()
    o_f = nc.dram_tensor("o_f", [A0_F['end'], TL], F32, kind="ExternalOutput").ap()
    ada_w1 = nc.dram_tensor("ada_w1", [1024, 6144], F32, kind="ExternalInput").ap()
    adab1 = nc.dram_tensor("adab1", [128, 48], F32, kind="ExternalInput").ap()
    mods_out = nc.dram_tensor("mods_out", [2, 128, 48, 2], F32, kind="ExternalOutput").ap()

    P = Prog(nc)
    hT = P.sb("hT", [128, 8, TL], F32)
    uT = P.sb("uT", [128, 8, TL], BF16)
    ones_bf = P.sb("ones_bf", [128, 128], BF16)
    work = dict(sq=P.sb("sq", [128, 8, 512], BF16), rs=P.sb("rs", [128, 512], F32),
                tmp=[P.sb("tmp%d" % i, [128, 512], F32) for i in range(2)])
    banks = [P.ps("bank%d" % i, [128, 512], F32) for i in range(6)]
    pm, ps_ssq = banks[0], banks[1]
    gb = banks[2:6]

    for k in range(8):
        P.dma('sp' if k % 2 == 0 else 'act', writes=['h:%d' % t for t in range(5)], out=hT[:, k, :], in_=xT[:, k, :])
    P.pool('memset', [], ['ones'], ap=ones_bf[:], constant=1.0)

    mod = emit_mods(P, condT, ada_w, adab, pm, '0')
    modB = emit_mods(P, condT, ada_w1, adab1, pm, '1')
    P.dma('sp', reads=['mod0'], out=mods_out[0], in_=mod[:])
    P.dma('sp', reads=['mod1'], out=mods_out[1], in_=modB[:])
    A = emit_affine(P, mod, 'mod0', gmix, 1, 'm0')
    for ti, tile in enumerate(TILES):
        emit_modulate_tile(P, hT, 'h', A, 'Am0', mod, 'mod0', 0, ti, tile, ones_bf, ps_ssq, work, uT=uT, ukey='u')

    gw_f = P.sb("gw_f", [16, 2, 256], F32)
    gw = P.sb("gw", [16, 2, 256], BF16)
    gbias = P.sb("gbias", [128, 2, 2], F32)
    ngb = P.sb("ngb", [128, 2, 2], F32)
    lbr = P.sb("lbr", [128, 2, 2, 4], F32)
    lbe = P.sb("lbe", [128, 2, 2, 4], F32)
    lb = P.sb("lb", [128, 2, 4], F32)
    oml = P.sb("oml", [128, 2, 4], F32)
    den = P.sb("den", [128, 2, 4], F32)
    P.dma('sp', writes=['gw_f'], out=gw_f[:], in_=gate_w)
    P.dma('sp', writes=['gbias'], out=gbias[:], in_=gate_b)
    P.dma('sp', writes=['lbr'], out=lbr[:], in_=hglb)
    P.dve('tensor_copy', ['gw_f'], ['gw'], out=gw[:], in_=gw_f[:])
    P.dve('tensor_scalar', ['gbias'], ['ngb'], out=ngb[:], in0=gbias[:], scalar1=-1.0, scalar2=None, op0=ALU.mult)
    P.act(lbe[:], lbr[:], AF.Exp, ['lbr'], ['lbe'])
    P.dve('tensor_tensor', ['lbe'], ['den'], out=den[:], in0=lbe[:, :, 0, :], in1=lbe[:, :, 1, :], op=ALU.add)
    P.dve('reciprocal', ['den'], ['den'], out=den[:], in_=den[:])
    P.dve('tensor_tensor', ['lbe', 'den'], ['lb'], out=lb[:], in0=lbe[:, :, 0, :], in1=den[:], op=ALU.mult)
    P.dve('tensor_scalar', ['lb'], ['oml'], out=oml[:], in0=lb[:], scalar1=-1.0, scalar2=1.0, op0=ALU.mult, op1=ALU.add)

    glrT = [P.sb("glrT%d" % d, [16, TL], BF16) for d in range(2)]
    ftmp = [P.sb("ftmp%d" % i, [128, 512], F32) for i in range(2)]
    C = ProjCtx(P, w_in, uT, 'u', gb, o_f, o_bf)

    def job_glr(col0, d):
        def post(ps, n, c0, c1, ti, bkey):
            P.dve('tensor_copy', [bkey], ['glrT%d' % d], out=glrT[d][:, c0:c1], in_=ps)
        C.load_w(col0, 16)
        C.chunk(col0, 16, post)

    def job_gate(d, row0):
        for j in range(2):
            st, sk = C.stage('f')
            for ti, (c0, c1, r) in enumerate(TILES):
                n = c1 - c0
                b, bkey = C.bank()
                P.mm(b[:, 0:n], gw[:, d, j * 128:(j + 1) * 128], glrT[d][:, c0:c1], True, True, ['gw', 'glrT%d' % d], [bkey])
                ft = ftmp[ti % 2]
                fk = 'ftmp%d' % (ti % 2)
                P.act(ft[:, 0:n], b[:, 0:n], AF.Exp, [bkey, 'ngb'], [fk], bias=ngb[:, d, j:j + 1], scale=-1.0)
                P.act(ft[:, 0:n], ft[:, 0:n], AF.Ln, [fk], [fk], bias=1.0, scale=1.0)
                P.dve('tensor_scalar', [fk], [sk], out=st[:, c0:c1], in0=ft[:, 0:n], scalar1=-1.0 / 16.0, scalar2=None, op0=ALU.mult)
            C.outdma('f', st, sk, row0 + j * 128, 128)

    def job_hgf(col0, d, row_k, row_la):
        C.load_w(col0, 512)
        for j in range(4):
            stb, skb = C.stage('b')
            stf, skf = C.stage('f')

            def post(ps, n, c0, c1, ti, bkey, stb=stb, skb=skb, stf=stf, skf=skf, j=j):
                ft = ftmp[ti % 2]
                fk = 'ftmp%d' % (ti % 2)
                P.act(ft[:, 0:n], ps, AF.Sigmoid, [bkey], [fk])
                P.dve('tensor_scalar', [fk, 'oml', 'lb'], [fk], out=ft[:, 0:n], in0=ft[:, 0:n], scalar1=oml[:, d, j:j + 1], scalar2=lb[:, d, j:j + 1], op0=ALU.mult, op1=ALU.add)
                P.pool('tensor_scalar', [fk], [skb], out=stb[:, c0:c1], in0=ft[:, 0:n], scalar1=-1.0, scalar2=1.0, op0=ALU.mult, op1=ALU.add)
                P.act(stf[:, c0:c1], ft[:, 0:n], AF.Ln, [fk], [skf])
            C.chunk(col0 + j * 128, 128, post)
            C.outdma('b', stb, skb, row_k + j * 128, 128)
            C.outdma('f', stf, skf, row_la + j * 128, 128)

    job_glr(AB['glr_f'], 0)
    job_glr(AB['glr_b'], 1)
    C.job_lin(AB['gq'], 256, A0_BF['gq'], 64 ** -0.5)
    C.job_lin(AB['gk'], 256, A0_BF['gk'], 1.0)
    C.job_lin(AB['gv'], 512, A0_BF['gv'], 1.0)
    C.job_silu(AB['gg'], 512, A0_F['gg'], 'f')
    job_gate(0, A0_F['gla_f'])
    job_gate(1, A0_F['gla_b'])
    C.job_silu(AB['hq'], 512, A0_BF['hq'], 'b')
    job_hgf(AB['hf_f'], 0, A0_BF['hk_f'], A0_F['hla_f'])
    job_hgf(AB['hf_b'], 1, A0_BF['hk_b'], A0_F['hla_b'])
    C.job_lin(AB['hi'], 512, A0_BF['hi'], 1.0)
    C.job_silu(AB['hg'], 512, A0_F['hg'], 'f')
    P.wait_all('sp')
    P.emit()
    P.close()
    return nc


def fm(a):
    T = a.shape[0]
    return np.ascontiguousarray(a.T.reshape(8, 128, T).transpose(1, 0, 2))


def colT(v, nk):
    return np.ascontiguousarray(np.asarray(v).reshape(nk, 128).T)


def core_tokens(x, ctx, i):
    b, q = i // 4, i % 4
    return np.concatenate([x[b, 2048 * q:2048 * (q + 1)], ctx[b, 64 * q:64 * (q + 1)]], axis=0)


def cond_T(inp, b):
    cond = np.stack([inp['c'][b], inp['c_ctx']], axis=-1)
    return np.ascontiguousarray(cond.reshape(8, 128, 2).transpose(1, 0, 2))


def host_A0_inputs(inp):
    maps = []
    x, ctx = inp['x'], inp['ctx']
    for i in range(NCORES):
        b = i // 4
        m = dict(
            xT=fm(core_tokens(x, ctx, i)),
            condT=cond_T(inp, b),
            ada_w=inp['ada_w'][0], adab=colT(inp['ada_b'][0], 48), gmix=colT(inp['norm_mix_g'][0], 8),
            ada_w1=inp['ada_w'][1], adab1=colT(inp['ada_b'][1], 48),
            w_in=inp['ab_w_in'][0],
            gate_w=np.ascontiguousarray(inp['gla_gate_w'][0].transpose(1, 0, 2)),
            gate_b=np.ascontiguousarray(inp['gla_gate_b'][0].reshape(2, 2, 128).transpose(2, 0, 1)),
            hglb=np.ascontiguousarray(inp['hg_lb'].reshape(2, 2, 4, 128).transpose(3, 0, 1, 2)),
        )
        maps.append(m)
    return maps


STOP_STAGE = 9
NSCAN = 4
VAR7 = 7
NCH = 132
SCH = 4
NSC = NCH // SCH


def scan_consts(P, tri_dram):
    tri = P.sb("tri_sb", [64, 6, 64], F32)
    P.dma('sp', writes=['tri'], out=tri[:], in_=tri_dram)
    return tri


def emit_scans(P, scans, tri, banks, nsteps=None):
    ns = len(scans)
    st = []
    for i, sc in enumerate(scans):
        dk, dv = sc['dk'], sc['dv']
        d = dict(sc)
        d['i'] = i
        d['in'] = []
        for par in range(2):
            n = "s%dp%d" % (i, par)
            d['in'].append(dict(
                qT=P.sb("qT" + n, [dk, SCH * 64], BF16), kT=P.sb("kT" + n, [dk, SCH * 64], BF16),
                ktm=P.sb("ktm" + n, [64, SCH, dk], BF16), vtm=P.sb("vtm" + n, [64, SCH, dv], BF16),
                latm=P.sb("latm" + n, [64, SCH, dk], F32), o=P.sb("o" + n, [64, SCH, dv], F32), key="in" + n, okey="o" + n))
        d['scr'] = []
        for par in range(2):
            n = "s%dq%d" % (i, par)
            d['scr'].append(dict(
                e1=P.sb("e1" + n, [dk, 64], F32), e2=P.sb("e2" + n, [dk, 64], F32), e3=P.sb("e3" + n, [64, dk], F32),
                qe=P.sb("qe" + n, [dk, 64], BF16), ke=P.sb("ke" + n, [dk, 64], BF16), kd=P.sb("kd" + n, [64, dk], BF16),
                attm=P.sb("attm" + n, [64, 64], BF16), dS=P.sb("dS" + n, [64, 64], F32), cc=P.sb("cc" + n, [64, 2], F32), n=n, bank=banks[2 * i + par]))
        d['Sf'] = P.sb("Sf%d" % i, [dk, dv], F32)
        d['Sb'] = P.sb("Sb%d" % i, [dk, dv], BF16)
        P.pool('memset', [], ['Sf%d' % i], ap=d['Sf'][:], constant=0.0)
        P.pool('memset', [], ['Sb%d' % i], ap=d['Sb'][:], constant=0.0)
        st.append(d)

    def sc_order(rev):
        if not rev:
            return [(s, list(range(SCH))) for s in range(NSC)]
        return [(0, list(range(SCH - 1, -1, -1)))] + [(s, list(range(SCH - 1, -1, -1))) for s in range(NSC - 1, 0, -1)]

    orders = [sc_order(d['rev']) for d in st]
    dq = [0]

    def load(d, step):
        s, _ = orders[d['i']][step]
        b = d['in'][step % 2]
        t0, t1 = s * SCH * 64, (s + 1) * SCH * 64
        c0, c1 = s * SCH, (s + 1) * SCH
        for nm, src in (('qT', d['qT'][:, t0:t1]), ('kT', d['kT'][:, t0:t1]), ('ktm', d['ktm'][:, c0:c1, :]), ('vtm', d['vtm'][:, c0:c1, :]), ('latm', d['latm'][:, c0:c1, :])):
            q = ['sp', 'act', 'pool'][dq[0] % 3]
            dq[0] += 1
            P.dma(q, reads=d.get('rkeys', []), writes=[b['key'] + nm], out=b[nm][:], in_=src)

    def store(d, step):
        s, _ = orders[d['i']][step]
        b = d['in'][step % 2]
        q = ['sp', 'act', 'pool'][dq[0] % 3]
        dq[0] += 1
        P.dma(q, reads=[b['okey']], writes=[d['okey']], out=d['o'][:, s * SCH:(s + 1) * SCH, :], in_=b['o'][:])

    for d in st:
        load(d, 0)
    cidx = 0
    NS = NSC if nsteps is None else nsteps
    for step in range(NS):
        for d in st:
            if step + 1 < NS:
                load(d, step + 1)
        for j in range(SCH):
            work = []
            for d in st:
                s, chs = orders[d['i']][step]
                c = chs[j]
                b = d['in'][step % 2]
                w = d['scr'][cidx % 2]
                work.append((d, b, w, c))
            cidx += 1
            for d, b, w, c in work:
                dk, dv, rev = d['dk'], d['dv'], d['rev']
                bk = w['bank']
                kb = 'bank' + w['n']
                lac = b['latm'][:, c, :]
                triI = tri[:, 1 if rev else 0, :]
                triS = tri[:, 3 if rev else 2, :]
                P.mm(bk[0:dk, 0:64], lac, triI, True, True, [b['key'] + 'latm', 'tri'], [kb])
                P.mm(bk[0:64, 128:128 + dk], triS, lac, True, True, [b['key'] + 'latm', 'tri'], [kb])
                if d['mode'] == 'scalar':
                    P.mm(bk[0:64, 320:322], triI, lac[:, 0:2], True, True, [b['key'] + 'latm', 'tri'], [kb])
            for d, b, w, c in (work if STOP_STAGE >= 2 else []):
                dk, dv, rev = d['dk'], d['dv'], d['rev']
                bk = w['bank']
                kb = 'bank' + w['n']
                n = w['n']
                P.act(w['e1'][:], bk[0:dk, 0:64], AF.Exp, [kb], ['e1' + n, kb])
                if d['mode'] == 'vec':
                    P.act(w['e2'][:], bk[0:dk, 0:64], AF.Exp, [kb], ['e2' + n, kb], scale=-1.0)
                P.act(w['e3'][:], bk[0:64, 128:128 + dk], AF.Exp, [kb], ['e3' + n, kb])
            for d, b, w, c in (work if STOP_STAGE >= 3 else []):
                dk, dv, rev = d['dk'], d['dv'], d['rev']
                n = w['n']
                bk = w['bank']
                kb = 'bank' + w['n']
                qc = b['qT'][:, c * 64:(c + 1) * 64]
                kc = b['kT'][:, c * 64:(c + 1) * 64]
                P.dve('tensor_tensor', [b['key'] + 'qT', 'e1' + n], ['qe' + n], out=w['qe'][:], in0=qc, in1=w['e1'][:], op=ALU.mult)
                if d['mode'] == 'vec':
                    P.dve('tensor_tensor', [b['key'] + 'kT', 'e2' + n], ['ke' + n], out=w['ke'][:], in0=kc, in1=w['e2'][:], op=ALU.mult)
                else:
                    P.dve('tensor_copy', [kb], ['cc' + n, kb], out=w['cc'][:], in_=bk[0:64, 320:322])
                    P.dve('scalar_tensor_tensor', [kb, 'cc' + n, 'tri'], ['dS' + n, kb], out=w['dS'][:], in0=bk[0:64, 0:64], scalar=w['cc'][:, 0:1],
                          in1=tri[:, 5 if rev else 4, :], op0=ALU.subtract, op1=ALU.add)
                P.pool('tensor_tensor', [b['key'] + 'ktm', 'e3' + n], ['kd' + n], out=w['kd'][:], in0=b['ktm'][:, c, :], in1=w['e3'][:], op=ALU.mult)
            for d, b, w, c in (work if STOP_STAGE >= 4 else []):
                dk = d['dk']
                n = w['n']
                bk = w['bank']
                kb = 'bank' + w['n']
                if d['mode'] == 'vec':
                    P.mm(bk[0:64, 64:128], w['ke'][:], w['qe'][:], True, True, ['ke' + n, 'qe' + n], [kb])
                else:
                    P.mm(bk[0:64, 64:128], b['kT'][:, c * 64:(c + 1) * 64], b['qT'][:, c * 64:(c + 1) * 64], True, True, [b['key'] + 'kT', b['key'] + 'qT'], [kb])
                    P.act(w['dS'][:], w['dS'][:], AF.Exp, ['dS' + n], ['dS' + n])
            for d, b, w, c in (work if STOP_STAGE >= 5 else []):
                n = w['n']
                bk = w['bank']
                kb = 'bank' + w['n']
                if d['mode'] == 'vec':
                    P.dve('tensor_tensor', [kb, 'tri'], ['attm' + n, kb], out=w['attm'][:], in0=bk[0:64, 64:128], in1=tri[:, 1 if d['rev'] else 0, :], op=ALU.mult)
                else:
                    P.dve('tensor_tensor', [kb, 'dS' + n], ['attm' + n, kb], out=w['attm'][:], in0=bk[0:64, 64:128], in1=w['dS'][:], op=ALU.mult)
            for d, b, w, c in (work if STOP_STAGE >= 6 else []):
                dk, dv = d['dk'], d['dv']
                n = w['n']
                i = d['i']
                bk = w['bank']
                kb = 'bank' + w['n']
                P.mm(bk[0:64, 384:384 + dv], w['qe'][:], d['Sb'][:], True, False, ['qe' + n, 'Sb%d' % i], [kb])
                P.mm(bk[0:64, 384:384 + dv], w['attm'][:], b['vtm'][:, c, :], False, True, ['attm' + n, b['key'] + 'vtm'], [kb])
                P.mm(bk[0:dk, 256:256 + dv], w['kd'][:], b['vtm'][:, c, :], True, True, ['kd' + n, b['key'] + 'vtm'], [kb])
            for d, b, w, c in (work if STOP_STAGE >= 7 else []):
                dk, dv, rev = d['dk'], d['dv'], d['rev']
                n = w['n']
                i = d['i']
                bk = w['bank']
                kb = 'bank' + w['n']
                if VAR7 & 1:
                    P.act(b['o'][:, c, :], bk[0:64, 384:384 + dv], AF.Copy, [kb], [b['okey'], kb])
                el = w['e1'][:, 0:1] if rev else w['e1'][:, 63:64]
                if VAR7 & 2:
                    P.dve('scalar_tensor_tensor', [kb, 'e1' + n, 'Sf%d' % i], ['Sf%d' % i, kb], out=d['Sf'][:], in0=d['Sf'][:], scalar=el, in1=bk[0:dk, 256:256 + dv], op0=ALU.mult, op1=ALU.add)
                if VAR7 & 4:
                    P.pool('tensor_copy', ['Sf%d' % i], ['Sb%d' % i], out=d['Sb'][:], in_=d['Sf'][:])
        for d in st:
            store(d, step)


def tri_consts():
    s = np.arange(64)[:, None]
    t = np.arange(64)[None, :]
    NEG = -30000.0
    m = np.stack([(s <= t), (s >= t), (s > t), (s < t)]).astype(np.float32)
    n = np.stack([np.where(s <= t, 0.0, NEG), np.where(s >= t, 0.0, NEG)]).astype(np.float32)
    return np.ascontiguousarray(np.concatenate([m, n], axis=0).transpose(1, 0, 2))


def build_B0(nsteps=None):
    nc = bass.Bass("TRN2", target_bir_lowering=False)
    T = NCH * 64
    tri_d = nc.dram_tensor("tri", [64, 6, 64], F32, kind="ExternalInput").ap()
    scans = []
    for nm, dk in (('g', 64), ('h', 128)):
        qT = nc.dram_tensor(nm + "_qT", [dk, T], BF16, kind="ExternalInput").ap()
        vtm = nc.dram_tensor(nm + "_vtm", [64, NCH, 128], BF16, kind="ExternalInput").ap()
        nk = 1 if nm == 'g' else 2
        kT = [nc.dram_tensor(nm + "_kT%d" % d, [dk, T], BF16, kind="ExternalInput").ap() for d in range(nk)]
        ktm = [nc.dram_tensor(nm + "_ktm%d" % d, [64, NCH, dk], BF16, kind="ExternalInput").ap() for d in range(nk)]
        for d in range(2):
            latm = nc.dram_tensor(nm + "_latm%d" % d, [64, NCH, dk], F32, kind="ExternalInput").ap()
            o = nc.dram_tensor(nm + "_o%d" % d, [64, NCH, 128], F32, kind="ExternalOutput").ap()
            scans.append(dict(dk=dk, dv=128, mode='vec', rev=(d == 1), qT=qT, kT=kT[d % nk], ktm=ktm[d % nk], vtm=vtm, latm=latm, o=o, okey='out_%s%d' % (nm, d)))
    P = Prog(nc)
    tri = scan_consts(P, tri_d)
    banks = [P.ps("bank%d" % i, [128, 512], F32) for i in range(8)]
    emit_scans(P, scans[:NSCAN], tri, banks, nsteps)
    P.wait_all('sp')
    P.emit()
    P.close()
    return nc


def joint_fm(outs, row0, nrows, b):
    parts_ctx = [outs[4 * b + q][row0:row0 + nrows, 2048:2112] for q in range(4)]
    parts_lat = [outs[4 * b + q][row0:row0 + nrows, 0:2048] for q in range(4)]
    return np.concatenate(parts_ctx + parts_lat, axis=1)


def to_tm(aT):
    w, T = aT.shape
    return np.ascontiguousarray(aT.T.reshape(T // 64, 64, w).transpose(1, 0, 2))


def host_B0_inputs(a0):
    obf = [r['o_bf'] for r in a0]
    of = [r['o_f'] for r in a0]
    tri = tri_consts()
    maps = []
    for i in range(NCORES):
        b, hd = i // 4, i % 4
        m = dict(tri=tri)
        gq = joint_fm(obf, A0_BF['gq'] + 64 * hd, 64, b)
        gk = joint_fm(obf, A0_BF['gk'] + 64 * hd, 64, b)
        gv = joint_fm(obf, A0_BF['gv'] + 128 * hd, 128, b)
        m['g_qT'] = gq
        m['g_kT0'] = gk
        m['g_ktm0'] = to_tm(gk)
        m['g_vtm'] = to_tm(gv)
        for d, nm in enumerate(('gla_f', 'gla_b')):
            m['g_latm%d' % d] = to_tm(joint_fm(of, A0_F[nm] + 64 * hd, 64, b))
        m['h_qT'] = joint_fm(obf, A0_BF['hq'] + 128 * hd, 128, b)
        m['h_vtm'] = to_tm(joint_fm(obf, A0_BF['hi'] + 128 * hd, 128, b))
        for d, (nk, nl) in enumerate((('hk_f', 'hla_f'), ('hk_b', 'hla_b'))):
            hk = joint_fm(obf, A0_BF[nk] + 128 * hd, 128, b)
            m['h_kT%d' % d] = hk
            m['h_ktm%d' % d] = to_tm(hk)
            m['h_latm%d' % d] = to_tm(joint_fm(of, A0_F[nl] + 128 * hd, 128, b))
        maps.append(m)
    return maps


CD = dict(hy=0, z=1536, xbc=2048, dt=3072, end=3088)
A1_BF = dict(hy=0, xbc=1536, end=2560)
A1_F = dict(zs=0, dt=512, la=528, end=544)


def barrier(P):
    for e in ENGS:
        P.wait_all(e)


def build_C(layer, nexp=16):
    nc = bass.Bass("TRN2", target_bir_lowering=False)
    T = TL if layer == 0 else 2048
    tiles = TILES if layer == 0 else TILES[:4]
    NT = len(tiles)
    din = lambda name, shape, dt=F32: nc.dram_tensor(name, list(shape), dt, kind="ExternalInput").ap()
    dout = lambda name, shape, dt=F32: nc.dram_tensor(name, list(shape), dt, kind="ExternalOutput").ap()
    hT_in = din("hT_in", [128, 8, T])
    mod_in = din("mod_in", [128, 48, 2])
    gffn = din("gffn", [128, 8])
    w_out = din("w_out", [1024, 1024])
    rw_d = din("router_w", [128, 8, 16])
    rb_d = din("router_b", [128, 16])
    sel_d = din("sel16", [16, 16, 128])
    identd = din("ident", [128, 128])
    wg_d = din("moe_wg", [16, 1024, 1024])
    wu_d = din("moe_wu", [16, 1024, 1024])
    wd_d = din("moe_wd", [16, 1024, 1024])
    if layer == 0:
        oT_d = din("oT", [8, 128, 2, T])
        gate_d = din("gateT", [8, 128, T])
        ng_d = din("ng", [128, 2])
        mod1_in = din("mod1_in", [128, 48, 2])
        gmix1 = din("gmix1", [128, 8])
        w_in1 = din("w_in1", [1024, 3088])
        dtp_d = din("dtp", [16, 2])
        hT_out = dout("hT_out", [128, 8, T])
        o_bf = dout("o_bf", [A1_BF['end'], T], BF16)
        o_f = dout("o_f", [A1_F['end'], T])
    else:
        hy_d = din("hyT", [128, 4, T])
        so_d = din("ssd_oT", [2, 128, 4, T])
        xs_d = din("xsT", [128, 4, T])
        zs_d = din("zsT", [128, 4, T])
        dsk_d = din("dsk", [128, 4])
        mg_d = din("mbg", [128, 4])
        gout_d = din("gout", [128, 8])
        y_out = dout("y_out", [128, 8, T])

    P = Prog(nc)
    hT = P.sb("hT", [128, 8, T], F32)
    fT = P.sb("fT", [128, 8, T], BF16)
    ones_bf = P.sb("ones_bf", [128, 128], BF16)
    ident = P.sb("ident_sb", [128, 128], F32)
    work = dict(sq=P.sb("sq", [128, 8, 512], BF16), rs=P.sb("rs", [128, 512], F32),
                tmp=[P.sb("tmp%d" % i, [128, 512], F32) for i in range(2)])
    banks = [P.ps("bank%d" % i, [128, 512], F32) for i in range(8)]
    pm, ps_ssq = banks[0], banks[1]
    for k in range(8):
        P.dma('sp' if k % 2 == 0 else 'act', writes=['h:%d' % t for t in range(NT)], out=hT[:, k, :], in_=hT_in[:, k, :])
    P.pool('memset', [], ['ones'], ap=ones_bf[:], constant=1.0)
    P.dma('sp', writes=['ident'], out=ident[:], in_=identd)
    mod = P.sb("sbmodL", [128, 48, 2], F32)
    P.dma('sp', writes=['modL'], out=mod[:], in_=mod_in)

    mark = len(P.stack)
    if layer == 0:
        ng = P.sb("ng_sb", [128, 2], F32)
        P.dma('sp', writes=['ng'], out=ng[:], in_=ng_d)
        ob = [P.sb("ob%d" % i, [128, 2, 512], F32) for i in range(2)]
        gbf = [P.sb("gbf%d" % i, [128, 512], F32) for i in range(2)]
        osum = [P.sb("osum%d" % i, [128, 512], F32) for i in range(2)]
        it = 0
        for hh in range(8):
            for ti, (c0, c1, r) in enumerate(tiles):
                n = c1 - c0
                i2 = it % 2
                it += 1
                P.dma('sp', writes=['ob%d' % i2], out=ob[i2][:, :, 0:n], in_=oT_d[hh, :, :, c0:c1])
                P.dma('act', writes=['gbf%d' % i2], out=gbf[i2][:, 0:n], in_=gate_d[hh, :, c0:c1])
                P.pool('tensor_tensor', ['ob%d' % i2], ['osum%d' % i2], out=osum[i2][:, 0:n], in0=ob[i2][:, 0, 0:n], in1=ob[i2][:, 1, 0:n], op=ALU.add)
                emit_rstd(P, osum[i2][:, 0:n].unsqueeze(1), 'osum%d' % i2, n, 1, ones_bf, ps_ssq, work['sq'], work['rs'], 1.0 / 128)
                tb = work['tmp'][i2]
                P.dve('scalar_tensor_tensor', ['osum%d' % i2, 'ng', 'rs'], ['tmp%d' % i2], out=tb[:, 0:n], in0=osum[i2][:, 0:n], scalar=ng[:, hh // 4:hh // 4 + 1], in1=work['rs'][:, 0:n], op0=ALU.mult, op1=ALU.mult)
                P.pool('tensor_tensor', ['tmp%d' % i2, 'gbf%d' % i2], ['f:%d' % ti], out=fT[:, hh, c0:c1], in0=tb[:, 0:n], in1=gbf[i2][:, 0:n], op=ALU.mult)
    else:
        dsk = P.sb("dsk_sb", [128, 4], F32)
        mg = P.sb("mg_sb", [128, 4], F32)
        P.dma('sp', writes=['dsk'], out=dsk[:], in_=dsk_d)
        P.dma('sp', writes=['mg'], out=mg[:], in_=mg_d)
        lb4 = [P.sb("lb4_%d" % i, [128, 4, 512], F32) for i in range(3)]
        yb = P.sb("yb", [128, 4, 512], F32)
        for ti, (c0, c1, r) in enumerate(tiles):
            n = c1 - c0
            P.dma('sp', writes=['lb4_0'], out=lb4[0][:, :, 0:n], in_=hy_d[:, :, c0:c1])
            P.pool('tensor_copy', ['lb4_0'], ['f:%d' % ti], out=fT[:, 0:4, c0:c1], in_=lb4[0][:, :, 0:n])
            P.dma('sp', writes=['lb4_1'], out=lb4[1][:, :, 0:n], in_=so_d[0, :, :, c0:c1])
            P.dma('act', writes=['lb4_2'], out=lb4[2][:, :, 0:n], in_=so_d[1, :, :, c0:c1])
            P.pool('tensor_tensor', ['lb4_1', 'lb4_2'], ['yb'], out=yb[:, :, 0:n], in0=lb4[1][:, :, 0:n], in1=lb4[2][:, :, 0:n], op=ALU.add)
            P.dma('sp', writes=['lb4_1'], out=lb4[1][:, :, 0:n], in_=xs_d[:, :, c0:c1])
            P.dma('act', writes=['lb4_2'], out=lb4[2][:, :, 0:n], in_=zs_d[:, :, c0:c1])
            for j in range(4):
                P.dve('scalar_tensor_tensor', ['lb4_1', 'dsk', 'yb'], ['yb'], out=yb[:, j, 0:n], in0=lb4[1][:, j, 0:n], scalar=dsk[:, j:j + 1], in1=yb[:, j, 0:n], op0=ALU.mult, op1=ALU.add)
            P.pool('tensor_tensor', ['yb', 'lb4_2'], ['yb'], out=yb[:, :, 0:n], in0=yb[:, :, 0:n], in1=lb4[2][:, :, 0:n], op=ALU.mult)
            for g in range(2):
                emit_rstd(P, yb[:, 2 * g:2 * g + 2, 0:n], 'yb', n, 2, ones_bf, ps_ssq, work['sq'], work['rs'], 1.0 / 256)
                for j in (2 * g, 2 * g + 1):
                    P.dve('scalar_tensor_tensor', ['yb', 'mg', 'rs'], ['f:%d' % ti], out=fT[:, 4 + j, c0:c1], in0=yb[:, j, 0:n], scalar=mg[:, j:j + 1], in1=work['rs'][:, 0:n], op0=ALU.mult, op1=ALU.mult)
    barrier(P)
    while len(P.stack) > mark:
        P.stack.pop().__exit__(None, None, None)

    wo = P.sb("wo", [128, 8, 1024], BF16)
    wov = w_out.rearrange("(k p) n -> p k n", p=128)
    for k in range(8):
        P.dma('pool', writes=['wo'], out=wo[:, k, :], in_=wov[:, k, :])
    gcnt = [0]
    gbk = banks[2:6]

    def bank():
        i = gcnt[0] % 4
        gcnt[0] += 1
        return gbk[i], 'gb%d' % i
    for dc in range(8):
        for ti, (c0, c1, r) in enumerate(tiles):
            n = c1 - c0
            b, bk = bank()
            for k in range(8):
                P.mm(b[:, 0:n], wo[:, k, dc * 128:(dc + 1) * 128], fT[:, k, c0:c1], k == 0, k == 7, ['wo', 'f:%d' % ti], [bk])
            P.dve('scalar_tensor_tensor', [bk, 'modL', 'h:%d' % ti], ['h:%d' % ti, bk], out=hT[:, dc, c0:c1], in0=b[:, 0:n], scalar=mod[:, 16 + dc, r:r + 1], in1=hT[:, dc, c0:c1], op0=ALU.mult, op1=ALU.add)
    barrier(P)
    P.stack.pop().__exit__(None, None, None)

    Af = emit_affine(P, mod, 'modL', gffn, 4, 'f')
    sel = P.sb("sel_sb", [16, 16, 128], F32)
    WT = P.sb("WT", [16, T], F32)
    mark = len(P.stack)
    u32 = P.sb("u32", [128, 8, 512], F32)
    rw = P.sb("rw", [128, 8, 16], F32)
    rb = P.sb("rb", [128, 16], F32)
    P.dma('sp', writes=['rw'], out=rw[:], in_=rw_d)
    P.dma('sp', writes=['rb'], out=rb[:], in_=rb_d)
    P.dma('sp', writes=['sel'], out=sel[:], in_=sel_d)
    rt = {nm: P.sb("rt_" + nm, [128, 16], F32) for nm in ('sc', 'sl', 'eq', 's2', 'ch', 'w')}
    r4 = {nm: P.sb("r4_" + nm, [128, 4], F32) for nm in ('m1', 'm2', 'gs', 'gm')}
    r1 = {nm: P.sb("r1_" + nm, [128, 1], F32) for nm in ('gx', 'ss')}
    v4 = lambda t_: t_[:].rearrange("p (g j) -> p g j", j=4)
    b4 = lambda t_: t_[:].unsqueeze(2).to_broadcast([128, 4, 4])
    rbank, rbk = banks[6], 'rbank'
    for ti, tile in enumerate(tiles):
        c0, c1, r = tile
        n = c1 - c0
        emit_modulate_tile(P, hT, 'h', Af, 'Af', mod, 'modL', 3, ti, tile, ones_bf, ps_ssq, work, uT=fT, ukey='v', u32=u32)
        for sub in range(n // 128 if n >= 128 else 1):
            m = min(128, n)
            s0 = sub * 128
            for k in range(8):
                P.mm(rbank[0:m, 0:16], u32[:, k, s0:s0 + m], rw[:, k, :], k == 0, k == 7, ['u32', 'rw'], [rbk])
            P.act(rt['sc'][0:m, :], rbank[0:m, 0:16], AF.Sigmoid, [rbk], ['rt_sc', rbk])
            P.dve('tensor_tensor', ['rt_sc', 'rb'], ['rt_sl'], out=rt['sl'][0:m], in0=rt['sc'][0:m], in1=rb[0:m], op=ALU.add)
            P.dve('tensor_reduce', ['rt_sl'], ['r4_m1'], out=r4['m1'][0:m], in_=v4(rt['sl'])[0:m], axis=AX.X, op=ALU.max)
            P.dve('tensor_tensor', ['rt_sl', 'r4_m1'], ['rt_eq'], out=v4(rt['eq'])[0:m], in0=v4(rt['sl'])[0:m], in1=b4(r4['m1'])[0:m], op=ALU.is_equal)
            P.dve('scalar_tensor_tensor', ['rt_eq', 'rt_sl'], ['rt_s2'], out=rt['s2'][0:m], in0=rt['eq'][0:m], scalar=-1e9, in1=rt['sl'][0:m], op0=ALU.mult, op1=ALU.add)
            P.dve('tensor_reduce', ['rt_s2'], ['r4_m2'], out=r4['m2'][0:m], in_=v4(rt['s2'])[0:m], axis=AX.X, op=ALU.max)
            P.dve('tensor_tensor', ['r4_m1', 'r4_m2'], ['r4_gs'], out=r4['gs'][0:m], in0=r4['m1'][0:m], in1=r4['m2'][0:m], op=ALU.add)
            P.dve('tensor_reduce', ['r4_gs'], ['r1_gx'], out=r1['gx'][0:m], in_=r4['gs'][0:m], axis=AX.X, op=ALU.max)
            P.dve('tensor_scalar', ['r4_gs', 'r1_gx'], ['r4_gm'], out=r4['gm'][0:m], in0=r4['gs'][0:m], scalar1=r1['gx'][0:m, 0:1], scalar2=None, op0=ALU.is_equal)
            P.dve('tensor_tensor', ['rt_sl', 'r4_m2'], ['rt_ch'], out=v4(rt['ch'])[0:m], in0=v4(rt['sl'])[0:m], in1=b4(r4['m2'])[0:m], op=ALU.is_ge)
            P.dve('tensor_tensor', ['rt_ch', 'r4_gm'], ['rt_ch'], out=v4(rt['ch'])[0:m], in0=v4(rt['ch'])[0:m], in1=b4(r4['gm'])[0:m], op=ALU.mult)
            P.dve('tensor_tensor', ['rt_ch', 'rt_sc'], ['rt_w'], out=rt['w'][0:m], in0=rt['ch'][0:m], in1=rt['sc'][0:m], op=ALU.mult)
            P.dve('tensor_reduce', ['rt_w'], ['r1_ss'], out=r1['ss'][0:m], in_=rt['w'][0:m], axis=AX.X, op=ALU.add)
            P.dve('reciprocal', ['r1_ss'], ['r1_ss'], out=r1['ss'][0:m], in_=r1['ss'][0:m])
            P.dve('tensor_scalar', ['rt_w', 'r1_ss'], ['rt_w'], out=rt['w'][0:m], in0=rt['w'][0:m], scalar1=r1['ss'][0:m, 0:1], scalar2=None, op0=ALU.mult)
            P.op('pe', 'transpose', ['rt_w', 'ident'], [rbk], out=rbank[0:16, 128:128 + m], in_=rt['w'][0:m, :], identity=ident[0:m, 0:m])
            P.act(WT[:, c0 + s0:c0 + s0 + m], rbank[0:16, 128:128 + m], AF.Copy, [rbk], ['WT', rbk])
    barrier(P)
    while len(P.stack) > mark:
        P.stack.pop().__exit__(None, None, None)

    barrier(P)
    while len(P.stack) > mark:
        P.stack.pop().__exit__(None, None, None)
    hid = P.sb("hid", [128, 4, T], BF16)
    wbc = P.sb("wbc", [128, T], F32)
    WG = [P.sb("WG%d" % k, [128, 512], BF16) for k in range(8)]
    WU = [P.sb("WU%d" % k, [128, 512], BF16) for k in range(8)]
    WD = [P.sb("WD%d" % k, [128, 1024], BF16) for k in range(4)]
    sg = [P.sb("sg%d" % i, [128, 512], F32) for i in range(2)]
    bA = [banks[2], banks[3]]
    bB = [banks[4], banks[5]]
    bY = [banks[6], banks[7]]
    it = 0
    for e in range(nexp):
        for ti, (c0, c1, r) in enumerate(tiles):
            n = c1 - c0
            P.mm(ps_ssq[:, 0:n], sel[:, e, :], WT[:, c0:c1], True, True, ['sel', 'WT'], ['ps_ssq'])
            P.act(wbc[:, c0:c1], ps_ssq[:, 0:n], AF.Copy, ['ps_ssq'], ['wbc:%d' % ti, 'ps_ssq'])
        for hf in range(2):
            for k in range(8):
                P.dma('pool', writes=['WG%d' % k], out=WG[k][:], in_=wg_d[e, k * 128:(k + 1) * 128, hf * 512:(hf + 1) * 512])
                P.dma('pool', writes=['WU%d' % k], out=WU[k][:], in_=wu_d[e, k * 128:(k + 1) * 128, hf * 512:(hf + 1) * 512])
            for k in range(4):
                P.dma('pool', writes=['WD%d' % k], out=WD[k][:], in_=wd_d[e, hf * 512 + k * 128:hf * 512 + (k + 1) * 128, :])
            for fc in range(4):
                for ti, (c0, c1, r) in enumerate(tiles):
                    n = c1 - c0
                    i2 = it % 2
                    it += 1
                    for k in range(8):
                        P.mm(bA[i2][:, 0:n], WG[k][:, fc * 128:(fc + 1) * 128], fT[:, k, c0:c1], k == 0, k == 7, ['WG%d' % k, 'v:%d' % ti], ['bA%d' % i2])
                    for k in range(8):
                        P.mm(bB[i2][:, 0:n], WU[k][:, fc * 128:(fc + 1) * 128], fT[:, k, c0:c1], k == 0, k == 7, ['WU%d' % k, 'v:%d' % ti], ['bB%d' % i2])
                    P.act(sg[i2][:, 0:n], bA[i2][:, 0:n], AF.Silu, ['bA%d' % i2], ['sg%d' % i2, 'bA%d' % i2])
                    P.dve('tensor_tensor', ['bB%d' % i2, 'sg%d' % i2], ['sg%d' % i2, 'bB%d' % i2], out=sg[i2][:, 0:n], in0=bB[i2][:, 0:n], in1=sg[i2][:, 0:n], op=ALU.mult)
                    P.pool('tensor_tensor', ['sg%d' % i2, 'wbc:%d' % ti], ['hid:%d' % ti], out=hid[:, fc, c0:c1], in0=sg[i2][:, 0:n], in1=wbc[:, c0:c1], op=ALU.mult)
            for dc in range(8):
                for ti, (c0, c1, r) in enumerate(tiles):
                    n = c1 - c0
                    i2 = it % 2
                    it += 1
                    for k in range(4):
                        P.mm(bY[i2][:, 0:n], WD[k][:, dc * 128:(dc + 1) * 128], hid[:, k, c0:c1], k == 0, k == 3, ['WD%d' % k, 'hid:%d' % ti], ['bY%d' % i2])
                    P.dve('scalar_tensor_tensor', ['bY%d' % i2, 'modL', 'h:%d' % ti], ['h:%d' % ti, 'bY%d' % i2], out=hT[:, dc, c0:c1], in0=bY[i2][:, 0:n], scalar=mod[:, 40 + dc, r:r + 1], in1=hT[:, dc, c0:c1], op0=ALU.mult, op1=ALU.add)
    barrier(P)
    while len(P.stack) > mark:
        P.stack.pop().__exit__(None, None, None)

    if layer == 0:
        for k in range(8):
            P.dma('sp' if k % 2 == 0 else 'act', reads=['h:%d' % t for t in range(NT)], out=hT_out[:, k, :], in_=hT[:, k, :])
        mod1 = P.sb("sbmod1", [128, 48, 2], F32)
        P.dma('sp', writes=['mod1'], out=mod1[:], in_=mod1_in)
        A1 = emit_affine(P, mod1, 'mod1', gmix1, 1, 'm1')
        for ti, tile in enumerate(tiles):
            emit_modulate_tile(P, hT, 'h', A1, 'Am1', mod1, 'mod1', 0, ti, tile, ones_bf, ps_ssq, work, uT=fT, ukey='u1')
        C = ProjCtx(P, w_in1, fT, 'u1', banks[2:6], o_f, o_bf)
        C.job_lin(CD['hy'], 1536, A1_BF['hy'], 1.0)
        C.job_lin(CD['xbc'], 1024, A1_BF['xbc'], 1.0)
        C.job_silu(CD['z'], 512, A1_F['zs'], 'f')
        dtp = P.sb("dtp_sb", [16, 2], F32)
        negA = P.sb("negA", [16, 1], F32)
        P.dma('sp', writes=['dtp'], out=dtp[:], in_=dtp_d)
        P.act(negA[:], dtp[:, 1:2], AF.Exp, ['dtp'], ['negA'])
        P.dve('tensor_scalar', ['negA'], ['negA'], out=negA[:], in0=negA[:], scalar1=-1.0, scalar2=None, op0=ALU.mult)
        st1, sk1 = C.stage('f')
        st2, sk2 = C.stage('f')
        C.load_w(CD['dt'], 16)

        def post(ps, n, c0, c1, ti, bkey):
            P.act(st1[0:16, c0:c1], ps, AF.Exp, [bkey, 'dtp'], [sk1, bkey], bias=dtp[:, 0:1], scale=1.0)
            P.act(st1[0:16, c0:c1], st1[0:16, c0:c1], AF.Ln, [sk1], [sk1], bias=1.0, scale=1.0)
            P.dve('tensor_scalar', [sk1, 'negA'], [sk2], out=st2[0:16, c0:c1], in0=st1[0:16, c0:c1], scalar1=negA[:, 0:1], scalar2=None, op0=ALU.mult)
        C.chunk(CD['dt'], 16, post)
        C.outdma('f', st1, sk1, A1_F['dt'], 16)
        C.outdma('f', st2, sk2, A1_F['la'], 16)
    else:
        gout = P.sb("gout_sb", [128, 8], F32)
        P.dma('sp', writes=['gout'], out=gout[:], in_=gout_d)
        yst = [P.sb("yst%d" % i, [128, 8, 512], F32) for i in range(2)]
        for ti, (c0, c1, r) in enumerate(tiles):
            n = c1 - c0
            emit_rstd(P, hT[:, :, c0:c1], 'h:%d' % ti, n, 8, ones_bf, ps_ssq, work['sq'], work['rs'], 1.0 / 1024)
            for k in range(8):
                P.dve('scalar_tensor_tensor', ['h:%d' % ti, 'gout', 'rs'], ['yst%d' % (ti % 2)], out=yst[ti % 2][:, k, 0:n], in0=hT[:, k, c0:c1], scalar=gout[:, k:k + 1], in1=work['rs'][:, 0:n], op0=ALU.mult, op1=ALU.mult)
            P.dma('sp' if ti % 2 == 0 else 'act', reads=['yst%d' % (ti % 2)], out=y_out[:, :, c0:c1], in_=yst[ti % 2][:, :, 0:n])
    P.wait_all('sp')
    P.emit()
    P.close()
    return nc


def build_C2(layer, nexp=16, CAP=640):
    nc = bass.Bass("TRN2", target_bir_lowering=False)
    T = TL if layer == 0 else 2048
    tiles = TILES if layer == 0 else TILES[:4]
    NT = len(tiles)
    din = lambda name, shape, dt=F32: nc.dram_tensor(name, list(shape), dt, kind="ExternalInput").ap()
    dout = lambda name, shape, dt=F32: nc.dram_tensor(name, list(shape), dt, kind="ExternalOutput").ap()
    hT_in = din("hT_in", [128, 8, T])
    mod_in = din("mod_in", [128, 48, 2])
    gffn = din("gffn", [128, 8])
    w_out = din("w_out", [1024, 1024])
    NSUB = (T + 127) // 128
    NSL = CAP // 128
    I32 = mybir.dt.int32
    tris_d = din("tris", [128, 128], BF16)
    tokhl_d = din("tokhl", [128, NSUB, 2], BF16)
    iota_d = din("iota_s", [128, CAP])
    eoff_d = din("eoff", [128, 16])
    vtm = nc.dram_tensor("vtm_scr", [NSUB * 128, 1024], BF16, kind="Internal").ap()
    y_all = nc.dram_tensor("yall_scr", [16 * CAP, 1024], F32, kind="Internal").ap()
    rw_d = din("router_w", [128, 8, 16])
    rb_d = din("router_b", [128, 16])
    identd = din("ident", [128, 128])
    wg_d = din("moe_wg", [16, 1024, 1024])
    wu_d = din("moe_wu", [16, 1024, 1024])
    wd_d = din("moe_wd", [16, 1024, 1024])
    if layer == 0:
        oT_d = din("oT", [8, 128, 2, T])
        gate_d = din("gateT", [8, 128, T])
        ng_d = din("ng", [128, 2])
        mod1_in = din("mod1_in", [128, 48, 2])
        gmix1 = din("gmix1", [128, 8])
        w_in1 = din("w_in1", [1024, 3088])
        dtp_d = din("dtp", [16, 2])
        hT_out = dout("hT_out", [128, 8, T])
        o_bf = dout("o_bf", [A1_BF['end'], T], BF16)
        o_f = dout("o_f", [A1_F['end'], T])
    else:
        hy_d = din("hyT", [128, 4, T])
        so_d = din("ssd_oT", [2, 128, 4, T])
        xs_d = din("xsT", [128, 4, T])
        zs_d = din("zsT", [128, 4, T])
        dsk_d = din("dsk", [128, 4])
        mg_d = din("mbg", [128, 4])
        gout_d = din("gout", [128, 8])
        y_out = dout("y_out", [128, 8, T])

    P = Prog(nc)
    hT = P.sb("hT", [128, 8, T], F32)
    ones_bf = P.sb("ones_bf", [128, 128], BF16)
    ident = P.sb("ident_sb", [128, 128], F32)
    work = dict(sq=P.sb("sq", [128, 8, 512], BF16), rs=P.sb("rs", [128, 512], F32),
                tmp=[P.sb("tmp%d" % i, [128, 512], F32) for i in range(2)])
    banks = [P.ps("bank%d" % i, [128, 512], F32) for i in range(8)]
    pm, ps_ssq = banks[0], banks[1]
    for k in range(8):
        P.dma('sp' if k % 2 == 0 else 'act', writes=['h:%d' % t for t in range(NT)], out=hT[:, k, :], in_=hT_in[:, k, :])
    P.pool('memset', [], ['ones'], ap=ones_bf[:], constant=1.0)
    P.dma('sp', writes=['ident'], out=ident[:], in_=identd)
    mod = P.sb("sbmodL", [128, 48, 2], F32)
    P.dma('sp', writes=['modL'], out=mod[:], in_=mod_in)

    mark_f = len(P.stack)
    fT = P.sb("fT", [128, 8, T], BF16)
    mark = len(P.stack)
    if layer == 0:
        ng = P.sb("ng_sb", [128, 2], F32)
        P.dma('sp', writes=['ng'], out=ng[:], in_=ng_d)
        ob = [P.sb("ob%d" % i, [128, 2, 512], F32) for i in range(2)]
        gbf = [P.sb("gbf%d" % i, [128, 512], F32) for i in range(2)]
        osum = [P.sb("osum%d" % i, [128, 512], F32) for i in range(2)]
        it = 0
        for hh in range(8):
            for ti, (c0, c1, r) in enumerate(tiles):
                n = c1 - c0
                i2 = it % 2
                it += 1
                P.dma('sp', writes=['ob%d' % i2], out=ob[i2][:, :, 0:n], in_=oT_d[hh, :, :, c0:c1])
                P.dma('act', writes=['gbf%d' % i2], out=gbf[i2][:, 0:n], in_=gate_d[hh, :, c0:c1])
                P.pool('tensor_tensor', ['ob%d' % i2], ['osum%d' % i2], out=osum[i2][:, 0:n], in0=ob[i2][:, 0, 0:n], in1=ob[i2][:, 1, 0:n], op=ALU.add)
                emit_rstd(P, osum[i2][:, 0:n].unsqueeze(1), 'osum%d' % i2, n, 1, ones_bf, ps_ssq, work['sq'], work['rs'], 1.0 / 128)
                tb = work['tmp'][i2]
                P.dve('scalar_tensor_tensor', ['osum%d' % i2, 'ng', 'rs'], ['tmp%d' % i2], out=tb[:, 0:n], in0=osum[i2][:, 0:n], scalar=ng[:, hh // 4:hh // 4 + 1], in1=work['rs'][:, 0:n], op0=ALU.mult, op1=ALU.mult)
                P.pool('tensor_tensor', ['tmp%d' % i2, 'gbf%d' % i2], ['f:%d' % ti], out=fT[:, hh, c0:c1], in0=tb[:, 0:n], in1=gbf[i2][:, 0:n], op=ALU.mult)
    else:
        dsk = P.sb("dsk_sb", [128, 4], F32)
        mg = P.sb("mg_sb", [128, 4], F32)
        P.dma('sp', writes=['dsk'], out=dsk[:], in_=dsk_d)
        P.dma('sp', writes=['mg'], out=mg[:], in_=mg_d)
        lb4 = [P.sb("lb4_%d" % i, [128, 4, 512], F32) for i in range(3)]
        yb = P.sb("yb", [128, 4, 512], F32)
        for ti, (c0, c1, r) in enumerate(tiles):
            n = c1 - c0
            P.dma('sp', writes=['lb4_0'], out=lb4[0][:, :, 0:n], in_=hy_d[:, :, c0:c1])
            P.pool('tensor_copy', ['lb4_0'], ['f:%d' % ti], out=fT[:, 0:4, c0:c1], in_=lb4[0][:, :, 0:n])
            P.dma('sp', writes=['lb4_1'], out=lb4[1][:, :, 0:n], in_=so_d[0, :, :, c0:c1])
            P.dma('act', writes=['lb4_2'], out=lb4[2][:, :, 0:n], in_=so_d[1, :, :, c0:c1])
            P.pool('tensor_tensor', ['lb4_1', 'lb4_2'], ['yb'], out=yb[:, :, 0:n], in0=lb4[1][:, :, 0:n], in1=lb4[2][:, :, 0:n], op=ALU.add)
            P.dma('sp', writes=['lb4_1'], out=lb4[1][:, :, 0:n], in_=xs_d[:, :, c0:c1])
            P.dma('act', writes=['lb4_2'], out=lb4[2][:, :, 0:n], in_=zs_d[:, :, c0:c1])
            for j in range(4):
                P.dve('scalar_tensor_tensor', ['lb4_1', 'dsk', 'yb'], ['yb'], out=yb[:, j, 0:n], in0=lb4[1][:, j, 0:n], scalar=dsk[:, j:j + 1], in1=yb[:, j, 0:n], op0=ALU.mult, op1=ALU.add)
            P.pool('tensor_tensor', ['yb', 'lb4_2'], ['yb'], out=yb[:, :, 0:n], in0=yb[:, :, 0:n], in1=lb4[2][:, :, 0:n], op=ALU.mult)
            for g in range(2):
                emit_rstd(P, yb[:, 2 * g:2 * g + 2, 0:n], 'yb', n, 2, ones_bf, ps_ssq, work['sq'], work['rs'], 1.0 / 256)
                for j in (2 * g, 2 * g + 1):
                    P.dve('scalar_tensor_tensor', ['yb', 'mg', 'rs'], ['f:%d' % ti], out=fT[:, 4 + j, c0:c1], in0=yb[:, j, 0:n], scalar=mg[:, j:j + 1], in1=work['rs'][:, 0:n], op0=ALU.mult, op1=ALU.mult)
    barrier(P)
    while len(P.stack) > mark:
        P.stack.pop().__exit__(None, None, None)

    wo = P.sb("wo", [128, 8, 1024], BF16)
    wov = w_out.rearrange("(k p) n -> p k n", p=128)
    for k in range(8):
        P.dma('pool', writes=['wo'], out=wo[:, k, :], in_=wov[:, k, :])
    gcnt = [0]
    gbk = banks[2:6]

    def bank():
        i = gcnt[0] % 4
        gcnt[0] += 1
        return gbk[i], 'gb%d' % i
    for dc in range(8):
        for ti, (c0, c1, r) in enumerate(tiles):
            n = c1 - c0
            b, bk = bank()
            for k in range(8):
                P.mm(b[:, 0:n], wo[:, k, dc * 128:(dc + 1) * 128], fT[:, k, c0:c1], k == 0, k == 7, ['wo', 'f:%d' % ti], [bk])
            P.dve('scalar_tensor_tensor', [bk, 'modL', 'h:%d' % ti], ['h:%d' % ti, bk], out=hT[:, dc, c0:c1], in0=b[:, 0:n], scalar=mod[:, 16 + dc, r:r + 1], in1=hT[:, dc, c0:c1], op0=ALU.mult, op1=ALU.add)
    barrier(P)
    while len(P.stack) > mark_f:
        P.stack.pop().__exit__(None, None, None)

    Af = emit_affine(P, mod, 'modL', gffn, 4, 'f')
    CH1 = P.sb("CH1", [128, NSUB, 16], F32)
    CH2 = P.sb("CH2", [128, NSUB, 16], F32)
    WW = P.sb("WW", [128, NSUB, 16], F32)
    CHb = P.sb("CHb", [128, NSUB, 16], BF16)
    RP = P.sb("RP", [128, NSUB, 16], F32)
    tris = P.sb("tris_sb", [128, 128], BF16)
    tokhl = P.sb("tokhl_sb", [128, NSUB, 2], BF16)
    iota_s = P.sb("iota_sb", [128, CAP], F32)
    eoff = P.sb("eoff_sb", [128, 16], F32)
    for t_, d_, k_ in ((tris, tris_d, 'tris'), (tokhl, tokhl_d, 'tokhl'), (iota_s, iota_d, 'iota'), (eoff, eoff_d, 'eoff')):
        P.dma('sp', writes=[k_], out=t_[:], in_=d_)
    for t_, k_ in ((CH1, 'CH1'), (CH2, 'CH2'), (WW, 'WW'), (CHb, 'CHb')):
        P.pool('memset', [], [k_], ap=t_[:], constant=0.0)
    mark = len(P.stack)
    u32 = P.sb("u32", [128, 8, 512], F32)
    rw = P.sb("rw", [128, 8, 16], F32)
    rb = P.sb("rb", [128, 16], F32)
    vrow = [P.sb("vrow%d" % i, [128, 1024], BF16) for i in range(2)]
    P.dma('sp', writes=['rw'], out=rw[:], in_=rw_d)
    P.dma('sp', writes=['rb'], out=rb[:], in_=rb_d)
    rt = {nm: P.sb("rt_" + nm, [128, 16], F32) for nm in ('sc', 'sl', 'eq', 's2', 'ch', 'w')}
    r4 = {nm: P.sb("r4_" + nm, [128, 4], F32) for nm in ('m1', 'm2', 'gs', 'gm')}
    r1 = {nm: P.sb("r1_" + nm, [128, 1], F32) for nm in ('gx', 'ss')}
    v4 = lambda t_: t_[:].rearrange("p (g j) -> p g j", j=4)
    b4 = lambda t_: t_[:].unsqueeze(2).to_broadcast([128, 4, 4])
    rbank, rbk = banks[6], 'rbank'
    jsub = 0
    for ti, tile in enumerate(tiles):
        c0, c1, r = tile
        n = c1 - c0
        emit_modulate_tile(P, hT, 'h', Af, 'Af', mod, 'modL', 3, ti, tile, ones_bf, ps_ssq, work, uT=None, ukey=None, u32=u32)
        for sub in range(n // 128 if n >= 128 else 1):
            m = min(128, n)
            s0 = sub * 128
            j = jsub
            jsub += 1
            vr, vk = vrow[j % 2], 'vrow%d' % (j % 2)
            for half in range(2):
                tb_, tk_ = banks[4 + half], 'tpb%d' % half
                for kk in range(4):
                    k = half * 4 + kk
                    P.op('pe', 'transpose', ['u32', 'ident'], [tk_], out=tb_[0:m, kk * 128:(kk + 1) * 128], in_=u32[:, k, s0:s0 + m], identity=ident[:])
                P.act(vr[0:m, half * 512:(half + 1) * 512], tb_[0:m, :], AF.Copy, [tk_], [vk, tk_])
            P.dma('sp' if j % 2 == 0 else 'act', reads=[vk], writes=['vtm'], out=vtm[j * 128:j * 128 + 128, :], in_=vr[:, :])
            for k in range(8):
                P.mm(rbank[0:m, 0:16], u32[:, k, s0:s0 + m], rw[:, k, :], k == 0, k == 7, ['u32', 'rw'], [rbk])
            P.act(rt['sc'][0:m, :], rbank[0:m, 0:16], AF.Sigmoid, [rbk], ['rt_sc', rbk])
            P.dve('tensor_tensor', ['rt_sc', 'rb'], ['rt_sl'], out=rt['sl'][0:m], in0=rt['sc'][0:m], in1=rb[0:m], op=ALU.add)
            P.dve('tensor_reduce', ['rt_sl'], ['r4_m1'], out=r4['m1'][0:m], in_=v4(rt['sl'])[0:m], axis=AX.X, op=ALU.max)
            P.dve('tensor_tensor', ['rt_sl', 'r4_m1'], ['rt_eq'], out=v4(rt['eq'])[0:m], in0=v4(rt['sl'])[0:m], in1=b4(r4['m1'])[0:m], op=ALU.is_equal)
            P.dve('scalar_tensor_tensor', ['rt_eq', 'rt_sl'], ['rt_s2'], out=rt['s2'][0:m], in0=rt['eq'][0:m], scalar=-1e9, in1=rt['sl'][0:m], op0=ALU.mult, op1=ALU.add)
            P.dve('tensor_reduce', ['rt_s2'], ['r4_m2'], out=r4['m2'][0:m], in_=v4(rt['s2'])[0:m], axis=AX.X, op=ALU.max)
            P.dve('tensor_tensor', ['r4_m1', 'r4_m2'], ['r4_gs'], out=r4['gs'][0:m], in0=r4['m1'][0:m], in1=r4['m2'][0:m], op=ALU.add)
            P.dve('tensor_reduce', ['r4_gs'], ['r1_gx'], out=r1['gx'][0:m], in_=r4['gs'][0:m], axis=AX.X, op=ALU.max)
            P.dve('tensor_scalar', ['r4_gs', 'r1_gx'], ['r4_gm'], out=r4['gm'][0:m], in0=r4['gs'][0:m], scalar1=r1['gx'][0:m, 0:1], scalar2=None, op0=ALU.is_equal)
            P.dve('tensor_tensor', ['rt_sl', 'r4_m2'], ['rt_ch'], out=v4(rt['ch'])[0:m], in0=v4(rt['sl'])[0:m], in1=b4(r4['m2'])[0:m], op=ALU.is_ge)
            P.dve('tensor_tensor', ['rt_ch', 'r4_gm'], ['rt_ch'], out=v4(rt['ch'])[0:m], in0=v4(rt['ch'])[0:m], in1=b4(r4['gm'])[0:m], op=ALU.mult)
            P.dve('tensor_tensor', ['rt_ch', 'rt_sc'], ['rt_w'], out=rt['w'][0:m], in0=rt['ch'][0:m], in1=rt['sc'][0:m], op=ALU.mult)
            P.dve('tensor_reduce', ['rt_w'], ['r1_ss'], out=r1['ss'][0:m], in_=rt['w'][0:m], axis=AX.X, op=ALU.add)
            P.dve('reciprocal', ['r1_ss'], ['r1_ss'], out=r1['ss'][0:m], in_=r1['ss'][0:m])
            P.dve('tensor_scalar', ['rt_w', 'r1_ss'], ['WW'], out=WW[0:m, j, :], in0=rt['w'][0:m], scalar1=r1['ss'][0:m, 0:1], scalar2=None, op0=ALU.mult)
            P.dve('tensor_tensor', ['rt_eq', 'r4_gm'], ['CH1'], out=CH1[0:m, j, :].rearrange("p (g j) -> p g j", j=4), in0=v4(rt['eq'])[0:m], in1=b4(r4['gm'])[0:m], op=ALU.mult)
            P.dve('tensor_tensor', ['rt_ch', 'CH1'], ['CH2'], out=CH2[0:m, j, :], in0=rt['ch'][0:m], in1=CH1[0:m, j, :], op=ALU.subtract)
            P.dve('tensor_copy', ['rt_ch'], ['CHb'], out=CHb[0:m, j, :], in_=rt['ch'][0:m])
    pbank, pbk = banks[6], 'rbank'
    for j in range(NSUB):
        P.mm(pbank[:, 0:16], tris[:], CHb[:, j, :], True, j == 0, ['tris', 'CHb'], [pbk])
        for jj in range(j):
            P.mm(pbank[:, 0:16], ones_bf[:], CHb[:, jj, :], False, jj == j - 1, ['ones', 'CHb'], [pbk])
        P.dve('tensor_tensor', [pbk, 'eoff'], ['RP', pbk], out=RP[:, j, :], in0=pbank[:, 0:16], in1=eoff[:], op=ALU.add)
    barrier(P)
    while len(P.stack) > mark:
        P.stack.pop().__exit__(None, None, None)

    identb = P.sb("identb_sb", [128, 128], BF16)
    P.dve('tensor_copy', ['ident'], ['identb'], out=identb[:], in_=ident[:])
    WG = [P.sb("WG%d" % k, [128, 1024], BF16) for k in range(8)]
    WU = [P.sb("WU%d" % k, [128, 1024], BF16) for k in range(8)]
    WD = [P.sb("WD%d" % k, [128, 1024], BF16) for k in range(8)]
    OH = [P.sb("OH%d" % i, [128, CAP], BF16) for i in range(3)]
    pe_ = P.sb("pe_e", [128, NSUB], F32)
    hl = P.sb("hl", [2, CAP], F32)
    idxf = P.sb("idxf", [128, NSL], F32)
    hls = P.sb("hls", [128, 2 * NSL], F32)
    idxi = [P.sb("idxi%d" % i, [128, NSL], I32) for i in range(2)]
    xg = [P.sb("xg%d" % i, [128, 1024], BF16) for i in range(2)]
    xsT2 = [P.sb("xsTe%d" % i, [128, 8, CAP], BF16) for i in range(2)]
    hid = P.sb("hid", [128, 8, CAP], BF16)
    sg = [P.sb("sg%d" % i, [128, 512], F32) for i in range(2)]
    ys = [P.sb("ys%d" % i, [128, 1024], F32) for i in range(2)]
    invA, invB = banks[0], banks[1]
    bA = [banks[2], banks[3]]
    bB = [banks[4], banks[5]]
    bY = [banks[6], banks[7]]
    ntl = [(0, min(512, CAP))] + ([(512, CAP)] if CAP > 512 else [])

    def indirect(out, in_, idx_ap, reads, writes):
        n_ = P.dma_n['pool']
        P.dma_n['pool'] += 1
        si = n_ % P.ndma
        key = ('d', 'pool', si)
        val = 16 * (n_ // P.ndma + 1)
        waits = P._deps('pool', list(reads), list(writes))
        if n_ >= P.ndma and P.seen['pool'].get(key, 0) < val - 16:
            waits.append((key, val - 16))
            P.seen['pool'][key] = val - 16
        P.q['pool'].append((waits, ('indirect_dma_start', dict(out=out, out_offset=None, in_=in_, in_offset=bass.IndirectOffsetOnAxis(ap=idx_ap, axis=0))), key))
        P._commit((key, val), list(reads), list(writes))

    def interleave(*gens):
        gens = [g_ for g_ in gens if g_ is not None]
        while gens:
            for g_ in list(gens):
                try:
                    next(g_)
                except StopIteration:
                    gens.remove(g_)

    state = dict(it=0, gcount=0)

    def prep_gen(e):
        xs_, xk_ = xsT2[e % 2], 'xsT%d' % (e % 2)
        P.dve('tensor_scalar', ['RP'], ['pe_e'], out=pe_[:], in0=RP[:, :, e], scalar1=float(1 - e * CAP), scalar2=None, op0=ALU.add)
        P.dve('tensor_tensor', ['pe_e', 'CHb'], ['pe_e'], out=pe_[:], in0=pe_[:], in1=CHb[:, :, e], op=ALU.mult)
        P.dve('tensor_scalar', ['pe_e'], ['pe_e'], out=pe_[:], in0=pe_[:], scalar1=-1.0, scalar2=None, op0=ALU.add)
        yield
        for j in range(NSUB):
            oh, ok_ = OH[j % 3], 'OH%d' % (j % 3)
            P.dve('tensor_scalar', ['iota', 'pe_e'], [ok_], out=oh[:], in0=iota_s[:], scalar1=pe_[:, j:j + 1], scalar2=None, op0=ALU.is_equal)
            for ni, (n0, n1) in enumerate(ntl):
                bk_, bkk = (invA, 'invA') if ni == 0 else (invB, 'invB')
                P.mm(bk_[0:2, 0:n1 - n0], tokhl[:, j, :], oh[:, n0:n1], j == 0, j == NSUB - 1, ['tokhl', ok_], [bkk])
            yield
        for ni, (n0, n1) in enumerate(ntl):
            bk_, bkk = (invA, 'invA') if ni == 0 else (invB, 'invB')
            P.act(hl[:, n0:n1], bk_[0:2, 0:n1 - n0], AF.Copy, [bkk], ['hl', bkk])
        yield
        for st in range(NSL):
            P.op('pe', 'transpose', ['hl', 'ident'], ['invB'], out=invB[:, 256 + 2 * st:256 + 2 * st + 2], in_=hl[0:2, st * 128:(st + 1) * 128], identity=ident[0:2, 0:2])
        P.act(hls[:], invB[:, 256:256 + 2 * NSL], AF.Copy, ['invB'], ['hls', 'invB'])
        hv = hls[:].rearrange("p (s c) -> p s c", c=2)
        P.dve('scalar_tensor_tensor', ['hls'], ['idxf'], out=idxf[:], in0=hv[:, :, 0], scalar=64.0, in1=hv[:, :, 1], op0=ALU.mult, op1=ALU.add)
        ii, ik = idxi[e % 2], 'idxi%d' % (e % 2)
        P.dve('tensor_copy', ['idxf'], [ik], out=ii[:], in_=idxf[:])
        yield
        for st in range(NSL):
            g_, gk_ = xg[state['gcount'] % 2], 'xg%d' % (state['gcount'] % 2)
            state['gcount'] += 1
            indirect(g_[:], vtm[:, :], ii[:, st:st + 1], [ik, 'vtm'], [gk_])
            tb_, tk_ = (invA, 'invA') if st % 2 == 0 else (invB, 'invB')
            tbb = tb_[:].bitcast(BF16)
            for k in range(8):
                P.op('pe', 'transpose', [gk_, 'identb'], [tk_], out=tbb[:, k * 128:(k + 1) * 128], in_=g_[:, k * 128:(k + 1) * 128], identity=identb[:])
            yield
            P.act(xs_[:, :, st * 128:(st + 1) * 128], tbb[:, 0:1024].rearrange("p (k s) -> p k s", k=8), AF.Copy, [tk_], [xk_, tk_])
            yield

    def ffn_gen(e):
        xs_, xk_ = xsT2[e % 2], 'xsT%d' % (e % 2)
        for k in range(8):
            P.dma('pool', writes=['WG%d' % k], out=WG[k][:], in_=wg_d[e, k * 128:(k + 1) * 128, :])
            P.dma('pool', writes=['WU%d' % k], out=WU[k][:], in_=wu_d[e, k * 128:(k + 1) * 128, :])
        for k in range(8):
            P.dma('pool', writes=['WD%d' % k], out=WD[k][:], in_=wd_d[e, k * 128:(k + 1) * 128, :])
        yield
        for fc in range(8):
            for (n0, n1) in ntl:
                n = n1 - n0
                i2 = state['it'] % 2
                state['it'] += 1
                for k in range(8):
                    P.mm(bA[i2][:, 0:n], WG[k][:, fc * 128:(fc + 1) * 128], xs_[:, k, n0:n1], k == 0, k == 7, ['WG%d' % k, xk_], ['bA%d' % i2])
                yield
                for k in range(8):
                    P.mm(bB[i2][:, 0:n], WU[k][:, fc * 128:(fc + 1) * 128], xs_[:, k, n0:n1], k == 0, k == 7, ['WU%d' % k, xk_], ['bB%d' % i2])
                yield
                P.act(sg[i2][:, 0:n], bA[i2][:, 0:n], AF.Silu, ['bA%d' % i2], ['sg%d' % i2, 'bA%d' % i2])
                P.dve('tensor_tensor', ['bB%d' % i2, 'sg%d' % i2], ['hid', 'bB%d' % i2], out=hid[:, fc, n0:n1], in0=bB[i2][:, 0:n], in1=sg[i2][:, 0:n], op=ALU.mult)
                yield
        for st in range(NSL):
            y_, yk = ys[st % 2], 'ys%d' % (st % 2)
            for dh in range(2):
                i2 = state['it'] % 2
                state['it'] += 1
                for k in range(8):
                    P.mm(bY[i2][:, :], hid[:, k, st * 128:(st + 1) * 128], WD[k][:, dh * 512:(dh + 1) * 512], k == 0, k == 7, ['WD%d' % k, 'hid'], ['bY%d' % i2])
                yield
                P.act(y_[:, dh * 512:(dh + 1) * 512], bY[i2][:, :], AF.Copy, ['bY%d' % i2], [yk, 'bY%d' % i2])
                yield
            P.dma('sp' if st % 2 == 0 else 'act', reads=[yk], writes=['yall'], out=y_all[e * CAP + st * 128:e * CAP + (st + 1) * 128, :], in_=y_[:])

    if nexp > 0:
        interleave(prep_gen(0))
    for e in range(nexp):
        interleave(ffn_gen(e), prep_gen(e + 1) if e + 1 < nexp else None)
    barrier(P)
    while len(P.stack) > mark:
        P.stack.pop().__exit__(None, None, None)

    gk2 = [P.sb("gk%d" % i, [128, 1024], F32) for i in range(4)]
    ytok = [P.sb("ytok%d" % i, [128, 1024], F32) for i in range(2)]
    t16 = P.sb("t16", [128, 16], F32)
    cs = {nm: P.sb("cs_" + nm, [128, 1], F32) for nm in ('row0', 'row1', 'w0', 'w1', 'eo', 'vl')}
    rowi = [P.sb("rowi%d" % i, [128, 1], I32) for i in range(4)]
    for j in range(NSUB):
        if layer == 0 and j == NSUB - 1:
            m, c0, r, ti = 64, 2048, 1, 4
        else:
            m, c0, r, ti = 128, j * 128, 0, j // 4
        for kk, CHk in enumerate((CH1, CH2)):
            rw_, w_ = cs['row%d' % kk], cs['w%d' % kk]
            P.dve('tensor_tensor', ['CH1', 'CH2', 'RP'], ['t16'], out=t16[:], in0=CHk[:, j, :], in1=RP[:, j, :], op=ALU.mult)
            P.dve('tensor_reduce', ['t16'], ['cs_row%d' % kk], out=rw_[:], in_=t16[:], axis=AX.X, op=ALU.add)
            P.dve('tensor_tensor', ['CH1', 'CH2', 'eoff'], ['t16'], out=t16[:], in0=CHk[:, j, :], in1=eoff[:], op=ALU.mult)
            P.dve('tensor_reduce', ['t16'], ['cs_eo'], out=cs['eo'][:], in_=t16[:], axis=AX.X, op=ALU.add)
            P.dve('tensor_tensor', ['CH1', 'CH2', 'WW'], ['t16'], out=t16[:], in0=CHk[:, j, :], in1=WW[:, j, :], op=ALU.mult)
            P.dve('tensor_reduce', ['t16'], ['cs_w%d' % kk], out=w_[:], in_=t16[:], axis=AX.X, op=ALU.add)
            P.dve('tensor_tensor', ['cs_row%d' % kk, 'cs_eo'], ['cs_row%d' % kk], out=rw_[:], in0=rw_[:], in1=cs['eo'][:], op=ALU.subtract)
            P.dve('tensor_scalar', ['cs_row%d' % kk], ['cs_vl'], out=cs['vl'][:], in0=rw_[:], scalar1=float(CAP) - 0.5, scalar2=None, op0=ALU.is_lt)
            P.dve('tensor_tensor', ['cs_w%d' % kk, 'cs_vl'], ['cs_w%d' % kk], out=w_[:], in0=w_[:], in1=cs['vl'][:], op=ALU.mult)
            P.dve('tensor_scalar', ['cs_row%d' % kk], ['cs_row%d' % kk], out=rw_[:], in0=rw_[:], scalar1=float(CAP - 1), scalar2=None, op0=ALU.min)
            P.dve('tensor_tensor', ['cs_row%d' % kk, 'cs_eo'], ['cs_row%d' % kk], out=rw_[:], in0=rw_[:], in1=cs['eo'][:], op=ALU.add)
            ri, rk_ = rowi[(2 * j + kk) % 4], 'rowi%d' % ((2 * j + kk) % 4)
            P.dve('tensor_copy', ['cs_row%d' % kk], [rk_], out=ri[:], in_=rw_[:])
            g_, gk_ = gk2[(2 * j + kk) % 4], 'gk%d' % ((2 * j + kk) % 4)
            indirect(g_[:], y_all[:, :], ri[:, 0:1], [rk_, 'yall'], [gk_])
        g0, g1 = gk2[(2 * j) % 4], gk2[(2 * j + 1) % 4]
        yt, ytk = ytok[j % 2], 'ytok%d' % (j % 2)
        P.dve('tensor_scalar', ['gk%d' % ((2 * j) % 4), 'cs_w0'], [ytk], out=yt[:], in0=g0[:], scalar1=cs['w0'][:, 0:1], scalar2=None, op0=ALU.mult)
        P.dve('scalar_tensor_tensor', ['gk%d' % ((2 * j + 1) % 4), 'cs_w1', ytk], [ytk], out=yt[:], in0=g1[:], scalar=cs['w1'][:, 0:1], in1=yt[:], op0=ALU.mult, op1=ALU.add)
        pb = (bA, bB)[j % 2]
        pk = ('bA%d', 'bB%d')[j % 2]
        for k in range(8):
            bb_, bbk = pb[k // 4], pk % (k // 4)
            P.op('pe', 'transpose', [ytk, 'ident'], [bbk], out=bb_[:, (k % 4) * 128:(k % 4) * 128 + m], in_=yt[0:m, k * 128:(k + 1) * 128], identity=ident[0:m, 0:m])
        for k in range(8):
            bb_, bbk = pb[k // 4], pk % (k // 4)
            P.dve('scalar_tensor_tensor', [bbk, 'modL', 'h:%d' % ti], ['h:%d' % ti, bbk], out=hT[:, k, c0:c0 + m], in0=bb_[:, (k % 4) * 128:(k % 4) * 128 + m], scalar=mod[:, 40 + k, r:r + 1], in1=hT[:, k, c0:c0 + m], op0=ALU.mult, op1=ALU.add)
    barrier(P)
    while len(P.stack) > mark:
        P.stack.pop().__exit__(None, None, None)

    if layer == 0:
        for k in range(8):
            P.dma('sp' if k % 2 == 0 else 'act', reads=['h:%d' % t for t in range(NT)], out=hT_out[:, k, :], in_=hT[:, k, :])
        mod1 = P.sb("sbmod1", [128, 48, 2], F32)
        P.dma('sp', writes=['mod1'], out=mod1[:], in_=mod1_in)
        A1 = emit_affine(P, mod1, 'mod1', gmix1, 1, 'm1')
        uT1 = P.sb("uT1", [128, 8, T], BF16)
        for ti, tile in enumerate(tiles):
            emit_modulate_tile(P, hT, 'h', A1, 'Am1', mod1, 'mod1', 0, ti, tile, ones_bf, ps_ssq, work, uT=uT1, ukey='u1')
        C = ProjCtx(P, w_in1, uT1, 'u1', banks[2:6], o_f, o_bf)
        C.job_lin(CD['hy'], 1536, A1_BF['hy'], 1.0)
        C.job_lin(CD['xbc'], 1024, A1_BF['xbc'], 1.0)
        C.job_silu(CD['z'], 512, A1_F['zs'], 'f')
        dtp = P.sb("dtp_sb", [16, 2], F32)
        negA = P.sb("negA", [16, 1], F32)
        P.dma('sp', writes=['dtp'], out=dtp[:], in_=dtp_d)
        P.act(negA[:], dtp[:, 1:2], AF.Exp, ['dtp'], ['negA'])
        P.dve('tensor_scalar', ['negA'], ['negA'], out=negA[:], in0=negA[:], scalar1=-1.0, scalar2=None, op0=ALU.mult)
        st1, sk1 = C.stage('f')
        st2, sk2 = C.stage('f')
        C.load_w(CD['dt'], 16)

        def post(ps, n, c0, c1, ti, bkey):
            P.act(st1[0:16, c0:c1], ps, AF.Exp, [bkey, 'dtp'], [sk1, bkey], bias=dtp[:, 0:1], scale=1.0)
            P.act(st1[0:16, c0:c1], st1[0:16, c0:c1], AF.Ln, [sk1], [sk1], bias=1.0, scale=1.0)
            P.dve('tensor_scalar', [sk1, 'negA'], [sk2], out=st2[0:16, c0:c1], in0=st1[0:16, c0:c1], scalar1=negA[:, 0:1], scalar2=None, op0=ALU.mult)
        C.chunk(CD['dt'], 16, post)
        C.outdma('f', st1, sk1, A1_F['dt'], 16)
        C.outdma('f', st2, sk2, A1_F['la'], 16)
    else:
        gout = P.sb("gout_sb", [128, 8], F32)
        P.dma('sp', writes=['gout'], out=gout[:], in_=gout_d)
        yst = [P.sb("yst%d" % i, [128, 8, 512], F32) for i in range(2)]
        for ti, (c0, c1, r) in enumerate(tiles):
            n = c1 - c0
            emit_rstd(P, hT[:, :, c0:c1], 'h:%d' % ti, n, 8, ones_bf, ps_ssq, work['sq'], work['rs'], 1.0 / 1024)
            for k in range(8):
                P.dve('scalar_tensor_tensor', ['h:%d' % ti, 'gout', 'rs'], ['yst%d' % (ti % 2)], out=yst[ti % 2][:, k, 0:n], in0=hT[:, k, c0:c1], scalar=gout[:, k:k + 1], in1=work['rs'][:, 0:n], op0=ALU.mult, op1=ALU.mult)
            P.dma('sp' if ti % 2 == 0 else 'act', reads=['yst%d' % (ti % 2)], out=y_out[:, :, c0:c1], in_=yst[ti % 2][:, :, 0:n])
    P.wait_all('sp')
    P.emit()
    P.close()
    return nc


def local_from_joint(a, q):
    return np.concatenate([a[256 + 2048 * q:256 + 2048 * (q + 1)], a[64 * q:64 * (q + 1)]], axis=0)


def tm_to_joint(o):
    return o.transpose(1, 0, 2).reshape(NCH * 64, o.shape[2])


CAP_ = 640


def tokhl_const(nsub):
    p = np.arange(128)[:, None]
    j = np.arange(nsub)[None, :]
    tid = j * 128 + p
    return np.ascontiguousarray(np.stack([tid // 64, tid % 64], axis=-1).astype(np.float32).astype(NPBF))


def common_C_inputs(inp, layer):
    sel = np.zeros((16, 16, 128), np.float32)
    for e in range(16):
        sel[e, e, :] = 1.0
    return dict(
        gffn=colT(inp['norm_ffn_g'][layer], 8),
        router_w=np.ascontiguousarray(inp['router_w'].reshape(8, 128, 16).transpose(1, 0, 2)),
        router_b=np.ascontiguousarray(np.broadcast_to(inp['router_b'][None, :], (128, 16))),
        sel16=sel, ident=np.eye(128, dtype=np.float32),
        tris=np.triu(np.ones((128, 128), np.float32), 1).astype(NPBF),
        iota_s=np.ascontiguousarray(np.broadcast_to(np.arange(CAP_, dtype=np.float32)[None, :], (128, CAP_))),
        eoff=np.ascontiguousarray(np.broadcast_to((np.arange(16, dtype=np.float32) * CAP_)[None, :], (128, 16))),
        tokhl=tokhl_const(17 if layer == 0 else 16),
        moe_wg=inp['moe_w_gate'][layer], moe_wu=inp['moe_w_up'][layer], moe_wd=inp['moe_w_down'][layer])


def host_C0_inputs(inp, a0, b0):
    maps = []
    com = common_C_inputs(inp, 0)
    ng = np.ascontiguousarray(np.stack([inp['gla_norm_g'][0], inp['hg_norm_g'][0]], axis=1))
    dtp = np.ascontiguousarray(np.stack([inp['mb_dt_bias'][0].reshape(16), inp['mb_a_log'][0].reshape(16)], axis=1))
    for i in range(NCORES):
        b, q = i // 4, i % 4
        oT = np.empty((8, 128, 2, TL), np.float32)
        for hh in range(8):
            src = b0[4 * b + hh % 4]
            for d in range(2):
                o = tm_to_joint(src[('g_o%d' if hh < 4 else 'h_o%d') % d])
                oT[hh, :, d, :] = local_from_joint(o, q).T
        m = dict(com)
        m.update(hT_in=fm(core_tokens(inp['x'], inp['ctx'], i)), mod_in=a0[i]['mods_out'][0], mod1_in=a0[i]['mods_out'][1], w_out=inp['ab_w_out'][0],
                 oT=oT, gateT=np.ascontiguousarray(a0[i]['o_f'][0:1024].reshape(8, 128, TL)), ng=ng,
                 gmix1=colT(inp['norm_mix_g'][1], 8),
                 w_in1=inp['cd_w_in'][0], dtp=dtp)
        maps.append(m)
    return maps


NFFT = 16384
PI = float(np.pi)


def fft_consts():
    a = np.arange(128, dtype=np.float64)
    th = 2 * np.pi * np.outer(a, a) / 128.0
    tw = 2 * np.pi * np.outer(a, a) / NFFT
    c, s = np.cos(th), np.sin(th)
    F64 = np.concatenate([c[:64], -s[:64]], axis=1)
    Fst = np.stack([c, -s, s], axis=1)
    G12 = np.stack([np.concatenate([c, s], 1), np.concatenate([-s, c], 1)], axis=1)
    Gst = np.stack([c[:, :64] / NFFT, -s[:, :64] / NFFT], axis=1)
    tr, ti = np.cos(tw), -np.sin(tw)
    TW = np.stack([np.tile(np.concatenate([tr, tr], 1), (1, 2)), np.tile(np.concatenate([ti, ti], 1), (1, 2))], axis=1)
    pr, pi_ = np.cos(tw), np.sin(tw)
    TWI = np.stack([np.tile(np.concatenate([pr, pr], 1), (1, 2)), np.tile(np.concatenate([pi_, pi_], 1), (1, 2))], axis=1)
    return dict(F64=F64.astype(NPBF), Fst=Fst.astype(NPBF), G12=G12.astype(NPBF), Gst=Gst.astype(NPBF),
                TW=TW.astype(np.float32), TWI=TWI.astype(np.float32))


def build_B1(nscan_steps=None, hy_groups=32, do_ssd=True, do_hy=True):
    nc = bass.Bass("TRN2", target_bir_lowering=False)
    T = NCH * 64
    din = lambda name, shape, dt=F32: nc.dram_tensor(name, list(shape), dt, kind="ExternalInput").ap()
    dout = lambda name, shape, dt=F32: nc.dram_tensor(name, list(shape), dt, kind="ExternalOutput").ap()
    dscr = lambda name, shape, dt=F32: nc.dram_tensor(name, list(shape), dt, kind="Internal").ap()
    tri_d = din("tri", [64, 6, 64])
    ident_d = din("identb", [128, 128], BF16)
    TP = 258 + 8194
    xin_d = din("ssd_xin", [128, 3, TP], BF16)
    scw_d = din("ssd_cw", [128, 3, 4])
    dttm_d = din("ssd_dttm", [64, NCH, 4])
    latm_d = [din("ssd_latm%d" % k, [64, NCH, 128]) for k in range(4)]
    s_qT = dscr("s_qT", [128, T], BF16)
    s_kT = dscr("s_kT", [128, T], BF16)
    s_ktm = dscr("s_ktm", [64, NCH, 128], BF16)
    s_vtm = [dscr("s_vtm%d" % k, [64, NCH, 64], BF16) for k in range(4)]
    ssd_o = [dout("ssd_o%d" % k, [64, NCH, 64]) for k in range(4)]
    xs_out = dout("xs_out", [128, T])
    hy_in_d = din("hy_in", [128, 3, 8194], BF16)
    hcw_d = din("hy_cw", [128, 3, 4])
    zT_d = din("hy_zT", [33, 8192])
    w1_d = din("hy_w1", [33, 64])
    w2_d = din("hy_w2", [64, 64])
    w3_d = din("hy_w3", [64, 4, 128])
    fp_d = din("hy_fp", [64, 3])
    rate_d = din("hy_rate", [64, 128])
    negt_d = din("hy_negt", [64, 128])
    hb_d = din("hy_bias", [64, 2, 128])
    F64_d = din("F64", [64, 256], BF16)
    Fst_d = din("Fst", [128, 3, 128], BF16)
    G12_d = din("G12", [128, 2, 256], BF16)
    Gst_d = din("Gst", [128, 2, 64], BF16)
    TW_d = din("TW", [128, 2, 512])
    TWI_d = din("TWI", [128, 2, 512])
    s_hy = dscr("s_hy", [3, 128, 8192], BF16)
    s_kf = dscr("s_kf", [2, 32, 128, 4, 2, 128])
    hy_out = dout("hy_out", [64, 128, 128])

    P = Prog(nc)
    banks = [P.ps("bank%d" % i, [128, 512], F32) for i in range(8)]
    dq = [0]

    def q3():
        dq[0] += 1
        return ['sp', 'act', 'pool'][dq[0] % 3]

    def dwconv(x, xkey, cw, part, col0, n, acc, acckey):
        P.act(acc[:, 0:n], x[:, part, col0 + 1:col0 + 1 + n], AF.Identity, [xkey, 'cw'], [acckey], scale=cw[:, part, 1:2], bias=cw[:, part, 3:4])
        P.dve('scalar_tensor_tensor', [xkey, 'cw', acckey], [acckey], out=acc[:, 0:n], in0=x[:, part, col0:col0 + n], scalar=cw[:, part, 0:1], in1=acc[:, 0:n], op0=ALU.mult, op1=ALU.add)
        P.dve('scalar_tensor_tensor', [xkey, 'cw', acckey], [acckey], out=acc[:, 0:n], in0=x[:, part, col0 + 2:col0 + 2 + n], scalar=cw[:, part, 2:3], in1=acc[:, 0:n], op0=ALU.mult, op1=ALU.add)

    if do_hy:
        mark0 = len(P.stack)
        xin = P.sb("hy_xin", [128, 3, 8194], BF16)
        cw = P.sb("hy_cw_sb", [128, 3, 4], F32)
        acc = [P.sb("hy_acc%d" % i, [128, 2048], F32) for i in range(2)]
        accb = [P.sb("hy_accb%d" % i, [128, 2048], BF16) for i in range(2)]
        for p in range(3):
            P.dma(q3(), writes=['hyxin'], out=xin[:, p, :], in_=hy_in_d[:, p, :])
        P.dma('sp', writes=['cw'], out=cw[:], in_=hcw_d)
        it = 0
        for p in range(3):
            for blk in range(4):
                i2 = it % 2
                it += 1
                dwconv(xin, 'hyxin', cw, p, blk * 2048, 2048, acc[i2], 'hyacc%d' % i2)
                P.pool('tensor_copy', ['hyacc%d' % i2], ['hyaccb%d' % i2], out=accb[i2][:], in_=acc[i2][:])
                P.dma(q3(), reads=['hyaccb%d' % i2], writes=['s_hy'], out=s_hy[p, :, blk * 2048:(blk + 1) * 2048], in_=accb[i2][:])
        barrier(P)
        while len(P.stack) > mark0:
            P.stack.pop().__exit__(None, None, None)

        F64 = P.sb("F64_sb", [64, 256], BF16)
        Fst = P.sb("Fst_sb", [128, 3, 128], BF16)
        G12 = P.sb("G12_sb", [128, 2, 256], BF16)
        Gst = P.sb("Gst_sb", [128, 2, 64], BF16)
        TW = P.sb("TW_sb", [128, 2, 512], F32)
        TWI = P.sb("TWI_sb", [128, 2, 512], F32)
        for t_, d_ in ((F64, F64_d), (Fst, Fst_d), (G12, G12_d), (Gst, Gst_d), (TW, TW_d), (TWI, TWI_d)):
            P.dma(q3(), writes=['fftc'], out=t_[:], in_=d_)
        m1 = [P.sb("m1_%d" % i, [128, 512], F32) for i in range(2)]
        m2 = [P.sb("m2_%d" % i, [128, 512], F32) for i in range(2)]
        Bt = [P.sb("Bt%d" % i, [128, 4, 2, 128], BF16) for i in range(2)]
        cnt = {'a': 0, 'b': 0}

        def twiddle(psrc, pkey, table, dst, dkey, pair):
            i2 = cnt['a'] % 2
            cnt['a'] += 1
            a1, a2 = m1[i2], m2[i2]
            P.dve('tensor_tensor', [pkey, 'fftc'], ['m1_%d' % i2, pkey], out=a1[:], in0=psrc, in1=table[:, 0, :], op=ALU.mult)
            yield
            P.dve('tensor_tensor', [pkey, 'fftc'], ['m2_%d' % i2, pkey], out=a2[:], in0=psrc, in1=table[:, 1, :], op=ALU.mult)
            yield
            v1 = a1[:].rearrange("p (c h k) -> p c h k", c=2, h=2)
            v2 = a2[:].rearrange("p (c h k) -> p c h k", c=2, h=2)
            P.pool('tensor_tensor', ['m1_%d' % i2, 'm2_%d' % i2], [dkey + 'r%d' % pair], out=dst[:, pair * 2:pair * 2 + 2, 0, :], in0=v1[:, :, 0, :], in1=v2[:, :, 1, :], op=ALU.subtract)
            yield
            P.pool('tensor_tensor', ['m1_%d' % i2, 'm2_%d' % i2], [dkey + 'i%d' % pair], out=dst[:, pair * 2:pair * 2 + 2, 1, :], in0=v2[:, :, 0, :], in1=v1[:, :, 1, :], op=ALU.add)
            yield

        def interleave(*gens):
            gens = [g_ for g_ in gens if g_ is not None]
            while gens:
                for g_ in list(gens):
                    try:
                        next(g_)
                    except StopIteration:
                        gens.remove(g_)

        def fft_fwd4(src, skey, c0, bXr, kXr, bXi, kXi):
            interleave(fft_fwd4_g(src, skey, c0, bXr, kXr, bXi, kXi))

        def fft_fwd4_g(src, skey, c0, bXr, kXr, bXi, kXi, extra=()):
            i2 = cnt['b'] % 2
            cnt['b'] += 1
            B = Bt[i2]
            bk = 'Bt%d' % i2
            for pair in range(2):
                pa, pk = banks[pair], 'psA%d' % pair
                for cc in range(2):
                    P.mm(pa[:, cc * 256:(cc + 1) * 256], src[0:64, c0 + pair * 2 + cc, :], F64[:], True, True, [skey, 'fftc'] + list(extra), [pk])
                yield
                yield from twiddle(pa[:], pk, TW, B, bk, pair)
            rk = [bk + 'r0', bk + 'r1', bk + 'i0', bk + 'i1', 'fftc']
            Br = B[:, :, 0, :]
            Bi = B[:, :, 1, :]
            P.mm(bXr[:], Fst[:, 0, :], Br, True, False, rk, [kXr])
            P.mm(bXr[:], Fst[:, 2, :], Bi, False, True, rk, [kXr])
            yield
            P.mm(bXi[:], Fst[:, 1, :], Br, True, False, rk, [kXi])
            P.mm(bXi[:], Fst[:, 0, :], Bi, False, True, rk, [kXi])
            yield

        mark1 = len(P.stack)
        zT = P.sb("zT_sb", [33, 8192], F32)
        w1 = P.sb("w1_sb", [33, 64], F32)
        w2 = P.sb("w2_sb", [64, 64], F32)
        w3 = P.sb("w3_sb", [64, 4, 128], F32)
        fp = P.sb("fp_sb", [64, 3], F32)
        fb = P.sb("fb_sb", [64, 2], F32)
        rate = P.sb("rate_sb", [64, 128], F32)
        negt = P.sb("negt_sb", [64, 128], F32)
        h1 = P.sb("h1_sb", [64, 8192], F32)
        h2 = P.sb("h2_sb", [64, 8192], F32)
        for t_, d_, k_ in ((zT, zT_d, 'zT'), (w1, w1_d, 'w1'), (w2, w2_d, 'w2'), (w3, w3_d, 'w3'), (fp, fp_d, 'fp'), (rate, rate_d, 'rate'), (negt, negt_d, 'negt')):
            P.dma(q3(), writes=[k_], out=t_[:], in_=d_)
        P.dve('tensor_scalar', ['fp'], ['fb'], out=fb[:], in0=fp[:, 1:3], scalar1=fp[:, 0:1], scalar2=None, op0=ALU.mult)
        gbank, gk = banks[6], 'gbank'
        wrp = [P.sb("wrp%d" % i, [64, 512], F32) for i in range(2)]
        for lyr, (wsb, wk, src, sk, dst, dk_, K_) in enumerate(((w1, 'w1', zT, 'zT', h1, 'h1', 33), (w2, 'w2', h1, 'h1', h2, 'h2', 64))):
            for blk in range(16):
                cs = slice(blk * 512, (blk + 1) * 512)
                P.mm(gbank[0:64, :], wsb[0:K_, :], src[0:K_, cs], True, True, [wk, sk], [gk])
                P.dve('tensor_scalar', [gk, 'fp', 'fb'], [dk_, gk], out=dst[:, cs], in0=gbank[0:64, :], scalar1=fp[:, 0:1], scalar2=fb[:, lyr:lyr + 1], op0=ALU.mult, op1=ALU.add)
                wa, wb_ = wrp[0][:, 0:512], wrp[1][:, 0:512]
                P.dve('tensor_scalar', [dk_], ['wrp0'], out=wa, in0=dst[:, cs], scalar1=PI, scalar2=-2 * PI, op0=ALU.is_gt, op1=ALU.mult)
                P.dve('tensor_scalar', [dk_], ['wrp1'], out=wb_, in0=dst[:, cs], scalar1=-PI, scalar2=2 * PI, op0=ALU.is_lt, op1=ALU.mult)
                P.dve('tensor_tensor', ['wrp0', 'wrp1'], ['wrp0'], out=wa, in0=wa, in1=wb_, op=ALU.add)
                P.dve('tensor_tensor', ['wrp0', dk_], [dk_], out=dst[:, cs], in0=dst[:, cs], in1=wa, op=ALU.add)
                P.dve('tensor_scalar', [dk_], [dk_], out=dst[:, cs], in0=dst[:, cs], scalar1=3.1415925, scalar2=-3.1415925, op0=ALU.min, op1=ALU.max)
                P.act(dst[:, cs], dst[:, cs], AF.Sin, [dk_], [dk_])
        hf = [P.sb("hf%d" % d, [64, 128, 128], BF16) for d in range(2)]
        dec = [P.sb("dec%d" % i, [64, 128], F32) for i in range(2)]
        kst = [P.sb("kst%d" % i, [128, 4, 2, 128], F32) for i in range(2)]
        xev = [P.sb("xev%d" % i, [128, 512], F32) for i in range(2)]
        h2v = h2[:].rearrange("j (a b) -> j a b", b=128)
        for o in range(2):
            for n2 in range(128):
                i2 = n2 % 2
                P.act(dec[i2][:], rate[:], AF.Exp, ['rate', 'negt'], ['dec%d' % i2], scale=negt[:, n2:n2 + 1])
                for d in range(2):
                    P.mm(gbank[0:64, 0:128], h2v[:, :, n2], w3[:, o * 2 + d, :], True, True, ['h2', 'w3'], [gk])
                    P.dve('tensor_tensor', [gk, 'dec%d' % i2], ['hf%d' % d, gk], out=hf[d][:, :, n2], in0=gbank[0:64, 0:128], in1=dec[i2][:], op=ALU.mult)
            for g in range(hy_groups):
                fft_fwd4(hf[0], 'hf0', g * 4, banks[2], 'bX0', banks[3], 'bX1')
                fft_fwd4(hf[1], 'hf1', g * 4, banks[4], 'bX2', banks[5], 'bX3')
                i2 = g % 2
                ks = kst[i2]
                kk = 'kst%d' % i2
                P.act(xev[0][:], banks[4][:], AF.Copy, ['bX2'], ['xev0', 'bX2'])
                P.act(xev[1][:], banks[5][:], AF.Copy, ['bX3'], ['xev1', 'bX3'])
                P.dve('tensor_tensor', ['bX0', 'xev0'], [kk, 'bX0'], out=ks[:, :, 0, :], in0=banks[2][:].rearrange("p (c k) -> p c k", c=4), in1=xev[0][:].rearrange("p (c k) -> p c k", c=4), op=ALU.add)
                P.dve('tensor_tensor', ['bX1', 'xev1'], [kk, 'bX1'], out=ks[:, :, 1, :], in0=banks[3][:].rearrange("p (c k) -> p c k", c=4), in1=xev[1][:].rearrange("p (c k) -> p c k", c=4), op=ALU.subtract)
                P.dma(q3(), reads=[kk], writes=['s_kf'], out=s_kf[o, g], in_=ks[:])
        barrier(P)
        while len(P.stack) > mark1:
            P.stack.pop().__exit__(None, None, None)

        xv = P.sb("xv", [64, 128, 128], BF16)
        x1 = P.sb("x1", [64, 128, 128], BF16)
        x2 = P.sb("x2", [64, 128, 128], BF16)
        hbias = P.sb("hbias", [64, 2, 128], F32)
        for p, t_ in enumerate((xv, x1, x2)):
            for hh in range(2):
                P.dma(q3(), reads=['s_hy'], writes=['xd%d' % p], out=t_[:, hh * 64:(hh + 1) * 64, :], in_=s_hy[p, hh * 64:(hh + 1) * 64, :].rearrange("c (a b) -> a c b", b=128))
        P.dma('sp', writes=['hbias'], out=hbias[:], in_=hb_d)
        kfb = [P.sb("kfb%d" % i, [128, 4, 2, 128], F32) for i in range(2)]
        Yt = [P.sb("Yt%d" % i, [128, 4, 2, 128], BF16) for i in range(2)]
        Dt = [P.sb("Dt%d" % i, [128, 4, 2, 128], BF16) for i in range(2)]
        pw = [P.sb("pw%d" % i, [128, 512], F32) for i in range(4)]
        ep = [P.sb("ep%d" % i, [64, 512], F32) for i in range(2)]
        ost = [P.sb("ost%d" % i, [64, 4, 128], F32) for i in range(2)]
        def fwd_gen(o, g):
            i2 = g % 2
            kf, kfk = kfb[i2], 'kfb%d' % i2
            P.dma(q3(), reads=['s_kf'], writes=[kfk], out=kf[:], in_=s_kf[o, g])
            yield from fft_fwd4_g(xv, 'xd0', g * 4, banks[2], 'bX0', banks[3], 'bX1', extra=['zz:%d' % g])
            Xr = banks[2][:].rearrange("p (c k) -> p c k", c=4)
            Xi = banks[3][:].rearrange("p (c k) -> p c k", c=4)
            Y = Yt[i2]
            yk = 'Yt%d' % i2
            pwv = [pw[i][:].rearrange("p (c k) -> p c k", c=4) for i in range(4)]
            P.dve('tensor_tensor', ['bX0', kfk, 'zz:%d' % g], ['pw0', 'bX0'], out=pwv[0], in0=Xr, in1=kf[:, :, 0, :], op=ALU.mult)
            yield
            P.dve('tensor_tensor', ['bX1', kfk], ['pw1', 'bX1'], out=pwv[1], in0=Xi, in1=kf[:, :, 1, :], op=ALU.mult)
            yield
            P.dve('tensor_tensor', ['bX0', kfk], ['pw2', 'bX0'], out=pwv[2], in0=Xr, in1=kf[:, :, 1, :], op=ALU.mult)
            yield
            P.dve('tensor_tensor', ['bX1', kfk], ['pw3', 'bX1'], out=pwv[3], in0=Xi, in1=kf[:, :, 0, :], op=ALU.mult)
            yield
            P.pool('tensor_tensor', ['pw0', 'pw1'], [yk], out=Y[:, :, 0, :], in0=pwv[0], in1=pwv[1], op=ALU.subtract)
            yield
            P.pool('tensor_tensor', ['pw2', 'pw3'], [yk], out=Y[:, :, 1, :], in0=pwv[2], in1=pwv[3], op=ALU.add)
            yield

        def inv_gen(o, g):
            i2 = g % 2
            Y = Yt[i2]
            yk = 'Yt%d' % i2
            D = Dt[i2]
            dk_ = 'Dt%d' % i2
            for pair in range(2):
                pc, pck = banks[4 + pair], 'psC%d' % pair
                for cc in range(2):
                    c = pair * 2 + cc
                    P.mm(pc[:, cc * 256:(cc + 1) * 256], Y[:, c, 0, :], G12[:, 0, :], True, False, [yk, 'fftc'], [pck])
                    P.mm(pc[:, cc * 256:(cc + 1) * 256], Y[:, c, 1, :], G12[:, 1, :], False, True, [yk, 'fftc'], [pck])
                    yield
                yield from twiddle(pc[:], pck, TWI, D, dk_, pair)
            rk = [dk_ + 'r0', dk_ + 'r1', dk_ + 'i0', dk_ + 'i1', 'fftc']
            yb, ybk = banks[6], 'ybank'
            P.mm(yb[0:64, :], Gst[:, 0, :], D[:, :, 0, :], True, False, rk, [ybk])
            P.mm(yb[0:64, :], Gst[:, 1, :], D[:, :, 1, :], False, True, rk, [ybk])
            yield
            cs = slice(g * 4, g * 4 + 4)
            e_ = ep[i2]
            ek = 'ep%d' % i2
            ev = e_[:].rearrange("p (c k) -> p c k", c=4)
            P.pool('tensor_tensor', ['xd0', 'zz:%d' % g, 'hbias'], [ek], out=ev, in0=xv[:, cs, :], in1=hbias[:, o, cs].unsqueeze(2).to_broadcast([64, 4, 128]), op=ALU.mult)
            yield
            P.dve('tensor_tensor', [ybk, ek], [ek, ybk], out=e_[:], in0=yb[0:64, :], in1=e_[:], op=ALU.add)
            yield
            if o == 0:
                P.dve('tensor_tensor', [ek, 'xd1'], ['zz:%d' % g], out=xv[:, cs, :], in0=ev, in1=x1[:, cs, :], op=ALU.mult)
            else:
                os_ = ost[i2]
                P.dve('tensor_tensor', [ek, 'xd2'], ['ost%d' % i2], out=os_[:], in0=ev, in1=x2[:, cs, :], op=ALU.mult)
                P.dma(q3(), reads=['ost%d' % i2], out=hy_out[:, cs, :], in_=os_[:])
            yield

        prev = None
        for o in range(2):
            for g in range(hy_groups):
                interleave(fwd_gen(o, g), prev)
                prev = inv_gen(o, g)
        interleave(prev)
        barrier(P)
        while len(P.stack) > mark0:
            P.stack.pop().__exit__(None, None, None)

    if do_ssd:
        mark2 = len(P.stack)
        cw = P.sb("ssd_cw_sb", [128, 3, 4], F32)
        identb = P.sb("identb_sb", [128, 128], BF16)
        ybf = P.sb("ssd_ybf", [128, 3, T], BF16)
        dttm = P.sb("dttm_sb", [64, NCH, 4], F32)
        mark3 = len(P.stack)
        xin = P.sb("ssd_xin_sb", [128, 3, TP], BF16)
        acc = [P.sb("ssd_acc%d" % i, [128, 2048], F32) for i in range(2)]
        for p in range(3):
            P.dma(q3(), writes=['sxin'], out=xin[:, p, :], in_=xin_d[:, p, :])
        P.dma('sp', writes=['cw'], out=cw[:], in_=scw_d)
        P.dma('sp', writes=['identb'], out=identb[:], in_=ident_d)
        P.dma('sp', writes=['dttm'], out=dttm[:], in_=dttm_d)
        segs = [(0, 0, 256)] + [(258 + i * 2048, 256 + i * 2048, 2048) for i in range(4)]
        it = 0
        for p in range(3):
            for (pc0, oc0, n) in segs:
                i2 = it % 2
                it += 1
                dwconv(xin, 'sxin', cw, p, pc0, n, acc[i2], 'sacc%d' % i2)
                P.act(acc[i2][:, 0:n], acc[i2][:, 0:n], AF.Silu, ['sacc%d' % i2], ['sacc%d' % i2])
                P.pool('tensor_copy', ['sacc%d' % i2], ['ybf%d' % p], out=ybf[:, p, oc0:oc0 + n], in_=acc[i2][:, 0:n])
                if p == 0:
                    P.dma(q3(), reads=['sacc%d' % i2], out=xs_out[:, oc0:oc0 + n], in_=acc[i2][:, 0:n])
        P.dma(q3(), reads=['ybf2'], writes=['s_qT'], out=s_qT, in_=ybf[:, 2, :])
        P.dma(q3(), reads=['ybf1'], writes=['s_kT'], out=s_kT, in_=ybf[:, 1, :])
        barrier(P)
        while len(P.stack) > mark3:
            P.stack.pop().__exit__(None, None, None)
        btm = P.sb("btm", [64, NCH, 128], BF16)
        xtm = P.sb("xtm", [64, NCH, 128], BF16)
        vt = [P.sb("vt%d" % i, [64, NCH, 64], BF16) for i in range(2)]
        tb = [banks[0], banks[1]]
        for part, dst, dk_ in ((1, btm, 'btm'), (0, xtm, 'xtm')):
            for c4 in range(NCH // 4):
                i2 = c4 % 2
                bkb = tb[i2][0:64, :].bitcast(BF16)
                for j in range(4):
                    c = c4 * 4 + j
                    P.op('pe', 'transpose', ['ybf%d' % part, 'identb'], ['tb%d' % i2], out=bkb[:, j * 128:(j + 1) * 128], in_=ybf[:, part, c * 64:(c + 1) * 64], identity=identb[:])
                P.act(dst[:, c4 * 4:(c4 + 1) * 4, :], bkb[:, 0:512].rearrange("p (c k) -> p c k", c=4), AF.Copy, ['tb%d' % i2], [dk_, 'tb%d' % i2])
        P.dma(q3(), reads=['btm'], writes=['s_ktm'], out=s_ktm, in_=btm[:])
        for k in range(4):
            d, j = k // 2, k % 2
            v_ = vt[k % 2]
            P.dve('tensor_tensor', ['xtm', 'dttm'], ['vt%d' % (k % 2)], out=v_[:], in0=xtm[:, :, j * 64:(j + 1) * 64], in1=dttm[:, :, k:k + 1].to_broadcast([64, NCH, 64]), op=ALU.mult)
            P.dma(q3(), reads=['vt%d' % (k % 2)], writes=['s_vtm%d' % k], out=s_vtm[k], in_=v_[:])
        barrier(P)
        while len(P.stack) > mark2:
            P.stack.pop().__exit__(None, None, None)
        tri = scan_consts(P, tri_d)
        scans = []
        for k in range(4):
            d, j = k // 2, k % 2
            scans.append(dict(dk=128, dv=64, mode='scalar', rev=(d == 1), qT=s_qT, kT=s_kT, ktm=s_ktm, vtm=s_vtm[k], latm=latm_d[k], o=ssd_o[k],
                              rkeys=['s_qT', 's_kT', 's_ktm', 's_vtm%d' % k], okey='ssd_out%d' % k))
        emit_scans(P, scans, tri, banks, nscan_steps)
    P.wait_all('sp')
    P.emit()
    P.close()
    return nc


HY_MIN_DECAY = float(np.log(1e-2) / 1.5)
HY_MAX_DECAY = float(np.log(1e-2) / 0.3)


def pad1(a):
    return np.concatenate([np.zeros_like(a[:, :1]), a, np.zeros_like(a[:, :1])], axis=1)


def hyena_pos_consts():
    n = 8192
    t = np.linspace(0.0, 1.0, n, dtype=np.float32)[:, None]
    bands = np.linspace(1e-4, 15, 16, dtype=np.float32)
    ang = (np.float32(2.0 * np.pi / n) * np.arange(n, dtype=np.float32)[:, None]) * bands
    z = np.concatenate([t, np.cos(ang), -np.sin(ang)], axis=-1).astype(np.float32)
    rates = np.abs(np.linspace(HY_MIN_DECAY, HY_MAX_DECAY, 512, dtype=np.float32))
    negt = -(np.arange(8192, dtype=np.float32) / np.float32(8191.0)).reshape(64, 128)
    return np.ascontiguousarray(z.T), rates, np.ascontiguousarray(negt)


def host_B1_inputs(inp, c0):
    obf = [r['o_bf'] for r in c0]
    of = [r['o_f'] for r in c0]
    fc = fft_consts()
    zT, rates, negt = hyena_pos_consts()
    tri = tri_consts()
    identb = np.eye(128, dtype=np.float32).astype(NPBF)
    cwall = np.concatenate([inp['mb_conv_w'][0], inp['mb_conv_b'][0][:, None]], axis=1)
    hcwall = np.concatenate([inp['hy_short_w'][0], inp['hy_short_b'][0][:, None]], axis=1)
    maps = []
    for i in range(NCORES):
        b, q = i // 4, i % 4
        g, hp = q // 2, q % 2
        h0 = 4 * g + 2 * hp
        rows = [64 * h0, 512 + 128 * g, 768 + 128 * g]
        m = dict(tri=tri, identb=identb)
        m.update(fc)
        xin = []
        for r0 in rows:
            a = joint_fm(obf, A1_BF['xbc'] + r0, 128, b)
            xin.append(np.concatenate([pad1(a[:, :256]), pad1(a[:, 256:])], axis=1))
        m['ssd_xin'] = np.ascontiguousarray(np.stack(xin, axis=1))
        m['ssd_cw'] = np.ascontiguousarray(np.stack([cwall[r0:r0 + 128] for r0 in rows], axis=1))
        dt4, la4 = [], []
        for k in range(4):
            d, j = k // 2, k % 2
            dt4.append(to_tm(joint_fm(of, A1_F['dt'] + d * 8 + h0 + j, 1, b)))
            la = to_tm(joint_fm(of, A1_F['la'] + d * 8 + h0 + j, 1, b))
            m['ssd_latm%d' % k] = np.ascontiguousarray(np.broadcast_to(la, (64, NCH, 128)))
        m['ssd_dttm'] = np.ascontiguousarray(np.concatenate(dt4, axis=2))
        hy = [pad1(joint_fm(obf, A1_BF['hy'] + p * 512 + 128 * q, 128, b)[:, 256:]) for p in range(3)]
        m['hy_in'] = np.ascontiguousarray(np.stack(hy, axis=1))
        m['hy_cw'] = np.ascontiguousarray(np.stack([hcwall[p * 512 + 128 * q:p * 512 + 128 * q + 128] for p in range(3)], axis=1))
        m['hy_zT'] = zT
        m['hy_w1'] = inp['hy_w1'][0]
        m['hy_w2'] = inp['hy_w2'][0]
        m['hy_w3'] = np.ascontiguousarray(inp['hy_w3'][0].reshape(64, 4, 512)[:, :, 128 * q:128 * q + 128])
        m['hy_fp'] = np.ascontiguousarray(np.stack([inp['hy_freq'][0], inp['hy_b1'][0], inp['hy_b2'][0]], axis=1))
        m['hy_rate'] = np.ascontiguousarray(np.broadcast_to(rates[128 * q:128 * q + 128][None, :], (64, 128)))
        m['hy_negt'] = negt
        m['hy_bias'] = np.ascontiguousarray(np.broadcast_to(inp['hy_bias'][0][:, 128 * q:128 * q + 128][None], (64, 2, 128)))
        maps.append(m)
    return maps


def host_C1_inputs(inp, a0, c0, b1):
    maps = []
    com = common_C_inputs(inp, 1)
    dsk = np.ascontiguousarray(np.repeat(inp['mb_d'][0], 64).reshape(4, 128).T)
    for i in range(NCORES):
        b, q = i // 4, i % 4
        ls = slice(256 + 2048 * q, 256 + 2048 * (q + 1))
        hyT = np.empty((128, 4, 2048), np.float32)
        soT = np.empty((2, 128, 4, 2048), np.float32)
        xsT = np.empty((128, 4, 2048), np.float32)
        for j in range(4):
            src = b1[4 * b + j]
            hy_ct = src['hy_out'].transpose(1, 0, 2).reshape(128, 8192)
            hyT[:, j, :] = hy_ct[:, 2048 * q:2048 * (q + 1)]
            xsT[:, j, :] = src['xs_out'][:, ls]
            for d in range(2):
                for jj in range(2):
                    o = tm_to_joint(src['ssd_o%d' % (d * 2 + jj)])
                    soT[d, jj * 64:(jj + 1) * 64, j, :] = o[ls].T
        m = dict(com)
        m.update(hT_in=np.ascontiguousarray(c0[i]['hT_out'][:, :, 0:2048]), mod_in=a0[i]['mods_out'][1], w_out=inp['cd_w_out'][0],
                 hyT=hyT, ssd_oT=soT, xsT=xsT,
                 zsT=np.ascontiguousarray(c0[i]['o_f'][0:512, 0:2048].reshape(4, 128, 2048).transpose(1, 0, 2)),
                 dsk=dsk, mbg=colT(inp['mb_norm_g'][0], 4), gout=colT(inp['norm_out_g'], 8))
        maps.append(m)
    return maps


_CACHE = {}


def _prog(name, fn):
    if name not in _CACHE:
        _CACHE[name] = fn()
    return _CACHE[name]


def _run(nc, maps):
    res = run_bass_kernel_spmd(nc, maps, core_ids=list(range(NCORES)))
    return [dict(r) for r in res.results]


def kernel(**inputs):
    inp = {k: np.asarray(v) for k, v in inputs.items()}
    a0 = _run(_prog('A0', build_A0), host_A0_inputs(inp))
    b0 = _run(_prog('B0', build_B0), host_B0_inputs(a0))
    c0 = _run(_prog('C0', lambda: build_C2(0)), host_C0_inputs(inp, a0, b0))
    b1 = _run(_prog('B1', build_B1), host_B1_inputs(inp, c0))
    c1 = _run(_prog('C1', lambda: build_C2(1)), host_C1_inputs(inp, a0, c0, b1))
    out = np.empty((2, 8192, 1024), np.float32)
    for i in range(NCORES):
        b, q = i // 4, i % 4
        out[b, 2048 * q:2048 * (q + 1), :] = c1[i]['y_out'].transpose(2, 1, 0).reshape(2048, 1024)
    return out
```

```python
import numpy as np
import ml_dtypes
import concourse.bass as bass
import concourse.mybir as mybir
from concourse.bass_utils import run_bass_kernel_spmd

F32 = mybir.dt.float32
BF16 = mybir.dt.bfloat16
AF = mybir.ActivationFunctionType
ALU = mybir.AluOpType
AX = mybir.AxisListType
NPBF = ml_dtypes.bfloat16

ENGS = ['pe', 'act', 'dve', 'pool', 'sp']
SAME_ENGINE_SYNC = True
NCORES = 8
TL = 2112
TILES = [(0, 512, 0), (512, 1024, 0), (1024, 1536, 0), (1536, 2048, 0), (2048, 2112, 1)]
EPS = 1e-6


class Prog:
    def __init__(self, nc, ndma_sems=6):
        self.nc = nc
        self.q = {e: [] for e in ENGS}
        self.cnt = {e: 0 for e in ENGS}
        self.seen = {e: {} for e in ENGS}
        self.lastw = {}
        self.reads = {}
        self.ndma = ndma_sems
        self.dma_n = {e: 0 for e in ENGS}
        self.stack = []
        self.uid = 0

    def enter(self, cm):
        v = cm.__enter__()
        self.stack.append(cm)
        return v

    def sb(self, name, shape, dt):
        return self.enter(self.nc.sbuf_tensor(name, list(shape), dt))

    def ps(self, name, shape, dt=F32):
        return self.enter(self.nc.psum_tensor(name, list(shape), dt))

    def close(self):
        while self.stack:
            self.stack.pop().__exit__(None, None, None)

    def _deps(self, eng, reads, writes):
        toks = set()
        for r in reads:
            t = self.lastw.get(r)
            if t is not None:
                toks.add(t)
        for w in writes:
            t = self.lastw.get(w)
            if t is not None:
                toks.add(t)
            for t in self.reads.get(w, ()):
                toks.add(t)
        need = {}
        for (k, v) in toks:
            if k == eng and (eng == 'pe' or not SAME_ENGINE_SYNC):
                continue
            if self.seen[eng].get(k, 0) >= v:
                continue
            if need.get(k, 0) < v:
                need[k] = v
        for k, v in need.items():
            self.seen[eng][k] = v
        return list(need.items())

    def _commit(self, tok, reads, writes):
        for r in reads:
            if r in writes:
                continue
            self.reads.setdefault(r, []).append(tok)
        for w in writes:
            self.lastw[w] = tok
            self.reads[w] = []

    def op(self, eng, name, reads=(), writes=(), **kw):
        reads = list(reads)
        writes = list(writes)
        waits = self._deps(eng, reads, writes)
        self.cnt[eng] += 1
        tok = (eng, self.cnt[eng])
        self.q[eng].append((waits, (name, kw), ('c', eng)))
        self._commit(tok, reads, writes)

    def mm(self, out, lhsT, rhs, start, stop, reads, writes):
        self.op('pe', 'matmul', reads, writes, out=out, lhsT=lhsT, rhs=rhs, start=start, stop=stop)

    def act(self, out, in_, func, reads, writes, **kw):
        self.op('act', 'activation', reads, writes, out=out, in_=in_, func=func, **kw)

    def dve(self, name, reads, writes, **kw):
        self.op('dve', name, reads, writes, **kw)

    def pool(self, name, reads, writes, **kw):
        self.op('pool', name, reads, writes, **kw)

    def dma(self, eng, reads=(), writes=(), **kw):
        fn = ('dma_start', kw)
        reads = list(reads)
        writes = list(writes)
        n = self.dma_n[eng]
        self.dma_n[eng] += 1
        si = n % self.ndma
        key = ('d', eng, si)
        val = 16 * (n // self.ndma + 1)
        waits = self._deps(eng, reads, writes)
        if n >= self.ndma and self.seen[eng].get(key, 0) < val - 16:
            waits.append((key, val - 16))
            self.seen[eng][key] = val - 16
        self.q[eng].append((waits, fn, key))
        self._commit((key, val), reads, writes)

    def wait_all(self, eng):
        need = {}
        for tok in self.lastw.values():
            k, v = tok
            if need.get(k, 0) < v:
                need[k] = v
        for toks in self.reads.values():
            for k, v in toks:
                if need.get(k, 0) < v:
                    need[k] = v
        waits = [(k, v) for k, v in need.items() if self.seen[eng].get(k, 0) < v and (k != eng or eng != 'sp')]
        for k, v in waits:
            self.seen[eng][k] = v
        self.q[eng].append((waits, None, None))

    def emit(self):
        nc = self.nc
        used = set()
        for e in ENGS:
            for waits, fn, inc in self.q[e]:
                for k, _ in waits:
                    used.add(k)
                if inc is not None:
                    used.add(inc if inc[0] == 'd' else inc[1])
        sem = {}
        for i, k in enumerate(sorted(used, key=str)):
            sem[k] = self.enter(nc.semaphore("sem%d" % i))
        block = self.enter(nc.Block())
        q = self.q

        def run(e, engine):
            for waits, fn, inc in q[e]:
                for k, v in waits:
                    engine.wait_ge(sem[k], v)
                if fn is None:
                    continue
                inst = getattr(engine, fn[0])(**fn[1])
                if inc[0] == 'd':
                    inst.then_inc(sem[inc], 16)
                else:
                    inst.then_inc(sem[inc[1]], 1)

        @block.tensor
        def _(eng):
            run('pe', eng)

        @block.scalar
        def _(eng):
            run('act', eng)

        @block.vector
        def _(eng):
            run('dve', eng)

        @block.gpsimd
        def _(eng):
            run('pool', eng)

        @block.sync
        def _(eng):
            run('sp', eng)


def emit_mods(P, condT, ada_w_l, adab, pm, tag):
    cond = P.sb("sbcond" + tag, [128, 8, 2], F32)
    sc = P.sb("sbsc" + tag, [128, 8, 2], F32)
    adab_sb = P.sb("sbadab" + tag, [128, 48], F32)
    mod = P.sb("sbmod" + tag, [128, 48, 2], F32)
    mark_ = len(P.stack)
    wb = [P.sb("sbadaw%d" % i + tag, [128, 8, 256], F32) for i in range(2)]
    P.dma('sp', writes=['cond' + tag], out=cond[:], in_=condT)
    P.dma('sp', writes=['adab' + tag], out=adab_sb[:], in_=adab)
    P.act(sc[:], cond[:], AF.Silu, ['cond' + tag], ['sc' + tag])
    wv = ada_w_l.rearrange("(k p) n -> p k n", p=128)
    for s in range(24):
        buf = wb[s % 2]
        bk = 'adaw%d' % (s % 2) + tag
        P.dma('sp' if s % 2 == 0 else 'act', writes=[bk], out=buf[:], in_=wv[:, :, s * 256:(s + 1) * 256])
        for m4 in range(2):
            m = s * 2 + m4
            for k in range(8):
                P.mm(pm[:, 2 * m:2 * m + 2], buf[:, k, m4 * 128:(m4 + 1) * 128], sc[:, k, :], k == 0, k == 7, [bk, 'sc' + tag], ['pm' + tag])
    P.dve('tensor_tensor', ['pm' + tag, 'adab' + tag], ['mod' + tag], out=mod[:], in0=pm[:, 0:96].rearrange("p (m r) -> p m r", r=2),
          in1=adab_sb[:].unsqueeze(2).to_broadcast([128, 48, 2]), op=ALU.add)
    barrier(P)
    while len(P.stack) > mark_:
        P.stack.pop().__exit__(None, None, None)
    return mod


def emit_affine(P, mod, modkey, gT_dram, kind_scale, tag):
    g = P.sb("sbg" + tag, [128, 8], F32)
    A = P.sb("sbA" + tag, [128, 8, 2], F32)
    P.dma('sp', writes=['g' + tag], out=g[:], in_=gT_dram)
    P.dve('scalar_tensor_tensor', [modkey, 'g' + tag], ['A' + tag], out=A[:], in0=mod[:, kind_scale * 8:kind_scale * 8 + 8, :], scalar=1.0,
          in1=g[:].unsqueeze(2).to_broadcast([128, 8, 2]), op0=ALU.add, op1=ALU.mult)
    return A


def emit_rstd(P, src, srckey, n, nk, ones_bf, ps_ssq, sq, rs, inv_n):
    P.act(sq[:, 0:nk, 0:n], src, AF.Square, [srckey], ['sq'])
    for k in range(nk):
        P.mm(ps_ssq[:, 0:n], ones_bf[:], sq[:, k, 0:n], k == 0, k == nk - 1, ['sq', 'ones'], ['ps_ssq'])
    P.dve('tensor_scalar', ['ps_ssq'], ['rs'], out=rs[:, 0:n], in0=ps_ssq[:, 0:n], scalar1=float(inv_n), scalar2=EPS, op0=ALU.mult, op1=ALU.add)
    P.act(rs[:, 0:n], rs[:, 0:n], AF.Sqrt, ['rs'], ['rs'])
    P.dve('reciprocal', ['rs'], ['rs'], out=rs[:, 0:n], in_=rs[:, 0:n])


def emit_modulate_tile(P, hT, hkey, A, Akey, mod, modkey, kind_shift, ti, tile, ones_bf, ps_ssq, work, uT=None, ukey=None, u32=None):
    sq, rs, tmp = work['sq'], work['rs'], work['tmp']
    c0, c1, r = tile
    n = c1 - c0
    emit_rstd(P, hT[:, :, c0:c1], hkey + ':%d' % ti, n, 8, ones_bf, ps_ssq, sq, rs, 1.0 / 1024)
    for k in range(8):
        tb = tmp[k % 2]
        tk = 'tmp%d' % (k % 2)
        P.dve('scalar_tensor_tensor', [hkey + ':%d' % ti, Akey, 'rs'], [tk], out=tb[:, 0:n], in0=hT[:, k, c0:c1], scalar=A[:, k, r:r + 1], in1=rs[:, 0:n], op0=ALU.mult, op1=ALU.mult)
        sh = mod[:, kind_shift * 8 + k, r:r + 1]
        if u32 is not None:
            P.act(u32[:, k, 0:n], tb[:, 0:n], AF.Identity, [tk, modkey], ['u32'], bias=sh, scale=1.0)
            if uT is not None:
                P.pool('tensor_copy', ['u32'], [ukey + ':%d' % ti], out=uT[:, k, c0:c1], in_=u32[:, k, 0:n])
        else:
            P.act(uT[:, k, c0:c1], tb[:, 0:n], AF.Identity, [tk, modkey], [ukey + ':%d' % ti], bias=sh, scale=1.0)


AB = dict(gq=0, gk=256, gv=512, gg=1024, glr_f=1536, glr_b=1552, hq=1568, hf_f=2080, hf_b=2592, hi=3104, hg=3616, end=4128)
A0_BF = dict(gq=0, gk=256, gv=512, hq=1024, hk_f=1536, hk_b=2048, hi=2560, end=3072)
A0_F = dict(gg=0, hg=512, gla_f=1024, gla_b=1280, hla_f=1536, hla_b=2048, end=2560)


class ProjCtx:
    def __init__(self, P, w_dram, uT, ukey, gb, o_f, o_bf, nst=3):
        self.P, self.uT, self.ukey, self.gb, self.o_f, self.o_bf = P, uT, ukey, gb, o_f, o_bf
        self.wv = w_dram.rearrange("(k p) n -> p k n", p=128)
        self.wblk = [P.sb("wblk%d" % i, [128, 8, 512], BF16) for i in range(2)]
        self.nw = 0
        self.W = None
        self.nst = nst
        self.stg_f = [P.sb("stgf%d" % i, [128, TL], F32) for i in range(nst)]
        self.stg_b = [P.sb("stgb%d" % i, [128, TL], BF16) for i in range(nst)]
        self.cnt = {'f': 0, 'b': 0, 'g': 0, 'q': 0}

    def stage(self, kind):
        i = self.cnt[kind] % self.nst
        self.cnt[kind] += 1
        return ((self.stg_f if kind == 'f' else self.stg_b)[i], 'stg%s%d' % (kind, i))

    def bank(self):
        i = self.cnt['g'] % len(self.gb)
        self.cnt['g'] += 1
        return self.gb[i], 'gb%d' % i

    def outdma(self, kind, st, sk, row0, nrows, ncols=TL):
        dst = (self.o_f if kind == 'f' else self.o_bf)
        q = ['sp', 'act'][self.cnt['q'] % 2]
        self.cnt['q'] += 1
        self.P.dma(q, reads=[sk], out=dst[row0:row0 + nrows, 0:ncols], in_=st[0:nrows, 0:ncols])

    def load_w(self, col0, ncols):
        i = self.nw % 2
        self.nw += 1
        self.W, self.Wkey, self.wcol0 = self.wblk[i], 'wblk%d' % i, col0
        self.P.dma('pool', writes=[self.Wkey], out=self.W[:, :, 0:ncols], in_=self.wv[:, :, col0:col0 + ncols])

    def chunk(self, col0, ncols, post, tiles=TILES):
        P = self.P
        col0 = col0 - self.wcol0
        for ti, (c0, c1, r) in enumerate(tiles):
            n = c1 - c0
            b, bkey = self.bank()
            for k in range(8):
                P.mm(b[0:ncols, 0:n], self.W[:, k, col0:col0 + ncols], self.uT[:, k, c0:c1], k == 0, k == 7, [self.Wkey, self.ukey + ':%d' % ti], [bkey])
            post(b[0:ncols, 0:n], n, c0, c1, ti, bkey)

    def job_lin(self, col0, ncols_total, row0, scale, kind='b', tiles=TILES, ncols_out=TL):
        P = self.P
        for j in range(ncols_total // 128):
            if j % 4 == 0:
                self.load_w(col0 + j * 128, min(512, ncols_total - j * 128))
            st, sk = self.stage(kind)

            def post(ps, n, c0, c1, ti, bkey, st=st, sk=sk):
                P.dve('tensor_scalar', [bkey], [sk, bkey], out=st[:, c0:c1], in0=ps, scalar1=float(scale), scalar2=None, op0=ALU.mult)
            self.chunk(col0 + j * 128, 128, post, tiles)
            self.outdma(kind, st, sk, row0 + j * 128, 128, ncols_out)

    def job_silu(self, col0, ncols_total, row0, kind, tiles=TILES, ncols_out=TL):
        P = self.P
        for j in range(ncols_total // 128):
            if j % 4 == 0:
                self.load_w(col0 + j * 128, min(512, ncols_total - j * 128))
            st, sk = self.stage(kind)

            def post(ps, n, c0, c1, ti, bkey, st=st, sk=sk):
                P.act(st[:, c0:c1], ps, AF.Silu, [bkey], [sk, bkey])
            self.chunk(col0 + j * 128, 128, post, tiles)
            self.outdma(kind, st, sk, row0 + j * 128, 128, ncols_out)


def build_A0():
    nc = bass.Bass("TRN2", target_bir_lowering=False)
    xT = nc.dram_tensor("xT", [128, 8, TL], F32, kind="ExternalInput").ap()
    condT = nc.dram_tensor("condT", [128, 8, 2], F32, kind="ExternalInput").ap()
    ada_w = nc.dram_tensor("ada_w", [1024, 6144], F32, kind="ExternalInput").ap()
    adab = nc.dram_tensor("adab", [128, 48], F32, kind="ExternalInput").ap()
    gmix = nc.dram_tensor("gmix", [128, 8], F32, kind="ExternalInput").ap()
    w_in = nc.dram_tensor("w_in", [1024, 4128], F32, kind="ExternalInput").ap()
    gate_w = nc.dram_tensor("gate_w", [16, 2, 256], F32, kind="ExternalInput").ap()
    gate_b = nc.dram_tensor("gate_b", [128, 2, 2], F32, kind="ExternalInput").ap()
    hglb = nc.dram_tensor("hglb", [128, 2, 2, 4], F32, kind="ExternalInput").ap()
    o_bf = nc.dram_tensor("o_bf", [A0_BF['end'], TL], BF16, kind="ExternalOutput").ap()
    o_f = nc.dram_tensor("o_f", [A0_F['end'], TL], F32, kind="ExternalOutput").ap()
    ada_w1 = nc.dram_tensor("ada_w1", [1024, 6144], F32, kind="ExternalInput").ap()
    adab1 = nc.dram_tensor("adab1", [128, 48], F32, kind="ExternalInput").ap()
    mods_out = nc.dram_tensor("mods_out", [2, 128, 48, 2], F32, kind="ExternalOutput").ap()

    P = Prog(nc)
    hT = P.sb("hT", [128, 8, TL], F32)
    uT = P.sb("uT", [128, 8, TL], BF16)
    ones_bf = P.sb("ones_bf", [128, 128], BF16)
    work = dict(sq=P.sb("sq", [128, 8, 512], BF16), rs=P.sb("rs", [128, 512], F32),
                tmp=[P.sb("tmp%d" % i, [128, 512], F32) for i in range(2)])
    banks = [P.ps("bank%d" % i, [128, 512], F32) for i in range(6)]
    pm, ps_ssq = banks[0], banks[1]
    gb = banks[2:6]

    for k in range(8):
        P.dma('sp' if k % 2 == 0 else 'act', writes=['h:%d' % t for t in range(5)], out=hT[:, k, :], in_=xT[:, k, :])
    P.pool('memset', [], ['ones'], ap=ones_bf[:], constant=1.0)

    mod = emit_mods(P, condT, ada_w, adab, pm, '0')
    modB = emit_mods(P, condT, ada_w1, adab1, pm, '1')
    P.dma('sp', reads=['mod0'], out=mods_out[0], in_=mod[:])
    P.dma('sp', reads=['mod1'], out=mods_out[1], in_=modB[:])
    A = emit_affine(P, mod, 'mod0', gmix, 1, 'm0')
    for ti, tile in enumerate(TILES):
        emit_modulate_tile(P, hT, 'h', A, 'Am0', mod, 'mod0', 0, ti, tile, ones_bf, ps_ssq, work, uT=uT, ukey='u')

    gw_f = P.sb("gw_f", [16, 2, 256], F32)
    gw = P.sb("gw", [16, 2, 256], BF16)
    gbias = P.sb("gbias", [128, 2, 2], F32)
    ngb = P.sb("ngb", [128, 2, 2], F32)
    lbr = P.sb("lbr", [128, 2, 2, 4], F32)
    lbe = P.sb("lbe", [128, 2, 2, 4], F32)
    lb = P.sb("lb", [128, 2, 4], F32)
    oml = P.sb("oml", [128, 2, 4], F32)
    den = P.sb("den", [128, 2, 4], F32)
    P.dma('sp', writes=['gw_f'], out=gw_f[:], in_=gate_w)
    P.dma('sp', writes=['gbias'], out=gbias[:], in_=gate_b)
    P.dma('sp', writes=['lbr'], out=lbr[:], in_=hglb)
    P.dve('tensor_copy', ['gw_f'], ['gw'], out=gw[:], in_=gw_f[:])
    P.dve('tensor_scalar', ['gbias'], ['ngb'], out=ngb[:], in0=gbias[:], scalar1=-1.0, scalar2=None, op0=ALU.mult)
    P.act(lbe[:], lbr[:], AF.Exp, ['lbr'], ['lbe'])
    P.dve('tensor_tensor', ['lbe'], ['den'], out=den[:], in0=lbe[:, :, 0, :], in1=lbe[:, :, 1, :], op=ALU.add)
    P.dve('reciprocal', ['den'], ['den'], out=den[:], in_=den[:])
    P.dve('tensor_tensor', ['lbe', 'den'], ['lb'], out=lb[:], in0=lbe[:, :, 0, :], in1=den[:], op=ALU.mult)
    P.dve('tensor_scalar', ['lb'], ['oml'], out=oml[:], in0=lb[:], scalar1=-1.0, scalar2=1.0, op0=ALU.mult, op1=ALU.add)

    glrT = [P.sb("glrT%d" % d, [16, TL], BF16) for d in range(2)]
    ftmp = [P.sb("ftmp%d" % i, [128, 512], F32) for i in range(2)]
    C = ProjCtx(P, w_in, uT, 'u', gb, o_f, o_bf)

    def job_glr(col0, d):
        def post(ps, n, c0, c1, ti, bkey):
            P.dve('tensor_copy', [bkey], ['glrT%d' % d], out=glrT[d][:, c0:c1], in_=ps)
        C.load_w(col0, 16)
        C.chunk(col0, 16, post)

    def job_gate(d, row0):
        for j in range(2):
            st, sk = C.stage('f')
            for ti, (c0, c1, r) in enumerate(TILES):
                n = c1 - c0
                b, bkey = C.bank()
                P.mm(b[:, 0:n], gw[:, d, j * 128:(j + 1) * 128], glrT[d][:, c0:c1], True, True, ['gw', 'glrT%d' % d], [bkey])
                ft = ftmp[ti % 2]
                fk = 'ftmp%d' % (ti % 2)
                P.act(ft[:, 0:n], b[:, 0:n], AF.Exp, [bkey, 'ngb'], [fk], bias=ngb[:, d, j:j + 1], scale=-1.0)
                P.act(ft[:, 0:n], ft[:, 0:n], AF.Ln, [fk], [fk], bias=1.0, scale=1.0)
                P.dve('tensor_scalar', [fk], [sk], out=st[:, c0:c1], in0=ft[:, 0:n], scalar1=-1.0 / 16.0, scalar2=None, op0=ALU.mult)
            C.outdma('f', st, sk, row0 + j * 128, 128)

    def job_hgf(col0, d, row_k, row_la):
        C.load_w(col0, 512)
        for j in range(4):
            stb, skb = C.stage('b')
            stf, skf = C.stage('f')

            def post(ps, n, c0, c1, ti, bkey, stb=stb, skb=skb, stf=stf, skf=skf, j=j):
                ft = ftmp[ti % 2]
                fk = 'ftmp%d' % (ti % 2)
                P.act(ft[:, 0:n], ps, AF.Sigmoid, [bkey], [fk])
                P.dve('tensor_scalar', [fk, 'oml', 'lb'], [fk], out=ft[:, 0:n], in0=ft[:, 0:n], scalar1=oml[:, d, j:j + 1], scalar2=lb[:, d, j:j + 1], op0=ALU.mult, op1=ALU.add)
                P.pool('tensor_scalar', [fk], [skb], out=stb[:, c0:c1], in0=ft[:, 0:n], scalar1=-1.0, scalar2=1.0, op0=ALU.mult, op1=ALU.add)
                P.act(stf[:, c0:c1], ft[:, 0:n], AF.Ln, [fk], [skf])
            C.chunk(col0 + j * 128, 128, post)
            C.outdma('b', stb, skb, row_k + j * 128, 128)
            C.outdma('f', stf, skf, row_la + j * 128, 128)

    job_glr(AB['glr_f'], 0)
    job_glr(AB['glr_b'], 1)
    C.job_lin(AB['gq'], 256, A0_BF['gq'], 64 ** -0.5)
    C.job_lin(AB['gk'], 256, A0_BF['gk'], 1.0)
    C.job_lin(AB['gv'], 512, A0_BF['gv'], 1.0)
    C.job_silu(AB['gg'], 512, A0_F['gg'], 'f')
    job_gate(0, A0_F['gla_f'])
    job_gate(1, A0_F['gla_b'])
    C.job_silu(AB['hq'], 512, A0_BF['hq'], 'b')
    job_hgf(AB['hf_f'], 0, A0_BF['hk_f'], A0_F['hla_f'])
    job_hgf(AB['hf_b'], 1, A0_BF['hk_b'], A0_F['hla_b'])
    C.job_lin(AB['hi'], 512, A0_BF['hi'], 1.0)
    C.job_silu(AB['hg'], 512, A0_F['hg'], 'f')
    P.wait_all('sp')
    P.emit()
    P.close()
    return nc


def fm(a):
    T = a.shape[0]
    return np.ascontiguousarray(a.T.reshape(8, 128, T).transpose(1, 0, 2))


def colT(v, nk):
    return np.ascontiguousarray(np.asarray(v).reshape(nk, 128).T)


def core_tokens(x, ctx, i):
    b, q = i // 4, i % 4
    return np.concatenate([x[b, 2048 * q:2048 * (q + 1)], ctx[b, 64 * q:64 * (q + 1)]], axis=0)


def cond_T(inp, b):
    cond = np.stack([inp['c'][b], inp['c_ctx']], axis=-1)
    return np.ascontiguousarray(cond.reshape(8, 128, 2).transpose(1, 0, 2))


def host_A0_inputs(inp):
    maps = []
    x, ctx = inp['x'], inp['ctx']
    for i in range(NCORES):
        b = i // 4
        m = dict(
            xT=fm(core_tokens(x, ctx, i)),
            condT=cond_T(inp, b),
            ada_w=inp['ada_w'][0], adab=colT(inp['ada_b'][0], 48), gmix=colT(inp['norm_mix_g'][0], 8),
            ada_w1=inp['ada_w'][1], adab1=colT(inp['ada_b'][1], 48),
            w_in=inp['ab_w_in'][0],
            gate_w=np.ascontiguousarray(inp['gla_gate_w'][0].transpose(1, 0, 2)),
            gate_b=np.ascontiguousarray(inp['gla_gate_b'][0].reshape(2, 2, 128).transpose(2, 0, 1)),
            hglb=np.ascontiguousarray(inp['hg_lb'].reshape(2, 2, 4, 128).transpose(3, 0, 1, 2)),
        )
        maps.append(m)
    return maps


STOP_STAGE = 9
NSCAN = 4
VAR7 = 7
NCH = 132
SCH = 4
NSC = NCH // SCH


def scan_consts(P, tri_dram):
    tri = P.sb("tri_sb", [64, 6, 64], F32)
    P.dma('sp', writes=['tri'], out=tri[:], in_=tri_dram)
    return tri


def emit_scans(P, scans, tri, banks, nsteps=None):
    ns = len(scans)
    st = []
    for i, sc in enumerate(scans):
        dk, dv = sc['dk'], sc['dv']
        d = dict(sc)
        d['i'] = i
        d['in'] = []
        for par in range(2):
            n = "s%dp%d" % (i, par)
            d['in'].append(dict(
                qT=P.sb("qT" + n, [dk, SCH * 64], BF16), kT=P.sb("kT" + n, [dk, SCH * 64], BF16),
                ktm=P.sb("ktm" + n, [64, SCH, dk], BF16), vtm=P.sb("vtm" + n, [64, SCH, dv], BF16),
                latm=P.sb("latm" + n, [64, SCH, dk], F32), o=P.sb("o" + n, [64, SCH, dv], F32), key="in" + n, okey="o" + n))
        d['scr'] = []
        for par in range(2):
            n = "s%dq%d" % (i, par)
            d['scr'].append(dict(
                e1=P.sb("e1" + n, [dk, 64], F32), e2=P.sb("e2" + n, [dk, 64], F32), e3=P.sb("e3" + n, [64, dk], F32),
                qe=P.sb("qe" + n, [dk, 64], BF16), ke=P.sb("ke" + n, [dk, 64], BF16), kd=P.sb("kd" + n, [64, dk], BF16),
                attm=P.sb("attm" + n, [64, 64], BF16), dS=P.sb("dS" + n, [64, 64], F32), cc=P.sb("cc" + n, [64, 2], F32), n=n, bank=banks[2 * i + par]))
        d['Sf'] = P.sb("Sf%d" % i, [dk, dv], F32)
        d['Sb'] = P.sb("Sb%d" % i, [dk, dv], BF16)
        P.pool('memset', [], ['Sf%d' % i], ap=d['Sf'][:], constant=0.0)
        P.pool('memset', [], ['Sb%d' % i], ap=d['Sb'][:], constant=0.0)
        st.append(d)

    def sc_order(rev):
        if not rev:
            return [(s, list(range(SCH))) for s in range(NSC)]
        return [(0, list(range(SCH - 1, -1, -1)))] + [(s, list(range(SCH - 1, -1, -1))) for s in range(NSC - 1, 0, -1)]

    orders = [sc_order(d['rev']) for d in st]
    dq = [0]

    def load(d, step):
        s, _ = orders[d['i']][step]
        b = d['in'][step % 2]
        t0, t1 = s * SCH * 64, (s + 1) * SCH * 64
        c0, c1 = s * SCH, (s + 1) * SCH
        for nm, src in (('qT', d['qT'][:, t0:t1]), ('kT', d['kT'][:, t0:t1]), ('ktm', d['ktm'][:, c0:c1, :]), ('vtm', d['vtm'][:, c0:c1, :]), ('latm', d['latm'][:, c0:c1, :])):
            q = ['sp', 'act', 'pool'][dq[0] % 3]
            dq[0] += 1
            P.dma(q, reads=d.get('rkeys', []), writes=[b['key'] + nm], out=b[nm][:], in_=src)

    def store(d, step):
        s, _ = orders[d['i']][step]
        b = d['in'][step % 2]
        q = ['sp', 'act', 'pool'][dq[0] % 3]
        dq[0] += 1
        P.dma(q, reads=[b['okey']], writes=[d['okey']], out=d['o'][:, s * SCH:(s + 1) * SCH, :], in_=b['o'][:])

    for d in st:
        load(d, 0)
    cidx = 0
    NS = NSC if nsteps is None else nsteps
    for step in range(NS):
        for d in st:
            if step + 1 < NS:
                load(d, step + 1)
        for j in range(SCH):
            work = []
            for d in st:
                s, chs = orders[d['i']][step]
                c = chs[j]
                b = d['in'][step % 2]
                w = d['scr'][cidx % 2]
                work.append((d, b, w, c))
            cidx += 1
            for d, b, w, c in work:
                dk, dv, rev = d['dk'], d['dv'], d['rev']
                bk = w['bank']
                kb = 'bank' + w['n']
                lac = b['latm'][:, c, :]
                triI = tri[:, 1 if rev else 0, :]
                triS = tri[:, 3 if rev else 2, :]
                P.mm(bk[0:dk, 0:64], lac, triI, True, True, [b['key'] + 'latm', 'tri'], [kb])
                P.mm(bk[0:64, 128:128 + dk], triS, lac, True, True, [b['key'] + 'latm', 'tri'], [kb])
                if d['mode'] == 'scalar':
                    P.mm(bk[0:64, 320:322], triI, lac[:, 0:2], True, True, [b['key'] + 'latm', 'tri'], [kb])
            for d, b, w, c in (work if STOP_STAGE >= 2 else []):
                dk, dv, rev = d['dk'], d['dv'], d['rev']
                bk = w['bank']
                kb = 'bank' + w['n']
                n = w['n']
                P.act(w['e1'][:], bk[0:dk, 0:64], AF.Exp, [kb], ['e1' + n, kb])
                if d['mode'] == 'vec':
                    P.act(w['e2'][:], bk[0:dk, 0:64], AF.Exp, [kb], ['e2' + n, kb], scale=-1.0)
                P.act(w['e3'][:], bk[0:64, 128:128 + dk], AF.Exp, [kb], ['e3' + n, kb])
            for d, b, w, c in (work if STOP_STAGE >= 3 else []):
                dk, dv, rev = d['dk'], d['dv'], d['rev']
                n = w['n']
                bk = w['bank']
                kb = 'bank' + w['n']
                qc = b['qT'][:, c * 64:(c + 1) * 64]
                kc = b['kT'][:, c * 64:(c + 1) * 64]
                P.dve('tensor_tensor', [b['key'] + 'qT', 'e1' + n], ['qe' + n], out=w['qe'][:], in0=qc, in1=w['e1'][:], op=ALU.mult)
                if d['mode'] == 'vec':
                    P.dve('tensor_tensor', [b['key'] + 'kT', 'e2' + n], ['ke' + n], out=w['ke'][:], in0=kc, in1=w['e2'][:], op=ALU.mult)
                else:
                    P.dve('tensor_copy', [kb], ['cc' + n, kb], out=w['cc'][:], in_=bk[0:64, 320:322])
                    P.dve('scalar_tensor_tensor', [kb, 'cc' + n, 'tri'], ['dS' + n, kb], out=w['dS'][:], in0=bk[0:64, 0:64], scalar=w['cc'][:, 0:1],
                          in1=tri[:, 5 if rev else 4, :], op0=ALU.subtract, op1=ALU.add)
                P.pool('tensor_tensor', [b['key'] + 'ktm', 'e3' + n], ['kd' + n], out=w['kd'][:], in0=b['ktm'][:, c, :], in1=w['e3'][:], op=ALU.mult)
            for d, b, w, c in (work if STOP_STAGE >= 4 else []):
                dk = d['dk']
                n = w['n']
                bk = w['bank']
                kb = 'bank' + w['n']
                if d['mode'] == 'vec':
                    P.mm(bk[0:64, 64:128], w['ke'][:], w['qe'][:], True, True, ['ke' + n, 'qe' + n], [kb])
                else:
                    P.mm(bk[0:64, 64:128], b['kT'][:, c * 64:(c + 1) * 64], b['qT'][:, c * 64:(c + 1) * 64], True, True, [b['key'] + 'kT', b['key'] + 'qT'], [kb])
                    P.act(w['dS'][:], w['dS'][:], AF.Exp, ['dS' + n], ['dS' + n])
            for d, b, w, c in (work if STOP_STAGE >= 5 else []):
                n = w['n']
                bk = w['bank']
                kb = 'bank' + w['n']
                if d['mode'] == 'vec':
                    P.dve('tensor_tensor', [kb, 'tri'], ['attm' + n, kb], out=w['attm'][:], in0=bk[0:64, 64:128], in1=tri[:, 1 if d['rev'] else 0, :], op=ALU.mult)
                else:
                    P.dve('tensor_tensor', [kb, 'dS' + n], ['attm' + n, kb], out=w['attm'][:], in0=bk[0:64, 64:128], in1=w['dS'][:], op=ALU.mult)
            for d, b, w, c in (work if STOP_STAGE >= 6 else []):
                dk, dv = d['dk'], d['dv']
                n = w['n']
                i = d['i']
                bk = w['bank']
                kb = 'bank' + w['n']
                P.mm(bk[0:64, 384:384 + dv], w['qe'][:], d['Sb'][:], True, False, ['qe' + n, 'Sb%d' % i], [kb])
                P.mm(bk[0:64, 384:384 + dv], w['attm'][:], b['vtm'][:, c, :], False, True, ['attm' + n, b['key'] + 'vtm'], [kb])
                P.mm(bk[0:dk, 256:256 + dv], w['kd'][:], b['vtm'][:, c, :], True, True, ['kd' + n, b['key'] + 'vtm'], [kb])
            for d, b, w, c in (work if STOP_STAGE >= 7 else []):
                dk, dv, rev = d['dk'], d['dv'], d['rev']
                n = w['n']
                i = d['i']
                bk = w['bank']
                kb = 'bank' + w['n']
                if VAR7 & 1:
                    P.act(b['o'][:, c, :], bk[0:64, 384:384 + dv], AF.Copy, [kb], [b['okey'], kb])
                el = w['e1'][:, 0:1] if rev else w['e1'][:, 63:64]
                if VAR7 & 2:
                    P.dve('scalar_tensor_tensor', [kb, 'e1' + n, 'Sf%d' % i], ['Sf%d' % i, kb], out=d['Sf'][:], in0=d['Sf'][:], scalar=el, in1=bk[0:dk, 256:256 + dv], op0=ALU.mult, op1=ALU.add)
                if VAR7 & 4:
                    P.pool('tensor_copy', ['Sf%d' % i], ['Sb%d' % i], out=d['Sb'][:], in_=d['Sf'][:])
        for d in st:
            store(d, step)


def tri_consts():
    s = np.arange(64)[:, None]
    t = np.arange(64)[None, :]
    NEG = -30000.0
    m = np.stack([(s <= t), (s >= t), (s > t), (s < t)]).astype(np.float32)
    n = np.stack([np.where(s <= t, 0.0, NEG), np.where(s >= t, 0.0, NEG)]).astype(np.float32)
    return np.ascontiguousarray(np.concatenate([m, n], axis=0).transpose(1, 0, 2))


def build_B0(nsteps=None):
    nc = bass.Bass("TRN2", target_bir_lowering=False)
    T = NCH * 64
    tri_d = nc.dram_tensor("tri", [64, 6, 64], F32, kind="ExternalInput").ap()
    scans = []
    for nm, dk in (('g', 64), ('h', 128)):
        qT = nc.dram_tensor(nm + "_qT", [dk, T], BF16, kind="ExternalInput").ap()
        vtm = nc.dram_tensor(nm + "_vtm", [64, NCH, 128], BF16, kind="ExternalInput").ap()
        nk = 1 if nm == 'g' else 2
        kT = [nc.dram_tensor(nm + "_kT%d" % d, [dk, T], BF16, kind="ExternalInput").ap() for d in range(nk)]
        ktm = [nc.dram_tensor(nm + "_ktm%d" % d, [64, NCH, dk], BF16, kind="ExternalInput").ap() for d in range(nk)]
        for d in range(2):
            latm = nc.dram_tensor(nm + "_latm%d" % d, [64, NCH, dk], F32, kind="ExternalInput").ap()
            o = nc.dram_tensor(nm + "_o%d" % d, [64, NCH, 128], F32, kind="ExternalOutput").ap()
            scans.append(dict(dk=dk, dv=128, mode='vec', rev=(d == 1), qT=qT, kT=kT[d % nk], ktm=ktm[d % nk], vtm=vtm, latm=latm, o=o, okey='out_%s%d' % (nm, d)))
    P = Prog(nc)
    tri = scan_consts(P, tri_d)
    banks = [P.ps("bank%d" % i, [128, 512], F32) for i in range(8)]
    emit_scans(P, scans[:NSCAN], tri, banks, nsteps)
    P.wait_all('sp')
    P.emit()
    P.close()
    return nc


def joint_fm(outs, row0, nrows, b):
    parts_ctx = [outs[4 * b + q][row0:row0 + nrows, 2048:2112] for q in range(4)]
    parts_lat = [outs[4 * b + q][row0:row0 + nrows, 0:2048] for q in range(4)]
    return np.concatenate(parts_ctx + parts_lat, axis=1)


def to_tm(aT):
    w, T = aT.shape
    return np.ascontiguousarray(aT.T.reshape(T // 64, 64, w).transpose(1, 0, 2))


def host_B0_inputs(a0):
    obf = [r['o_bf'] for r in a0]
    of = [r['o_f'] for r in a0]
    tri = tri_consts()
    maps = []
    for i in range(NCORES):
        b, hd = i // 4, i % 4
        m = dict(tri=tri)
        gq = joint_fm(obf, A0_BF['gq'] + 64 * hd, 64, b)
        gk = joint_fm(obf, A0_BF['gk'] + 64 * hd, 64, b)
        gv = joint_fm(obf, A0_BF['gv'] + 128 * hd, 128, b)
        m['g_qT'] = gq
        m['g_kT0'] = gk
        m['g_ktm0'] = to_tm(gk)
        m['g_vtm'] = to_tm(gv)
        for d, nm in enumerate(('gla_f', 'gla_b')):
            m['g_latm%d' % d] = to_tm(joint_fm(of, A0_F[nm] + 64 * hd, 64, b))
        m['h_qT'] = joint_fm(obf, A0_BF['hq'] + 128 * hd, 128, b)
        m['h_vtm'] = to_tm(joint_fm(obf, A0_BF['hi'] + 128 * hd, 128, b))
        for d, (nk, nl) in enumerate((('hk_f', 'hla_f'), ('hk_b', 'hla_b'))):
            hk = joint_fm(obf, A0_BF[nk] + 128 * hd, 128, b)
            m['h_kT%d' % d] = hk
            m['h_ktm%d' % d] = to_tm(hk)
            m['h_latm%d' % d] = to_tm(joint_fm(of, A0_F[nl] + 128 * hd, 128, b))
        maps.append(m)
    return maps


CD = dict(hy=0, z=1536, xbc=2048, dt=3072, end=3088)
A1_BF = dict(hy=0, xbc=1536, end=2560)
A1_F = dict(zs=0, dt=512, la=528, end=544)


def barrier(P):
    for e in ENGS:
        P.wait_all(e)


def build_C(layer, nexp=16):
    nc = bass.Bass("TRN2", target_bir_lowering=False)
    T = TL if layer == 0 else 2048
    tiles = TILES if layer == 0 else TILES[:4]
    NT = len(tiles)
    din = lambda name, shape, dt=F32: nc.dram_tensor(name, list(shape), dt, kind="ExternalInput").ap()
    dout = lambda name, shape, dt=F32: nc.dram_tensor(name, list(shape), dt, kind="ExternalOutput").ap()
    hT_in = din("hT_in", [128, 8, T])
    mod_in = din("mod_in", [128, 48, 2])
    gffn = din("gffn", [128, 8])
    w_out = din("w_out", [1024, 1024])
    rw_d = din("router_w", [128, 8, 16])
    rb_d = din("router_b", [128, 16])
    sel_d = din("sel16", [16, 16, 128])
    identd = din("ident", [128, 128])
    wg_d = din("moe_wg", [16, 1024, 1024])
    wu_d = din("moe_wu", [16, 1024, 1024])
    wd_d = din("moe_wd", [16, 1024, 1024])
    if layer == 0:
        oT_d = din("oT", [8, 128, 2, T])
        gate_d = din("gateT", [8, 128, T])
        ng_d = din("ng", [128, 2])
        mod1_in = din("mod1_in", [128, 48, 2])
        gmix1 = din("gmix1", [128, 8])
        w_in1 = din("w_in1", [1024, 3088])
        dtp_d = din("dtp", [16, 2])
        hT_out = dout("hT_out", [128, 8, T])
        o_bf = dout("o_bf", [A1_BF['end'], T], BF16)
        o_f = dout("o_f", [A1_F['end'], T])
    else:
        hy_d = din("hyT", [128, 4, T])
        so_d = din("ssd_oT", [2, 128, 4, T])
        xs_d = din("xsT", [128, 4, T])
        zs_d = din("zsT", [128, 4, T])
        dsk_d = din("dsk", [128, 4])
        mg_d = din("mbg", [128, 4])
        gout_d = din("gout", [128, 8])
        y_out = dout("y_out", [128, 8, T])

    P = Prog(nc)
    hT = P.sb("hT", [128, 8, T], F32)
    fT = P.sb("fT", [128, 8, T], BF16)
    ones_bf = P.sb("ones_bf", [128, 128], BF16)
    ident = P.sb("ident_sb", [128, 128], F32)
    work = dict(sq=P.sb("sq", [128, 8, 512], BF16), rs=P.sb("rs", [128, 512], F32),
                tmp=[P.sb("tmp%d" % i, [128, 512], F32) for i in range(2)])
    banks = [P.ps("bank%d" % i, [128, 512], F32) for i in range(8)]
    pm, ps_ssq = banks[0], banks[1]
    for k in range(8):
        P.dma('sp' if k % 2 == 0 else 'act', writes=['h:%d' % t for t in range(NT)], out=hT[:, k, :], in_=hT_in[:, k, :])
    P.pool('memset', [], ['ones'], ap=ones_bf[:], constant=1.0)
    P.dma('sp', writes=['ident'], out=ident[:], in_=identd)
    mod = P.sb("sbmodL", [128, 48, 2], F32)
    P.dma('sp', writes=['modL'], out=mod[:], in_=mod_in)

    mark = len(P.stack)
    if layer == 0:
        ng = P.sb("ng_sb", [128, 2], F32)
        P.dma('sp', writes=['ng'], out=ng[:], in_=ng_d)
        ob = [P.sb("ob%d" % i, [128, 2, 512], F32) for i in range(2)]
        gbf = [P.sb("gbf%d" % i, [128, 512], F32) for i in range(2)]
        osum = [P.sb("osum%d" % i, [128, 512], F32) for i in range(2)]
        it = 0
        for hh in range(8):
            for ti, (c0, c1, r) in enumerate(tiles):
                n = c1 - c0
                i2 = it % 2
                it += 1
                P.dma('sp', writes=['ob%d' % i2], out=ob[i2][:, :, 0:n], in_=oT_d[hh, :, :, c0:c1])
                P.dma('act', writes=['gbf%d' % i2], out=gbf[i2][:, 0:n], in_=gate_d[hh, :, c0:c1])
                P.pool('tensor_tensor', ['ob%d' % i2], ['osum%d' % i2], out=osum[i2][:, 0:n], in0=ob[i2][:, 0, 0:n], in1=ob[i2][:, 1, 0:n], op=ALU.add)
                emit_rstd(P, osum[i2][:, 0:n].unsqueeze(1), 'osum%d' % i2, n, 1, ones_bf, ps_ssq, work['sq'], work['rs'], 1.0 / 128)
                tb = work['tmp'][i2]
                P.dve('scalar_tensor_tensor', ['osum%d' % i2, 'ng', 'rs'], ['tmp%d' % i2], out=tb[:, 0:n], in0=osum[i2][:, 0:n], scalar=ng[:, hh // 4:hh // 4 + 1], in1=work['rs'][:, 0:n], op0=ALU.mult, op1=ALU.mult)
                P.pool('tensor_tensor', ['tmp%d' % i2, 'gbf%d' % i2], ['f:%d' % ti], out=fT[:, hh, c0:c1], in0=tb[:, 0:n], in1=gbf[i2][:, 0:n], op=ALU.mult)
    else:
        dsk = P.sb("dsk_sb", [128, 4], F32)
        mg = P.sb("mg_sb", [128, 4], F32)
        P.dma('sp', writes=['dsk'], out=dsk[:], in_=dsk_d)
        P.dma('sp', writes=['mg'], out=mg[:], in_=mg_d)
        lb4 = [P.sb("lb4_%d" % i, [128, 4, 512], F32) for i in range(3)]
        yb = P.sb("yb", [128, 4, 512], F32)
        for ti, (c0, c1, r) in enumerate(tiles):
            n = c1 - c0
            P.dma('sp', writes=['lb4_0'], out=lb4[0][:, :, 0:n], in_=hy_d[:, :, c0:c1])
            P.pool('tensor_copy', ['lb4_0'], ['f:%d' % ti], out=fT[:, 0:4, c0:c1], in_=lb4[0][:, :, 0:n])
            P.dma('sp', writes=['lb4_1'], out=lb4[1][:, :, 0:n], in_=so_d[0, :, :, c0:c1])
            P.dma('act', writes=['lb4_2'], out=lb4[2][:, :, 0:n], in_=so_d[1, :, :, c0:c1])
            P.pool('tensor_tensor', ['lb4_1', 'lb4_2'], ['yb'], out=yb[:, :, 0:n], in0=lb4[1][:, :, 0:n], in1=lb4[2][:, :, 0:n], op=ALU.add)
            P.dma('sp', writes=['lb4_1'], out=lb4[1][:, :, 0:n], in_=xs_d[:, :, c0:c1])
            P.dma('act', writes=['lb4_2'], out=lb4[2][:, :, 0:n], in_=zs_d[:, :, c0:c1])
            for j in range(4):
                P.dve('scalar_tensor_tensor', ['lb4_1', 'dsk', 'yb'], ['yb'], out=yb[:, j, 0:n], in0=lb4[1][:, j, 0:n], scalar=dsk[:, j:j + 1], in1=yb[:, j, 0:n], op0=ALU.mult, op1=ALU.add)
            P.pool('tensor_tensor', ['yb', 'lb4_2'], ['yb'], out=yb[:, :, 0:n], in0=yb[:, :, 0:n], in1=lb4[2][:, :, 0:n], op=ALU.mult)
            for g in range(2):
                emit_rstd(P, yb[:, 2 * g:2 * g + 2, 0:n], 'yb', n, 2, ones_bf, ps_ssq, work['sq'], work['rs'], 1.0 / 256)
                for j in (2 * g, 2 * g + 1):
                    P.dve('scalar_tensor_tensor', ['yb', 'mg', 'rs'], ['f:%d' % ti], out=fT[:, 4 + j, c0:c1], in0=yb[:, j, 0:n], scalar=mg[:, j:j + 1], in1=work['rs'][:, 0:n], op0=ALU.mult, op1=ALU.mult)
    barrier(P)
    while len(P.stack) > mark:
        P.stack.pop().__exit__(None, None, None)

    wo = P.sb("wo", [128, 8, 1024], BF16)
    wov = w_out.rearrange("(k p) n -> p k n", p=128)
    for k in range(8):
        P.dma('pool', writes=['wo'], out=wo[:, k, :], in_=wov[:, k, :])
    gcnt = [0]
    gbk = banks[2:6]

    def bank():
        i = gcnt[0] % 4
        gcnt[0] += 1
        return gbk[i], 'gb%d' % i
    for dc in range(8):
        for ti, (c0, c1, r) in enumerate(tiles):
            n = c1 - c0
            b, bk = bank()
            for k in range(8):
                P.mm(b[:, 0:n], wo[:, k, dc * 128:(dc + 1) * 128], fT[:, k, c0:c1], k == 0, k == 7, ['wo', 'f:%d' % ti], [bk])
            P.dve('scalar_tensor_tensor', [bk, 'modL', 'h:%d' % ti], ['h:%d' % ti, bk], out=hT[:, dc, c0:c1], in0=b[:, 0:n], scalar=mod[:, 16 + dc, r:r + 1], in1=hT[:, dc, c0:c1], op0=ALU.mult, op1=ALU.add)
    barrier(P)
    P.stack.pop().__exit__(None, None, None)

    Af = emit_affine(P, mod, 'modL', gffn, 4, 'f')
    sel = P.sb("sel_sb", [16, 16, 128], F32)
    WT = P.sb("WT", [16, T], F32)
    mark = len(P.stack)
    u32 = P.sb("u32", [128, 8, 512], F32)
    rw = P.sb("rw", [128, 8, 16], F32)
    rb = P.sb("rb", [128, 16], F32)
    P.dma('sp', writes=['rw'], out=rw[:], in_=rw_d)
    P.dma('sp', writes=['rb'], out=rb[:], in_=rb_d)
    P.dma('sp', writes=['sel'], out=sel[:], in_=sel_d)
    rt = {nm: P.sb("rt_" + nm, [128, 16], F32) for nm in ('sc', 'sl', 'eq', 's2', 'ch', 'w')}
    r4 = {nm: P.sb("r4_" + nm, [128, 4], F32) for nm in ('m1', 'm2', 'gs', 'gm')}
    r1 = {nm: P.sb("r1_" + nm, [128, 1], F32) for nm in ('gx', 'ss')}
    v4 = lambda t_: t_[:].rearrange("p (g j) -> p g j", j=4)
    b4 = lambda t_: t_[:].unsqueeze(2).to_broadcast([128, 4, 4])
    rbank, rbk = banks[6], 'rbank'
    for ti, tile in enumerate(tiles):
        c0, c1, r = tile
        n = c1 - c0
        emit_modulate_tile(P, hT, 'h', Af, 'Af', mod, 'modL', 3, ti, tile, ones_bf, ps_ssq, work, uT=fT, ukey='v', u32=u32)
        for sub in range(n // 128 if n >= 128 else 1):
            m = min(128, n)
            s0 = sub * 128
            for k in range(8):
                P.mm(rbank[0:m, 0:16], u32[:, k, s0:s0 + m], rw[:, k, :], k == 0, k == 7, ['u32', 'rw'], [rbk])
            P.act(rt['sc'][0:m, :], rbank[0:m, 0:16], AF.Sigmoid, [rbk], ['rt_sc', rbk])
            P.dve('tensor_tensor', ['rt_sc', 'rb'], ['rt_sl'], out=rt['sl'][0:m], in0=rt['sc'][0:m], in1=rb[0:m], op=ALU.add)
            P.dve('tensor_reduce', ['rt_sl'], ['r4_m1'], out=r4['m1'][0:m], in_=v4(rt['sl'])[0:m], axis=AX.X, op=ALU.max)
            P.dve('tensor_tensor', ['rt_sl', 'r4_m1'], ['rt_eq'], out=v4(rt['eq'])[0:m], in0=v4(rt['sl'])[0:m], in1=b4(r4['m1'])[0:m], op=ALU.is_equal)
            P.dve('scalar_tensor_tensor', ['rt_eq', 'rt_sl'], ['rt_s2'], out=rt['s2'][0:m], in0=rt['eq'][0:m], scalar=-1e9, in1=rt['sl'][0:m], op0=ALU.mult, op1=ALU.add)
            P.dve('tensor_reduce', ['rt_s2'], ['r4_m2'], out=r4['m2'][0:m], in_=v4(rt['s2'])[0:m], axis=AX.X, op=ALU.max)
            P.dve('tensor_tensor', ['r4_m1', 'r4_m2'], ['r4_gs'], out=r4['gs'][0:m], in0=r4['m1'][0:m], in1=r4['m2'][0:m], op=ALU.add)
            P.dve('tensor_reduce', ['r4_gs'], ['r1_gx'], out=r1['gx'][0:m], in_=r4['gs'][0:m], axis=AX.X, op=ALU.max)
            P.dve('tensor_scalar', ['r4_gs', 'r1_gx'], ['r4_gm'], out=r4['gm'][0:m], in0=r4['gs'][0:m], scalar1=r1['gx'][0:m, 0:1], scalar2=None, op0=ALU.is_equal)
            P.dve('tensor_tensor', ['rt_sl', 'r4_m2'], ['rt_ch'], out=v4(rt['ch'])[0:m], in0=v4(rt['sl'])[0:m], in1=b4(r4['m2'])[0:m], op=ALU.is_ge)
            P.dve('tensor_tensor', ['rt_ch', 'r4_gm'], ['rt_ch'], out=v4(rt['ch'])[0:m], in0=v4(rt['ch'])[0:m], in1=b4(r4['gm'])[0:m], op=ALU.mult)
            P.dve('tensor_tensor', ['rt_ch', 'rt_sc'], ['rt_w'], out=rt['w'][0:m], in0=rt['ch'][0:m], in1=rt['sc'][0:m], op=ALU.mult)
            P.dve('tensor_reduce', ['rt_w'], ['r1_ss'], out=r1['ss'][0:m], in_=rt['w'][0:m], axis=AX.X, op=ALU.add)
            P.dve('reciprocal', ['r1_ss'], ['r1_ss'], out=r1['ss'][0:m], in_=r1['ss'][0:m])
            P.dve('tensor_scalar', ['rt_w', 'r1_ss'], ['rt_w'], out=rt['w'][0:m], in0=rt['w'][0:m], scalar1=r1['ss'][0:m, 0:1], scalar2=None, op0=ALU.mult)
            P.op('pe', 'transpose', ['rt_w', 'ident'], [rbk], out=rbank[0:16, 128:128 + m], in_=rt['w'][0:m, :], identity=ident[0:m, 0:m])
            P.act(WT[:, c0 + s0:c0 + s0 + m], rbank[0:16, 128:128 + m], AF.Copy, [rbk], ['WT', rbk])
    barrier(P)
    while len(P.stack) > mark:
        P.stack.pop().__exit__(None, None, None)

    barrier(P)
    while len(P.stack) > mark:
        P.stack.pop().__exit__(None, None, None)
    hid = P.sb("hid", [128, 4, T], BF16)
    wbc = P.sb("wbc", [128, T], F32)
    WG = [P.sb("WG%d" % k, [128, 512], BF16) for k in range(8)]
    WU = [P.sb("WU%d" % k, [128, 512], BF16) for k in range(8)]
    WD = [P.sb("WD%d" % k, [128, 1024], BF16) for k in range(4)]
    sg = [P.sb("sg%d" % i, [128, 512], F32) for i in range(2)]
    bA = [banks[2], banks[3]]
    bB = [banks[4], banks[5]]
    bY = [banks[6], banks[7]]
    it = 0
    for e in range(nexp):
        for ti, (c0, c1, r) in enumerate(tiles):
            n = c1 - c0
            P.mm(ps_ssq[:, 0:n], sel[:, e, :], WT[:, c0:c1], True, True, ['sel', 'WT'], ['ps_ssq'])
            P.act(wbc[:, c0:c1], ps_ssq[:, 0:n], AF.Copy, ['ps_ssq'], ['wbc:%d' % ti, 'ps_ssq'])
        for hf in range(2):
            for k in range(8):
                P.dma('pool', writes=['WG%d' % k], out=WG[k][:], in_=wg_d[e, k * 128:(k + 1) * 128, hf * 512:(hf + 1) * 512])
                P.dma('pool', writes=['WU%d' % k], out=WU[k][:], in_=wu_d[e, k * 128:(k + 1) * 128, hf * 512:(hf + 1) * 512])
            for k in range(4):
                P.dma('pool', writes=['WD%d' % k], out=WD[k][:], in_=wd_d[e, hf * 512 + k * 128:hf * 512 + (k + 1) * 128, :])
            for fc in range(4):
                for ti, (c0, c1, r) in enumerate(tiles):
                    n = c1 - c0
                    i2 = it % 2
                    it += 1
                    for k in range(8):
                        P.mm(bA[i2][:, 0:n], WG[k][:, fc * 128:(fc + 1) * 128], fT[:, k, c0:c1], k == 0, k == 7, ['WG%d' % k, 'v:%d' % ti], ['bA%d' % i2])
                    for k in range(8):
                        P.mm(bB[i2][:, 0:n], WU[k][:, fc * 128:(fc + 1) * 128], fT[:, k, c0:c1], k == 0, k == 7, ['WU%d' % k, 'v:%d' % ti], ['bB%d' % i2])
                    P.act(sg[i2][:, 0:n], bA[i2][:, 0:n], AF.Silu, ['bA%d' % i2], ['sg%d' % i2, 'bA%d' % i2])
                    P.dve('tensor_tensor', ['bB%d' % i2, 'sg%d' % i2], ['sg%d' % i2, 'bB%d' % i2], out=sg[i2][:, 0:n], in0=bB[i2][:, 0:n], in1=sg[i2][:, 0:n], op=ALU.mult)
                    P.pool('tensor_tensor', ['sg%d' % i2, 'wbc:%d' % ti], ['hid:%d' % ti], out=hid[:, fc, c0:c1], in0=sg[i2][:, 0:n], in1=wbc[:, c0:c1], op=ALU.mult)
            for dc in range(8):
                for ti, (c0, c1, r) in enumerate(tiles):
                    n = c1 - c0
                    i2 = it % 2
                    it += 1
                    for k in range(4):
                        P.mm(bY[i2][:, 0:n], WD[k][:, dc * 128:(dc + 1) * 128], hid[:, k, c0:c1], k == 0, k == 3, ['WD%d' % k, 'hid:%d' % ti], ['bY%d' % i2])
                    P.dve('scalar_tensor_tensor', ['bY%d' % i2, 'modL', 'h:%d' % ti], ['h:%d' % ti, 'bY%d' % i2], out=hT[:, dc, c0:c1], in0=bY[i2][:, 0:n], scalar=mod[:, 40 + dc, r:r + 1], in1=hT[:, dc, c0:c1], op0=ALU.mult, op1=ALU.add)
    barrier(P)
    while len(P.stack) > mark:
        P.stack.pop().__exit__(None, None, None)

    if layer == 0:
        for k in range(8):
            P.dma('sp' if k % 2 == 0 else 'act', reads=['h:%d' % t for t in range(NT)], out=hT_out[:, k, :], in_=hT[:, k, :])
        mod1 = P.sb("sbmod1", [128, 48, 2], F32)
        P.dma('sp', writes=['mod1'], out=mod1[:], in_=mod1_in)
        A1 = emit_affine(P, mod1, 'mod1', gmix1, 1, 'm1')
        for ti, tile in enumerate(tiles):
            emit_modulate_tile(P, hT, 'h', A1, 'Am1', mod1, 'mod1', 0, ti, tile, ones_bf, ps_ssq, work, uT=fT, ukey='u1')
        C = ProjCtx(P, w_in1, fT, 'u1', banks[2:6], o_f, o_bf)
        C.job_lin(CD['hy'], 1536, A1_BF['hy'], 1.0)
        C.job_lin(CD['xbc'], 1024, A1_BF['xbc'], 1.0)
        C.job_silu(CD['z'], 512, A1_F['zs'], 'f')
        dtp = P.sb("dtp_sb", [16, 2], F32)
        negA = P.sb("negA", [16, 1], F32)
        P.dma('sp', writes=['dtp'], out=dtp[:], in_=dtp_d)
        P.act(negA[:], dtp[:, 1:2], AF.Exp, ['dtp'], ['negA'])
        P.dve('tensor_scalar', ['negA'], ['negA'], out=negA[:], in0=negA[:], scalar1=-1.0, scalar2=None, op0=ALU.mult)
        st1, sk1 = C.stage('f')
        st2, sk2 = C.stage('f')
        C.load_w(CD['dt'], 16)

        def post(ps, n, c0, c1, ti, bkey):
            P.act(st1[0:16, c0:c1], ps, AF.Exp, [bkey, 'dtp'], [sk1, bkey], bias=dtp[:, 0:1], scale=1.0)
            P.act(st1[0:16, c0:c1], st1[0:16, c0:c1], AF.Ln, [sk1], [sk1], bias=1.0, scale=1.0)
            P.dve('tensor_scalar', [sk1, 'negA'], [sk2], out=st2[0:16, c0:c1], in0=st1[0:16, c0:c1], scalar1=negA[:, 0:1], scalar2=None, op0=ALU.mult)
        C.chunk(CD['dt'], 16, post)
        C.outdma('f', st1, sk1, A1_F['dt'], 16)
        C.outdma('f', st2, sk2, A1_F['la'], 16)
    else:
        gout = P.sb("gout_sb", [128, 8], F32)
        P.dma('sp', writes=['gout'], out=gout[:], in_=gout_d)
        yst = [P.sb("yst%d" % i, [128, 8, 512], F32) for i in range(2)]
        for ti, (c0, c1, r) in enumerate(tiles):
            n = c1 - c0
            emit_rstd(P, hT[:, :, c0:c1], 'h:%d' % ti, n, 8, ones_bf, ps_ssq, work['sq'], work['rs'], 1.0 / 1024)
            for k in range(8):
                P.dve('scalar_tensor_tensor', ['h:%d' % ti, 'gout', 'rs'], ['yst%d' % (ti % 2)], out=yst[ti % 2][:, k, 0:n], in0=hT[:, k, c0:c1], scalar=gout[:, k:k + 1], in1=work['rs'][:, 0:n], op0=ALU.mult, op1=ALU.mult)
            P.dma('sp' if ti % 2 == 0 else 'act', reads=['yst%d' % (ti % 2)], out=y_out[:, :, c0:c1], in_=yst[ti % 2][:, :, 0:n])
    P.wait_all('sp')
    P.emit()
    P.close()
    return nc


def build_C2(layer, nexp=16, CAP=640):
    nc = bass.Bass("TRN2", target_bir_lowering=False)
    T = TL if layer == 0 else 2048
    tiles = TILES if layer == 0 else TILES[:4]
    NT = len(tiles)
    din = lambda name, shape, dt=F32: nc.dram_tensor(name, list(shape), dt, kind="ExternalInput").ap()
    dout = lambda name, shape, dt=F32: nc.dram_tensor(name, list(shape), dt, kind="ExternalOutput").ap()
    hT_in = din("hT_in", [128, 8, T])
    mod_in = din("mod_in", [128, 48, 2])
    gffn = din("gffn", [128, 8])
    w_out = din("w_out", [1024, 1024])
    NSUB = (T + 127) // 128
    NSL = CAP // 128
    I32 = mybir.dt.int32
    tris_d = din("tris", [128, 128], BF16)
    tokhl_d = din("tokhl", [128, NSUB, 2], BF16)
    iota_d = din("iota_s", [128, CAP])
    eoff_d = din("eoff", [128, 16])
    vtm = nc.dram_tensor("vtm_scr", [NSUB * 128, 1024], BF16, kind="Internal").ap()
    y_all = nc.dram_tensor("yall_scr", [16 * CAP, 1024], F32, kind="Internal").ap()
    rw_d = din("router_w", [128, 8, 16])
    rb_d = din("router_b", [128, 16])
    identd = din("ident", [128, 128])
    wg_d = din("moe_wg", [16, 1024, 1024])
    wu_d = din("moe_wu", [16, 1024, 1024])
    wd_d = din("moe_wd", [16, 1024, 1024])
    if layer == 0:
        oT_d = din("oT", [8, 128, 2, T])
        gate_d = din("gateT", [8, 128, T])
        ng_d = din("ng", [128, 2])
        mod1_in = din("mod1_in", [128, 48, 2])
        gmix1 = din("gmix1", [128, 8])
        w_in1 = din("w_in1", [1024, 3088])
        dtp_d = din("dtp", [16, 2])
        hT_out = dout("hT_out", [128, 8, T])
        o_bf = dout("o_bf", [A1_BF['end'], T], BF16)
        o_f = dout("o_f", [A1_F['end'], T])
    else:
        hy_d = din("hyT", [128, 4, T])
        so_d = din("ssd_oT", [2, 128, 4, T])
        xs_d = din("xsT", [128, 4, T])
        zs_d = din("zsT", [128, 4, T])
        dsk_d = din("dsk", [128, 4])
        mg_d = din("mbg", [128, 4])
        gout_d = din("gout", [128, 8])
        y_out = dout("y_out", [128, 8, T])

    P = Prog(nc)
    hT = P.sb("hT", [128, 8, T], F32)
    ones_bf = P.sb("ones_bf", [128, 128], BF16)
    ident = P.sb("ident_sb", [128, 128], F32)
    work = dict(sq=P.sb("sq", [128, 8, 512], BF16), rs=P.sb("rs", [128, 512], F32),
                tmp=[P.sb("tmp%d" % i, [128, 512], F32) for i in range(2)])
    banks = [P.ps("bank%d" % i, [128, 512], F32) for i in range(8)]
    pm, ps_ssq = banks[0], banks[1]
    for k in range(8):
        P.dma('sp' if k % 2 == 0 else 'act', writes=['h:%d' % t for t in range(NT)], out=hT[:, k, :], in_=hT_in[:, k, :])
    P.pool('memset', [], ['ones'], ap=ones_bf[:], constant=1.0)
    P.dma('sp', writes=['ident'], out=ident[:], in_=identd)
    mod = P.sb("sbmodL", [128, 48, 2], F32)
    P.dma('sp', writes=['modL'], out=mod[:], in_=mod_in)

    mark_f = len(P.stack)
    fT = P.sb("fT", [128, 8, T], BF16)
    mark = len(P.stack)
    if layer == 0:
        ng = P.sb("ng_sb", [128, 2], F32)
        P.dma('sp', writes=['ng'], out=ng[:], in_=ng_d)
        ob = [P.sb("ob%d" % i, [128, 2, 512], F32) for i in range(2)]
        gbf = [P.sb("gbf%d" % i, [128, 512], F32) for i in range(2)]
        osum = [P.sb("osum%d" % i, [128, 512], F32) for i in range(2)]
        it = 0
        for hh in range(8):
            for ti, (c0, c1, r) in enumerate(tiles):
                n = c1 - c0
                i2 = it % 2
                it += 1
                P.dma('sp', writes=['ob%d' % i2], out=ob[i2][:, :, 0:n], in_=oT_d[hh, :, :, c0:c1])
                P.dma('act', writes=['gbf%d' % i2], out=gbf[i2][:, 0:n], in_=gate_d[hh, :, c0:c1])
                P.pool('tensor_tensor', ['ob%d' % i2], ['osum%d' % i2], out=osum[i2][:, 0:n], in0=ob[i2][:, 0, 0:n], in1=ob[i2][:, 1, 0:n], op=ALU.add)
                emit_rstd(P, osum[i2][:, 0:n].unsqueeze(1), 'osum%d' % i2, n, 1, ones_bf, ps_ssq, work['sq'], work['rs'], 1.0 / 128)
                tb = work['tmp'][i2]
                P.dve('scalar_tensor_tensor', ['osum%d' % i2, 'ng', 'rs'], ['tmp%d' % i2], out=tb[:, 0:n], in0=osum[i2][:, 0:n], scalar=ng[:, hh // 4:hh // 4 + 1], in1=work['rs'][:, 0:n], op0=ALU.mult, op1=ALU.mult)
                P.pool('tensor_tensor', ['tmp%d' % i2, 'gbf%d' % i2], ['f:%d' % ti], out=fT[:, hh, c0:c1], in0=tb[:, 0:n], in1=gbf[i2][:, 0:n], op=ALU.mult)
    else:
        dsk = P.sb("dsk_sb", [128, 4], F32)
        mg = P.sb("mg_sb", [128, 4], F32)
        P.dma('sp', writes=['dsk'], out=dsk[:], in_=dsk_d)
        P.dma('sp', writes=['mg'], out=mg[:], in_=mg_d)
        lb4 = [P.sb("lb4_%d" % i, [128, 4, 512], F32) for i in range(3)]
        yb = P.sb("yb", [128, 4, 512], F32)
        for ti, (c0, c1, r) in enumerate(tiles):
            n = c1 - c0
            P.dma('sp', writes=['lb4_0'], out=lb4[0][:, :, 0:n], in_=hy_d[:, :, c0:c1])
            P.pool('tensor_copy', ['lb4_0'], ['f:%d' % ti], out=fT[:, 0:4, c0:c1], in_=lb4[0][:, :, 0:n])
            P.dma('sp', writes=['lb4_1'], out=lb4[1][:, :, 0:n], in_=so_d[0, :, :, c0:c1])
            P.dma('act', writes=['lb4_2'], out=lb4[2][:, :, 0:n], in_=so_d[1, :, :, c0:c1])
            P.pool('tensor_tensor', ['lb4_1', 'lb4_2'], ['yb'], out=yb[:, :, 0:n], in0=lb4[1][:, :, 0:n], in1=lb4[2][:, :, 0:n], op=ALU.add)
            P.dma('sp', writes=['lb4_1'], out=lb4[1][:, :, 0:n], in_=xs_d[:, :, c0:c1])
            P.dma('act', writes=['lb4_2'], out=lb4[2][:, :, 0:n], in_=zs_d[:, :, c0:c1])
            for j in range(4):
                P.dve('scalar_tensor_tensor', ['lb4_1', 'dsk', 'yb'], ['yb'], out=yb[:, j, 0:n], in0=lb4[1][:, j, 0:n], scalar=dsk[:, j:j + 1], in1=yb[:, j, 0:n], op0=ALU.mult, op1=ALU.add)
            P.pool('tensor_tensor', ['yb', 'lb4_2'], ['yb'], out=yb[:, :, 0:n], in0=yb[:, :, 0:n], in1=lb4[2][:, :, 0:n], op=ALU.mult)
            for g in range(2):
                emit_rstd(P, yb[:, 2 * g:2 * g + 2, 0:n], 'yb', n, 2, ones_bf, ps_ssq, work['sq'], work['rs'], 1.0 / 256)
                for j in (2 * g, 2 * g + 1):
                    P.dve('scalar_tensor_tensor', ['yb', 'mg', 'rs'], ['f:%d' % ti], out=fT[:, 4 + j, c0:c1], in0=yb[:, j, 0:n], scalar=mg[:, j:j + 1], in1=work['rs'][:, 0:n], op0=ALU.mult, op1=ALU.mult)
    barrier(P)
    while len(P.stack) > mark:
        P.stack.pop().__exit__(None, None, None)

    wo = P.sb("wo", [128, 8, 1024], BF16)
    wov = w_out.rearrange("(k p) n -> p k n", p=128)
    for k in range(8):
        P.dma('pool', writes=['wo'], out=wo[:, k, :], in_=wov[:, k, :])
    gcnt = [0]
    gbk = banks[2:6]

    def bank():
        i = gcnt[0] % 4
        gcnt[0] += 1
        return gbk[i], 'gb%d' % i
    for dc in range(8):
        for ti, (c0, c1, r) in enumerate(tiles):
            n = c1 - c0
            b, bk = bank()
            for k in range(8):
                P.mm(b[:, 0:n], wo[:, k, dc * 128:(dc + 1) * 128], fT[:, k, c0:c1], k == 0, k == 7, ['wo', 'f:%d' % ti], [bk])
            P.dve('scalar_tensor_tensor', [bk, 'modL', 'h:%d' % ti], ['h:%d' % ti, bk], out=hT[:, dc, c0:c1], in0=b[:, 0:n], scalar=mod[:, 16 + dc, r:r + 1], in1=hT[:, dc, c0:c1], op0=ALU.mult, op1=ALU.add)
    barrier(P)
    while len(P.stack) > mark_f:
        P.stack.pop().__exit__(None, None, None)

    Af = emit_affine(P, mod, 'modL', gffn, 4, 'f')
    CH1 = P.sb("CH1", [128, NSUB, 16], F32)
    CH2 = P.sb("CH2", [128, NSUB, 16], F32)
    WW = P.sb("WW", [128, NSUB, 16], F32)
    CHb = P.sb("CHb", [128, NSUB, 16], BF16)
    RP = P.sb("RP", [128, NSUB, 16], F32)
    tris = P.sb("tris_sb", [128, 128], BF16)
    tokhl = P.sb("tokhl_sb", [128, NSUB, 2], BF16)
    iota_s = P.sb("iota_sb", [128, CAP], F32)
    eoff = P.sb("eoff_sb", [128, 16], F32)
    for t_, d_, k_ in ((tris, tris_d, 'tris'), (tokhl, tokhl_d, 'tokhl'), (iota_s, iota_d, 'iota'), (eoff, eoff_d, 'eoff')):
        P.dma('sp', writes=[k_], out=t_[:], in_=d_)
    for t_, k_ in ((CH1, 'CH1'), (CH2, 'CH2'), (WW, 'WW'), (CHb, 'CHb')):
        P.pool('memset', [], [k_], ap=t_[:], constant=0.0)
    mark = len(P.stack)
    u32 = P.sb("u32", [128, 8, 512], F32)
    rw = P.sb("rw", [128, 8, 16], F32)
    rb = P.sb("rb", [128, 16], F32)
    vrow = [P.sb("vrow%d" % i, [128, 1024], BF16) for i in range(2)]
    P.dma('sp', writes=['rw'], out=rw[:], in_=rw_d)
    P.dma('sp', writes=['rb'], out=rb[:], in_=rb_d)
    rt = {nm: P.sb("rt_" + nm, [128, 16], F32) for nm in ('sc', 'sl', 'eq', 's2', 'ch', 'w')}
    r4 = {nm: P.sb("r4_" + nm, [128, 4], F32) for nm in ('m1', 'm2', 'gs', 'gm')}
    r1 = {nm: P.sb("r1_" + nm, [128, 1], F32) for nm in ('gx', 'ss')}
    v4 = lambda t_: t_[:].rearrange("p (g j) -> p g j", j=4)
    b4 = lambda t_: t_[:].unsqueeze(2).to_broadcast([128, 4, 4])
    rbank, rbk = banks[6], 'rbank'
    jsub = 0
    for ti, tile in enumerate(tiles):
        c0, c1, r = tile
        n = c1 - c0
        emit_modulate_tile(P, hT, 'h', Af, 'Af', mod, 'modL', 3, ti, tile, ones_bf, ps_ssq, work, uT=None, ukey=None, u32=u32)
        for sub in range(n // 128 if n >= 128 else 1):
            m = min(128, n)
            s0 = sub * 128
            j = jsub
            jsub += 1
            vr, vk = vrow[j % 2], 'vrow%d' % (j % 2)
            for half in range(2):
                tb_, tk_ = banks[4 + half], 'tpb%d' % half
                for kk in range(4):
                    k = half * 4 + kk
                    P.op('pe', 'transpose', ['u32', 'ident'], [tk_], out=tb_[0:m, kk * 128:(kk + 1) * 128], in_=u32[:, k, s0:s0 + m], identity=ident[:])
                P.act(vr[0:m, half * 512:(half + 1) * 512], tb_[0:m, :], AF.Copy, [tk_], [vk, tk_])
            P.dma('sp' if j % 2 == 0 else 'act', reads=[vk], writes=['vtm'], out=vtm[j * 128:j * 128 + 128, :], in_=vr[:, :])
            for k in range(8):
                P.mm(rbank[0:m, 0:16], u32[:, k, s0:s0 + m], rw[:, k, :], k == 0, k == 7, ['u32', 'rw'], [rbk])
            P.act(rt['sc'][0:m, :], rbank[0:m, 0:16], AF.Sigmoid, [rbk], ['rt_sc', rbk])
            P.dve('tensor_tensor', ['rt_sc', 'rb'], ['rt_sl'], out=rt['sl'][0:m], in0=rt['sc'][0:m], in1=rb[0:m], op=ALU.add)
            P.dve('tensor_reduce', ['rt_sl'], ['r4_m1'], out=r4['m1'][0:m], in_=v4(rt['sl'])[0:m], axis=AX.X, op=ALU.max)
            P.dve('tensor_tensor', ['rt_sl', 'r4_m1'], ['rt_eq'], out=v4(rt['eq'])[0:m], in0=v4(rt['sl'])[0:m], in1=b4(r4['m1'])[0:m], op=ALU.is_equal)
            P.dve('scalar_tensor_tensor', ['rt_eq', 'rt_sl'], ['rt_s2'], out=rt['s2'][0:m], in0=rt['eq'][0:m], scalar=-1e9, in1=rt['sl'][0:m], op0=ALU.mult, op1=ALU.add)
            P.dve('tensor_reduce', ['rt_s2'], ['r4_m2'], out=r4['m2'][0:m], in_=v4(rt['s2'])[0:m], axis=AX.X, op=ALU.max)
            P.dve('tensor_tensor', ['r4_m1', 'r4_m2'], ['r4_gs'], out=r4['gs'][0:m], in0=r4['m1'][0:m], in1=r4['m2'][0:m], op=ALU.add)
            P.dve('tensor_reduce', ['r4_gs'], ['r1_gx'], out=r1['gx'][0:m], in_=r4['gs'][0:m], axis=AX.X, op=ALU.max)
            P.dve('tensor_scalar', ['r4_gs', 'r1_gx'], ['r4_gm'], out=r4['gm'][0:m], in0=r4['gs'][0:m], scalar1=r1['gx'][0:m, 0:1], scalar2=None, op0=ALU.is_equal)
            P.dve('tensor_tensor', ['rt_sl', 'r4_m2'], ['rt_ch'], out=v4(rt['ch'])[0:m], in0=v4(rt['sl'])[0:m], in1=b4(r4['m2'])[0:m], op=ALU.is_ge)
            P.dve('tensor_tensor', ['rt_ch', 'r4_gm'], ['rt_ch'], out=v4(rt['ch'])[0:m], in0=v4(rt['ch'])[0:m], in1=b4(r4['gm'])[0:m], op=ALU.mult)
            P.dve('tensor_tensor', ['rt_ch', 'rt_sc'], ['rt_w'], out=rt['w'][0:m], in0=rt['ch'][0:m], in1=rt['sc'][0:m], op=ALU.mult)
            P.dve('tensor_reduce', ['rt_w'], ['r1_ss'], out=r1['ss'][0:m], in_=rt['w'][0:m], axis=AX.X, op=ALU.add)
            P.dve('reciprocal', ['r1_ss'], ['r1_ss'], out=r1['ss'][0:m], in_=r1['ss'][0:m])
            P.dve('tensor_scalar', ['rt_w', 'r1_ss'], ['WW'], out=WW[0:m, j, :], in0=rt['w'][0:m], scalar1=r1['ss'][0:m, 0:1], scalar2=None, op0=ALU.mult)
            P.dve('tensor_tensor', ['rt_eq', 'r4_gm'], ['CH1'], out=CH1[0:m, j, :].rearrange("p (g j) -> p g j", j=4), in0=v4(rt['eq'])[0:m], in1=b4(r4['gm'])[0:m], op=ALU.mult)
            P.dve('tensor_tensor', ['rt_ch', 'CH1'], ['CH2'], out=CH2[0:m, j, :], in0=rt['ch'][0:m], in1=CH1[0:m, j, :], op=ALU.subtract)
            P.dve('tensor_copy', ['rt_ch'], ['CHb'], out=CHb[0:m, j, :], in_=rt['ch'][0:m])
    pbank, pbk = banks[6], 'rbank'
    for j in range(NSUB):
        P.mm(pbank[:, 0:16], tris[:], CHb[:, j, :], True, j == 0, ['tris', 'CHb'], [pbk])
        for jj in range(j):
            P.mm(pbank[:, 0:16], ones_bf[:], CHb[:, jj, :], False, jj == j - 1, ['ones', 'CHb'], [pbk])
        P.dve('tensor_tensor', [pbk, 'eoff'], ['RP', pbk], out=RP[:, j, :], in0=pbank[:, 0:16], in1=eoff[:], op=ALU.add)
    barrier(P)
    while len(P.stack) > mark:
        P.stack.pop().__exit__(None, None, None)

    identb = P.sb("identb_sb", [128, 128], BF16)
    P.dve('tensor_copy', ['ident'], ['identb'], out=identb[:], in_=ident[:])
    WG = [P.sb("WG%d" % k, [128, 1024], BF16) for k in range(8)]
    WU = [P.sb("WU%d" % k, [128, 1024], BF16) for k in range(8)]
    WD = [P.sb("WD%d" % k, [128, 1024], BF16) for k in range(8)]
    OH = [P.sb("OH%d" % i, [128, CAP], BF16) for i in range(3)]
    pe_ = P.sb("pe_e", [128, NSUB], F32)
    hl = P.sb("hl", [2, CAP], F32)
    idxf = P.sb("idxf", [128, NSL], F32)
    hls = P.sb("hls", [128, 2 * NSL], F32)
    idxi = [P.sb("idxi%d" % i, [128, NSL], I32) for i in range(2)]
    xg = [P.sb("xg%d" % i, [128, 1024], BF16) for i in range(2)]
    xsT2 = [P.sb("xsTe%d" % i, [128, 8, CAP], BF16) for i in range(2)]
    hid = P.sb("hid", [128, 8, CAP], BF16)
    sg = [P.sb("sg%d" % i, [128, 512], F32) for i in range(2)]
    ys = [P.sb("ys%d" % i, [128, 1024], F32) for i in range(2)]
    invA, invB = banks[0], banks[1]
    bA = [banks[2], banks[3]]
    bB = [banks[4], banks[5]]
    bY = [banks[6], banks[7]]
    ntl = [(0, min(512, CAP))] + ([(512, CAP)] if CAP > 512 else [])

    def indirect(out, in_, idx_ap, reads, writes):
        n_ = P.dma_n['pool']
        P.dma_n['pool'] += 1
        si = n_ % P.ndma
        key = ('d', 'pool', si)
        val = 16 * (n_ // P.ndma + 1)
        waits = P._deps('pool', list(reads), list(writes))
        if n_ >= P.ndma and P.seen['pool'].get(key, 0) < val - 16:
            waits.append((key, val - 16))
            P.seen['pool'][key] = val - 16
        P.q['pool'].append((waits, ('indirect_dma_start', dict(out=out, out_offset=None, in_=in_, in_offset=bass.IndirectOffsetOnAxis(ap=idx_ap, axis=0))), key))
        P._commit((key, val), list(reads), list(writes))

    def interleave(*gens):
        gens = [g_ for g_ in gens if g_ is not None]
        while gens:
            for g_ in list(gens):
                try:
                    next(g_)
                except StopIteration:
                    gens.remove(g_)

    state = dict(it=0, gcount=0)

    def prep_gen(e):
        xs_, xk_ = xsT2[e % 2], 'xsT%d' % (e % 2)
        P.dve('tensor_scalar', ['RP'], ['pe_e'], out=pe_[:], in0=RP[:, :, e], scalar1=float(1 - e * CAP), scalar2=None, op0=ALU.add)
        P.dve('tensor_tensor', ['pe_e', 'CHb'], ['pe_e'], out=pe_[:], in0=pe_[:], in1=CHb[:, :, e], op=ALU.mult)
        P.dve('tensor_scalar', ['pe_e'], ['pe_e'], out=pe_[:], in0=pe_[:], scalar1=-1.0, scalar2=None, op0=ALU.add)
        yield
        for jj in range(NSUB + 2):
            if jj < NSUB:
                oh, ok_ = OH[jj % 3], 'OH%d' % (jj % 3)
                P.dve('tensor_scalar', ['iota', 'pe_e'], [ok_], out=oh[:], in0=iota_s[:], scalar1=pe_[:, jj:jj + 1], scalar2=None, op0=ALU.is_equal)
            j = jj - 2
            if j >= 0:
                oh, ok_ = OH[j % 3], 'OH%d' % (j % 3)
                for ni, (n0, n1) in enumerate(ntl):
                    bk_, bkk = (invA, 'invA') if ni == 0 else (invB, 'invB')
                    P.mm(bk_[0:2, 0:n1 - n0], tokhl[:, j, :], oh[:, n0:n1], j == 0, j == NSUB - 1, ['tokhl', ok_], [bkk])
            yield
        yield
        for ni, (n0, n1) in enumerate(ntl):
            bk_, bkk = (invA, 'invA') if ni == 0 else (invB, 'invB')
            P.act(hl[:, n0:n1], bk_[0:2, 0:n1 - n0], AF.Copy, [bkk], ['hl', bkk])
        yield
        yield
        for st in range(NSL):
            P.op('pe', 'transpose', ['hl', 'ident'], ['invB'], out=invB[:, 256 + 2 * st:256 + 2 * st + 2], in_=hl[0:2, st * 128:(st + 1) * 128], identity=ident[0:2, 0:2])
        yield
        P.act(hls[:], invB[:, 256:256 + 2 * NSL], AF.Copy, ['invB'], ['hls', 'invB'])
        yield
        hv = hls[:].rearrange("p (s c) -> p s c", c=2)
        P.dve('scalar_tensor_tensor', ['hls'], ['idxf'], out=idxf[:], in0=hv[:, :, 0], scalar=64.0, in1=hv[:, :, 1], op0=ALU.mult, op1=ALU.add)
        ii, ik = idxi[e % 2], 'idxi%d' % (e % 2)
        P.dve('tensor_copy', ['idxf'], [ik], out=ii[:], in_=idxf[:])
        yield

        def gather(st):
            g_, gk_ = xg[st % 2], 'xg%d' % (st % 2)
            indirect(g_[:], vtm[:, :], ii[:, st:st + 1], [ik, 'vtm'], [gk_])
        gather(0)
        for st in range(NSL):
            if st + 1 < NSL:
                gather(st + 1)
            yield
            yield
            g_, gk_ = xg[st % 2], 'xg%d' % (st % 2)
            tb_, tk_ = (invA, 'invA') if st % 2 == 0 else (invB, 'invB')
            tbb = tb_[:].bitcast(BF16)
            for k in range(8):
                P.op('pe', 'transpose', [gk_, 'identb'], [tk_], out=tbb[:, k * 128:(k + 1) * 128], in_=g_[:, k * 128:(k + 1) * 128], identity=identb[:])
            yield
            P.act(xs_[:, :, st * 128:(st + 1) * 128], tbb[:, 0:1024].rearrange("p (k s) -> p k s", k=8), AF.Copy, [tk_], [xk_, tk_])
            yield

    def ffn_gen(e):
        xs_, xk_ = xsT2[e % 2], 'xsT%d' % (e % 2)
        for k in range(8):
            P.dma('pool', writes=['WG%d' % k], out=WG[k][:], in_=wg_d[e, k * 128:(k + 1) * 128, :])
            P.dma('pool', writes=['WU%d' % k], out=WU[k][:], in_=wu_d[e, k * 128:(k + 1) * 128, :])
        for k in range(8):
            P.dma('pool', writes=['WD%d' % k], out=WD[k][:], in_=wd_d[e, k * 128:(k + 1) * 128, :])
        yield
        for fc in range(8):
            for (n0, n1) in ntl:
                n = n1 - n0
                i2 = state['it'] % 2
                state['it'] += 1
                for k in range(8):
                    P.mm(bA[i2][:, 0:n], WG[k][:, fc * 128:(fc + 1) * 128], xs_[:, k, n0:n1], k == 0, k == 7, ['WG%d' % k, xk_], ['bA%d' % i2])
                yield
                for k in range(8):
                    P.mm(bB[i2][:, 0:n], WU[k][:, fc * 128:(fc + 1) * 128], xs_[:, k, n0:n1], k == 0, k == 7, ['WU%d' % k, xk_], ['bB%d' % i2])
                yield
                P.act(sg[i2][:, 0:n], bA[i2][:, 0:n], AF.Silu, ['bA%d' % i2], ['sg%d' % i2, 'bA%d' % i2])
                P.dve('tensor_tensor', ['bB%d' % i2, 'sg%d' % i2], ['hid', 'bB%d' % i2], out=hid[:, fc, n0:n1], in0=bB[i2][:, 0:n], in1=sg[i2][:, 0:n], op=ALU.mult)
                yield
        for st in range(NSL):
            y_, yk = ys[st % 2], 'ys%d' % (st % 2)
            for dh in range(2):
                i2 = state['it'] % 2
                state['it'] += 1
                for k in range(8):
                    P.mm(bY[i2][:, :], hid[:, k, st * 128:(st + 1) * 128], WD[k][:, dh * 512:(dh + 1) * 512], k == 0, k == 7, ['WD%d' % k, 'hid'], ['bY%d' % i2])
                yield
                P.act(y_[:, dh * 512:(dh + 1) * 512], bY[i2][:, :], AF.Copy, ['bY%d' % i2], [yk, 'bY%d' % i2])
                yield
            P.dma('sp' if st % 2 == 0 else 'act', reads=[yk], writes=['yall'], out=y_all[e * CAP + st * 128:e * CAP + (st + 1) * 128, :], in_=y_[:])

    if nexp > 0:
        interleave(prep_gen(0))
    for e in range(nexp):
        interleave(ffn_gen(e), prep_gen(e + 1) if e + 1 < nexp else None)
    barrier(P)
    while len(P.stack) > mark:
        P.stack.pop().__exit__(None, None, None)

    gk2 = [P.sb("gk%d" % i, [128, 1024], F32) for i in range(4)]
    ytok = [P.sb("ytok%d" % i, [128, 1024], F32) for i in range(2)]
    t16 = P.sb("t16", [128, 16], F32)
    cs = {nm: P.sb("cs_" + nm, [128, 1], F32) for nm in ('row0', 'row1', 'w0', 'w1', 'eo', 'vl')}
    rowi = [P.sb("rowi%d" % i, [128, 1], I32) for i in range(4)]
    for j in range(NSUB):
        if layer == 0 and j == NSUB - 1:
            m, c0, r, ti = 64, 2048, 1, 4
        else:
            m, c0, r, ti = 128, j * 128, 0, j // 4
        for kk, CHk in enumerate((CH1, CH2)):
            rw_, w_ = cs['row%d' % kk], cs['w%d' % kk]
            P.dve('tensor_tensor', ['CH1', 'CH2', 'RP'], ['t16'], out=t16[:], in0=CHk[:, j, :], in1=RP[:, j, :], op=ALU.mult)
            P.dve('tensor_reduce', ['t16'], ['cs_row%d' % kk], out=rw_[:], in_=t16[:], axis=AX.X, op=ALU.add)
            P.dve('tensor_tensor', ['CH1', 'CH2', 'eoff'], ['t16'], out=t16[:], in0=CHk[:, j, :], in1=eoff[:], op=ALU.mult)
            P.dve('tensor_reduce', ['t16'], ['cs_eo'], out=cs['eo'][:], in_=t16[:], axis=AX.X, op=ALU.add)
            P.dve('tensor_tensor', ['CH1', 'CH2', 'WW'], ['t16'], out=t16[:], in0=CHk[:, j, :], in1=WW[:, j, :], op=ALU.mult)
            P.dve('tensor_reduce', ['t16'], ['cs_w%d' % kk], out=w_[:], in_=t16[:], axis=AX.X, op=ALU.add)
            P.dve('tensor_tensor', ['cs_row%d' % kk, 'cs_eo'], ['cs_row%d' % kk], out=rw_[:], in0=rw_[:], in1=cs['eo'][:], op=ALU.subtract)
            P.dve('tensor_scalar', ['cs_row%d' % kk], ['cs_vl'], out=cs['vl'][:], in0=rw_[:], scalar1=float(CAP) - 0.5, scalar2=None, op0=ALU.is_lt)
            P.dve('tensor_tensor', ['cs_w%d' % kk, 'cs_vl'], ['cs_w%d' % kk], out=w_[:], in0=w_[:], in1=cs['vl'][:], op=ALU.mult)
            P.dve('tensor_scalar', ['cs_row%d' % kk], ['cs_row%d' % kk], out=rw_[:], in0=rw_[:], scalar1=float(CAP - 1), scalar2=None, op0=ALU.min)
            P.dve('tensor_tensor', ['cs_row%d' % kk, 'cs_eo'], ['cs_row%d' % kk], out=rw_[:], in0=rw_[:], in1=cs['eo'][:], op=ALU.add)
            ri, rk_ = rowi[(2 * j + kk) % 4], 'rowi%d' % ((2 * j + kk) % 4)
            P.dve('tensor_copy', ['cs_row%d' % kk], [rk_], out=ri[:], in_=rw_[:])
            g_, gk_ = gk2[(2 * j + kk) % 4], 'gk%d' % ((2 * j + kk) % 4)
            indirect(g_[:], y_all[:, :], ri[:, 0:1], [rk_, 'yall'], [gk_])
        g0, g1 = gk2[(2 * j) % 4], gk2[(2 * j + 1) % 4]
        yt, ytk = ytok[j % 2], 'ytok%d' % (j % 2)
        P.dve('tensor_scalar', ['gk%d' % ((2 * j) % 4), 'cs_w0'], [ytk], out=yt[:], in0=g0[:], scalar1=cs['w0'][:, 0:1], scalar2=None, op0=ALU.mult)
        P.dve('scalar_tensor_tensor', ['gk%d' % ((2 * j + 1) % 4), 'cs_w1', ytk], [ytk], out=yt[:], in0=g1[:], scalar=cs['w1'][:, 0:1], in1=yt[:], op0=ALU.mult, op1=ALU.add)
        pb = (bA, bB)[j % 2]
        pk = ('bA%d', 'bB%d')[j % 2]
        for k in range(8):
            bb_, bbk = pb[k // 4], pk % (k // 4)
            P.op('pe', 'transpose', [ytk, 'ident'], [bbk], out=bb_[:, (k % 4) * 128:(k % 4) * 128 + m], in_=yt[0:m, k * 128:(k + 1) * 128], identity=ident[0:m, 0:m])
        for k in range(8):
            bb_, bbk = pb[k // 4], pk % (k // 4)
            P.dve('scalar_tensor_tensor', [bbk, 'modL', 'h:%d' % ti], ['h:%d' % ti, bbk], out=hT[:, k, c0:c0 + m], in0=bb_[:, (k % 4) * 128:(k % 4) * 128 + m], scalar=mod[:, 40 + k, r:r + 1], in1=hT[:, k, c0:c0 + m], op0=ALU.mult, op1=ALU.add)
    barrier(P)
    while len(P.stack) > mark:
        P.stack.pop().__exit__(None, None, None)

    if layer == 0:
        for k in range(8):
            P.dma('sp' if k % 2 == 0 else 'act', reads=['h:%d' % t for t in range(NT)], out=hT_out[:, k, :], in_=hT[:, k, :])
        mod1 = P.sb("sbmod1", [128, 48, 2], F32)
        P.dma('sp', writes=['mod1'], out=mod1[:], in_=mod1_in)
        A1 = emit_affine(P, mod1, 'mod1', gmix1, 1, 'm1')
        uT1 = P.sb("uT1", [128, 8, T], BF16)
        for ti, tile in enumerate(tiles):
            emit_modulate_tile(P, hT, 'h', A1, 'Am1', mod1, 'mod1', 0, ti, tile, ones_bf, ps_ssq, work, uT=uT1, ukey='u1')
        C = ProjCtx(P, w_in1, uT1, 'u1', banks[2:6], o_f, o_bf)
        C.job_lin(CD['hy'], 1536, A1_BF['hy'], 1.0)
        C.job_lin(CD['xbc'], 1024, A1_BF['xbc'], 1.0)
        C.job_silu(CD['z'], 512, A1_F['zs'], 'f')
        dtp = P.sb("dtp_sb", [16, 2], F32)
        negA = P.sb("negA", [16, 1], F32)
        P.dma('sp', writes=['dtp'], out=dtp[:], in_=dtp_d)
        P.act(negA[:], dtp[:, 1:2], AF.Exp, ['dtp'], ['negA'])
        P.dve('tensor_scalar', ['negA'], ['negA'], out=negA[:], in0=negA[:], scalar1=-1.0, scalar2=None, op0=ALU.mult)
        st1, sk1 = C.stage('f')
        st2, sk2 = C.stage('f')
        C.load_w(CD['dt'], 16)

        def post(ps, n, c0, c1, ti, bkey):
            P.act(st1[0:16, c0:c1], ps, AF.Exp, [bkey, 'dtp'], [sk1, bkey], bias=dtp[:, 0:1], scale=1.0)
            P.act(st1[0:16, c0:c1], st1[0:16, c0:c1], AF.Ln, [sk1], [sk1], bias=1.0, scale=1.0)
            P.dve('tensor_scalar', [sk1, 'negA'], [sk2], out=st2[0:16, c0:c1], in0=st1[0:16, c0:c1], scalar1=negA[:, 0:1], scalar2=None, op0=ALU.mult)
        C.chunk(CD['dt'], 16, post)
        C.outdma('f', st1, sk1, A1_F['dt'], 16)
        C.outdma('f', st2, sk2, A1_F['la'], 16)
    else:
        gout = P.sb("gout_sb", [128, 8], F32)
        P.dma('sp', writes=['gout'], out=gout[:], in_=gout_d)
        yst = [P.sb("yst%d" % i, [128, 8, 512], F32) for i in range(2)]
        for ti, (c0, c1, r) in enumerate(tiles):
            n = c1 - c0
            emit_rstd(P, hT[:, :, c0:c1], 'h:%d' % ti, n, 8, ones_bf, ps_ssq, work['sq'], work['rs'], 1.0 / 1024)
            for k in range(8):
                P.dve('scalar_tensor_tensor', ['h:%d' % ti, 'gout', 'rs'], ['yst%d' % (ti % 2)], out=yst[ti % 2][:, k, 0:n], in0=hT[:, k, c0:c1], scalar=gout[:, k:k + 1], in1=work['rs'][:, 0:n], op0=ALU.mult, op1=ALU.mult)
            P.dma('sp' if ti % 2 == 0 else 'act', reads=['yst%d' % (ti % 2)], out=y_out[:, :, c0:c1], in_=yst[ti % 2][:, :, 0:n])
    P.wait_all('sp')
    P.emit()
    P.close()
    return nc


def local_from_joint(a, q):
    return np.concatenate([a[256 + 2048 * q:256 + 2048 * (q + 1)], a[64 * q:64 * (q + 1)]], axis=0)


def tm_to_joint(o):
    return o.transpose(1, 0, 2).reshape(NCH * 64, o.shape[2])


CAP_ = 640


def tokhl_const(nsub):
    p = np.arange(128)[:, None]
    j = np.arange(nsub)[None, :]
    tid = j * 128 + p
    return np.ascontiguousarray(np.stack([tid // 64, tid % 64], axis=-1).astype(np.float32).astype(NPBF))


def common_C_inputs(inp, layer):
    sel = np.zeros((16, 16, 128), np.float32)
    for e in range(16):
        sel[e, e, :] = 1.0
    return dict(
        gffn=colT(inp['norm_ffn_g'][layer], 8),
        router_w=np.ascontiguousarray(inp['router_w'].reshape(8, 128, 16).transpose(1, 0, 2)),
        router_b=np.ascontiguousarray(np.broadcast_to(inp['router_b'][None, :], (128, 16))),
        sel16=sel, ident=np.eye(128, dtype=np.float32),
        tris=np.triu(np.ones((128, 128), np.float32), 1).astype(NPBF),
        iota_s=np.ascontiguousarray(np.broadcast_to(np.arange(CAP_, dtype=np.float32)[None, :], (128, CAP_))),
        eoff=np.ascontiguousarray(np.broadcast_to((np.arange(16, dtype=np.float32) * CAP_)[None, :], (128, 16))),
        tokhl=tokhl_const(17 if layer == 0 else 16),
        moe_wg=inp['moe_w_gate'][layer], moe_wu=inp['moe_w_up'][layer], moe_wd=inp['moe_w_down'][layer])


def host_C0_inputs(inp, a0, b0):
    maps = []
    com = common_C_inputs(inp, 0)
    ng = np.ascontiguousarray(np.stack([inp['gla_norm_g'][0], inp['hg_norm_g'][0]], axis=1))
    dtp = np.ascontiguousarray(np.stack([inp['mb_dt_bias'][0].reshape(16), inp['mb_a_log'][0].reshape(16)], axis=1))
    for i in range(NCORES):
        b, q = i // 4, i % 4
        oT = np.empty((8, 128, 2, TL), np.float32)
        for hh in range(8):
            src = b0[4 * b + hh % 4]
            for d in range(2):
                o = tm_to_joint(src[('g_o%d' if hh < 4 else 'h_o%d') % d])
                oT[hh, :, d, :] = local_from_joint(o, q).T
        m = dict(com)
        m.update(hT_in=fm(core_tokens(inp['x'], inp['ctx'], i)), mod_in=a0[i]['mods_out'][0], mod1_in=a0[i]['mods_out'][1], w_out=inp['ab_w_out'][0],
                 oT=oT, gateT=np.ascontiguousarray(a0[i]['o_f'][0:1024].reshape(8, 128, TL)), ng=ng,
                 gmix1=colT(inp['norm_mix_g'][1], 8),
                 w_in1=inp['cd_w_in'][0], dtp=dtp)
        maps.append(m)
    return maps


NFFT = 16384
PI = float(np.pi)


def fft_consts():
    a = np.arange(128, dtype=np.float64)
    th = 2 * np.pi * np.outer(a, a) / 128.0
    tw = 2 * np.pi * np.outer(a, a) / NFFT
    c, s = np.cos(th), np.sin(th)
    F64 = np.concatenate([c[:64], -s[:64]], axis=1)
    Fst = np.stack([c, -s, s], axis=1)
    G12 = np.stack([np.concatenate([c, s], 1), np.concatenate([-s, c], 1)], axis=1)
    Gst = np.stack([c[:, :64] / NFFT, -s[:, :64] / NFFT], axis=1)
    tr, ti = np.cos(tw), -np.sin(tw)
    TW = np.stack([np.tile(np.concatenate([tr, tr], 1), (1, 2)), np.tile(np.concatenate([ti, ti], 1), (1, 2))], axis=1)
    pr, pi_ = np.cos(tw), np.sin(tw)
    TWI = np.stack([np.tile(np.concatenate([pr, pr], 1), (1, 2)), np.tile(np.concatenate([pi_, pi_], 1), (1, 2))], axis=1)
    return dict(F64=F64.astype(NPBF), Fst=Fst.astype(NPBF), G12=G12.astype(NPBF), Gst=Gst.astype(NPBF),
                TW=TW.astype(np.float32), TWI=TWI.astype(np.float32))


def build_B1(nscan_steps=None, hy_groups=32, do_ssd=True, do_hy=True):
    nc = bass.Bass("TRN2", target_bir_lowering=False)
    T = NCH * 64
    din = lambda name, shape, dt=F32: nc.dram_tensor(name, list(shape), dt, kind="ExternalInput").ap()
    dout = lambda name, shape, dt=F32: nc.dram_tensor(name, list(shape), dt, kind="ExternalOutput").ap()
    dscr = lambda name, shape, dt=F32: nc.dram_tensor(name, list(shape), dt, kind="Internal").ap()
    tri_d = din("tri", [64, 6, 64])
    ident_d = din("identb", [128, 128], BF16)
    TP = 258 + 8194
    xin_d = din("ssd_xin", [128, 3, TP], BF16)
    scw_d = din("ssd_cw", [128, 3, 4])
    dttm_d = din("ssd_dttm", [64, NCH, 4])
    latm_d = [din("ssd_latm%d" % k, [64, NCH, 128]) for k in range(4)]
    s_qT = dscr("s_qT", [128, T], BF16)
    s_kT = dscr("s_kT", [128, T], BF16)
    s_ktm = dscr("s_ktm", [64, NCH, 128], BF16)
    s_vtm = [dscr("s_vtm%d" % k, [64, NCH, 64], BF16) for k in range(4)]
    ssd_o = [dout("ssd_o%d" % k, [64, NCH, 64]) for k in range(4)]
    xs_out = dout("xs_out", [128, T])
    hy_in_d = din("hy_in", [128, 3, 8194], BF16)
    hcw_d = din("hy_cw", [128, 3, 4])
    zT_d = din("hy_zT", [33, 8192])
    w1_d = din("hy_w1", [33, 64])
    w2_d = din("hy_w2", [64, 64])
    w3_d = din("hy_w3", [64, 4, 128])
    fp_d = din("hy_fp", [64, 3])
    rate_d = din("hy_rate", [64, 128])
    negt_d = din("hy_negt", [64, 128])
    hb_d = din("hy_bias", [64, 2, 128])
    F64_d = din("F64", [64, 256], BF16)
    Fst_d = din("Fst", [128, 3, 128], BF16)
    G12_d = din("G12", [128, 2, 256], BF16)
    Gst_d = din("Gst", [128, 2, 64], BF16)
    TW_d = din("TW", [128, 2, 512])
    TWI_d = din("TWI", [128, 2, 512])
    s_hy = dscr("s_hy", [3, 128, 8192], BF16)
    s_kf = dscr("s_kf", [2, 32, 128, 4, 2, 128])
    hy_out = dout("hy_out", [64, 128, 128])

    P = Prog(nc)
    banks = [P.ps("bank%d" % i, [128, 512], F32) for i in range(8)]
    dq = [0]

    def q3():
        dq[0] += 1
        return ['sp', 'act', 'pool'][dq[0] % 3]

    def dwconv(x, xkey, cw, part, col0, n, acc, acckey):
        P.act(acc[:, 0:n], x[:, part, col0 + 1:col0 + 1 + n], AF.Identity, [xkey, 'cw'], [acckey], scale=cw[:, part, 1:2], bias=cw[:, part, 3:4])
        P.dve('scalar_tensor_tensor', [xkey, 'cw', acckey], [acckey], out=acc[:, 0:n], in0=x[:, part, col0:col0 + n], scalar=cw[:, part, 0:1], in1=acc[:, 0:n], op0=ALU.mult, op1=ALU.add)
        P.dve('scalar_tensor_tensor', [xkey, 'cw', acckey], [acckey], out=acc[:, 0:n], in0=x[:, part, col0 + 2:col0 + 2 + n], scalar=cw[:, part, 2:3], in1=acc[:, 0:n], op0=ALU.mult, op1=ALU.add)

    if do_hy:
        mark0 = len(P.stack)
        xin = P.sb("hy_xin", [128, 3, 8194], BF16)
        cw = P.sb("hy_cw_sb", [128, 3, 4], F32)
        acc = [P.sb("hy_acc%d" % i, [128, 2048], F32) for i in range(2)]
        accb = [P.sb("hy_accb%d" % i, [128, 2048], BF16) for i in range(2)]
        for p in range(3):
            P.dma(q3(), writes=['hyxin'], out=xin[:, p, :], in_=hy_in_d[:, p, :])
        P.dma('sp', writes=['cw'], out=cw[:], in_=hcw_d)
        it = 0
        for p in range(3):
            for blk in range(4):
                i2 = it % 2
                it += 1
                dwconv(xin, 'hyxin', cw, p, blk * 2048, 2048, acc[i2], 'hyacc%d' % i2)
                P.pool('tensor_copy', ['hyacc%d' % i2], ['hyaccb%d' % i2], out=accb[i2][:], in_=acc[i2][:])
                P.dma(q3(), reads=['hyaccb%d' % i2], writes=['s_hy'], out=s_hy[p, :, blk * 2048:(blk + 1) * 2048], in_=accb[i2][:])
        barrier(P)
        while len(P.stack) > mark0:
            P.stack.pop().__exit__(None, None, None)

        F64 = P.sb("F64_sb", [64, 256], BF16)
        Fst = P.sb("Fst_sb", [128, 3, 128], BF16)
        G12 = P.sb("G12_sb", [128, 2, 256], BF16)
        Gst = P.sb("Gst_sb", [128, 2, 64], BF16)
        TW = P.sb("TW_sb", [128, 2, 512], F32)
        TWI = P.sb("TWI_sb", [128, 2, 512], F32)
        for t_, d_ in ((F64, F64_d), (Fst, Fst_d), (G12, G12_d), (Gst, Gst_d), (TW, TW_d), (TWI, TWI_d)):
            P.dma(q3(), writes=['fftc'], out=t_[:], in_=d_)
        m1 = [P.sb("m1_%d" % i, [128, 512], F32) for i in range(2)]
        m2 = [P.sb("m2_%d" % i, [128, 512], F32) for i in range(2)]
        Bt = [P.sb("Bt%d" % i, [128, 4, 2, 128], BF16) for i in range(2)]
        cnt = {'a': 0, 'b': 0}

        def twiddle(psrc, pkey, table, dst, dkey, pair):
            i2 = cnt['a'] % 2
            cnt['a'] += 1
            a1, a2 = m1[i2], m2[i2]
            P.dve('tensor_tensor', [pkey, 'fftc'], ['m1_%d' % i2, pkey], out=a1[:], in0=psrc, in1=table[:, 0, :], op=ALU.mult)
            yield
            P.dve('tensor_tensor', [pkey, 'fftc'], ['m2_%d' % i2, pkey], out=a2[:], in0=psrc, in1=table[:, 1, :], op=ALU.mult)
            yield
            v1 = a1[:].rearrange("p (c h k) -> p c h k", c=2, h=2)
            v2 = a2[:].rearrange("p (c h k) -> p c h k", c=2, h=2)
            P.pool('tensor_tensor', ['m1_%d' % i2, 'm2_%d' % i2], [dkey + 'r%d' % pair], out=dst[:, pair * 2:pair * 2 + 2, 0, :], in0=v1[:, :, 0, :], in1=v2[:, :, 1, :], op=ALU.subtract)
            yield
            P.pool('tensor_tensor', ['m1_%d' % i2, 'm2_%d' % i2], [dkey + 'i%d' % pair], out=dst[:, pair * 2:pair * 2 + 2, 1, :], in0=v2[:, :, 0, :], in1=v1[:, :, 1, :], op=ALU.add)
            yield

        def interleave(*gens):
            gens = [g_ for g_ in gens if g_ is not None]
            while gens:
                for g_ in list(gens):
                    try:
                        next(g_)
                    except StopIteration:
                        gens.remove(g_)

        def fft_fwd4(src, skey, c0, bXr, kXr, bXi, kXi):
            interleave(fft_fwd4_g(src, skey, c0, bXr, kXr, bXi, kXi))

        def fft_fwd4_g(src, skey, c0, bXr, kXr, bXi, kXi, extra=()):
            i2 = cnt['b'] % 2
            cnt['b'] += 1
            B = Bt[i2]
            bk = 'Bt%d' % i2
            for pair in range(2):
                pa, pk = banks[pair], 'psA%d' % pair
                for cc in range(2):
                    P.mm(pa[:, cc * 256:(cc + 1) * 256], src[0:64, c0 + pair * 2 + cc, :], F64[:], True, True, [skey, 'fftc'] + list(extra), [pk])
                yield
                yield from twiddle(pa[:], pk, TW, B, bk, pair)
            rk = [bk + 'r0', bk + 'r1', bk + 'i0', bk + 'i1', 'fftc']
            Br = B[:, :, 0, :]
            Bi = B[:, :, 1, :]
            P.mm(bXr[:], Fst[:, 0, :], Br, True, False, rk, [kXr])
            P.mm(bXr[:], Fst[:, 2, :], Bi, False, True, rk, [kXr])
            yield
            P.mm(bXi[:], Fst[:, 1, :], Br, True, False, rk, [kXi])
            P.mm(bXi[:], Fst[:, 0, :], Bi, False, True, rk, [kXi])
            yield

        mark1 = len(P.stack)
        zT = P.sb("zT_sb", [33, 8192], F32)
        w1 = P.sb("w1_sb", [33, 64], F32)
        w2 = P.sb("w2_sb", [64, 64], F32)
        w3 = P.sb("w3_sb", [64, 4, 128], F32)
        fp = P.sb("fp_sb", [64, 3], F32)
        fb = P.sb("fb_sb", [64, 2], F32)
        rate = P.sb("rate_sb", [64, 128], F32)
        negt = P.sb("negt_sb", [64, 128], F32)
        h1 = P.sb("h1_sb", [64, 8192], F32)
        h2 = P.sb("h2_sb", [64, 8192], F32)
        for t_, d_, k_ in ((zT, zT_d, 'zT'), (w1, w1_d, 'w1'), (w2, w2_d, 'w2'), (w3, w3_d, 'w3'), (fp, fp_d, 'fp'), (rate, rate_d, 'rate'), (negt, negt_d, 'negt')):
            P.dma(q3(), writes=[k_], out=t_[:], in_=d_)
        P.dve('tensor_scalar', ['fp'], ['fb'], out=fb[:], in0=fp[:, 1:3], scalar1=fp[:, 0:1], scalar2=None, op0=ALU.mult)
        gbank, gk = banks[6], 'gbank'
        wrp = [P.sb("wrp%d" % i, [64, 512], F32) for i in range(2)]
        for lyr, (wsb, wk, src, sk, dst, dk_, K_) in enumerate(((w1, 'w1', zT, 'zT', h1, 'h1', 33), (w2, 'w2', h1, 'h1', h2, 'h2', 64))):
            for blk in range(16):
                cs = slice(blk * 512, (blk + 1) * 512)
                P.mm(gbank[0:64, :], wsb[0:K_, :], src[0:K_, cs], True, True, [wk, sk], [gk])
                P.dve('tensor_scalar', [gk, 'fp', 'fb'], [dk_, gk], out=dst[:, cs], in0=gbank[0:64, :], scalar1=fp[:, 0:1], scalar2=fb[:, lyr:lyr + 1], op0=ALU.mult, op1=ALU.add)
                wa, wb_ = wrp[0][:, 0:512], wrp[1][:, 0:512]
                P.dve('tensor_scalar', [dk_], ['wrp0'], out=wa, in0=dst[:, cs], scalar1=PI, scalar2=-2 * PI, op0=ALU.is_gt, op1=ALU.mult)
                P.dve('tensor_scalar', [dk_], ['wrp1'], out=wb_, in0=dst[:, cs], scalar1=-PI, scalar2=2 * PI, op0=ALU.is_lt, op1=ALU.mult)
                P.dve('tensor_tensor', ['wrp0', 'wrp1'], ['wrp0'], out=wa, in0=wa, in1=wb_, op=ALU.add)
                P.dve('tensor_tensor', ['wrp0', dk_], [dk_], out=dst[:, cs], in0=dst[:, cs], in1=wa, op=ALU.add)
                P.dve('tensor_scalar', [dk_], [dk_], out=dst[:, cs], in0=dst[:, cs], scalar1=3.1415925, scalar2=-3.1415925, op0=ALU.min, op1=ALU.max)
                P.act(dst[:, cs], dst[:, cs], AF.Sin, [dk_], [dk_])
        hf = [P.sb("hf%d" % d, [64, 128, 128], BF16) for d in range(2)]
        dec = [P.sb("dec%d" % i, [64, 128], F32) for i in range(2)]
        kst = [P.sb("kst%d" % i, [128, 4, 2, 128], F32) for i in range(2)]
        xev = [P.sb("xev%d" % i, [128, 512], F32) for i in range(2)]
        h2v = h2[:].rearrange("j (a b) -> j a b", b=128)
        for o in range(2):
            for n2 in range(128):
                i2 = n2 % 2
                P.act(dec[i2][:], rate[:], AF.Exp, ['rate', 'negt'], ['dec%d' % i2], scale=negt[:, n2:n2 + 1])
                for d in range(2):
                    P.mm(gbank[0:64, 0:128], h2v[:, :, n2], w3[:, o * 2 + d, :], True, True, ['h2', 'w3'], [gk])
                    P.dve('tensor_tensor', [gk, 'dec%d' % i2], ['hf%d' % d, gk], out=hf[d][:, :, n2], in0=gbank[0:64, 0:128], in1=dec[i2][:], op=ALU.mult)
            for g in range(hy_groups):
                fft_fwd4(hf[0], 'hf0', g * 4, banks[2], 'bX0', banks[3], 'bX1')
                fft_fwd4(hf[1], 'hf1', g * 4, banks[4], 'bX2', banks[5], 'bX3')
                i2 = g % 2
                ks = kst[i2]
                kk = 'kst%d' % i2
                P.act(xev[0][:], banks[4][:], AF.Copy, ['bX2'], ['xev0', 'bX2'])
                P.act(xev[1][:], banks[5][:], AF.Copy, ['bX3'], ['xev1', 'bX3'])
                P.dve('tensor_tensor', ['bX0', 'xev0'], [kk, 'bX0'], out=ks[:, :, 0, :], in0=banks[2][:].rearrange("p (c k) -> p c k", c=4), in1=xev[0][:].rearrange("p (c k) -> p c k", c=4), op=ALU.add)
                P.dve('tensor_tensor', ['bX1', 'xev1'], [kk, 'bX1'], out=ks[:, :, 1, :], in0=banks[3][:].rearrange("p (c k) -> p c k", c=4), in1=xev[1][:].rearrange("p (c k) -> p c k", c=4), op=ALU.subtract)
                P.dma(q3(), reads=[kk], writes=['s_kf'], out=s_kf[o, g], in_=ks[:])
        barrier(P)
        while len(P.stack) > mark1:
            P.stack.pop().__exit__(None, None, None)

        xv = P.sb("xv", [64, 128, 128], BF16)
        x1 = P.sb("x1", [64, 128, 128], BF16)
        x2 = P.sb("x2", [64, 128, 128], BF16)
        hbias = P.sb("hbias", [64, 2, 128], F32)
        for p, t_ in enumerate((xv, x1, x2)):
            for hh in range(2):
                P.dma(q3(), reads=['s_hy'], writes=['xd%d' % p], out=t_[:, hh * 64:(hh + 1) * 64, :], in_=s_hy[p, hh * 64:(hh + 1) * 64, :].rearrange("c (a b) -> a c b", b=128))
        P.dma('sp', writes=['hbias'], out=hbias[:], in_=hb_d)
        kfb = [P.sb("kfb%d" % i, [128, 4, 2, 128], F32) for i in range(2)]
        Yt = [P.sb("Yt%d" % i, [128, 4, 2, 128], BF16) for i in range(2)]
        Dt = [P.sb("Dt%d" % i, [128, 4, 2, 128], BF16) for i in range(2)]
        pw = [P.sb("pw%d" % i, [128, 512], F32) for i in range(4)]
        ep = [P.sb("ep%d" % i, [64, 512], F32) for i in range(2)]
        ost = [P.sb("ost%d" % i, [64, 4, 128], F32) for i in range(2)]
        def fwd_gen(o, g):
            i2 = g % 2
            kf, kfk = kfb[i2], 'kfb%d' % i2
            P.dma(q3(), reads=['s_kf'], writes=[kfk], out=kf[:], in_=s_kf[o, g])
            yield from fft_fwd4_g(xv, 'xd0', g * 4, banks[2], 'bX0', banks[3], 'bX1', extra=['zz:%d' % g])
            Xr = banks[2][:].rearrange("p (c k) -> p c k", c=4)
            Xi = banks[3][:].rearrange("p (c k) -> p c k", c=4)
            Y = Yt[i2]
            yk = 'Yt%d' % i2
            pwv = [pw[i][:].rearrange("p (c k) -> p c k", c=4) for i in range(4)]
            P.dve('tensor_tensor', ['bX0', kfk, 'zz:%d' % g], ['pw0', 'bX0'], out=pwv[0], in0=Xr, in1=kf[:, :, 0, :], op=ALU.mult)
            yield
            P.dve('tensor_tensor', ['bX1', kfk], ['pw1', 'bX1'], out=pwv[1], in0=Xi, in1=kf[:, :, 1, :], op=ALU.mult)
            yield
            P.dve('tensor_tensor', ['bX0', kfk], ['pw2', 'bX0'], out=pwv[2], in0=Xr, in1=kf[:, :, 1, :], op=ALU.mult)
            yield
            P.dve('tensor_tensor', ['bX1', kfk], ['pw3', 'bX1'], out=pwv[3], in0=Xi, in1=kf[:, :, 0, :], op=ALU.mult)
            yield
            P.pool('tensor_tensor', ['pw0', 'pw1'], [yk], out=Y[:, :, 0, :], in0=pwv[0], in1=pwv[1], op=ALU.subtract)
            yield
            P.pool('tensor_tensor', ['pw2', 'pw3'], [yk], out=Y[:, :, 1, :], in0=pwv[2], in1=pwv[3], op=ALU.add)
            yield

        def inv_gen(o, g):
            i2 = g % 2
            Y = Yt[i2]
            yk = 'Yt%d' % i2
            D = Dt[i2]
            dk_ = 'Dt%d' % i2
            for pair in range(2):
                pc, pck = banks[4 + pair], 'psC%d' % pair
                for cc in range(2):
                    c = pair * 2 + cc
                    P.mm(pc[:, cc * 256:(cc + 1) * 256], Y[:, c, 0, :], G12[:, 0, :], True, False, [yk, 'fftc'], [pck])
                    P.mm(pc[:, cc * 256:(cc + 1) * 256], Y[:, c, 1, :], G12[:, 1, :], False, True, [yk, 'fftc'], [pck])
                    yield
                yield from twiddle(pc[:], pck, TWI, D, dk_, pair)
            rk = [dk_ + 'r0', dk_ + 'r1', dk_ + 'i0', dk_ + 'i1', 'fftc']
            yb, ybk = banks[6], 'ybank'
            P.mm(yb[0:64, :], Gst[:, 0, :], D[:, :, 0, :], True, False, rk, [ybk])
            P.mm(yb[0:64, :], Gst[:, 1, :], D[:, :, 1, :], False, True, rk, [ybk])
            yield
            cs = slice(g * 4, g * 4 + 4)
            e_ = ep[i2]
            ek = 'ep%d' % i2
            ev = e_[:].rearrange("p (c k) -> p c k", c=4)
            P.pool('tensor_tensor', ['xd0', 'zz:%d' % g, 'hbias'], [ek], out=ev, in0=xv[:, cs, :], in1=hbias[:, o, cs].unsqueeze(2).to_broadcast([64, 4, 128]), op=ALU.mult)
            yield
            P.dve('tensor_tensor', [ybk, ek], [ek, ybk], out=e_[:], in0=yb[0:64, :], in1=e_[:], op=ALU.add)
            yield
            if o == 0:
                P.dve('tensor_tensor', [ek, 'xd1'], ['zz:%d' % g], out=xv[:, cs, :], in0=ev, in1=x1[:, cs, :], op=ALU.mult)
            else:
                os_ = ost[i2]
                P.dve('tensor_tensor', [ek, 'xd2'], ['ost%d' % i2], out=os_[:], in0=ev, in1=x2[:, cs, :], op=ALU.mult)
                P.dma(q3(), reads=['ost%d' % i2], out=hy_out[:, cs, :], in_=os_[:])
            yield

        prev = None
        for o in range(2):
            for g in range(hy_groups):
                interleave(fwd_gen(o, g), prev)
                prev = inv_gen(o, g)
        interleave(prev)
        barrier(P)
        while len(P.stack) > mark0:
            P.stack.pop().__exit__(None, None, None)

    if do_ssd:
        mark2 = len(P.stack)
        cw = P.sb("ssd_cw_sb", [128, 3, 4], F32)
        identb = P.sb("identb_sb", [128, 128], BF16)
        ybf = P.sb("ssd_ybf", [128, 3, T], BF16)
        dttm = P.sb("dttm_sb", [64, NCH, 4], F32)
        mark3 = len(P.stack)
        xin = P.sb("ssd_xin_sb", [128, 3, TP], BF16)
        acc = [P.sb("ssd_acc%d" % i, [128, 2048], F32) for i in range(2)]
        for p in range(3):
            P.dma(q3(), writes=['sxin'], out=xin[:, p, :], in_=xin_d[:, p, :])
        P.dma('sp', writes=['cw'], out=cw[:], in_=scw_d)
        P.dma('sp', writes=['identb'], out=identb[:], in_=ident_d)
        P.dma('sp', writes=['dttm'], out=dttm[:], in_=dttm_d)
        segs = [(0, 0, 256)] + [(258 + i * 2048, 256 + i * 2048, 2048) for i in range(4)]
        it = 0
        for p in range(3):
            for (pc0, oc0, n) in segs:
                i2 = it % 2
                it += 1
                dwconv(xin, 'sxin', cw, p, pc0, n, acc[i2], 'sacc%d' % i2)
                P.act(acc[i2][:, 0:n], acc[i2][:, 0:n], AF.Silu, ['sacc%d' % i2], ['sacc%d' % i2])
                P.pool('tensor_copy', ['sacc%d' % i2], ['ybf%d' % p], out=ybf[:, p, oc0:oc0 + n], in_=acc[i2][:, 0:n])
                if p == 0:
                    P.dma(q3(), reads=['sacc%d' % i2], out=xs_out[:, oc0:oc0 + n], in_=acc[i2][:, 0:n])
        P.dma(q3(), reads=['ybf2'], writes=['s_qT'], out=s_qT, in_=ybf[:, 2, :])
        P.dma(q3(), reads=['ybf1'], writes=['s_kT'], out=s_kT, in_=ybf[:, 1, :])
        barrier(P)
        while len(P.stack) > mark3:
            P.stack.pop().__exit__(None, None, None)
        btm = P.sb("btm", [64, NCH, 128], BF16)
        xtm = P.sb("xtm", [64, NCH, 128], BF16)
        vt = [P.sb("vt%d" % i, [64, NCH, 64], BF16) for i in range(2)]
        tb = [banks[0], banks[1]]
        for part, dst, dk_ in ((1, btm, 'btm'), (0, xtm, 'xtm')):
            for c4 in range(NCH // 4):
                i2 = c4 % 2
                bkb = tb[i2][0:64, :].bitcast(BF16)
                for j in range(4):
                    c = c4 * 4 + j
                    P.op('pe', 'transpose', ['ybf%d' % part, 'identb'], ['tb%d' % i2], out=bkb[:, j * 128:(j + 1) * 128], in_=ybf[:, part, c * 64:(c + 1) * 64], identity=identb[:])
                P.act(dst[:, c4 * 4:(c4 + 1) * 4, :], bkb[:, 0:512].rearrange("p (c k) -> p c k", c=4), AF.Copy, ['tb%d' % i2], [dk_, 'tb%d' % i2])
        P.dma(q3(), reads=['btm'], writes=['s_ktm'], out=s_ktm, in_=btm[:])
        for k in range(4):
            d, j = k // 2, k % 2
            v_ = vt[k % 2]
            P.dve('tensor_tensor', ['xtm', 'dttm'], ['vt%d' % (k % 2)], out=v_[:], in0=xtm[:, :, j * 64:(j + 1) * 64], in1=dttm[:, :, k:k + 1].to_broadcast([64, NCH, 64]), op=ALU.mult)
            P.dma(q3(), reads=['vt%d' % (k % 2)], writes=['s_vtm%d' % k], out=s_vtm[k], in_=v_[:])
        barrier(P)
        while len(P.stack) > mark2:
            P.stack.pop().__exit__(None, None, None)
        tri = scan_consts(P, tri_d)
        scans = []
        for k in range(4):
            d, j = k // 2, k % 2
            scans.append(dict(dk=128, dv=64, mode='scalar', rev=(d == 1), qT=s_qT, kT=s_kT, ktm=s_ktm, vtm=s_vtm[k], latm=latm_d[k], o=ssd_o[k],
                              rkeys=['s_qT', 's_kT', 's_ktm', 's_vtm%d' % k], okey='ssd_out%d' % k))
        emit_scans(P, scans, tri, banks, nscan_steps)
    P.wait_all('sp')
    P.emit()
    P.close()
    return nc


HY_MIN_DECAY = float(np.log(1e-2) / 1.5)
HY_MAX_DECAY = float(np.log(1e-2) / 0.3)


def pad1(a):
    return np.concatenate([np.zeros_like(a[:, :1]), a, np.zeros_like(a[:, :1])], axis=1)


def hyena_pos_consts():
    n = 8192
    t = np.linspace(0.0, 1.0, n, dtype=np.float32)[:, None]
    bands = np.linspace(1e-4, 15, 16, dtype=np.float32)
    ang = (np.float32(2.0 * np.pi / n) * np.arange(n, dtype=np.float32)[:, None]) * bands
    z = np.concatenate([t, np.cos(ang), -np.sin(ang)], axis=-1).astype(np.float32)
    rates = np.abs(np.linspace(HY_MIN_DECAY, HY_MAX_DECAY, 512, dtype=np.float32))
    negt = -(np.arange(8192, dtype=np.float32) / np.float32(8191.0)).reshape(64, 128)
    return np.ascontiguousarray(z.T), rates, np.ascontiguousarray(negt)


def host_B1_inputs(inp, c0):
    obf = [r['o_bf'] for r in c0]
    of = [r['o_f'] for r in c0]
    fc = fft_consts()
    zT, rates, negt = hyena_pos_consts()
    tri = tri_consts()
    identb = np.eye(128, dtype=np.float32).astype(NPBF)
    cwall = np.concatenate([inp['mb_conv_w'][0], inp['mb_conv_b'][0][:, None]], axis=1)
    hcwall = np.concatenate([inp['hy_short_w'][0], inp['hy_short_b'][0][:, None]], axis=1)
    maps = []
    for i in range(NCORES):
        b, q = i // 4, i % 4
        g, hp = q // 2, q % 2
        h0 = 4 * g + 2 * hp
        rows = [64 * h0, 512 + 128 * g, 768 + 128 * g]
        m = dict(tri=tri, identb=identb)
        m.update(fc)
        xin = []
        for r0 in rows:
            a = joint_fm(obf, A1_BF['xbc'] + r0, 128, b)
            xin.append(np.concatenate([pad1(a[:, :256]), pad1(a[:, 256:])], axis=1))
        m['ssd_xin'] = np.ascontiguousarray(np.stack(xin, axis=1))
        m['ssd_cw'] = np.ascontiguousarray(np.stack([cwall[r0:r0 + 128] for r0 in rows], axis=1))
        dt4, la4 = [], []
        for k in range(4):
            d, j = k // 2, k % 2
            dt4.append(to_tm(joint_fm(of, A1_F['dt'] + d * 8 + h0 + j, 1, b)))
            la = to_tm(joint_fm(of, A1_F['la'] + d * 8 + h0 + j, 1, b))
            m['ssd_latm%d' % k] = np.ascontiguousarray(np.broadcast_to(la, (64, NCH, 128)))
        m['ssd_dttm'] = np.ascontiguousarray(np.concatenate(dt4, axis=2))
        hy = [pad1(joint_fm(obf, A1_BF['hy'] + p * 512 + 128 * q, 128, b)[:, 256:]) for p in range(3)]
        m['hy_in'] = np.ascontiguousarray(np.stack(hy, axis=1))
        m['hy_cw'] = np.ascontiguousarray(np.stack([hcwall[p * 512 + 128 * q:p * 512 + 128 * q + 128] for p in range(3)], axis=1))
        m['hy_zT'] = zT
        m['hy_w1'] = inp['hy_w1'][0]
        m['hy_w2'] = inp['hy_w2'][0]
        m['hy_w3'] = np.ascontiguousarray(inp['hy_w3'][0].reshape(64, 4, 512)[:, :, 128 * q:128 * q + 128])
        m['hy_fp'] = np.ascontiguousarray(np.stack([inp['hy_freq'][0], inp['hy_b1'][0], inp['hy_b2'][0]], axis=1))
        m['hy_rate'] = np.ascontiguousarray(np.broadcast_to(rates[128 * q:128 * q + 128][None, :], (64, 128)))
        m['hy_negt'] = negt
        m['hy_bias'] = np.ascontiguousarray(np.broadcast_to(inp['hy_bias'][0][:, 128 * q:128 * q + 128][None], (64, 2, 128)))
        maps.append(m)
    return maps


def host_C1_inputs(inp, a0, c0, b1):
    maps = []
    com = common_C_inputs(inp, 1)
    dsk = np.ascontiguousarray(np.repeat(inp['mb_d'][0], 64).reshape(4, 128).T)
    for i in range(NCORES):
        b, q = i // 4, i % 4
        ls = slice(256 + 2048 * q, 256 + 2048 * (q + 1))
        hyT = np.empty((128, 4, 2048), np.float32)
        soT = np.empty((2, 128, 4, 2048), np.float32)
        xsT = np.empty((128, 4, 2048), np.float32)
        for j in range(4):
            src = b1[4 * b + j]
            hy_ct = src['hy_out'].transpose(1, 0, 2).reshape(128, 8192)
            hyT[:, j, :] = hy_ct[:, 2048 * q:2048 * (q + 1)]
            xsT[:, j, :] = src['xs_out'][:, ls]
            for d in range(2):
                for jj in range(2):
                    o = tm_to_joint(src['ssd_o%d' % (d * 2 + jj)])
                    soT[d, jj * 64:(jj + 1) * 64, j, :] = o[ls].T
        m = dict(com)
        m.update(hT_in=np.ascontiguousarray(c0[i]['hT_out'][:, :, 0:2048]), mod_in=a0[i]['mods_out'][1], w_out=inp['cd_w_out'][0],
                 hyT=hyT, ssd_oT=soT, xsT=xsT,
                 zsT=np.ascontiguousarray(c0[i]['o_f'][0:512, 0:2048].reshape(4, 128, 2048).transpose(1, 0, 2)),
                 dsk=dsk, mbg=colT(inp['mb_norm_g'][0], 4), gout=colT(inp['norm_out_g'], 8))
        maps.append(m)
    return maps


_CACHE = {}


def _prog(name, fn):
    if name not in _CACHE:
        _CACHE[name] = fn()
    return _CACHE[name]


def _run(nc, maps):
    res = run_bass_kernel_spmd(nc, maps, core_ids=list(range(NCORES)))
    return [dict(r) for r in res.results]


def kernel(**inputs):
    inp = {k: np.asarray(v) for k, v in inputs.items()}
    a0 = _run(_prog('A0', build_A0), host_A0_inputs(inp))
    b0 = _run(_prog('B0', build_B0), host_B0_inputs(a0))
    c0 = _run(_prog('C0', lambda: build_C2(0)), host_C0_inputs(inp, a0, b0))
    b1 = _run(_prog('B1', build_B1), host_B1_inputs(inp, c0))
    c1 = _run(_prog('C1', lambda: build_C2(1)), host_C1_inputs(inp, a0, c0, b1))
    out = np.empty((2, 8192, 1024), np.float32)
    for i in range(NCORES):
        b, q = i // 4, i % 4
        out[b, 2048 * q:2048 * (q + 1), :] = c1[i]['y_out'].transpose(2, 1, 0).reshape(2048, 1024)
    return out
```

```python
import numpy as np
import ml_dtypes
import concourse.bass as bass
import concourse.mybir as mybir
from concourse.bass_utils import run_bass_kernel_spmd

F32 = mybir.dt.float32
BF16 = mybir.dt.bfloat16
AF = mybir.ActivationFunctionType
ALU = mybir.AluOpType
AX = mybir.AxisListType
NPBF = ml_dtypes.bfloat16

ENGS = ['pe', 'act', 'dve', 'pool', 'sp']
SAME_ENGINE_SYNC = True
NCORES = 8
TL = 2112
TILES = [(0, 512, 0), (512, 1024, 0), (1024, 1536, 0), (1536, 2048, 0), (2048, 2112, 1)]
EPS = 1e-6


class Prog:
    def __init__(self, nc, ndma_sems=6):
        self.nc = nc
        self.q = {e: [] for e in ENGS}
        self.cnt = {e: 0 for e in ENGS}
        self.seen = {e: {} for e in ENGS}
        self.lastw = {}
        self.reads = {}
        self.ndma = ndma_sems
        self.dma_n = {e: 0 for e in ENGS}
        self.stack = []
        self.uid = 0

    def enter(self, cm):
        v = cm.__enter__()
        self.stack.append(cm)
        return v

    def sb(self, name, shape, dt):
        return self.enter(self.nc.sbuf_tensor(name, list(shape), dt))

    def ps(self, name, shape, dt=F32):
        return self.enter(self.nc.psum_tensor(name, list(shape), dt))

    def close(self):
        while self.stack:
            self.stack.pop().__exit__(None, None, None)

    def _deps(self, eng, reads, writes):
        toks = set()
        for r in reads:
            t = self.lastw.get(r)
            if t is not None:
                toks.add(t)
        for w in writes:
            t = self.lastw.get(w)
            if t is not None:
                toks.add(t)
            for t in self.reads.get(w, ()):
                toks.add(t)
        need = {}
        for (k, v) in toks:
            if k == eng and (eng == 'pe' or not SAME_ENGINE_SYNC):
                continue
            if self.seen[eng].get(k, 0) >= v:
                continue
            if need.get(k, 0) < v:
                need[k] = v
        for k, v in need.items():
            self.seen[eng][k] = v
        return list(need.items())

    def _commit(self, tok, reads, writes):
        for r in reads:
            if r in writes:
                continue
            self.reads.setdefault(r, []).append(tok)
        for w in writes:
            self.lastw[w] = tok
            self.reads[w] = []

    def op(self, eng, name, reads=(), writes=(), **kw):
        reads = list(reads)
        writes = list(writes)
        waits = self._deps(eng, reads, writes)
        self.cnt[eng] += 1
        tok = (eng, self.cnt[eng])
        self.q[eng].append((waits, (name, kw), ('c', eng)))
        self._commit(tok, reads, writes)

    def mm(self, out, lhsT, rhs, start, stop, reads, writes):
        self.op('pe', 'matmul', reads, writes, out=out, lhsT=lhsT, rhs=rhs, start=start, stop=stop)

    def act(self, out, in_, func, reads, writes, **kw):
        self.op('act', 'activation', reads, writes, out=out, in_=in_, func=func, **kw)

    def dve(self, name, reads, writes, **kw):
        self.op('dve', name, reads, writes, **kw)

    def pool(self, name, reads, writes, **kw):
        self.op('pool', name, reads, writes, **kw)

    def dma(self, eng, reads=(), writes=(), **kw):
        fn = ('dma_start', kw)
        reads = list(reads)
        writes = list(writes)
        n = self.dma_n[eng]
        self.dma_n[eng] += 1
        si = n % self.ndma
        key = ('d', eng, si)
        val = 16 * (n // self.ndma + 1)
        waits = self._deps(eng, reads, writes)
        if n >= self.ndma and self.seen[eng].get(key, 0) < val - 16:
            waits.append((key, val - 16))
            self.seen[eng][key] = val - 16
        self.q[eng].append((waits, fn, key))
        self._commit((key, val), reads, writes)

    def wait_all(self, eng):
        need = {}
        for tok in self.lastw.values():
            k, v = tok
            if need.get(k, 0) < v:
                need[k] = v
        for toks in self.reads.values():
            for k, v in toks:
                if need.get(k, 0) < v:
                    need[k] = v
        waits = [(k, v) for k, v in need.items() if self.seen[eng].get(k, 0) < v and (k != eng or eng != 'sp')]
        for k, v in waits:
            self.seen[eng][k] = v
        self.q[eng].append((waits, None, None))

    def emit(self):
        nc = self.nc
        used = set()
        for e in ENGS:
            for waits, fn, inc in self.q[e]:
                for k, _ in waits:
                    used.add(k)
                if inc is not None:
                    used.add(inc if inc[0] == 'd' else inc[1])
        sem = {}
        for i, k in enumerate(sorted(used, key=str)):
            sem[k] = self.enter(nc.semaphore("sem%d" % i))
        block = self.enter(nc.Block())
        q = self.q

        def run(e, engine):
            for waits, fn, inc in q[e]:
                for k, v in waits:
                    engine.wait_ge(sem[k], v)
                if fn is None:
                    continue
                inst = getattr(engine, fn[0])(**fn[1])
                if inc[0] == 'd':
                    inst.then_inc(sem[inc], 16)
                else:
                    inst.then_inc(sem[inc[1]], 1)

        @block.tensor
        def _(eng):
            run('pe', eng)

        @block.scalar
        def _(eng):
            run('act', eng)

        @block.vector
        def _(eng):
            run('dve', eng)

        @block.gpsimd
        def _(eng):
            run('pool', eng)

        @block.sync
        def _(eng):
            run('sp', eng)


def emit_mods(P, condT, ada_w_l, adab, pm, tag):
    cond = P.sb("sbcond" + tag, [128, 8, 2], F32)
    sc = P.sb("sbsc" + tag, [128, 8, 2], F32)
    adab_sb = P.sb("sbadab" + tag, [128, 48], F32)
    mod = P.sb("sbmod" + tag, [128, 48, 2], F32)
    mark_ = len(P.stack)
    wb = [P.sb("sbadaw%d" % i + tag, [128, 8, 256], F32) for i in range(2)]
    P.dma('sp', writes=['cond' + tag], out=cond[:], in_=condT)
    P.dma('sp', writes=['adab' + tag], out=adab_sb[:], in_=adab)
    P.act(sc[:], cond[:], AF.Silu, ['cond' + tag], ['sc' + tag])
    wv = ada_w_l.rearrange("(k p) n -> p k n", p=128)
    for s in range(24):
        buf = wb[s % 2]
        bk = 'adaw%d' % (s % 2) + tag
        P.dma('sp' if s % 2 == 0 else 'act', writes=[bk], out=buf[:], in_=wv[:, :, s * 256:(s + 1) * 256])
        for m4 in range(2):
            m = s * 2 + m4
            for k in range(8):
                P.mm(pm[:, 2 * m:2 * m + 2], buf[:, k, m4 * 128:(m4 + 1) * 128], sc[:, k, :], k == 0, k == 7, [bk, 'sc' + tag], ['pm' + tag])
    P.dve('tensor_tensor', ['pm' + tag, 'adab' + tag], ['mod' + tag], out=mod[:], in0=pm[:, 0:96].rearrange("p (m r) -> p m r", r=2),
          in1=adab_sb[:].unsqueeze(2).to_broadcast([128, 48, 2]), op=ALU.add)
    barrier(P)
    while len(P.stack) > mark_:
        P.stack.pop().__exit__(None, None, None)
    return mod


def emit_affine(P, mod, modkey, gT_dram, kind_scale, tag):
    g = P.sb("sbg" + tag, [128, 8], F32)
    A = P.sb("sbA" + tag, [128, 8, 2], F32)
    P.dma('sp', writes=['g' + tag], out=g[:], in_=gT_dram)
    P.dve('scalar_tensor_tensor', [modkey, 'g' + tag], ['A' + tag], out=A[:], in0=mod[:, kind_scale * 8:kind_scale * 8 + 8, :], scalar=1.0,
          in1=g[:].unsqueeze(2).to_broadcast([128, 8, 2]), op0=ALU.add, op1=ALU.mult)
    return A


def emit_rstd(P, src, srckey, n, nk, ones_bf, ps_ssq, sq, rs, inv_n):
    P.act(sq[:, 0:nk, 0:n], src, AF.Square, [srckey], ['sq'])
    for k in range(nk):
        P.mm(ps_ssq[:, 0:n], ones_bf[:], sq[:, k, 0:n], k == 0, k == nk - 1, ['sq', 'ones'], ['ps_ssq'])
    P.dve('tensor_scalar', ['ps_ssq'], ['rs'], out=rs[:, 0:n], in0=ps_ssq[:, 0:n], scalar1=float(inv_n), scalar2=EPS, op0=ALU.mult, op1=ALU.add)
    P.act(rs[:, 0:n], rs[:, 0:n], AF.Sqrt, ['rs'], ['rs'])
    P.dve('reciprocal', ['rs'], ['rs'], out=rs[:, 0:n], in_=rs[:, 0:n])


def emit_modulate_tile(P, hT, hkey, A, Akey, mod, modkey, kind_shift, ti, tile, ones_bf, ps_ssq, work, uT=None, ukey=None, u32=None):
    sq, rs, tmp = work['sq'], work['rs'], work['tmp']
    c0, c1, r = tile
    n = c1 - c0
    emit_rstd(P, hT[:, :, c0:c1], hkey + ':%d' % ti, n, 8, ones_bf, ps_ssq, sq, rs, 1.0 / 1024)
    for k in range(8):
        tb = tmp[k % 2]
        tk = 'tmp%d' % (k % 2)
        P.dve('scalar_tensor_tensor', [hkey + ':%d' % ti, Akey, 'rs'], [tk], out=tb[:, 0:n], in0=hT[:, k, c0:c1], scalar=A[:, k, r:r + 1], in1=rs[:, 0:n], op0=ALU.mult, op1=ALU.mult)
        sh = mod[:, kind_shift * 8 + k, r:r + 1]
        if u32 is not None:
            P.act(u32[:, k, 0:n], tb[:, 0:n], AF.Identity, [tk, modkey], ['u32'], bias=sh, scale=1.0)
            if uT is not None:
                P.pool('tensor_copy', ['u32'], [ukey + ':%d' % ti], out=uT[:, k, c0:c1], in_=u32[:, k, 0:n])
        else:
            P.act(uT[:, k, c0:c1], tb[:, 0:n], AF.Identity, [tk, modkey], [ukey + ':%d' % ti], bias=sh, scale=1.0)


AB = dict(gq=0, gk=256, gv=512, gg=1024, glr_f=1536, glr_b=1552, hq=1568, hf_f=2080, hf_b=2592, hi=3104, hg=3616, end=4128)
A0_BF = dict(gq=0, gk=256, gv=512, hq=1024, hk_f=1536, hk_b=2048, hi=2560, end=3072)
A0_F = dict(gg=0, hg=512, gla_f=1024, gla_b=1280, hla_f=1536, hla_b=2048, end=2560)


class ProjCtx:
    def __init__(self, P, w_dram, uT, ukey, gb, o_f, o_bf, nst=3):
        self.P, self.uT, self.ukey, self.gb, self.o_f, self.o_bf = P, uT, ukey, gb, o_f, o_bf
        self.wv = w_dram.rearrange("(k p) n -> p k n", p=128)
        self.wblk = [P.sb("wblk%d" % i, [128, 8, 512], BF16) for i in range(2)]
        self.nw = 0
        self.W = None
        self.nst = nst
        self.stg_f = [P.sb("stgf%d" % i, [128, TL], F32) for i in range(nst)]
        self.stg_b = [P.sb("stgb%d" % i, [128, TL], BF16) for i in range(nst)]
        self.cnt = {'f': 0, 'b': 0, 'g': 0, 'q': 0}

    def stage(self, kind):
        i = self.cnt[kind] % self.nst
        self.cnt[kind] += 1
        return ((self.stg_f if kind == 'f' else self.stg_b)[i], 'stg%s%d' % (kind, i))

    def bank(self):
        i = self.cnt['g'] % len(self.gb)
        self.cnt['g'] += 1
        return self.gb[i], 'gb%d' % i

    def outdma(self, kind, st, sk, row0, nrows, ncols=TL):
        dst = (self.o_f if kind == 'f' else self.o_bf)
        q = ['sp', 'act'][self.cnt['q'] % 2]
        self.cnt['q'] += 1
        self.P.dma(q, reads=[sk], out=dst[row0:row0 + nrows, 0:ncols], in_=st[0:nrows, 0:ncols])

    def load_w(self, col0, ncols):
        i = self.nw % 2
        self.nw += 1
        self.W, self.Wkey, self.wcol0 = self.wblk[i], 'wblk%d' % i, col0
        self.P.dma('pool', writes=[self.Wkey], out=self.W[:, :, 0:ncols], in_=self.wv[:, :, col0:col0 + ncols])

    def chunk(self, col0, ncols, post, tiles=TILES):
        P = self.P
        col0 = col0 - self.wcol0
        for ti, (c0, c1, r) in enumerate(tiles):
            n = c1 - c0
            b, bkey = self.bank()
            for k in range(8):
                P.mm(b[0:ncols, 0:n], self.W[:, k, col0:col0 + ncols], self.uT[:, k, c0:c1], k == 0, k == 7, [self.Wkey, self.ukey + ':%d' % ti], [bkey])
            post(b[0:ncols, 0:n], n, c0, c1, ti, bkey)

    def job_lin(self, col0, ncols_total, row0, scale, kind='b', tiles=TILES, ncols_out=TL):
        P = self.P
        for j in range(ncols_total // 128):
            if j % 4 == 0:
                self.load_w(col0 + j * 128, min(512, ncols_total - j * 128))
            st, sk = self.stage(kind)

            def post(ps, n, c0, c1, ti, bkey, st=st, sk=sk):
                P.dve('tensor_scalar', [bkey], [sk, bkey], out=st[:, c0:c1], in0=ps, scalar1=float(scale), scalar2=None, op0=ALU.mult)
            self.chunk(col0 + j * 128, 128, post, tiles)
            self.outdma(kind, st, sk, row0 + j * 128, 128, ncols_out)

    def job_silu(self, col0, ncols_total, row0, kind, tiles=TILES, ncols_out=TL):
        P = self.P
        for j in range(ncols_total // 128):
            if j % 4 == 0:
                self.load_w(col0 + j * 128, min(512, ncols_total - j * 128))
            st, sk = self.stage(kind)

            def post(ps, n, c0, c1, ti, bkey, st=st, sk=sk):
                P.act(st[:, c0:c1], ps, AF.Silu, [bkey], [sk, bkey])
            self.chunk(col0 + j * 128, 128, post, tiles)
            self.outdma(kind, st, sk, row0 + j * 128, 128, ncols_out)


def build_A0():
    nc = bass.Bass("TRN2", target_bir_lowering=False)
    xT = nc.dram_tensor("xT", [128, 8, TL], F32, kind="ExternalInput").ap()
    condT = nc.dram_tensor("condT", [128, 8, 2], F32, kind="ExternalInput").ap()
    ada_w = nc.dram_tensor("ada_w", [1024, 6144], F32, kind="ExternalInput").ap()
    adab = nc.dram_tensor("adab", [128, 48], F32, kind="ExternalInput").ap()
    gmix = nc.dram_tensor("gmix", [128, 8], F32, kind="ExternalInput").ap()
    w_in = nc.dram_tensor("w_in", [1024, 4128], F32, kind="ExternalInput").ap()
    gate_w = nc.dram_tensor("gate_w", [16, 2, 256], F32, kind="ExternalInput").ap()
    gate_b = nc.dram_tensor("gate_b", [128, 2, 2], F32, kind="ExternalInput").ap()
    hglb = nc.dram_tensor("hglb", [128, 2, 2, 4], F32, kind="ExternalInput").ap()
    o_bf = nc.dram_tensor("o_bf", [A0_BF['end'], TL], BF16, kind="ExternalOutput").ap()
    o_f = nc.dram_tensor("o_f", [A0_F['end'], TL], F32, kind="ExternalOutput").ap()
    ada_w1 = nc.dram_tensor("ada_w1", [1024, 6144], F32, kind="ExternalInput").ap()
    adab1 = nc.dram_tensor("adab1", [128, 48], F32, kind="ExternalInput").ap()
    mods_out = nc.dram_tensor("mods_out", [2, 128, 48, 2], F32, kind="ExternalOutput").ap()

    P = Prog(nc)
    hT = P.sb("hT", [128, 8, TL], F32)
    uT = P.sb("uT", [128, 8, TL], BF16)
    ones_bf = P.sb("ones_bf", [128, 128], BF16)
    work = dict(sq=P.sb("sq", [128, 8, 512], BF16), rs=P.sb("rs", [128, 512], F32),
                tmp=[P.sb("tmp%d" % i, [128, 512], F32) for i in range(2)])
    banks = [P.ps("bank%d" % i, [128, 512], F32) for i in range(6)]
    pm, ps_ssq = banks[0], banks[1]
    gb = banks[2:6]

    for k in range(8):
        P.dma('sp' if k % 2 == 0 else 'act', writes=['h:%d' % t for t in range(5)], out=hT[:, k, :], in_=xT[:, k, :])
    P.pool('memset', [], ['ones'], ap=ones_bf[:], constant=1.0)

    mod = emit_mods(P, condT, ada_w, adab, pm, '0')
    modB = emit_mods(P, condT, ada_w1, adab1, pm, '1')
    P.dma('sp', reads=['mod0'], out=mods_out[0], in_=mod[:])
    P.dma('sp', reads=['mod1'], out=mods_out[1], in_=modB[:])
    A = emit_affine(P, mod, 'mod0', gmix, 1, 'm0')
    for ti, tile in enumerate(TILES):
        emit_modulate_tile(P, hT, 'h', A, 'Am0', mod, 'mod0', 0, ti, tile, ones_bf, ps_ssq, work, uT=uT, ukey='u')

    gw_f = P.sb("gw_f", [16, 2, 256], F32)
    gw = P.sb("gw", [16, 2, 256], BF16)
    gbias = P.sb("gbias", [128, 2, 2], F32)
    ngb = P.sb("ngb", [128, 2, 2], F32)
    lbr = P.sb("lbr", [128, 2, 2, 4], F32)
    lbe = P.sb("lbe", [128, 2, 2, 4], F32)
    lb = P.sb("lb", [128, 2, 4], F32)
    oml = P.sb("oml", [128, 2, 4], F32)
    den = P.sb("den", [128, 2, 4], F32)
    P.dma('sp', writes=['gw_f'], out=gw_f[:], in_=gate_w)
    P.dma('sp', writes=['gbias'], out=gbias[:], in_=gate_b)
    P.dma('sp', writes=['lbr'], out=lbr[:], in_=hglb)
    P.dve('tensor_copy', ['gw_f'], ['gw'], out=gw[:], in_=gw_f[:])
    P.dve('tensor_scalar', ['gbias'], ['ngb'], out=ngb[:], in0=gbias[:], scalar1=-1.0, scalar2=None, op0=ALU.mult)
    P.act(lbe[:], lbr[:], AF.Exp, ['lbr'], ['lbe'])
    P.dve('tensor_tensor', ['lbe'], ['den'], out=den[:], in0=lbe[:, :, 0, :], in1=lbe[:, :, 1, :], op=ALU.add)
    P.dve('reciprocal', ['den'], ['den'], out=den[:], in_=den[:])
    P.dve('tensor_tensor', ['lbe', 'den'], ['lb'], out=lb[:], in0=lbe[:, :, 0, :], in1=den[:], op=ALU.mult)
    P.dve('tensor_scalar', ['lb'], ['oml'], out=oml[:], in0=lb[:], scalar1=-1.0, scalar2=1.0, op0=ALU.mult, op1=ALU.add)

    glrT = [P.sb("glrT%d" % d, [16, TL], BF16) for d in range(2)]
    ftmp = [P.sb("ftmp%d" % i, [128, 512], F32) for i in range(2)]
    C = ProjCtx(P, w_in, uT, 'u', gb, o_f, o_bf)

    def job_glr(col0, d):
        def post(ps, n, c0, c1, ti, bkey):
            P.dve('tensor_copy', [bkey], ['glrT%d' % d], out=glrT[d][:, c0:c1], in_=ps)
        C.load_w(col0, 16)
        C.chunk(col0, 16, post)

    def job_gate(d, row0):
        for j in range(2):
            st, sk = C.stage('f')
            for ti, (c0, c1, r) in enumerate(TILES):
                n = c1 - c0
                b, bkey = C.bank()
                P.mm(b[:, 0:n], gw[:, d, j * 128:(j + 1) * 128], glrT[d][:, c0:c1], True, True, ['gw', 'glrT%d' % d], [bkey])
                ft = ftmp[ti % 2]
                fk = 'ftmp%d' % (ti % 2)
                P.act(ft[:, 0:n], b[:, 0:n], AF.Exp, [bkey, 'ngb'], [fk], bias=ngb[:, d, j:j + 1], scale=-1.0)
                P.act(ft[:, 0:n], ft[:, 0:n], AF.Ln, [fk], [fk], bias=1.0, scale=1.0)
                P.dve('tensor_scalar', [fk], [sk], out=st[:, c0:c1], in0=ft[:, 0:n], scalar1=-1.0 / 16.0, scalar2=None, op0=ALU.mult)
            C.outdma('f', st, sk, row0 + j * 128, 128)

    def job_hgf(col0, d, row_k, row_la):
        C.load_w(col0, 512)
        for j in range(4):
            stb, skb = C.stage('b')
            stf, skf = C.stage('f')

            def post(ps, n, c0, c1, ti, bkey, stb=stb, skb=skb, stf=stf, skf=skf, j=j):
                ft = ftmp[ti % 2]
                fk = 'ftmp%d' % (ti % 2)
                P.act(ft[:, 0:n], ps, AF.Sigmoid, [bkey], [fk])
                P.dve('tensor_scalar', [fk, 'oml', 'lb'], [fk], out=ft[:, 0:n], in0=ft[:, 0:n], scalar1=oml[:, d, j:j + 1], scalar2=lb[:, d, j:j + 1], op0=ALU.mult, op1=ALU.add)
                P.pool('tensor_scalar', [fk], [skb], out=stb[:, c0:c1], in0=ft[:, 0:n], scalar1=-1.0, scalar2=1.0, op0=ALU.mult, op1=ALU.add)
                P.act(stf[:, c0:c1], ft[:, 0:n], AF.Ln, [fk], [skf])
            C.chunk(col0 + j * 128, 128, post)
            C.outdma('b', stb, skb, row_k + j * 128, 128)
            C.outdma('f', stf, skf, row_la + j * 128, 128)

    job_glr(AB['glr_f'], 0)
    job_glr(AB['glr_b'], 1)
    C.job_lin(AB['gq'], 256, A0_BF['gq'], 64 ** -0.5)
    C.job_lin(AB['gk'], 256, A0_BF['gk'], 1.0)
    C.job_lin(AB['gv'], 512, A0_BF['gv'], 1.0)
    C.job_silu(AB['gg'], 512, A0_F['gg'], 'f')
    job_gate(0, A0_F['gla_f'])
    job_gate(1, A0_F['gla_b'])
    C.job_silu(AB['hq'], 512, A0_BF['hq'], 'b')
    job_hgf(AB['hf_f'], 0, A0_BF['hk_f'], A0_F['hla_f'])
    job_hgf(AB['hf_b'], 1, A0_BF['hk_b'], A0_F['hla_b'])
    C.job_lin(AB['hi'], 512, A0_BF['hi'], 1.0)
    C.job_silu(AB['hg'], 512, A0_F['hg'], 'f')
    P.wait_all('sp')
    P.emit()
    P.close()
    return nc


def fm(a):
    T = a.shape[0]
    return np.ascontiguousarray(a.T.reshape(8, 128, T).transpose(1, 0, 2))


def colT(v, nk):
    return np.ascontiguousarray(np.asarray(v).reshape(nk, 128).T)


def core_tokens(x, ctx, i):
    b, q = i // 4, i % 4
    return np.concatenate([x[b, 2048 * q:2048 * (q + 1)], ctx[b, 64 * q:64 * (q + 1)]], axis=0)


def cond_T(inp, b):
    cond = np.stack([inp['c'][b], inp['c_ctx']], axis=-1)
    return np.ascontiguousarray(cond.reshape(8, 128, 2).transpose(1, 0, 2))


def host_A0_inputs(inp):
    maps = []
    x, ctx = inp['x'], inp['ctx']
    for i in range(NCORES):
        b = i // 4
        m = dict(
            xT=fm(core_tokens(x, ctx, i)),
            condT=cond_T(inp, b),
            ada_w=inp['ada_w'][0], adab=colT(inp['ada_b'][0], 48), gmix=colT(inp['norm_mix_g'][0], 8),
            ada_w1=inp['ada_w'][1], adab1=colT(inp['ada_b'][1], 48),
            w_in=inp['ab_w_in'][0],
            gate_w=np.ascontiguousarray(inp['gla_gate_w'][0].transpose(1, 0, 2)),
            gate_b=np.ascontiguousarray(inp['gla_gate_b'][0].reshape(2, 2, 128).transpose(2, 0, 1)),
            hglb=np.ascontiguousarray(inp['hg_lb'].reshape(2, 2, 4, 128).transpose(3, 0, 1, 2)),
        )
        maps.append(m)
    return maps


STOP_STAGE = 9
NSCAN = 4
VAR7 = 7
NCH = 132
SCH = 4
NSC = NCH // SCH


def scan_consts(P, tri_dram):
    tri = P.sb("tri_sb", [64, 6, 64], F32)
    P.dma('sp', writes=['tri'], out=tri[:], in_=tri_dram)
    return tri


def emit_scans(P, scans, tri, banks, nsteps=None):
    ns = len(scans)
    st = []
    for i, sc in enumerate(scans):
        dk, dv = sc['dk'], sc['dv']
        d = dict(sc)
        d['i'] = i
        d['in'] = []
        for par in range(2):
            n = "s%dp%d" % (i, par)
            d['in'].append(dict(
                qT=P.sb("qT" + n, [dk, SCH * 64], BF16), kT=P.sb("kT" + n, [dk, SCH * 64], BF16),
                ktm=P.sb("ktm" + n, [64, SCH, dk], BF16), vtm=P.sb("vtm" + n, [64, SCH, dv], BF16),
                latm=P.sb("latm" + n, [64, SCH, dk], F32), o=P.sb("o" + n, [64, SCH, dv], F32), key="in" + n, okey="o" + n))
        d['scr'] = []
        for par in range(2):
            n = "s%dq%d" % (i, par)
            d['scr'].append(dict(
                e1=P.sb("e1" + n, [dk, 64], F32), e2=P.sb("e2" + n, [dk, 64], F32), e3=P.sb("e3" + n, [64, dk], F32),
                qe=P.sb("qe" + n, [dk, 64], BF16), ke=P.sb("ke" + n, [dk, 64], BF16), kd=P.sb("kd" + n, [64, dk], BF16),
                attm=P.sb("attm" + n, [64, 64], BF16), dS=P.sb("dS" + n, [64, 64], F32), cc=P.sb("cc" + n, [64, 2], F32), n=n, bank=banks[2 * i + par]))
        d['Sf'] = P.sb("Sf%d" % i, [dk, dv], F32)
        d['Sb'] = P.sb("Sb%d" % i, [dk, dv], BF16)
        P.pool('memset', [], ['Sf%d' % i], ap=d['Sf'][:], constant=0.0)
        P.pool('memset', [], ['Sb%d' % i], ap=d['Sb'][:], constant=0.0)
        st.append(d)

    def sc_order(rev):
        if not rev:
            return [(s, list(range(SCH))) for s in range(NSC)]
        return [(0, list(range(SCH - 1, -1, -1)))] + [(s, list(range(SCH - 1, -1, -1))) for s in range(NSC - 1, 0, -1)]

    orders = [sc_order(d['rev']) for d in st]
    dq = [0]

    def load(d, step):
        s, _ = orders[d['i']][step]
        b = d['in'][step % 2]
        t0, t1 = s * SCH * 64, (s + 1) * SCH * 64
        c0, c1 = s * SCH, (s + 1) * SCH
        for nm, src in (('qT', d['qT'][:, t0:t1]), ('kT', d['kT'][:, t0:t1]), ('ktm', d['ktm'][:, c0:c1, :]), ('vtm', d['vtm'][:, c0:c1, :]), ('latm', d['latm'][:, c0:c1, :])):
            q = ['sp', 'act', 'pool'][dq[0] % 3]
            dq[0] += 1
            P.dma(q, reads=d.get('rkeys', []), writes=[b['key'] + nm], out=b[nm][:], in_=src)

    def store(d, step):
        s, _ = orders[d['i']][step]
        b = d['in'][step % 2]
        q = ['sp', 'act', 'pool'][dq[0] % 3]
        dq[0] += 1
        P.dma(q, reads=[b['okey']], writes=[d['okey']], out=d['o'][:, s * SCH:(s + 1) * SCH, :], in_=b['o'][:])

    for d in st:
        load(d, 0)
    cidx = 0
    NS = NSC if nsteps is None else nsteps
    for step in range(NS):
        for d in st:
            if step + 1 < NS:
                load(d, step + 1)
        for j in range(SCH):
            work = []
            for d in st:
                s, chs = orders[d['i']][step]
                c = chs[j]
                b = d['in'][step % 2]
                w = d['scr'][cidx % 2]
                work.append((d, b, w, c))
            cidx += 1
            for d, b, w, c in work:
                dk, dv, rev = d['dk'], d['dv'], d['rev']
                bk = w['bank']
                kb = 'bank' + w['n']
                lac = b['latm'][:, c, :]
                triI = tri[:, 1 if rev else 0, :]
                triS = tri[:, 3 if rev else 2, :]
                P.mm(bk[0:dk, 0:64], lac, triI, True, True, [b['key'] + 'latm', 'tri'], [kb])
                P.mm(bk[0:64, 128:128 + dk], triS, lac, True, True, [b['key'] + 'latm', 'tri'], [kb])
                if d['mode'] == 'scalar':
                    P.mm(bk[0:64, 320:322], triI, lac[:, 0:2], True, True, [b['key'] + 'latm', 'tri'], [kb])
            for d, b, w, c in (work if STOP_STAGE >= 2 else []):
                dk, dv, rev = d['dk'], d['dv'], d['rev']
                bk = w['bank']
                kb = 'bank' + w['n']
                n = w['n']
                P.act(w['e1'][:], bk[0:dk, 0:64], AF.Exp, [kb], ['e1' + n, kb])
                if d['mode'] == 'vec':
                    P.act(w['e2'][:], bk[0:dk, 0:64], AF.Exp, [kb], ['e2' + n, kb], scale=-1.0)
                P.act(w['e3'][:], bk[0:64, 128:128 + dk], AF.Exp, [kb], ['e3' + n, kb])
            for d, b, w, c in (work if STOP_STAGE >= 3 else []):
                dk, dv, rev = d['dk'], d['dv'], d['rev']
                n = w['n']
                bk = w['bank']
                kb = 'bank' + w['n']
                qc = b['qT'][:, c * 64:(c + 1) * 64]
                kc = b['kT'][:, c * 64:(c + 1) * 64]
                P.dve('tensor_tensor', [b['key'] + 'qT', 'e1' + n], ['qe' + n], out=w['qe'][:], in0=qc, in1=w['e1'][:], op=ALU.mult)
                if d['mode'] == 'vec':
                    P.dve('tensor_tensor', [b['key'] + 'kT', 'e2' + n], ['ke' + n], out=w['ke'][:], in0=kc, in1=w['e2'][:], op=ALU.mult)
                else:
                    P.dve('tensor_copy', [kb], ['cc' + n, kb], out=w['cc'][:], in_=bk[0:64, 320:322])
                    P.dve('scalar_tensor_tensor', [kb, 'cc' + n, 'tri'], ['dS' + n, kb], out=w['dS'][:], in0=bk[0:64, 0:64], scalar=w['cc'][:, 0:1],
                          in1=tri[:, 5 if rev else 4, :], op0=ALU.subtract, op1=ALU.add)
                P.pool('tensor_tensor', [b['key'] + 'ktm', 'e3' + n], ['kd' + n], out=w['kd'][:], in0=b['ktm'][:, c, :], in1=w['e3'][:], op=ALU.mult)
            for d, b, w, c in (work if STOP_STAGE >= 4 else []):
                dk = d['dk']
                n = w['n']
                bk = w['bank']
                kb = 'bank' + w['n']
                if d['mode'] == 'vec':
                    P.mm(bk[0:64, 64:128], w['ke'][:], w['qe'][:], True, True, ['ke' + n, 'qe' + n], [kb])
                else:
                    P.mm(bk[0:64, 64:128], b['kT'][:, c * 64:(c + 1) * 64], b['qT'][:, c * 64:(c + 1) * 64], True, True, [b['key'] + 'kT', b['key'] + 'qT'], [kb])
                    P.act(w['dS'][:], w['dS'][:], AF.Exp, ['dS' + n], ['dS' + n])
            for d, b, w, c in (work if STOP_STAGE >= 5 else []):
                n = w['n']
                bk = w['bank']
                kb = 'bank' + w['n']
                if d['mode'] == 'vec':
                    P.dve('tensor_tensor', [kb, 'tri'], ['attm' + n, kb], out=w['attm'][:], in0=bk[0:64, 64:128], in1=tri[:, 1 if d['rev'] else 0, :], op=ALU.mult)
                else:
                    P.dve('tensor_tensor', [kb, 'dS' + n], ['attm' + n, kb], out=w['attm'][:], in0=bk[0:64, 64:128], in1=w['dS'][:], op=ALU.mult)
            for d, b, w, c in (work if STOP_STAGE >= 6 else []):
                dk, dv = d['dk'], d['dv']
                n = w['n']
                i = d['i']
                bk = w['bank']
                kb = 'bank' + w['n']
                P.mm(bk[0:64, 384:384 + dv], w['qe'][:], d['Sb'][:], True, False, ['qe' + n, 'Sb%d' % i], [kb])
                P.mm(bk[0:64, 384:384 + dv], w['attm'][:], b['vtm'][:, c, :], False, True, ['attm' + n, b['key'] + 'vtm'], [kb])
                P.mm(bk[0:dk, 256:256 + dv], w['kd'][:], b['vtm'][:, c, :], True, True, ['kd' + n, b['key'] + 'vtm'], [kb])
            for d, b, w, c in (work if STOP_STAGE >= 7 else []):
                dk, dv, rev = d['dk'], d['dv'], d['rev']
                n = w['n']
                i = d['i']
                bk = w['bank']
                kb = 'bank' + w['n']
                if VAR7 & 1:
                    P.act(b['o'][:, c, :], bk[0:64, 384:384 + dv], AF.Copy, [kb], [b['okey'], kb])
                el = w['e1'][:, 0:1] if rev else w['e1'][:, 63:64]
                if VAR7 & 2:
                    P.dve('scalar_tensor_tensor', [kb, 'e1' + n, 'Sf%d' % i], ['Sf%d' % i, kb], out=d['Sf'][:], in0=d['Sf'][:], scalar=el, in1=bk[0:dk, 256:256 + dv], op0=ALU.mult, op1=ALU.add)
                if VAR7 & 4:
                    P.pool('tensor_copy', ['Sf%d' % i], ['Sb%d' % i], out=d['Sb'][:], in_=d['Sf'][:])
        for d in st:
            store(d, step)


def tri_consts():
    s = np.arange(64)[:, None]
    t = np.arange(64)[None, :]
    NEG = -30000.0
    m = np.stack([(s <= t), (s >= t), (s > t), (s < t)]).astype(np.float32)
    n = np.stack([np.where(s <= t, 0.0, NEG), np.where(s >= t, 0.0, NEG)]).astype(np.float32)
    return np.ascontiguousarray(np.concatenate([m, n], axis=0).transpose(1, 0, 2))


def build_B0(nsteps=None):
    nc = bass.Bass("TRN2", target_bir_lowering=False)
    T = NCH * 64
    tri_d = nc.dram_tensor("tri", [64, 6, 64], F32, kind="ExternalInput").ap()
    scans = []
    for nm, dk in (('g', 64), ('h', 128)):
        qT = nc.dram_tensor(nm + "_qT", [dk, T], BF16, kind="ExternalInput").ap()
        vtm = nc.dram_tensor(nm + "_vtm", [64, NCH, 128], BF16, kind="ExternalInput").ap()
        nk = 1 if nm == 'g' else 2
        kT = [nc.dram_tensor(nm + "_kT%d" % d, [dk, T], BF16, kind="ExternalInput").ap() for d in range(nk)]
        ktm = [nc.dram_tensor(nm + "_ktm%d" % d, [64, NCH, dk], BF16, kind="ExternalInput").ap() for d in range(nk)]
        for d in range(2):
            latm = nc.dram_tensor(nm + "_latm%d" % d, [64, NCH, dk], F32, kind="ExternalInput").ap()
            o = nc.dram_tensor(nm + "_o%d" % d, [64, NCH, 128], F32, kind="ExternalOutput").ap()
            scans.append(dict(dk=dk, dv=128, mode='vec', rev=(d == 1), qT=qT, kT=kT[d % nk], ktm=ktm[d % nk], vtm=vtm, latm=latm, o=o, okey='out_%s%d' % (nm, d)))
    P = Prog(nc)
    tri = scan_consts(P, tri_d)
    banks = [P.ps("bank%d" % i, [128, 512], F32) for i in range(8)]
    emit_scans(P, scans[:NSCAN], tri, banks, nsteps)
    P.wait_all('sp')
    P.emit()
    P.close()
    return nc


def joint_fm(outs, row0, nrows, b):
    parts_ctx = [outs[4 * b + q][row0:row0 + nrows, 2048:2112] for q in range(4)]
    parts_lat = [outs[4 * b + q][row0:row0 + nrows, 0:2048] for q in range(4)]
    return np.concatenate(parts_ctx + parts_lat, axis=1)


def to_tm(aT):
    w, T = aT.shape
    return np.ascontiguousarray(aT.T.reshape(T // 64, 64, w).transpose(1, 0, 2))


def host_B0_inputs(a0):
    obf = [r['o_bf'] for r in a0]
    of = [r['o_f'] for r in a0]
    tri = tri_consts()
    maps = []
    for i in range(NCORES):
        b, hd = i // 4, i % 4
        m = dict(tri=tri)
        gq = joint_fm(obf, A0_BF['gq'] + 64 * hd, 64, b)
        gk = joint_fm(obf, A0_BF['gk'] + 64 * hd, 64, b)
        gv = joint_fm(obf, A0_BF['gv'] + 128 * hd, 128, b)
        m['g_qT'] = gq
        m['g_kT0'] = gk
        m['g_ktm0'] = to_tm(gk)
        m['g_vtm'] = to_tm(gv)
        for d, nm in enumerate(('gla_f', 'gla_b')):
            m['g_latm%d' % d] = to_tm(joint_fm(of, A0_F[nm] + 64 * hd, 64, b))
        m['h_qT'] = joint_fm(obf, A0_BF['hq'] + 128 * hd, 128, b)
        m['h_vtm'] = to_tm(joint_fm(obf, A0_BF['hi'] + 128 * hd, 128, b))
        for d, (nk, nl) in enumerate((('hk_f', 'hla_f'), ('hk_b', 'hla_b'))):
            hk = joint_fm(obf, A0_BF[nk] + 128 * hd, 128, b)
            m['h_kT%d' % d] = hk
            m['h_ktm%d' % d] = to_tm(hk)
            m['h_latm%d' % d] = to_tm(joint_fm(of, A0_F[nl] + 128 * hd, 128, b))
        maps.append(m)
    return maps


CD = dict(hy=0, z=1536, xbc=2048, dt=3072, end=3088)
A1_BF = dict(hy=0, xbc=1536, end=2560)
A1_F = dict(zs=0, dt=512, la=528, end=544)


def barrier(P):
    for e in ENGS:
        P.wait_all(e)


def build_C(layer, nexp=16):
    nc = bass.Bass("TRN2", target_bir_lowering=False)
    T = TL if layer == 0 else 2048
    tiles = TILES if layer == 0 else TILES[:4]
    NT = len(tiles)
    din = lambda name, shape, dt=F32: nc.dram_tensor(name, list(shape), dt, kind="ExternalInput").ap()
    dout = lambda name, shape, dt=F32: nc.dram_tensor(name, list(shape), dt, kind="ExternalOutput").ap()
    hT_in = din("hT_in", [128, 8, T])
    mod_in = din("mod_in", [128, 48, 2])
    gffn = din("gffn", [128, 8])
    w_out = din("w_out", [1024, 1024])
    rw_d = din("router_w", [128, 8, 16])
    rb_d = din("router_b", [128, 16])
    sel_d = din("sel16", [16, 16, 128])
    identd = din("ident", [128, 128])
    wg_d = din("moe_wg", [16, 1024, 1024])
    wu_d = din("moe_wu", [16, 1024, 1024])
    wd_d = din("moe_wd", [16, 1024, 1024])
    if layer == 0:
        oT_d = din("oT", [8, 128, 2, T])
        gate_d = din("gateT", [8, 128, T])
        ng_d = din("ng", [128, 2])
        mod1_in = din("mod1_in", [128, 48, 2])
        gmix1 = din("gmix1", [128, 8])
        w_in1 = din("w_in1", [1024, 3088])
        dtp_d = din("dtp", [16, 2])
        hT_out = dout("hT_out", [128, 8, T])
        o_bf = dout("o_bf", [A1_BF['end'], T], BF16)
        o_f = dout("o_f", [A1_F['end'], T])
    else:
        hy_d = din("hyT", [128, 4, T])
        so_d = din("ssd_oT", [2, 128, 4, T])
        xs_d = din("xsT", [128, 4, T])
        zs_d = din("zsT", [128, 4, T])
        dsk_d = din("dsk", [128, 4])
        mg_d = din("mbg", [128, 4])
        gout_d = din("gout", [128, 8])
        y_out = dout("y_out", [128, 8, T])

    P = Prog(nc)
    hT = P.sb("hT", [128, 8, T], F32)
    fT = P.sb("fT", [128, 8, T], BF16)
    ones_bf = P.sb("ones_bf", [128, 128], BF16)
    ident = P.sb("ident_sb", [128, 128], F32)
    work = dict(sq=P.sb("sq", [128, 8, 512], BF16), rs=P.sb("rs", [128, 512], F32),
                tmp=[P.sb("tmp%d" % i, [128, 512], F32) for i in range(2)])
    banks = [P.ps("bank%d" % i, [128, 512], F32) for i in range(8)]
    pm, ps_ssq = banks[0], banks[1]
    for k in range(8):
        P.dma('sp' if k % 2 == 0 else 'act', writes=['h:%d' % t for t in range(NT)], out=hT[:, k, :], in_=hT_in[:, k, :])
    P.pool('memset', [], ['ones'], ap=ones_bf[:], constant=1.0)
    P.dma('sp', writes=['ident'], out=ident[:], in_=identd)
    mod = P.sb("sbmodL", [128, 48, 2], F32)
    P.dma('sp', writes=['modL'], out=mod[:], in_=mod_in)

    mark = len(P.stack)
    if layer == 0:
        ng = P.sb("ng_sb", [128, 2], F32)
        P.dma('sp', writes=['ng'], out=ng[:], in_=ng_d)
        ob = [P.sb("ob%d" % i, [128, 2, 512], F32) for i in range(2)]
        gbf = [P.sb("gbf%d" % i, [128, 512], F32) for i in range(2)]
        osum = [P.sb("osum%d" % i, [128, 512], F32) for i in range(2)]
        it = 0
        for hh in range(8):
            for ti, (c0, c1, r) in enumerate(tiles):
                n = c1 - c0
                i2 = it % 2
                it += 1
                P.dma('sp', writes=['ob%d' % i2], out=ob[i2][:, :, 0:n], in_=oT_d[hh, :, :, c0:c1])
                P.dma('act', writes=['gbf%d' % i2], out=gbf[i2][:, 0:n], in_=gate_d[hh, :, c0:c1])
                P.pool('tensor_tensor', ['ob%d' % i2], ['osum%d' % i2], out=osum[i2][:, 0:n], in0=ob[i2][:, 0, 0:n], in1=ob[i2][:, 1, 0:n], op=ALU.add)
                emit_rstd(P, osum[i2][:, 0:n].unsqueeze(1), 'osum%d' % i2, n, 1, ones_bf, ps_ssq, work['sq'], work['rs'], 1.0 / 128)
                tb = work['tmp'][i2]
                P.dve('scalar_tensor_tensor', ['osum%d' % i2, 'ng', 'rs'], ['tmp%d' % i2], out=tb[:, 0:n], in0=osum[i2][:, 0:n], scalar=ng[:, hh // 4:hh // 4 + 1], in1=work['rs'][:, 0:n], op0=ALU.mult, op1=ALU.mult)
                P.pool('tensor_tensor', ['tmp%d' % i2, 'gbf%d' % i2], ['f:%d' % ti], out=fT[:, hh, c0:c1], in0=tb[:, 0:n], in1=gbf[i2][:, 0:n], op=ALU.mult)
    else:
        dsk = P.sb("dsk_sb", [128, 4], F32)
        mg = P.sb("mg_sb", [128, 4], F32)
        P.dma('sp', writes=['dsk'], out=dsk[:], in_=dsk_d)
        P.dma('sp', writes=['mg'], out=mg[:], in_=mg_d)
        lb4 = [P.sb("lb4_%d" % i, [128, 4, 512], F32) for i in range(3)]
        yb = P.sb("yb", [128, 4, 512], F32)
        for ti, (c0, c1, r) in enumerate(tiles):
            n = c1 - c0
            P.dma('sp', writes=['lb4_0'], out=lb4[0][:, :, 0:n], in_=hy_d[:, :, c0:c1])
            P.pool('tensor_copy', ['lb4_0'], ['f:%d' % ti], out=fT[:, 0:4, c0:c1], in_=lb4[0][:, :, 0:n])
            P.dma('sp', writes=['lb4_1'], out=lb4[1][:, :, 0:n], in_=so_d[0, :, :, c0:c1])
            P.dma('act', writes=['lb4_2'], out=lb4[2][:, :, 0:n], in_=so_d[1, :, :, c0:c1])
            P.pool('tensor_tensor', ['lb4_1', 'lb4_2'], ['yb'], out=yb[:, :, 0:n], in0=lb4[1][:, :, 0:n], in1=lb4[2][:, :, 0:n], op=ALU.add)
            P.dma('sp', writes=['lb4_1'], out=lb4[1][:, :, 0:n], in_=xs_d[:, :, c0:c1])
            P.dma('act', writes=['lb4_2'], out=lb4[2][:, :, 0:n], in_=zs_d[:, :, c0:c1])
            for j in range(4):
                P.dve('scalar_tensor_tensor', ['lb4_1', 'dsk', 'yb'], ['yb'], out=yb[:, j, 0:n], in0=lb4[1][:, j, 0:n], scalar=dsk[:, j:j + 1], in1=yb[:, j, 0:n], op0=ALU.mult, op1=ALU.add)
            P.pool('tensor_tensor', ['yb', 'lb4_2'], ['yb'], out=yb[:, :, 0:n], in0=yb[:, :, 0:n], in1=lb4[2][:, :, 0:n], op=ALU.mult)
            for g in range(2):
                emit_rstd(P, yb[:, 2 * g:2 * g + 2, 0:n], 'yb', n, 2, ones_bf, ps_ssq, work['sq'], work['rs'], 1.0 / 256)
                for j in (2 * g, 2 * g + 1):
                    P.dve('scalar_tensor_tensor', ['yb', 'mg', 'rs'], ['f:%d' % ti], out=fT[:, 4 + j, c0:c1], in0=yb[:, j, 0:n], scalar=mg[:, j:j + 1], in1=work['rs'][:, 0:n], op0=ALU.mult, op1=ALU.mult)
    barrier(P)
    while len(P.stack) > mark:
        P.stack.pop().__exit__(None, None, None)

    wo = P.sb("wo", [128, 8, 1024], BF16)
    wov = w_out.rearrange("(k p) n -> p k n", p=128)
    for k in range(8):
        P.dma('pool', writes=['wo'], out=wo[:, k, :], in_=wov[:, k, :])
    gcnt = [0]
    gbk = banks[2:6]

    def bank():
        i = gcnt[0] % 4
        gcnt[0] += 1
        return gbk[i], 'gb%d' % i
    for dc in range(8):
        for ti, (c0, c1, r) in enumerate(tiles):
            n = c1 - c0
            b, bk = bank()
            for k in range(8):
                P.mm(b[:, 0:n], wo[:, k, dc * 128:(dc + 1) * 128], fT[:, k, c0:c1], k == 0, k == 7, ['wo', 'f:%d' % ti], [bk])
            P.dve('scalar_tensor_tensor', [bk, 'modL', 'h:%d' % ti], ['h:%d' % ti, bk], out=hT[:, dc, c0:c1], in0=b[:, 0:n], scalar=mod[:, 16 + dc, r:r + 1], in1=hT[:, dc, c0:c1], op0=ALU.mult, op1=ALU.add)
    barrier(P)
    P.stack.pop().__exit__(None, None, None)

    Af = emit_affine(P, mod, 'modL', gffn, 4, 'f')
    sel = P.sb("sel_sb", [16, 16, 128], F32)
    WT = P.sb("WT", [16, T], F32)
    mark = len(P.stack)
    u32 = P.sb("u32", [128, 8, 512], F32)
    rw = P.sb("rw", [128, 8, 16], F32)
    rb = P.sb("rb", [128, 16], F32)
    P.dma('sp', writes=['rw'], out=rw[:], in_=rw_d)
    P.dma('sp', writes=['rb'], out=rb[:], in_=rb_d)
    P.dma('sp', writes=['sel'], out=sel[:], in_=sel_d)
    rt = {nm: P.sb("rt_" + nm, [128, 16], F32) for nm in ('sc', 'sl', 'eq', 's2', 'ch', 'w')}
    r4 = {nm: P.sb("r4_" + nm, [128, 4], F32) for nm in ('m1', 'm2', 'gs', 'gm')}
    r1 = {nm: P.sb("r1_" + nm, [128, 1], F32) for nm in ('gx', 'ss')}
    v4 = lambda t_: t_[:].rearrange("p (g j) -> p g j", j=4)
    b4 = lambda t_: t_[:].unsqueeze(2).to_broadcast([128, 4, 4])
    rbank, rbk = banks[6], 'rbank'
    for ti, tile in enumerate(tiles):
        c0, c1, r = tile
        n = c1 - c0
        emit_modulate_tile(P, hT, 'h', Af, 'Af', mod, 'modL', 3, ti, tile, ones_bf, ps_ssq, work, uT=fT, ukey='v', u32=u32)
        for sub in range(n // 128 if n >= 128 else 1):
            m = min(128, n)
            s0 = sub * 128
            for k in range(8):
                P.mm(rbank[0:m, 0:16], u32[:, k, s0:s0 + m], rw[:, k, :], k == 0, k == 7, ['u32', 'rw'], [rbk])
            P.act(rt['sc'][0:m, :], rbank[0:m, 0:16], AF.Sigmoid, [rbk], ['rt_sc', rbk])
            P.dve('tensor_tensor', ['rt_sc', 'rb'], ['rt_sl'], out=rt['sl'][0:m], in0=rt['sc'][0:m], in1=rb[0:m], op=ALU.add)
            P.dve('tensor_reduce', ['rt_sl'], ['r4_m1'], out=r4['m1'][0:m], in_=v4(rt['sl'])[0:m], axis=AX.X, op=ALU.max)
            P.dve('tensor_tensor', ['rt_sl', 'r4_m1'], ['rt_eq'], out=v4(rt['eq'])[0:m], in0=v4(rt['sl'])[0:m], in1=b4(r4['m1'])[0:m], op=ALU.is_equal)
            P.dve('scalar_tensor_tensor', ['rt_eq', 'rt_sl'], ['rt_s2'], out=rt['s2'][0:m], in0=rt['eq'][0:m], scalar=-1e9, in1=rt['sl'][0:m], op0=ALU.mult, op1=ALU.add)
            P.dve('tensor_reduce', ['rt_s2'], ['r4_m2'], out=r4['m2'][0:m], in_=v4(rt['s2'])[0:m], axis=AX.X, op=ALU.max)
            P.dve('tensor_tensor', ['r4_m1', 'r4_m2'], ['r4_gs'], out=r4['gs'][0:m], in0=r4['m1'][0:m], in1=r4['m2'][0:m], op=ALU.add)
            P.dve('tensor_reduce', ['r4_gs'], ['r1_gx'], out=r1['gx'][0:m], in_=r4['gs'][0:m], axis=AX.X, op=ALU.max)
            P.dve('tensor_scalar', ['r4_gs', 'r1_gx'], ['r4_gm'], out=r4['gm'][0:m], in0=r4['gs'][0:m], scalar1=r1['gx'][0:m, 0:1], scalar2=None, op0=ALU.is_equal)
            P.dve('tensor_tensor', ['rt_sl', 'r4_m2'], ['rt_ch'], out=v4(rt['ch'])[0:m], in0=v4(rt['sl'])[0:m], in1=b4(r4['m2'])[0:m], op=ALU.is_ge)
            P.dve('tensor_tensor', ['rt_ch', 'r4_gm'], ['rt_ch'], out=v4(rt['ch'])[0:m], in0=v4(rt['ch'])[0:m], in1=b4(r4['gm'])[0:m], op=ALU.mult)
            P.dve('tensor_tensor', ['rt_ch', 'rt_sc'], ['rt_w'], out=rt['w'][0:m], in0=rt['ch'][0:m], in1=rt['sc'][0:m], op=ALU.mult)
            P.dve('tensor_reduce', ['rt_w'], ['r1_ss'], out=r1['ss'][0:m], in_=rt['w'][0:m], axis=AX.X, op=ALU.add)
            P.dve('reciprocal', ['r1_ss'], ['r1_ss'], out=r1['ss'][0:m], in_=r1['ss'][0:m])
            P.dve('tensor_scalar', ['rt_w', 'r1_ss'], ['rt_w'], out=rt['w'][0:m], in0=rt['w'][0:m], scalar1=r1['ss'][0:m, 0:1], scalar2=None, op0=ALU.mult)
            P.op('pe', 'transpose', ['rt_w', 'ident'], [rbk], out=rbank[0:16, 128:128 + m], in_=rt['w'][0:m, :], identity=ident[0:m, 0:m])
            P.act(WT[:, c0 + s0:c0 + s0 + m], rbank[0:16, 128:128 + m], AF.Copy, [rbk], ['WT', rbk])
    barrier(P)
    while len(P.stack) > mark:
        P.stack.pop().__exit__(None, None, None)

    barrier(P)
    while len(P.stack) > mark:
        P.stack.pop().__exit__(None, None, None)
    hid = P.sb("hid", [128, 4, T], BF16)
    wbc = P.sb("wbc", [128, T], F32)
    WG = [P.sb("WG%d" % k, [128, 512], BF16) for k in range(8)]
    WU = [P.sb("WU%d" % k, [128, 512], BF16) for k in range(8)]
    WD = [P.sb("WD%d" % k, [128, 1024], BF16) for k in range(4)]
    sg = [P.sb("sg%d" % i, [128, 512], F32) for i in range(2)]
    bA = [banks[2], banks[3]]
    bB = [banks[4], banks[5]]
    bY = [banks[6], banks[7]]
    it = 0
    for e in range(nexp):
        for ti, (c0, c1, r) in enumerate(tiles):
            n = c1 - c0
            P.mm(ps_ssq[:, 0:n], sel[:, e, :], WT[:, c0:c1], True, True, ['sel', 'WT'], ['ps_ssq'])
            P.act(wbc[:, c0:c1], ps_ssq[:, 0:n], AF.Copy, ['ps_ssq'], ['wbc:%d' % ti, 'ps_ssq'])
        for hf in range(2):
            for k in range(8):
                P.dma('pool', writes=['WG%d' % k], out=WG[k][:], in_=wg_d[e, k * 128:(k + 1) * 128, hf * 512:(hf + 1) * 512])
                P.dma('pool', writes=['WU%d' % k], out=WU[k][:], in_=wu_d[e, k * 128:(k + 1) * 128, hf * 512:(hf + 1) * 512])
            for k in range(4):
                P.dma('pool', writes=['WD%d' % k], out=WD[k][:], in_=wd_d[e, hf * 512 + k * 128:hf * 512 + (k + 1) * 128, :])
            for fc in range(4):
                for ti, (c0, c1, r) in enumerate(tiles):
                    n = c1 - c0
                    i2 = it % 2
                    it += 1
                    for k in range(8):
                        P.mm(bA[i2][:, 0:n], WG[k][:, fc * 128:(fc + 1) * 128], fT[:, k, c0:c1], k == 0, k == 7, ['WG%d' % k, 'v:%d' % ti], ['bA%d' % i2])
                    for k in range(8):
                        P.mm(bB[i2][:, 0:n], WU[k][:, fc * 128:(fc + 1) * 128], fT[:, k, c0:c1], k == 0, k == 7, ['WU%d' % k, 'v:%d' % ti], ['bB%d' % i2])
                    P.act(sg[i2][:, 0:n], bA[i2][:, 0:n], AF.Silu, ['bA%d' % i2], ['sg%d' % i2, 'bA%d' % i2])
                    P.dve('tensor_tensor', ['bB%d' % i2, 'sg%d' % i2], ['sg%d' % i2, 'bB%d' % i2], out=sg[i2][:, 0:n], in0=bB[i2][:, 0:n], in1=sg[i2][:, 0:n], op=ALU.mult)
                    P.pool('tensor_tensor', ['sg%d' % i2, 'wbc:%d' % ti], ['hid:%d' % ti], out=hid[:, fc, c0:c1], in0=sg[i2][:, 0:n], in1=wbc[:, c0:c1], op=ALU.mult)
            for dc in range(8):
                for ti, (c0, c1, r) in enumerate(tiles):
                    n = c1 - c0
                    i2 = it % 2
                    it += 1
                    for k in range(4):
                        P.mm(bY[i2][:, 0:n], WD[k][:, dc * 128:(dc + 1) * 128], hid[:, k, c0:c1], k == 0, k == 3, ['WD%d' % k, 'hid:%d' % ti], ['bY%d' % i2])
                    P.dve('scalar_tensor_tensor', ['bY%d' % i2, 'modL', 'h:%d' % ti], ['h:%d' % ti, 'bY%d' % i2], out=hT[:, dc, c0:c1], in0=bY[i2][:, 0:n], scalar=mod[:, 40 + dc, r:r + 1], in1=hT[:, dc, c0:c1], op0=ALU.mult, op1=ALU.add)
    barrier(P)
    while len(P.stack) > mark:
        P.stack.pop().__exit__(None, None, None)

    if layer == 0:
        for k in range(8):
            P.dma('sp' if k % 2 == 0 else 'act', reads=['h:%d' % t for t in range(NT)], out=hT_out[:, k, :], in_=hT[:, k, :])
        mod1 = P.sb("sbmod1", [128, 48, 2], F32)
        P.dma('sp', writes=['mod1'], out=mod1[:], in_=mod1_in)
        A1 = emit_affine(P, mod1, 'mod1', gmix1, 1, 'm1')
        for ti, tile in enumerate(tiles):
            emit_modulate_tile(P, hT, 'h', A1, 'Am1', mod1, 'mod1', 0, ti, tile, ones_bf, ps_ssq, work, uT=fT, ukey='u1')
        C = ProjCtx(P, w_in1, fT, 'u1', banks[2:6], o_f, o_bf)
        C.job_lin(CD['hy'], 1536, A1_BF['hy'], 1.0)
        C.job_lin(CD['xbc'], 1024, A1_BF['xbc'], 1.0)
        C.job_silu(CD['z'], 512, A1_F['zs'], 'f')
        dtp = P.sb("dtp_sb", [16, 2], F32)
        negA = P.sb("negA", [16, 1], F32)
        P.dma('sp', writes=['dtp'], out=dtp[:], in_=dtp_d)
        P.act(negA[:], dtp[:, 1:2], AF.Exp, ['dtp'], ['negA'])
        P.dve('tensor_scalar', ['negA'], ['negA'], out=negA[:], in0=negA[:], scalar1=-1.0, scalar2=None, op0=ALU.mult)
        st1, sk1 = C.stage('f')
        st2, sk2 = C.stage('f')
        C.load_w(CD['dt'], 16)

        def post(ps, n, c0, c1, ti, bkey):
            P.act(st1[0:16, c0:c1], ps, AF.Exp, [bkey, 'dtp'], [sk1, bkey], bias=dtp[:, 0:1], scale=1.0)
            P.act(st1[0:16, c0:c1], st1[0:16, c0:c1], AF.Ln, [sk1], [sk1], bias=1.0, scale=1.0)
            P.dve('tensor_scalar', [sk1, 'negA'], [sk2], out=st2[0:16, c0:c1], in0=st1[0:16, c0:c1], scalar1=negA[:, 0:1], scalar2=None, op0=ALU.mult)
        C.chunk(CD['dt'], 16, post)
        C.outdma('f', st1, sk1, A1_F['dt'], 16)
        C.outdma('f', st2, sk2, A1_F['la'], 16)
    else:
        gout = P.sb("gout_sb", [128, 8], F32)
        P.dma('sp', writes=['gout'], out=gout[:], in_=gout_d)
        yst = [P.sb("yst%d" % i, [128, 8, 512], F32) for i in range(2)]
        for ti, (c0, c1, r) in enumerate(tiles):
            n = c1 - c0
            emit_rstd(P, hT[:, :, c0:c1], 'h:%d' % ti, n, 8, ones_bf, ps_ssq, work['sq'], work['rs'], 1.0 / 1024)
            for k in range(8):
                P.dve('scalar_tensor_tensor', ['h:%d' % ti, 'gout', 'rs'], ['yst%d' % (ti % 2)], out=yst[ti % 2][:, k, 0:n], in0=hT[:, k, c0:c1], scalar=gout[:, k:k + 1], in1=work['rs'][:, 0:n], op0=ALU.mult, op1=ALU.mult)
            P.dma('sp' if ti % 2 == 0 else 'act', reads=['yst%d' % (ti % 2)], out=y_out[:, :, c0:c1], in_=yst[ti % 2][:, :, 0:n])
    P.wait_all('sp')
    P.emit()
    P.close()
    return nc


def build_C2(layer, nexp=16, CAP=640):
    nc = bass.Bass("TRN2", target_bir_lowering=False)
    T = TL if layer == 0 else 2048
    tiles = TILES if layer == 0 else TILES[:4]
    NT = len(tiles)
    din = lambda name, shape, dt=F32: nc.dram_tensor(name, list(shape), dt, kind="ExternalInput").ap()
    dout = lambda name, shape, dt=F32: nc.dram_tensor(name, list(shape), dt, kind="ExternalOutput").ap()
    hT_in = din("hT_in", [128, 8, T])
    mod_in = din("mod_in", [128, 48, 2])
    gffn = din("gffn", [128, 8])
    w_out = din("w_out", [1024, 1024])
    NSUB = (T + 127) // 128
    NSL = CAP // 128
    I32 = mybir.dt.int32
    tris_d = din("tris", [128, 128], BF16)
    tokhl_d = din("tokhl", [128, NSUB, 2], BF16)
    iota_d = din("iota_s", [128, CAP])
    eoff_d = din("eoff", [128, 16])
    vtm = nc.dram_tensor("vtm_scr", [NSUB * 128, 1024], BF16, kind="Internal").ap()
    y_all = nc.dram_tensor("yall_scr", [16 * CAP, 1024], F32, kind="Internal").ap()
    rw_d = din("router_w", [128, 8, 16])
    rb_d = din("router_b", [128, 16])
    identd = din("ident", [128, 128])
    wg_d = din("moe_wg", [16, 1024, 1024])
    wu_d = din("moe_wu", [16, 1024, 1024])
    wd_d = din("moe_wd", [16, 1024, 1024])
    if layer == 0:
        oT_d = din("oT", [8, 128, 2, T])
        gate_d = din("gateT", [8, 128, T])
        ng_d = din("ng", [128, 2])
        mod1_in = din("mod1_in", [128, 48, 2])
        gmix1 = din("gmix1", [128, 8])
        w_in1 = din("w_in1", [1024, 3088])
        dtp_d = din("dtp", [16, 2])
        hT_out = dout("hT_out", [128, 8, T])
        o_bf = dout("o_bf", [A1_BF['end'], T], BF16)
        o_f = dout("o_f", [A1_F['end'], T])
    else:
        hy_d = din("hyT", [128, 4, T])
        so_d = din("ssd_oT", [2, 128, 4, T])
        xs_d = din("xsT", [128, 4, T])
        zs_d = din("zsT", [128, 4, T])
        dsk_d = din("dsk", [128, 4])
        mg_d = din("mbg", [128, 4])
        gout_d = din("gout", [128, 8])
        y_out = dout("y_out", [128, 8, T])

    P = Prog(nc)
    hT = P.sb("hT", [128, 8, T], F32)
    ones_bf = P.sb("ones_bf", [128, 128], BF16)
    ident = P.sb("ident_sb", [128, 128], F32)
    work = dict(sq=P.sb("sq", [128, 8, 512], BF16), rs=P.sb("rs", [128, 512], F32),
                tmp=[P.sb("tmp%d" % i, [128, 512], F32) for i in range(2)])
    banks = [P.ps("bank%d" % i, [128, 512], F32) for i in range(8)]
    pm, ps_ssq = banks[0], banks[1]
    for k in range(8):
        P.dma('sp' if k % 2 == 0 else 'act', writes=['h:%d' % t for t in range(NT)], out=hT[:, k, :], in_=hT_in[:, k, :])
    P.pool('memset', [], ['ones'], ap=ones_bf[:], constant=1.0)
    P.dma('sp', writes=['ident'], out=ident[:], in_=identd)
    mod = P.sb("sbmodL", [128, 48, 2], F32)
    P.dma('sp', writes=['modL'], out=mod[:], in_=mod_in)

    mark_f = len(P.stack)
    fT = P.sb("fT", [128, 8, T], BF16)
    mark = len(P.stack)
    if layer == 0:
        ng = P.sb("ng_sb", [128, 2], F32)
        P.dma('sp', writes=['ng'], out=ng[:], in_=ng_d)
        ob = [P.sb("ob%d" % i, [128, 2, 512], F32) for i in range(2)]
        gbf = [P.sb("gbf%d" % i, [128, 512], F32) for i in range(2)]
        osum = [P.sb("osum%d" % i, [128, 512], F32) for i in range(2)]
        it = 0
        for hh in range(8):
            for ti, (c0, c1, r) in enumerate(tiles):
                n = c1 - c0
                i2 = it % 2
                it += 1
                P.dma('sp', writes=['ob%d' % i2], out=ob[i2][:, :, 0:n], in_=oT_d[hh, :, :, c0:c1])
                P.dma('act', writes=['gbf%d' % i2], out=gbf[i2][:, 0:n], in_=gate_d[hh, :, c0:c1])
                P.pool('tensor_tensor', ['ob%d' % i2], ['osum%d' % i2], out=osum[i2][:, 0:n], in0=ob[i2][:, 0, 0:n], in1=ob[i2][:, 1, 0:n], op=ALU.add)
                emit_rstd(P, osum[i2][:, 0:n].unsqueeze(1), 'osum%d' % i2, n, 1, ones_bf, ps_ssq, work['sq'], work['rs'], 1.0 / 128)
                tb = work['tmp'][i2]
                P.dve('scalar_tensor_tensor', ['osum%d' % i2, 'ng', 'rs'], ['tmp%d' % i2], out=tb[:, 0:n], in0=osum[i2][:, 0:n], scalar=ng[:, hh // 4:hh // 4 + 1], in1=work['rs'][:, 0:n], op0=ALU.mult, op1=ALU.mult)
                P.pool('tensor_tensor', ['tmp%d' % i2, 'gbf%d' % i2], ['f:%d' % ti], out=fT[:, hh, c0:c1], in0=tb[:, 0:n], in1=gbf[i2][:, 0:n], op=ALU.mult)
    else:
        dsk = P.sb("dsk_sb", [128, 4], F32)
        mg = P.sb("mg_sb", [128, 4], F32)
        P.dma('sp', writes=['dsk'], out=dsk[:], in_=dsk_d)
        P.dma('sp', writes=['mg'], out=mg[:], in_=mg_d)
        lb4 = [P.sb("lb4_%d" % i, [128, 4, 512], F32) for i in range(3)]
        yb = P.sb("yb", [128, 4, 512], F32)
        for ti, (c0, c1, r) in enumerate(tiles):
            n = c1 - c0
            P.dma('sp', writes=['lb4_0'], out=lb4[0][:, :, 0:n], in_=hy_d[:, :, c0:c1])
            P.pool('tensor_copy', ['lb4_0'], ['f:%d' % ti], out=fT[:, 0:4, c0:c1], in_=lb4[0][:, :, 0:n])
            P.dma('sp', writes=['lb4_1'], out=lb4[1][:, :, 0:n], in_=so_d[0, :, :, c0:c1])
            P.dma('act', writes=['lb4_2'], out=lb4[2][:, :, 0:n], in_=so_d[1, :, :, c0:c1])
            P.pool('tensor_tensor', ['lb4_1', 'lb4_2'], ['yb'], out=yb[:, :, 0:n], in0=lb4[1][:, :, 0:n], in1=lb4[2][:, :, 0:n], op=ALU.add)
            P.dma('sp', writes=['lb4_1'], out=lb4[1][:, :, 0:n], in_=xs_d[:, :, c0:c1])
            P.dma('act', writes=['lb4_2'], out=lb4[2][:, :, 0:n], in_=zs_d[:, :, c0:c1])
            for j in range(4):
                P.dve('scalar_tensor_tensor', ['lb4_1', 'dsk', 'yb'], ['yb'], out=yb[:, j, 0:n], in0=lb4[1][:, j, 0:n], scalar=dsk[:, j:j + 1], in1=yb[:, j, 0:n], op0=ALU.mult, op1=ALU.add)
            P.pool('tensor_tensor', ['yb', 'lb4_2'], ['yb'], out=yb[:, :, 0:n], in0=yb[:, :, 0:n], in1=lb4[2][:, :, 0:n], op=ALU.mult)
            for g in range(2):
                emit_rstd(P, yb[:, 2 * g:2 * g + 2, 0:n], 'yb', n, 2, ones_bf, ps_ssq, work['sq'], work['rs'], 1.0 / 256)
                for j in (2 * g, 2 * g + 1):
                    P.dve('scalar_tensor_tensor', ['yb', 'mg', 'rs'], ['f:%d' % ti], out=fT[:, 4 + j, c0:c1], in0=yb[:, j, 0:n], scalar=mg[:, j:j + 1], in1=work['rs'][:, 0:n], op0=ALU.mult, op1=ALU.mult)
    barrier(P)
    while len(P.stack) > mark:
        P.stack.pop().__exit__(None, None, None)

    wo = P.sb("wo", [128, 8, 1024], BF16)
    wov = w_out.rearrange("(k p) n -> p k n", p=128)
    for k in range(8):
        P.dma('pool', writes=['wo'], out=wo[:, k, :], in_=wov[:, k, :])
    gcnt = [0]
    gbk = banks[2:6]

    def bank():
        i = gcnt[0] % 4
        gcnt[0] += 1
        return gbk[i], 'gb%d' % i
    for dc in range(8):
        for ti, (c0, c1, r) in enumerate(tiles):
            n = c1 - c0
            b, bk = bank()
            for k in range(8):
                P.mm(b[:, 0:n], wo[:, k, dc * 128:(dc + 1) * 128], fT[:, k, c0:c1], k == 0, k == 7, ['wo', 'f:%d' % ti], [bk])
            P.dve('scalar_tensor_tensor', [bk, 'modL', 'h:%d' % ti], ['h:%d' % ti, bk], out=hT[:, dc, c0:c1], in0=b[:, 0:n], scalar=mod[:, 16 + dc, r:r + 1], in1=hT[:, dc, c0:c1], op0=ALU.mult, op1=ALU.add)
    barrier(P)
    while len(P.stack) > mark_f:
        P.stack.pop().__exit__(None, None, None)

    Af = emit_affine(P, mod, 'modL', gffn, 4, 'f')
    CH1 = P.sb("CH1", [128, NSUB, 16], F32)
    CH2 = P.sb("CH2", [128, NSUB, 16], F32)
    WW = P.sb("WW", [128, NSUB, 16], F32)
    CHb = P.sb("CHb", [128, NSUB, 16], BF16)
    RP = P.sb("RP", [128, NSUB, 16], F32)
    tris = P.sb("tris_sb", [128, 128], BF16)
    tokhl = P.sb("tokhl_sb", [128, NSUB, 2], BF16)
    iota_s = P.sb("iota_sb", [128, CAP], F32)
    eoff = P.sb("eoff_sb", [128, 16], F32)
    for t_, d_, k_ in ((tris, tris_d, 'tris'), (tokhl, tokhl_d, 'tokhl'), (iota_s, iota_d, 'iota'), (eoff, eoff_d, 'eoff')):
        P.dma('sp', writes=[k_], out=t_[:], in_=d_)
    for t_, k_ in ((CH1, 'CH1'), (CH2, 'CH2'), (WW, 'WW'), (CHb, 'CHb')):
        P.pool('memset', [], [k_], ap=t_[:], constant=0.0)
    mark = len(P.stack)
    u32 = P.sb("u32", [128, 8, 512], F32)
    rw = P.sb("rw", [128, 8, 16], F32)
    rb = P.sb("rb", [128, 16], F32)
    vrow = [P.sb("vrow%d" % i, [128, 1024], BF16) for i in range(2)]
    P.dma('sp', writes=['rw'], out=rw[:], in_=rw_d)
    P.dma('sp', writes=['rb'], out=rb[:], in_=rb_d)
    rt = {nm: P.sb("rt_" + nm, [128, 16], F32) for nm in ('sc', 'sl', 'eq', 's2', 'ch', 'w')}
    r4 = {nm: P.sb("r4_" + nm, [128, 4], F32) for nm in ('m1', 'm2', 'gs', 'gm')}
    r1 = {nm: P.sb("r1_" + nm, [128, 1], F32) for nm in ('gx', 'ss')}
    v4 = lambda t_: t_[:].rearrange("p (g j) -> p g j", j=4)
    b4 = lambda t_: t_[:].unsqueeze(2).to_broadcast([128, 4, 4])
    rbank, rbk = banks[6], 'rbank'
    jsub = 0
    for ti, tile in enumerate(tiles):
        c0, c1, r = tile
        n = c1 - c0
        emit_modulate_tile(P, hT, 'h', Af, 'Af', mod, 'modL', 3, ti, tile, ones_bf, ps_ssq, work, uT=None, ukey=None, u32=u32)
        for sub in range(n // 128 if n >= 128 else 1):
            m = min(128, n)
            s0 = sub * 128
            j = jsub
            jsub += 1
            vr, vk = vrow[j % 2], 'vrow%d' % (j % 2)
            for half in range(2):
                tb_, tk_ = banks[4 + half], 'tpb%d' % half
                for kk in range(4):
                    k = half * 4 + kk
                    P.op('pe', 'transpose', ['u32', 'ident'], [tk_], out=tb_[0:m, kk * 128:(kk + 1) * 128], in_=u32[:, k, s0:s0 + m], identity=ident[:])
                P.act(vr[0:m, half * 512:(half + 1) * 512], tb_[0:m, :], AF.Copy, [tk_], [vk, tk_])
            P.dma('sp' if j % 2 == 0 else 'act', reads=[vk], writes=['vtm'], out=vtm[j * 128:j * 128 + 128, :], in_=vr[:, :])
            for k in range(8):
                P.mm(rbank[0:m, 0:16], u32[:, k, s0:s0 + m], rw[:, k, :], k == 0, k == 7, ['u32', 'rw'], [rbk])
            P.act(rt['sc'][0:m, :], rbank[0:m, 0:16], AF.Sigmoid, [rbk], ['rt_sc', rbk])
            P.dve('tensor_tensor', ['rt_sc', 'rb'], ['rt_sl'], out=rt['sl'][0:m], in0=rt['sc'][0:m], in1=rb[0:m], op=ALU.add)
            P.dve('tensor_reduce', ['rt_sl'], ['r4_m1'], out=r4['m1'][0:m], in_=v4(rt['sl'])[0:m], axis=AX.X, op=ALU.max)
            P.dve('tensor_tensor', ['rt_sl', 'r4_m1'], ['rt_eq'], out=v4(rt['eq'])[0:m], in0=v4(rt['sl'])[0:m], in1=b4(r4['m1'])[0:m], op=ALU.is_equal)
            P.dve('scalar_tensor_tensor', ['rt_eq', 'rt_sl'], ['rt_s2'], out=rt['s2'][0:m], in0=rt['eq'][0:m], scalar=-1e9, in1=rt['sl'][0:m], op0=ALU.mult, op1=ALU.add)
            P.dve('tensor_reduce', ['rt_s2'], ['r4_m2'], out=r4['m2'][0:m], in_=v4(rt['s2'])[0:m], axis=AX.X, op=ALU.max)
            P.dve('tensor_tensor', ['r4_m1', 'r4_m2'], ['r4_gs'], out=r4['gs'][0:m], in0=r4['m1'][0:m], in1=r4['m2'][0:m], op=ALU.add)
            P.dve('tensor_reduce', ['r4_gs'], ['r1_gx'], out=r1['gx'][0:m], in_=r4['gs'][0:m], axis=AX.X, op=ALU.max)
            P.dve('tensor_scalar', ['r4_gs', 'r1_gx'], ['r4_gm'], out=r4['gm'][0:m], in0=r4['gs'][0:m], scalar1=r1['gx'][0:m, 0:1], scalar2=None, op0=ALU.is_equal)
            P.dve('tensor_tensor', ['rt_sl', 'r4_m2'], ['rt_ch'], out=v4(rt['ch'])[0:m], in0=v4(rt['sl'])[0:m], in1=b4(r4['m2'])[0:m], op=ALU.is_ge)
            P.dve('tensor_tensor', ['rt_ch', 'r4_gm'], ['rt_ch'], out=v4(rt['ch'])[0:m], in0=v4(rt['ch'])[0:m], in1=b4(r4['gm'])[0:m], op=ALU.mult)
            P.dve('tensor_tensor', ['rt_ch', 'rt_sc'], ['rt_w'], out=rt['w'][0:m], in0=rt['ch'][0:m], in1=rt['sc'][0:m], op=ALU.mult)
            P.dve('tensor_reduce', ['rt_w'], ['r1_ss'], out=r1['ss'][0:m], in_=rt['w'][0:m], axis=AX.X, op=ALU.add)
            P.dve('reciprocal', ['r1_ss'], ['r1_ss'], out=r1['ss'][0:m], in_=r1['ss'][0:m])
            P.dve('tensor_scalar', ['rt_w', 'r1_ss'], ['WW'], out=WW[0:m, j, :], in0=rt['w'][0:m], scalar1=r1['ss'][0:m, 0:1], scalar2=None, op0=ALU.mult)
            P.dve('tensor_tensor', ['rt_eq', 'r4_gm'], ['CH1'], out=CH1[0:m, j, :].rearrange("p (g j) -> p g j", j=4), in0=v4(rt['eq'])[0:m], in1=b4(r4['gm'])[0:m], op=ALU.mult)
            P.dve('tensor_tensor', ['rt_ch', 'CH1'], ['CH2'], out=CH2[0:m, j, :], in0=rt['ch'][0:m], in1=CH1[0:m, j, :], op=ALU.subtract)
            P.dve('tensor_copy', ['rt_ch'], ['CHb'], out=CHb[0:m, j, :], in_=rt['ch'][0:m])
    for j in range(NSUB):
        pbank, pbk = banks[6 + j % 2], ('rbank' if j % 2 == 0 else 'pbank1')
        P.mm(pbank[:, 0:16], tris[:], CHb[:, j, :], True, j == 0, ['tris', 'CHb'], [pbk])
        for jj in range(j):
            P.mm(pbank[:, 0:16], ones_bf[:], CHb[:, jj, :], False, jj == j - 1, ['ones', 'CHb'], [pbk])
        P.dve('tensor_tensor', [pbk, 'eoff'], ['RP', pbk], out=RP[:, j, :], in0=pbank[:, 0:16], in1=eoff[:], op=ALU.add)
    barrier(P)
    while len(P.stack) > mark:
        P.stack.pop().__exit__(None, None, None)

    identb = P.sb("identb_sb", [128, 128], BF16)
    P.dve('tensor_copy', ['ident'], ['identb'], out=identb[:], in_=ident[:])
    WG = [P.sb("WG%d" % k, [128, 1024], BF16) for k in range(8)]
    WU = [P.sb("WU%d" % k, [128, 1024], BF16) for k in range(8)]
    WD = [P.sb("WD%d" % k, [128, 1024], BF16) for k in range(8)]
    OH = [P.sb("OH%d" % i, [128, CAP], BF16) for i in range(3)]
    pe_ = P.sb("pe_e", [128, NSUB], F32)
    hl = P.sb("hl", [2, CAP], F32)
    idxf = P.sb("idxf", [128, NSL], F32)
    hls = P.sb("hls", [128, 2 * NSL], F32)
    idxi = [P.sb("idxi%d" % i, [128, NSL], I32) for i in range(2)]
    xg = [P.sb("xg%d" % i, [128, 1024], BF16) for i in range(2)]
    xsT2 = [P.sb("xsTe%d" % i, [128, 8, CAP], BF16) for i in range(2)]
    hid = P.sb("hid", [128, 8, CAP], BF16)
    sg = [P.sb("sg%d" % i, [128, 512], F32) for i in range(2)]
    ys = [P.sb("ys%d" % i, [128, 1024], F32) for i in range(2)]
    invA, invB = banks[0], banks[1]
    bA = [banks[2], banks[3]]
    bB = [banks[4], banks[5]]
    bY = [banks[6], banks[7]]
    ntl = [(0, min(512, CAP))] + ([(512, CAP)] if CAP > 512 else [])

    def indirect(out, in_, idx_ap, reads, writes):
        n_ = P.dma_n['pool']
        P.dma_n['pool'] += 1
        si = n_ % P.ndma
        key = ('d', 'pool', si)
        val = 16 * (n_ // P.ndma + 1)
        waits = P._deps('pool', list(reads), list(writes))
        if n_ >= P.ndma and P.seen['pool'].get(key, 0) < val - 16:
            waits.append((key, val - 16))
            P.seen['pool'][key] = val - 16
        P.q['pool'].append((waits, ('indirect_dma_start', dict(out=out, out_offset=None, in_=in_, in_offset=bass.IndirectOffsetOnAxis(ap=idx_ap, axis=0))), key))
        P._commit((key, val), list(reads), list(writes))

    def interleave(*gens):
        gens = [g_ for g_ in gens if g_ is not None]
        while gens:
            for g_ in list(gens):
                try:
                    next(g_)
                except StopIteration:
                    gens.remove(g_)

    state = dict(it=0, gcount=0)

    def prep_gen(e):
        xs_, xk_ = xsT2[e % 2], 'xsT%d' % (e % 2)
        P.dve('tensor_scalar', ['RP'], ['pe_e'], out=pe_[:], in0=RP[:, :, e], scalar1=float(1 - e * CAP), scalar2=None, op0=ALU.add)
        P.dve('tensor_tensor', ['pe_e', 'CHb'], ['pe_e'], out=pe_[:], in0=pe_[:], in1=CHb[:, :, e], op=ALU.mult)
        P.dve('tensor_scalar', ['pe_e'], ['pe_e'], out=pe_[:], in0=pe_[:], scalar1=-1.0, scalar2=None, op0=ALU.add)
        yield
        for jj in range(NSUB + 2):
            if jj < NSUB:
                oh, ok_ = OH[jj % 3], 'OH%d' % (jj % 3)
                P.dve('tensor_scalar', ['iota', 'pe_e'], [ok_], out=oh[:], in0=iota_s[:], scalar1=pe_[:, jj:jj + 1], scalar2=None, op0=ALU.is_equal)
            j = jj - 2
            if j >= 0:
                oh, ok_ = OH[j % 3], 'OH%d' % (j % 3)
                for ni, (n0, n1) in enumerate(ntl):
                    bk_, bkk = (invA, 'invA') if ni == 0 else (invB, 'invB')
                    P.mm(bk_[0:2, 0:n1 - n0], tokhl[:, j, :], oh[:, n0:n1], j == 0, j == NSUB - 1, ['tokhl', ok_], [bkk])
            yield
        yield
        for ni, (n0, n1) in enumerate(ntl):
            bk_, bkk = (invA, 'invA') if ni == 0 else (invB, 'invB')
            P.act(hl[:, n0:n1], bk_[0:2, 0:n1 - n0], AF.Copy, [bkk], ['hl', bkk])
        yield
        yield
        for st in range(NSL):
            P.op('pe', 'transpose', ['hl', 'ident'], ['invB'], out=invB[:, 256 + 2 * st:256 + 2 * st + 2], in_=hl[0:2, st * 128:(st + 1) * 128], identity=ident[0:2, 0:2])
        yield
        P.act(hls[:], invB[:, 256:256 + 2 * NSL], AF.Copy, ['invB'], ['hls', 'invB'])
        yield
        hv = hls[:].rearrange("p (s c) -> p s c", c=2)
        P.dve('scalar_tensor_tensor', ['hls'], ['idxf'], out=idxf[:], in0=hv[:, :, 0], scalar=64.0, in1=hv[:, :, 1], op0=ALU.mult, op1=ALU.add)
        ii, ik = idxi[e % 2], 'idxi%d' % (e % 2)
        P.dve('tensor_copy', ['idxf'], [ik], out=ii[:], in_=idxf[:])
        yield

        def gather(st):
            g_, gk_ = xg[st % 2], 'xg%d' % (st % 2)
            indirect(g_[:], vtm[:, :], ii[:, st:st + 1], [ik, 'vtm'], [gk_])
        gather(0)
        for st in range(NSL):
            if st + 1 < NSL:
                gather(st + 1)
            yield
            yield
            g_, gk_ = xg[st % 2], 'xg%d' % (st % 2)
            tb_, tk_ = (invA, 'invA') if st % 2 == 0 else (invB, 'invB')
            tbb = tb_[:].bitcast(BF16)
            for k in range(8):
                P.op('pe', 'transpose', [gk_, 'identb'], [tk_], out=tbb[:, k * 128:(k + 1) * 128], in_=g_[:, k * 128:(k + 1) * 128], identity=identb[:])
            yield
            P.act(xs_[:, :, st * 128:(st + 1) * 128], tbb[:, 0:1024].rearrange("p (k s) -> p k s", k=8), AF.Copy, [tk_], [xk_, tk_])
            yield

    def ffn_gen(e):
        xs_, xk_ = xsT2[e % 2], 'xsT%d' % (e % 2)
        for k in range(8):
            P.dma('pool', writes=['WG%d' % k], out=WG[k][:], in_=wg_d[e, k * 128:(k + 1) * 128, :])
            P.dma('pool', writes=['WU%d' % k], out=WU[k][:], in_=wu_d[e, k * 128:(k + 1) * 128, :])
        for k in range(8):
            P.dma('pool', writes=['WD%d' % k], out=WD[k][:], in_=wd_d[e, k * 128:(k + 1) * 128, :])
        yield
        for fc in range(8):
            for (n0, n1) in ntl:
                n = n1 - n0
                i2 = state['it'] % 2
                state['it'] += 1
                for k in range(8):
                    P.mm(bA[i2][:, 0:n], WG[k][:, fc * 128:(fc + 1) * 128], xs_[:, k, n0:n1], k == 0, k == 7, ['WG%d' % k, xk_], ['bA%d' % i2])
                yield
                for k in range(8):
                    P.mm(bB[i2][:, 0:n], WU[k][:, fc * 128:(fc + 1) * 128], xs_[:, k, n0:n1], k == 0, k == 7, ['WU%d' % k, xk_], ['bB%d' % i2])
                yield
                P.act(sg[i2][:, 0:n], bA[i2][:, 0:n], AF.Silu, ['bA%d' % i2], ['sg%d' % i2, 'bA%d' % i2])
                P.dve('tensor_tensor', ['bB%d' % i2, 'sg%d' % i2], ['hid', 'bB%d' % i2], out=hid[:, fc, n0:n1], in0=bB[i2][:, 0:n], in1=sg[i2][:, 0:n], op=ALU.mult)
                yield
        for st in range(NSL):
            y_, yk = ys[st % 2], 'ys%d' % (st % 2)
            for dh in range(2):
                i2 = state['it'] % 2
                state['it'] += 1
                for k in range(8):
                    P.mm(bY[i2][:, :], hid[:, k, st * 128:(st + 1) * 128], WD[k][:, dh * 512:(dh + 1) * 512], k == 0, k == 7, ['WD%d' % k, 'hid'], ['bY%d' % i2])
                yield
                P.act(y_[:, dh * 512:(dh + 1) * 512], bY[i2][:, :], AF.Copy, ['bY%d' % i2], [yk, 'bY%d' % i2])
                yield
            P.dma('sp' if st % 2 == 0 else 'act', reads=[yk], writes=['yall'], out=y_all[e * CAP + st * 128:e * CAP + (st + 1) * 128, :], in_=y_[:])

    if nexp > 0:
        interleave(prep_gen(0))
    for e in range(nexp):
        interleave(ffn_gen(e), prep_gen(e + 1) if e + 1 < nexp else None)
    barrier(P)
    while len(P.stack) > mark:
        P.stack.pop().__exit__(None, None, None)

    gk2 = [P.sb("gk%d" % i, [128, 1024], F32) for i in range(4)]
    ytok = [P.sb("ytok%d" % i, [128, 1024], F32) for i in range(2)]
    t16 = P.sb("t16", [128, 16], F32)
    cs = {nm: P.sb("cs_" + nm, [128, 1], F32) for nm in ('row0', 'row1', 'w0', 'w1', 'eo', 'vl')}
    rowi = [P.sb("rowi%d" % i, [128, 1], I32) for i in range(4)]
    for j in range(NSUB):
        if layer == 0 and j == NSUB - 1:
            m, c0, r, ti = 64, 2048, 1, 4
        else:
            m, c0, r, ti = 128, j * 128, 0, j // 4
        for kk, CHk in enumerate((CH1, CH2)):
            rw_, w_ = cs['row%d' % kk], cs['w%d' % kk]
            P.dve('tensor_tensor', ['CH1', 'CH2', 'RP'], ['t16'], out=t16[:], in0=CHk[:, j, :], in1=RP[:, j, :], op=ALU.mult)
            P.dve('tensor_reduce', ['t16'], ['cs_row%d' % kk], out=rw_[:], in_=t16[:], axis=AX.X, op=ALU.add)
            P.dve('tensor_tensor', ['CH1', 'CH2', 'eoff'], ['t16'], out=t16[:], in0=CHk[:, j, :], in1=eoff[:], op=ALU.mult)
            P.dve('tensor_reduce', ['t16'], ['cs_eo'], out=cs['eo'][:], in_=t16[:], axis=AX.X, op=ALU.add)
            P.dve('tensor_tensor', ['CH1', 'CH2', 'WW'], ['t16'], out=t16[:], in0=CHk[:, j, :], in1=WW[:, j, :], op=ALU.mult)
            P.dve('tensor_reduce', ['t16'], ['cs_w%d' % kk], out=w_[:], in_=t16[:], axis=AX.X, op=ALU.add)
            P.dve('tensor_tensor', ['cs_row%d' % kk, 'cs_eo'], ['cs_row%d' % kk], out=rw_[:], in0=rw_[:], in1=cs['eo'][:], op=ALU.subtract)
            P.dve('tensor_scalar', ['cs_row%d' % kk], ['cs_vl'], out=cs['vl'][:], in0=rw_[:], scalar1=float(CAP) - 0.5, scalar2=None, op0=ALU.is_lt)
            P.dve('tensor_tensor', ['cs_w%d' % kk, 'cs_vl'], ['cs_w%d' % kk], out=w_[:], in0=w_[:], in1=cs['vl'][:], op=ALU.mult)
            P.dve('tensor_scalar', ['cs_row%d' % kk], ['cs_row%d' % kk], out=rw_[:], in0=rw_[:], scalar1=float(CAP - 1), scalar2=None, op0=ALU.min)
            P.dve('tensor_tensor', ['cs_row%d' % kk, 'cs_eo'], ['cs_row%d' % kk], out=rw_[:], in0=rw_[:], in1=cs['eo'][:], op=ALU.add)
            ri, rk_ = rowi[(2 * j + kk) % 4], 'rowi%d' % ((2 * j + kk) % 4)
            P.dve('tensor_copy', ['cs_row%d' % kk], [rk_], out=ri[:], in_=rw_[:])
            g_, gk_ = gk2[(2 * j + kk) % 4], 'gk%d' % ((2 * j + kk) % 4)
            indirect(g_[:], y_all[:, :], ri[:, 0:1], [rk_, 'yall'], [gk_])
        g0, g1 = gk2[(2 * j) % 4], gk2[(2 * j + 1) % 4]
        yt, ytk = ytok[j % 2], 'ytok%d' % (j % 2)
        P.dve('tensor_scalar', ['gk%d' % ((2 * j) % 4), 'cs_w0'], [ytk], out=yt[:], in0=g0[:], scalar1=cs['w0'][:, 0:1], scalar2=None, op0=ALU.mult)
        P.dve('scalar_tensor_tensor', ['gk%d' % ((2 * j + 1) % 4), 'cs_w1', ytk], [ytk], out=yt[:], in0=g1[:], scalar=cs['w1'][:, 0:1], in1=yt[:], op0=ALU.mult, op1=ALU.add)
        pb = (bA, bB)[j % 2]
        pk = ('bA%d', 'bB%d')[j % 2]
        for k in range(8):
            bb_, bbk = pb[k // 4], pk % (k // 4)
            P.op('pe', 'transpose', [ytk, 'ident'], [bbk], out=bb_[:, (k % 4) * 128:(k % 4) * 128 + m], in_=yt[0:m, k * 128:(k + 1) * 128], identity=ident[0:m, 0:m])
        for k in range(8):
            bb_, bbk = pb[k // 4], pk % (k // 4)
            P.dve('scalar_tensor_tensor', [bbk, 'modL', 'h:%d' % ti], ['h:%d' % ti, bbk], out=hT[:, k, c0:c0 + m], in0=bb_[:, (k % 4) * 128:(k % 4) * 128 + m], scalar=mod[:, 40 + k, r:r + 1], in1=hT[:, k, c0:c0 + m], op0=ALU.mult, op1=ALU.add)
    barrier(P)
    while len(P.stack) > mark:
        P.stack.pop().__exit__(None, None, None)

    if layer == 0:
        for k in range(8):
            P.dma('sp' if k % 2 == 0 else 'act', reads=['h:%d' % t for t in range(NT)], out=hT_out[:, k, :], in_=hT[:, k, :])
        mod1 = P.sb("sbmod1", [128, 48, 2], F32)
        P.dma('sp', writes=['mod1'], out=mod1[:], in_=mod1_in)
        A1 = emit_affine(P, mod1, 'mod1', gmix1, 1, 'm1')
        uT1 = P.sb("uT1", [128, 8, T], BF16)
        for ti, tile in enumerate(tiles):
            emit_modulate_tile(P, hT, 'h', A1, 'Am1', mod1, 'mod1', 0, ti, tile, ones_bf, ps_ssq, work, uT=uT1, ukey='u1')
        C = ProjCtx(P, w_in1, uT1, 'u1', banks[2:6], o_f, o_bf)
        C.job_lin(CD['hy'], 1536, A1_BF['hy'], 1.0)
        C.job_lin(CD['xbc'], 1024, A1_BF['xbc'], 1.0)
        C.job_silu(CD['z'], 512, A1_F['zs'], 'f')
        dtp = P.sb("dtp_sb", [16, 2], F32)
        negA = P.sb("negA", [16, 1], F32)
        P.dma('sp', writes=['dtp'], out=dtp[:], in_=dtp_d)
        P.act(negA[:], dtp[:, 1:2], AF.Exp, ['dtp'], ['negA'])
        P.dve('tensor_scalar', ['negA'], ['negA'], out=negA[:], in0=negA[:], scalar1=-1.0, scalar2=None, op0=ALU.mult)
        st1, sk1 = C.stage('f')
        st2, sk2 = C.stage('f')
        C.load_w(CD['dt'], 16)

        def post(ps, n, c0, c1, ti, bkey):
            P.act(st1[0:16, c0:c1], ps, AF.Exp, [bkey, 'dtp'], [sk1, bkey], bias=dtp[:, 0:1], scale=1.0)
            P.act(st1[0:16, c0:c1], st1[0:16, c0:c1], AF.Ln, [sk1], [sk1], bias=1.0, scale=1.0)
            P.dve('tensor_scalar', [sk1, 'negA'], [sk2], out=st2[0:16, c0:c1], in0=st1[0:16, c0:c1], scalar1=negA[:, 0:1], scalar2=None, op0=ALU.mult)
        C.chunk(CD['dt'], 16, post)
        C.outdma('f', st1, sk1, A1_F['dt'], 16)
        C.outdma('f', st2, sk2, A1_F['la'], 16)
    else:
        gout = P.sb("gout_sb", [128, 8], F32)
        P.dma('sp', writes=['gout'], out=gout[:], in_=gout_d)
        yst = [P.sb("yst%d" % i, [128, 8, 512], F32) for i in range(2)]
        for ti, (c0, c1, r) in enumerate(tiles):
            n = c1 - c0
            emit_rstd(P, hT[:, :, c0:c1], 'h:%d' % ti, n, 8, ones_bf, ps_ssq, work['sq'], work['rs'], 1.0 / 1024)
            for k in range(8):
                P.dve('scalar_tensor_tensor', ['h:%d' % ti, 'gout', 'rs'], ['yst%d' % (ti % 2)], out=yst[ti % 2][:, k, 0:n], in0=hT[:, k, c0:c1], scalar=gout[:, k:k + 1], in1=work['rs'][:, 0:n], op0=ALU.mult, op1=ALU.mult)
            P.dma('sp' if ti % 2 == 0 else 'act', reads=['yst%d' % (ti % 2)], out=y_out[:, :, c0:c1], in_=yst[ti % 2][:, :, 0:n])
    P.wait_all('sp')
    P.emit()
    P.close()
    return nc


def local_from_joint(a, q):
    return np.concatenate([a[256 + 2048 * q:256 + 2048 * (q + 1)], a[64 * q:64 * (q + 1)]], axis=0)


def tm_to_joint(o):
    return o.transpose(1, 0, 2).reshape(NCH * 64, o.shape[2])


CAP_ = 640


def tokhl_const(nsub):
    p = np.arange(128)[:, None]
    j = np.arange(nsub)[None, :]
    tid = j * 128 + p
    return np.ascontiguousarray(np.stack([tid // 64, tid % 64], axis=-1).astype(np.float32).astype(NPBF))


def common_C_inputs(inp, layer):
    sel = np.zeros((16, 16, 128), np.float32)
    for e in range(16):
        sel[e, e, :] = 1.0
    return dict(
        gffn=colT(inp['norm_ffn_g'][layer], 8),
        router_w=np.ascontiguousarray(inp['router_w'].reshape(8, 128, 16).transpose(1, 0, 2)),
        router_b=np.ascontiguousarray(np.broadcast_to(inp['router_b'][None, :], (128, 16))),
        sel16=sel, ident=np.eye(128, dtype=np.float32),
        tris=np.triu(np.ones((128, 128), np.float32), 1).astype(NPBF),
        iota_s=np.ascontiguousarray(np.broadcast_to(np.arange(CAP_, dtype=np.float32)[None, :], (128, CAP_))),
        eoff=np.ascontiguousarray(np.broadcast_to((np.arange(16, dtype=np.float32) * CAP_)[None, :], (128, 16))),
        tokhl=tokhl_const(17 if layer == 0 else 16),
        moe_wg=inp['moe_w_gate'][layer], moe_wu=inp['moe_w_up'][layer], moe_wd=inp['moe_w_down'][layer])


def host_C0_inputs(inp, a0, b0):
    maps = []
    com = common_C_inputs(inp, 0)
    ng = np.ascontiguousarray(np.stack([inp['gla_norm_g'][0], inp['hg_norm_g'][0]], axis=1))
    dtp = np.ascontiguousarray(np.stack([inp['mb_dt_bias'][0].reshape(16), inp['mb_a_log'][0].reshape(16)], axis=1))
    for i in range(NCORES):
        b, q = i // 4, i % 4
        oT = np.empty((8, 128, 2, TL), np.float32)
        for hh in range(8):
            src = b0[4 * b + hh % 4]
            for d in range(2):
                o = tm_to_joint(src[('g_o%d' if hh < 4 else 'h_o%d') % d])
                oT[hh, :, d, :] = local_from_joint(o, q).T
        m = dict(com)
        m.update(hT_in=fm(core_tokens(inp['x'], inp['ctx'], i)), mod_in=a0[i]['mods_out'][0], mod1_in=a0[i]['mods_out'][1], w_out=inp['ab_w_out'][0],
                 oT=oT, gateT=np.ascontiguousarray(a0[i]['o_f'][0:1024].reshape(8, 128, TL)), ng=ng,
                 gmix1=colT(inp['norm_mix_g'][1], 8),
                 w_in1=inp['cd_w_in'][0], dtp=dtp)
        maps.append(m)
    return maps


NFFT = 16384
PI = float(np.pi)


def fft_consts():
    a = np.arange(128, dtype=np.float64)
    th = 2 * np.pi * np.outer(a, a) / 128.0
    tw = 2 * np.pi * np.outer(a, a) / NFFT
    c, s = np.cos(th), np.sin(th)
    F64 = np.concatenate([c[:64], -s[:64]], axis=1)
    Fst = np.stack([c, -s, s], axis=1)
    G12 = np.stack([np.concatenate([c, s], 1), np.concatenate([-s, c], 1)], axis=1)
    Gst = np.stack([c[:, :64] / NFFT, -s[:, :64] / NFFT], axis=1)
    tr, ti = np.cos(tw), -np.sin(tw)
    TW = np.stack([np.tile(np.concatenate([tr, tr], 1), (1, 2)), np.tile(np.concatenate([ti, ti], 1), (1, 2))], axis=1)
    pr, pi_ = np.cos(tw), np.sin(tw)
    TWI = np.stack([np.tile(np.concatenate([pr, pr], 1), (1, 2)), np.tile(np.concatenate([pi_, pi_], 1), (1, 2))], axis=1)
    return dict(F64=F64.astype(NPBF), Fst=Fst.astype(NPBF), G12=G12.astype(NPBF), Gst=Gst.astype(NPBF),
                TW=TW.astype(np.float32), TWI=TWI.astype(np.float32))


def build_B1(nscan_steps=None, hy_groups=32, do_ssd=True, do_hy=True):
    nc = bass.Bass("TRN2", target_bir_lowering=False)
    T = NCH * 64
    din = lambda name, shape, dt=F32: nc.dram_tensor(name, list(shape), dt, kind="ExternalInput").ap()
    dout = lambda name, shape, dt=F32: nc.dram_tensor(name, list(shape), dt, kind="ExternalOutput").ap()
    dscr = lambda name, shape, dt=F32: nc.dram_tensor(name, list(shape), dt, kind="Internal").ap()
    tri_d = din("tri", [64, 6, 64])
    ident_d = din("identb", [128, 128], BF16)
    TP = 258 + 8194
    xin_d = din("ssd_xin", [128, 3, TP], BF16)
    scw_d = din("ssd_cw", [128, 3, 4])
    dttm_d = din("ssd_dttm", [64, NCH, 4])
    latm_d = [din("ssd_latm%d" % k, [64, NCH, 128]) for k in range(4)]
    s_qT = dscr("s_qT", [128, T], BF16)
    s_kT = dscr("s_kT", [128, T], BF16)
    s_ktm = dscr("s_ktm", [64, NCH, 128], BF16)
    s_vtm = [dscr("s_vtm%d" % k, [64, NCH, 64], BF16) for k in range(4)]
    ssd_o = [dout("ssd_o%d" % k, [64, NCH, 64]) for k in range(4)]
    xs_out = dout("xs_out", [128, T])
    hy_in_d = din("hy_in", [128, 3, 8194], BF16)
    hcw_d = din("hy_cw", [128, 3, 4])
    zT_d = din("hy_zT", [33, 8192])
    w1_d = din("hy_w1", [33, 64])
    w2_d = din("hy_w2", [64, 64])
    w3_d = din("hy_w3", [64, 4, 128])
    fp_d = din("hy_fp", [64, 3])
    rate_d = din("hy_rate", [64, 128])
    negt_d = din("hy_negt", [64, 128])
    hb_d = din("hy_bias", [64, 2, 128])
    F64_d = din("F64", [64, 256], BF16)
    Fst_d = din("Fst", [128, 3, 128], BF16)
    G12_d = din("G12", [128, 2, 256], BF16)
    Gst_d = din("Gst", [128, 2, 64], BF16)
    TW_d = din("TW", [128, 2, 512])
    TWI_d = din("TWI", [128, 2, 512])
    s_hy = dscr("s_hy", [3, 128, 8192], BF16)
    s_kf = dscr("s_kf", [2, 32, 128, 4, 2, 128])
    hy_out = dout("hy_out", [64, 128, 128])

    P = Prog(nc)
    banks = [P.ps("bank%d" % i, [128, 512], F32) for i in range(8)]
    dq = [0]

    def q3():
        dq[0] += 1
        return ['sp', 'act', 'pool'][dq[0] % 3]

    def dwconv(x, xkey, cw, part, col0, n, acc, acckey):
        P.act(acc[:, 0:n], x[:, part, col0 + 1:col0 + 1 + n], AF.Identity, [xkey, 'cw'], [acckey], scale=cw[:, part, 1:2], bias=cw[:, part, 3:4])
        P.dve('scalar_tensor_tensor', [xkey, 'cw', acckey], [acckey], out=acc[:, 0:n], in0=x[:, part, col0:col0 + n], scalar=cw[:, part, 0:1], in1=acc[:, 0:n], op0=ALU.mult, op1=ALU.add)
        P.dve('scalar_tensor_tensor', [xkey, 'cw', acckey], [acckey], out=acc[:, 0:n], in0=x[:, part, col0 + 2:col0 + 2 + n], scalar=cw[:, part, 2:3], in1=acc[:, 0:n], op0=ALU.mult, op1=ALU.add)

    if do_hy:
        mark0 = len(P.stack)
        xin = P.sb("hy_xin", [128, 3, 8194], BF16)
        cw = P.sb("hy_cw_sb", [128, 3, 4], F32)
        acc = [P.sb("hy_acc%d" % i, [128, 2048], F32) for i in range(2)]
        accb = [P.sb("hy_accb%d" % i, [128, 2048], BF16) for i in range(2)]
        for p in range(3):
            P.dma(q3(), writes=['hyxin'], out=xin[:, p, :], in_=hy_in_d[:, p, :])
        P.dma('sp', writes=['cw'], out=cw[:], in_=hcw_d)
        it = 0
        for p in range(3):
            for blk in range(4):
                i2 = it % 2
                it += 1
                dwconv(xin, 'hyxin', cw, p, blk * 2048, 2048, acc[i2], 'hyacc%d' % i2)
                P.pool('tensor_copy', ['hyacc%d' % i2], ['hyaccb%d' % i2], out=accb[i2][:], in_=acc[i2][:])
                P.dma(q3(), reads=['hyaccb%d' % i2], writes=['s_hy'], out=s_hy[p, :, blk * 2048:(blk + 1) * 2048], in_=accb[i2][:])
        barrier(P)
        while len(P.stack) > mark0:
            P.stack.pop().__exit__(None, None, None)

        F64 = P.sb("F64_sb", [64, 256], BF16)
        Fst = P.sb("Fst_sb", [128, 3, 128], BF16)
        G12 = P.sb("G12_sb", [128, 2, 256], BF16)
        Gst = P.sb("Gst_sb", [128, 2, 64], BF16)
        TW = P.sb("TW_sb", [128, 2, 512], F32)
        TWI = P.sb("TWI_sb", [128, 2, 512], F32)
        for t_, d_ in ((F64, F64_d), (Fst, Fst_d), (G12, G12_d), (Gst, Gst_d), (TW, TW_d), (TWI, TWI_d)):
            P.dma(q3(), writes=['fftc'], out=t_[:], in_=d_)
        m1 = [P.sb("m1_%d" % i, [128, 512], F32) for i in range(2)]
        m2 = [P.sb("m2_%d" % i, [128, 512], F32) for i in range(2)]
        Bt = [P.sb("Bt%d" % i, [128, 4, 2, 128], BF16) for i in range(2)]
        cnt = {'a': 0, 'b': 0}

        def twiddle(psrc, pkey, table, dst, dkey, pair):
            i2 = cnt['a'] % 2
            cnt['a'] += 1
            a1, a2 = m1[i2], m2[i2]
            P.dve('tensor_tensor', [pkey, 'fftc'], ['m1_%d' % i2, pkey], out=a1[:], in0=psrc, in1=table[:, 0, :], op=ALU.mult)
            yield
            P.dve('tensor_tensor', [pkey, 'fftc'], ['m2_%d' % i2, pkey], out=a2[:], in0=psrc, in1=table[:, 1, :], op=ALU.mult)
            yield
            v1 = a1[:].rearrange("p (c h k) -> p c h k", c=2, h=2)
            v2 = a2[:].rearrange("p (c h k) -> p c h k", c=2, h=2)
            P.pool('tensor_tensor', ['m1_%d' % i2, 'm2_%d' % i2], [dkey + 'r%d' % pair], out=dst[:, pair * 2:pair * 2 + 2, 0, :], in0=v1[:, :, 0, :], in1=v2[:, :, 1, :], op=ALU.subtract)
            yield
            P.pool('tensor_tensor', ['m1_%d' % i2, 'm2_%d' % i2], [dkey + 'i%d' % pair], out=dst[:, pair * 2:pair * 2 + 2, 1, :], in0=v2[:, :, 0, :], in1=v1[:, :, 1, :], op=ALU.add)
            yield

        def interleave(*gens):
            gens = [g_ for g_ in gens if g_ is not None]
            while gens:
                for g_ in list(gens):
                    try:
                        next(g_)
                    except StopIteration:
                        gens.remove(g_)

        def fft_fwd4(src, skey, c0, bXr, kXr, bXi, kXi):
            interleave(fft_fwd4_g(src, skey, c0, bXr, kXr, bXi, kXi))

        def fft_fwd4_g(src, skey, c0, bXr, kXr, bXi, kXi, extra=(), flip=0):
            i2 = cnt['b'] % 2
            cnt['b'] += 1
            B = Bt[i2]
            bk = 'Bt%d' % i2
            for pair in range(2):
                pa, pk = banks[pair ^ flip], 'psA%d' % (pair ^ flip)
                for cc in range(2):
                    P.mm(pa[:, cc * 256:(cc + 1) * 256], src[0:64, c0 + pair * 2 + cc, :], F64[:], True, True, [skey, 'fftc'] + list(extra), [pk])
                yield
                yield from twiddle(pa[:], pk, TW, B, bk, pair)
            rk = [bk + 'r0', bk + 'r1', bk + 'i0', bk + 'i1', 'fftc']
            Br = B[:, :, 0, :]
            Bi = B[:, :, 1, :]
            P.mm(bXr[:], Fst[:, 0, :], Br, True, False, rk, [kXr])
            P.mm(bXr[:], Fst[:, 2, :], Bi, False, True, rk, [kXr])
            yield
            P.mm(bXi[:], Fst[:, 1, :], Br, True, False, rk, [kXi])
            P.mm(bXi[:], Fst[:, 0, :], Bi, False, True, rk, [kXi])
            yield

        mark1 = len(P.stack)
        zT = P.sb("zT_sb", [33, 8192], F32)
        w1 = P.sb("w1_sb", [33, 64], F32)
        w2 = P.sb("w2_sb", [64, 64], F32)
        w3 = P.sb("w3_sb", [64, 4, 128], F32)
        fp = P.sb("fp_sb", [64, 3], F32)
        fb = P.sb("fb_sb", [64, 2], F32)
        rate = P.sb("rate_sb", [64, 128], F32)
        negt = P.sb("negt_sb", [64, 128], F32)
        h1 = P.sb("h1_sb", [64, 8192], F32)
        h2 = P.sb("h2_sb", [64, 8192], F32)
        for t_, d_, k_ in ((zT, zT_d, 'zT'), (w1, w1_d, 'w1'), (w2, w2_d, 'w2'), (w3, w3_d, 'w3'), (fp, fp_d, 'fp'), (rate, rate_d, 'rate'), (negt, negt_d, 'negt')):
            P.dma(q3(), writes=[k_], out=t_[:], in_=d_)
        P.dve('tensor_scalar', ['fp'], ['fb'], out=fb[:], in0=fp[:, 1:3], scalar1=fp[:, 0:1], scalar2=None, op0=ALU.mult)
        gbank, gk = banks[6], 'gbank'
        wrp = [P.sb("wrp%d" % i, [64, 512], F32) for i in range(2)]
        for lyr, (wsb, wk, src, sk, dst, dk_, K_) in enumerate(((w1, 'w1', zT, 'zT', h1, 'h1', 33), (w2, 'w2', h1, 'h1', h2, 'h2', 64))):
            for blk in range(16):
                cs = slice(blk * 512, (blk + 1) * 512)
                gbank, gk = banks[6 + blk % 2], 'gbank%d' % (blk % 2)
                P.mm(gbank[0:64, :], wsb[0:K_, :], src[0:K_, cs], True, True, [wk, sk], [gk])
                P.dve('tensor_scalar', [gk, 'fp', 'fb'], [dk_, gk], out=dst[:, cs], in0=gbank[0:64, :], scalar1=fp[:, 0:1], scalar2=fb[:, lyr:lyr + 1], op0=ALU.mult, op1=ALU.add)
                wa, wb_ = wrp[0][:, 0:512], wrp[1][:, 0:512]
                P.dve('tensor_scalar', [dk_], ['wrp0'], out=wa, in0=dst[:, cs], scalar1=PI, scalar2=-2 * PI, op0=ALU.is_gt, op1=ALU.mult)
                P.dve('tensor_scalar', [dk_], ['wrp1'], out=wb_, in0=dst[:, cs], scalar1=-PI, scalar2=2 * PI, op0=ALU.is_lt, op1=ALU.mult)
                P.dve('tensor_tensor', ['wrp0', 'wrp1'], ['wrp0'], out=wa, in0=wa, in1=wb_, op=ALU.add)
                P.dve('tensor_tensor', ['wrp0', dk_], [dk_], out=dst[:, cs], in0=dst[:, cs], in1=wa, op=ALU.add)
                P.dve('tensor_scalar', [dk_], [dk_], out=dst[:, cs], in0=dst[:, cs], scalar1=3.1415925, scalar2=-3.1415925, op0=ALU.min, op1=ALU.max)
                P.act(dst[:, cs], dst[:, cs], AF.Sin, [dk_], [dk_])
        hf = [P.sb("hf%d" % d, [64, 128, 128], BF16) for d in range(2)]
        dec = [P.sb("dec%d" % i, [64, 128], F32) for i in range(2)]
        kst = [P.sb("kst%d" % i, [128, 4, 2, 128], F32) for i in range(2)]
        xev = [P.sb("xev%d" % i, [128, 512], F32) for i in range(2)]
        h2v = h2[:].rearrange("j (a b) -> j a b", b=128)
        for o in range(2):
            for n2 in range(128):
                i2 = n2 % 2
                P.act(dec[i2][:], rate[:], AF.Exp, ['rate', 'negt'], ['dec%d' % i2], scale=negt[:, n2:n2 + 1])
                for d in range(2):
                    gbank, gk = banks[6 + d], 'gbank%d' % d
                    P.mm(gbank[0:64, 0:128], h2v[:, :, n2], w3[:, o * 2 + d, :], True, True, ['h2', 'w3'], [gk])
                    P.dve('tensor_tensor', [gk, 'dec%d' % i2], ['hf%d' % d, gk], out=hf[d][:, :, n2], in0=gbank[0:64, 0:128], in1=dec[i2][:], op=ALU.mult)
            for g in range(hy_groups):
                interleave(fft_fwd4_g(hf[0], 'hf0', g * 4, banks[2], 'bX0', banks[3], 'bX1'), fft_fwd4_g(hf[1], 'hf1', g * 4, banks[4], 'bX2', banks[5], 'bX3', flip=1))
                i2 = g % 2
                ks = kst[i2]
                kk = 'kst%d' % i2
                P.act(xev[0][:], banks[4][:], AF.Copy, ['bX2'], ['xev0', 'bX2'])
                P.act(xev[1][:], banks[5][:], AF.Copy, ['bX3'], ['xev1', 'bX3'])
                P.dve('tensor_tensor', ['bX0', 'xev0'], [kk, 'bX0'], out=ks[:, :, 0, :], in0=banks[2][:].rearrange("p (c k) -> p c k", c=4), in1=xev[0][:].rearrange("p (c k) -> p c k", c=4), op=ALU.add)
                P.dve('tensor_tensor', ['bX1', 'xev1'], [kk, 'bX1'], out=ks[:, :, 1, :], in0=banks[3][:].rearrange("p (c k) -> p c k", c=4), in1=xev[1][:].rearrange("p (c k) -> p c k", c=4), op=ALU.subtract)
                P.dma(q3(), reads=[kk], writes=['s_kf'], out=s_kf[o, g], in_=ks[:])
        barrier(P)
        while len(P.stack) > mark1:
            P.stack.pop().__exit__(None, None, None)

        xv = P.sb("xv", [64, 128, 128], BF16)
        x1 = P.sb("x1", [64, 128, 128], BF16)
        x2 = P.sb("x2", [64, 128, 128], BF16)
        hbias = P.sb("hbias", [64, 2, 128], F32)
        for p, t_ in enumerate((xv, x1, x2)):
            for hh in range(2):
                P.dma(q3(), reads=['s_hy'], writes=['xd%d' % p], out=t_[:, hh * 64:(hh + 1) * 64, :], in_=s_hy[p, hh * 64:(hh + 1) * 64, :].rearrange("c (a b) -> a c b", b=128))
        P.dma('sp', writes=['hbias'], out=hbias[:], in_=hb_d)
        kfb = [P.sb("kfb%d" % i, [128, 4, 2, 128], F32) for i in range(2)]
        Yt = [P.sb("Yt%d" % i, [128, 4, 2, 128], BF16) for i in range(2)]
        Dt = [P.sb("Dt%d" % i, [128, 4, 2, 128], BF16) for i in range(2)]
        pw = [P.sb("pw%d" % i, [128, 512], F32) for i in range(4)]
        ep = [P.sb("ep%d" % i, [64, 512], F32) for i in range(2)]
        ost = [P.sb("ost%d" % i, [64, 4, 128], F32) for i in range(2)]
        def fwd_gen(o, g):
            i2 = g % 2
            kf, kfk = kfb[i2], 'kfb%d' % i2
            P.dma(q3(), reads=['s_kf'], writes=[kfk], out=kf[:], in_=s_kf[o, g])
            yield from fft_fwd4_g(xv, 'xd0', g * 4, banks[2], 'bX0', banks[3], 'bX1', extra=['zz:%d' % g])
            Xr = banks[2][:].rearrange("p (c k) -> p c k", c=4)
            Xi = banks[3][:].rearrange("p (c k) -> p c k", c=4)
            Y = Yt[i2]
            yk = 'Yt%d' % i2
            pwv = [pw[i][:].rearrange("p (c k) -> p c k", c=4) for i in range(4)]
            P.dve('tensor_tensor', ['bX0', kfk, 'zz:%d' % g], ['pw0', 'bX0'], out=pwv[0], in0=Xr, in1=kf[:, :, 0, :], op=ALU.mult)
            yield
            P.dve('tensor_tensor', ['bX1', kfk], ['pw1', 'bX1'], out=pwv[1], in0=Xi, in1=kf[:, :, 1, :], op=ALU.mult)
            yield
            P.dve('tensor_tensor', ['bX0', kfk], ['pw2', 'bX0'], out=pwv[2], in0=Xr, in1=kf[:, :, 1, :], op=ALU.mult)
            yield
            P.dve('tensor_tensor', ['bX1', kfk], ['pw3', 'bX1'], out=pwv[3], in0=Xi, in1=kf[:, :, 0, :], op=ALU.mult)
            yield
            P.pool('tensor_tensor', ['pw0', 'pw1'], [yk], out=Y[:, :, 0, :], in0=pwv[0], in1=pwv[1], op=ALU.subtract)
            yield
            P.pool('tensor_tensor', ['pw2', 'pw3'], [yk], out=Y[:, :, 1, :], in0=pwv[2], in1=pwv[3], op=ALU.add)
            yield

        def inv_gen(o, g):
            i2 = g % 2
            Y = Yt[i2]
            yk = 'Yt%d' % i2
            D = Dt[i2]
            dk_ = 'Dt%d' % i2
            for pair in range(2):
                pc, pck = banks[4 + pair], 'psC%d' % pair
                for cc in range(2):
                    c = pair * 2 + cc
                    P.mm(pc[:, cc * 256:(cc + 1) * 256], Y[:, c, 0, :], G12[:, 0, :], True, False, [yk, 'fftc'], [pck])
                    P.mm(pc[:, cc * 256:(cc + 1) * 256], Y[:, c, 1, :], G12[:, 1, :], False, True, [yk, 'fftc'], [pck])
                    yield
                yield from twiddle(pc[:], pck, TWI, D, dk_, pair)
            rk = [dk_ + 'r0', dk_ + 'r1', dk_ + 'i0', dk_ + 'i1', 'fftc']
            yb, ybk = banks[6 + i2], 'ybank%d' % i2
            P.mm(yb[0:64, :], Gst[:, 0, :], D[:, :, 0, :], True, False, rk, [ybk])
            P.mm(yb[0:64, :], Gst[:, 1, :], D[:, :, 1, :], False, True, rk, [ybk])
            yield
            cs = slice(g * 4, g * 4 + 4)
            e_ = ep[i2]
            ek = 'ep%d' % i2
            ev = e_[:].rearrange("p (c k) -> p c k", c=4)
            P.pool('tensor_tensor', ['xd0', 'zz:%d' % g, 'hbias'], [ek], out=ev, in0=xv[:, cs, :], in1=hbias[:, o, cs].unsqueeze(2).to_broadcast([64, 4, 128]), op=ALU.mult)
            yield
            P.dve('tensor_tensor', [ybk, ek], [ek, ybk], out=e_[:], in0=yb[0:64, :], in1=e_[:], op=ALU.add)
            yield
            if o == 0:
                P.dve('tensor_tensor', [ek, 'xd1'], ['zz:%d' % g], out=xv[:, cs, :], in0=ev, in1=x1[:, cs, :], op=ALU.mult)
            else:
                os_ = ost[i2]
                P.dve('tensor_tensor', [ek, 'xd2'], ['ost%d' % i2], out=os_[:], in0=ev, in1=x2[:, cs, :], op=ALU.mult)
                P.dma(q3(), reads=['ost%d' % i2], out=hy_out[:, cs, :], in_=os_[:])
            yield

        prev = None
        for o in range(2):
            for g in range(hy_groups):
                interleave(fwd_gen(o, g), prev)
                prev = inv_gen(o, g)
        interleave(prev)
        barrier(P)
        while len(P.stack) > mark0:
            P.stack.pop().__exit__(None, None, None)

    if do_ssd:
        mark2 = len(P.stack)
        cw = P.sb("ssd_cw_sb", [128, 3, 4], F32)
        identb = P.sb("identb_sb", [128, 128], BF16)
        ybf = P.sb("ssd_ybf", [128, 3, T], BF16)
        dttm = P.sb("dttm_sb", [64, NCH, 4], F32)
        mark3 = len(P.stack)
        xin = P.sb("ssd_xin_sb", [128, 3, TP], BF16)
        acc = [P.sb("ssd_acc%d" % i, [128, 2048], F32) for i in range(2)]
        for p in range(3):
            P.dma(q3(), writes=['sxin'], out=xin[:, p, :], in_=xin_d[:, p, :])
        P.dma('sp', writes=['cw'], out=cw[:], in_=scw_d)
        P.dma('sp', writes=['identb'], out=identb[:], in_=ident_d)
        P.dma('sp', writes=['dttm'], out=dttm[:], in_=dttm_d)
        segs = [(0, 0, 256)] + [(258 + i * 2048, 256 + i * 2048, 2048) for i in range(4)]
        it = 0
        for p in range(3):
            for (pc0, oc0, n) in segs:
                i2 = it % 2
                it += 1
                dwconv(xin, 'sxin', cw, p, pc0, n, acc[i2], 'sacc%d' % i2)
                P.act(acc[i2][:, 0:n], acc[i2][:, 0:n], AF.Silu, ['sacc%d' % i2], ['sacc%d' % i2])
                P.pool('tensor_copy', ['sacc%d' % i2], ['ybf%d' % p], out=ybf[:, p, oc0:oc0 + n], in_=acc[i2][:, 0:n])
                if p == 0:
                    P.dma(q3(), reads=['sacc%d' % i2], out=xs_out[:, oc0:oc0 + n], in_=acc[i2][:, 0:n])
        P.dma(q3(), reads=['ybf2'], writes=['s_qT'], out=s_qT, in_=ybf[:, 2, :])
        P.dma(q3(), reads=['ybf1'], writes=['s_kT'], out=s_kT, in_=ybf[:, 1, :])
        barrier(P)
        while len(P.stack) > mark3:
            P.stack.pop().__exit__(None, None, None)
        btm = P.sb("btm", [64, NCH, 128], BF16)
        xtm = P.sb("xtm", [64, NCH, 128], BF16)
        vt = [P.sb("vt%d" % i, [64, NCH, 64], BF16) for i in range(2)]
        tb = [banks[0], banks[1]]
        for part, dst, dk_ in ((1, btm, 'btm'), (0, xtm, 'xtm')):
            for c4 in range(NCH // 4):
                i2 = c4 % 2
                bkb = tb[i2][0:64, :].bitcast(BF16)
                for j in range(4):
                    c = c4 * 4 + j
                    P.op('pe', 'transpose', ['ybf%d' % part, 'identb'], ['tb%d' % i2], out=bkb[:, j * 128:(j + 1) * 128], in_=ybf[:, part, c * 64:(c + 1) * 64], identity=identb[:])
                P.act(dst[:, c4 * 4:(c4 + 1) * 4, :], bkb[:, 0:512].rearrange("p (c k) -> p c k", c=4), AF.Copy, ['tb%d' % i2], [dk_, 'tb%d' % i2])
        P.dma(q3(), reads=['btm'], writes=['s_ktm'], out=s_ktm, in_=btm[:])
        for k in range(4):
            d, j = k // 2, k % 2
            v_ = vt[k % 2]
            P.dve('tensor_tensor', ['xtm', 'dttm'], ['vt%d' % (k % 2)], out=v_[:], in0=xtm[:, :, j * 64:(j + 1) * 64], in1=dttm[:, :, k:k + 1].to_broadcast([64, NCH, 64]), op=ALU.mult)
            P.dma(q3(), reads=['vt%d' % (k % 2)], writes=['s_vtm%d' % k], out=s_vtm[k], in_=v_[:])
        barrier(P)
        while len(P.stack) > mark2:
            P.stack.pop().__exit__(None, None, None)
        tri = scan_consts(P, tri_d)
        scans = []
        for k in range(4):
            d, j = k // 2, k % 2
            scans.append(dict(dk=128, dv=64, mode='scalar', rev=(d == 1), qT=s_qT, kT=s_kT, ktm=s_ktm, vtm=s_vtm[k], latm=latm_d[k], o=ssd_o[k],
                              rkeys=['s_qT', 's_kT', 's_ktm', 's_vtm%d' % k], okey='ssd_out%d' % k))
        emit_scans(P, scans, tri, banks, nscan_steps)
    P.wait_all('sp')
    P.emit()
    P.close()
    return nc


HY_MIN_DECAY = float(np.log(1e-2) / 1.5)
HY_MAX_DECAY = float(np.log(1e-2) / 0.3)


def pad1(a):
    return np.concatenate([np.zeros_like(a[:, :1]), a, np.zeros_like(a[:, :1])], axis=1)


def hyena_pos_consts():
    n = 8192
    t = np.linspace(0.0, 1.0, n, dtype=np.float32)[:, None]
    bands = np.linspace(1e-4, 15, 16, dtype=np.float32)
    ang = (np.float32(2.0 * np.pi / n) * np.arange(n, dtype=np.float32)[:, None]) * bands
    z = np.concatenate([t, np.cos(ang), -np.sin(ang)], axis=-1).astype(np.float32)
    rates = np.abs(np.linspace(HY_MIN_DECAY, HY_MAX_DECAY, 512, dtype=np.float32))
    negt = -(np.arange(8192, dtype=np.float32) / np.float32(8191.0)).reshape(64, 128)
    return np.ascontiguousarray(z.T), rates, np.ascontiguousarray(negt)


def host_B1_inputs(inp, c0):
    obf = [r['o_bf'] for r in c0]
    of = [r['o_f'] for r in c0]
    fc = fft_consts()
    zT, rates, negt = hyena_pos_consts()
    tri = tri_consts()
    identb = np.eye(128, dtype=np.float32).astype(NPBF)
    cwall = np.concatenate([inp['mb_conv_w'][0], inp['mb_conv_b'][0][:, None]], axis=1)
    hcwall = np.concatenate([inp['hy_short_w'][0], inp['hy_short_b'][0][:, None]], axis=1)
    maps = []
    for i in range(NCORES):
        b, q = i // 4, i % 4
        g, hp = q // 2, q % 2
        h0 = 4 * g + 2 * hp
        rows = [64 * h0, 512 + 128 * g, 768 + 128 * g]
        m = dict(tri=tri, identb=identb)
        m.update(fc)
        xin = []
        for r0 in rows:
            a = joint_fm(obf, A1_BF['xbc'] + r0, 128, b)
            xin.append(np.concatenate([pad1(a[:, :256]), pad1(a[:, 256:])], axis=1))
        m['ssd_xin'] = np.ascontiguousarray(np.stack(xin, axis=1))
        m['ssd_cw'] = np.ascontiguousarray(np.stack([cwall[r0:r0 + 128] for r0 in rows], axis=1))
        dt4, la4 = [], []
        for k in range(4):
            d, j = k // 2, k % 2
            dt4.append(to_tm(joint_fm(of, A1_F['dt'] + d * 8 + h0 + j, 1, b)))
            la = to_tm(joint_fm(of, A1_F['la'] + d * 8 + h0 + j, 1, b))
            m['ssd_latm%d' % k] = np.ascontiguousarray(np.broadcast_to(la, (64, NCH, 128)))
        m['ssd_dttm'] = np.ascontiguousarray(np.concatenate(dt4, axis=2))
        hy = [pad1(joint_fm(obf, A1_BF['hy'] + p * 512 + 128 * q, 128, b)[:, 256:]) for p in range(3)]
        m['hy_in'] = np.ascontiguousarray(np.stack(hy, axis=1))
        m['hy_cw'] = np.ascontiguousarray(np.stack([hcwall[p * 512 + 128 * q:p * 512 + 128 * q + 128] for p in range(3)], axis=1))
        m['hy_zT'] = zT
        m['hy_w1'] = inp['hy_w1'][0]
        m['hy_w2'] = inp['hy_w2'][0]
        m['hy_w3'] = np.ascontiguousarray(inp['hy_w3'][0].reshape(64, 4, 512)[:, :, 128 * q:128 * q + 128])
        m['hy_fp'] = np.ascontiguousarray(np.stack([inp['hy_freq'][0], inp['hy_b1'][0], inp['hy_b2'][0]], axis=1))
        m['hy_rate'] = np.ascontiguousarray(np.broadcast_to(rates[128 * q:128 * q + 128][None, :], (64, 128)))
        m['hy_negt'] = negt
        m['hy_bias'] = np.ascontiguousarray(np.broadcast_to(inp['hy_bias'][0][:, 128 * q:128 * q + 128][None], (64, 2, 128)))
        maps.append(m)
    return maps


def host_C1_inputs(inp, a0, c0, b1):
    maps = []
    com = common_C_inputs(inp, 1)
    dsk = np.ascontiguousarray(np.repeat(inp['mb_d'][0], 64).reshape(4, 128).T)
    for i in range(NCORES):
        b, q = i // 4, i % 4
        ls = slice(256 + 2048 * q, 256 + 2048 * (q + 1))
        hyT = np.empty((128, 4, 2048), np.float32)
        soT = np.empty((2, 128, 4, 2048), np.float32)
        xsT = np.empty((128, 4, 2048), np.float32)
        for j in range(4):
            src = b1[4 * b + j]
            hy_ct = src['hy_out'].transpose(1, 0, 2).reshape(128, 8192)
            hyT[:, j, :] = hy_ct[:, 2048 * q:2048 * (q + 1)]
            xsT[:, j, :] = src['xs_out'][:, ls]
            for d in range(2):
                for jj in range(2):
                    o = tm_to_joint(src['ssd_o%d' % (d * 2 + jj)])
                    soT[d, jj * 64:(jj + 1) * 64, j, :] = o[ls].T
        m = dict(com)
        m.update(hT_in=np.ascontiguousarray(c0[i]['hT_out'][:, :, 0:2048]), mod_in=a0[i]['mods_out'][1], w_out=inp['cd_w_out'][0],
                 hyT=hyT, ssd_oT=soT, xsT=xsT,
                 zsT=np.ascontiguousarray(c0[i]['o_f'][0:512, 0:2048].reshape(4, 128, 2048).transpose(1, 0, 2)),
                 dsk=dsk, mbg=colT(inp['mb_norm_g'][0], 4), gout=colT(inp['norm_out_g'], 8))
        maps.append(m)
    return maps


_CACHE = {}


def _prog(name, fn):
    if name not in _CACHE:
        _CACHE[name] = fn()
    return _CACHE[name]


def _run(nc, maps):
    res = run_bass_kernel_spmd(nc, maps, core_ids=list(range(NCORES)))
    return [dict(r) for r in res.results]


def kernel(**inputs):
    inp = {k: np.asarray(v) for k, v in inputs.items()}
    a0 = _run(_prog('A0', build_A0), host_A0_inputs(inp))
    b0 = _run(_prog('B0', build_B0), host_B0_inputs(a0))
    c0 = _run(_prog('C0', lambda: build_C2(0)), host_C0_inputs(inp, a0, b0))
    b1 = _run(_prog('B1', build_B1), host_B1_inputs(inp, c0))
    c1 = _run(_prog('C1', lambda: build_C2(1)), host_C1_inputs(inp, a0, c0, b1))
    out = np.empty((2, 8192, 1024), np.float32)
    for i in range(NCORES):
        b, q = i // 4, i % 4
        out[b, 2048 * q:2048 * (q + 1), :] = c1[i]['y_out'].transpose(2, 1, 0).reshape(2048, 1024)
    return out
```
